# Optimizing a Trainium2 kernel written in Bass

```python
import math
import jax, jax.numpy as jnp
from jax import lax
import numpy as np

D_MODEL = 1024
BATCH = 8
SEQ = 2048
DEPTH = 2

D_FF = 2816
RMS_EPS = 1e-6
LN_EPS = 1e-5
Q_BLOCK = 128
N_BRANCH = 3
DIFF_HEADS = 4
DIFF_HEAD_DIM = 64
DIFF_V_DIM = 2 * DIFF_HEAD_DIM
DIFF_QK_WIDTH = DIFF_HEADS * 2 * DIFF_HEAD_DIM
DIFF_V_WIDTH = DIFF_HEADS * DIFF_V_DIM
FOX_HEADS = 8
FOX_HEAD_DIM = 64
FOX_WIDTH = FOX_HEADS * FOX_HEAD_DIM
MLSTM_HEADS = 4
MLSTM_HEAD_DIM = 128
MLSTM_WIDTH = MLSTM_HEADS * MLSTM_HEAD_DIM
MLSTM_CONV = 4
MLSTM_CHUNK = 128
SPLIT_SIZES = (DIFF_QK_WIDTH, DIFF_QK_WIDTH, DIFF_V_WIDTH,
               FOX_WIDTH, FOX_WIDTH, FOX_WIDTH, FOX_HEADS,
               MLSTM_WIDTH, MLSTM_WIDTH,
               N_BRANCH * D_MODEL)
N_IN = sum(SPLIT_SIZES)

kernel_name = "hybrid_diffattn_fox_mlstm_macaron"


def rms_norm(x, w):
    x32 = x.astype(jnp.float32)
    y = x32 * lax.rsqrt(jnp.mean(x32 * x32, axis=-1, keepdims=True) + RMS_EPS)
    return (y * w.astype(jnp.float32)).astype(x.dtype)


def swiglu(x, w_gate, w_up, w_down):
    return (jax.nn.silu(x @ w_gate) * (x @ w_up)) @ w_down


def split_columns(p):
    offsets = []
    acc = 0
    for s in SPLIT_SIZES[:-1]:
        acc += s
        offsets.append(acc)
    return jnp.split(p, offsets, axis=-1)


def alibi_slopes(n):
    return np.array([2.0 ** (-8.0 * (h + 1) / n) for h in range(n)], dtype=np.float32)


def causal_block_attention(q, k, v, bias_fn):
    B, H, S, dk = q.shape
    dv = v.shape[-1]
    nb = S // Q_BLOCK
    qb = q.reshape(B, H, nb, Q_BLOCK, dk).transpose(2, 0, 1, 3, 4)
    k_pos = jnp.arange(S)
    scale = dk ** -0.5

    def one_block(args):
        blk, q_i = args
        q_pos = blk * Q_BLOCK + jnp.arange(Q_BLOCK)
        logits = jnp.einsum('bhtd,bhsd->bhts', q_i, k).astype(jnp.float32) * scale
        logits = logits + bias_fn(blk, q_pos, k_pos)
        logits = jnp.where(k_pos[None, :] <= q_pos[:, None], logits, -jnp.inf)
        p = jax.nn.softmax(logits, axis=-1).astype(v.dtype)
        return jnp.einsum('bhts,bhse->bhte', p, v)

    out = lax.map(one_block, (jnp.arange(nb), qb))
    return out.transpose(1, 2, 0, 3, 4).reshape(B, H, S, dv)


def causal_depthwise_conv(x, w, b):
    C = x.shape[-1]
    y = lax.conv_general_dilated(x, w[:, None, :].astype(x.dtype), window_strides=(1,),
                                 padding=[(w.shape[0] - 1, 0)],
                                 dimension_numbers=('NWC', 'WIO', 'NWC'),
                                 feature_group_count=C)
    return y + b


def mlstm_chunkwise(q, k, v, log_i, log_f):
    B, H, S, dk = q.shape
    dv = v.shape[-1]
    L = MLSTM_CHUNK
    nc = S // L
    f32 = jnp.float32

    def chunks(a):
        return a.astype(f32).reshape(B, H, nc, L, a.shape[-1]).transpose(2, 0, 1, 3, 4)

    def gchunks(a):
        return a.astype(f32).reshape(B, H, nc, L).transpose(2, 0, 1, 3)

    qc, kc, vc = chunks(q), chunks(k * dk ** -0.5), chunks(v)
    lic, lfc = gchunks(log_i), gchunks(log_f)
    causal = jnp.tril(jnp.ones((L, L), dtype=bool))

    def step(carry, xs):
        C, n, m = carry
        q_c, k_c, v_c, li, lf = xs
        b = jnp.cumsum(lf, axis=-1)
        b_last = b[..., -1]
        D = b[..., :, None] - b[..., None, :] + li[..., None, :]
        D = jnp.where(causal, D, -jnp.inf)
        inter = b + m[..., None]
        m_t = jnp.maximum(inter, jnp.max(D, axis=-1))
        w_intra = jnp.exp(D - m_t[..., None])
        w_inter = jnp.exp(inter - m_t)
        s = jnp.einsum('bhtd,bhsd->bhts', q_c, k_c) * w_intra
        num = w_inter[..., None] * jnp.einsum('bhtd,bhde->bhte', q_c, C) + jnp.einsum('bhts,bhse->bhte', s, v_c)
        den = w_inter * jnp.einsum('bhtd,bhd->bht', q_c, n) + jnp.sum(s, axis=-1)
        h = num / jnp.maximum(jnp.abs(den), jnp.exp(-m_t))[..., None]
        g = b_last[..., None] - b + li
        m_new = jnp.maximum(b_last + m, jnp.max(g, axis=-1))
        wk = jnp.exp(g - m_new[..., None])
        dec = jnp.exp(b_last + m - m_new)
        C_new = dec[..., None, None] * C + jnp.einsum('bhs,bhsd,bhse->bhde', wk, k_c, v_c)
        n_new = dec[..., None] * n + jnp.einsum('bhs,bhsd->bhd', wk, k_c)
        return (C_new, n_new, m_new), h

    init = (jnp.zeros((B, H, dk, dv), f32), jnp.zeros((B, H, dk), f32), jnp.zeros((B, H), f32))
    _, hs = lax.scan(step, init, (qc, kc, vc, lic, lfc))
    return hs.transpose(1, 2, 0, 3, 4).reshape(B, H, S, dv)


def hybrid_mixer(h, layer_idx, w_in, gate_bias,
                 diff_lq1, diff_lk1, diff_lq2, diff_lk2, diff_subln,
                 fox_b_f,
                 mlstm_conv_w, mlstm_conv_b, mlstm_wq, mlstm_wk, mlstm_wv, mlstm_w_if, mlstm_b_if,
                 mlstm_norm, mlstm_skip,
                 w_branch_diff, w_branch_fox, w_branch_mlstm, w_out):
    B, S, _ = h.shape
    f32 = jnp.float32
    proj = h @ w_in
    d_q, d_k, d_v, f_q, f_k, f_v, f_f, m_x, m_z, g_pre = split_columns(proj)

    dq = d_q.reshape(B, S, DIFF_HEADS, 2, DIFF_HEAD_DIM)
    dk_ = d_k.reshape(B, S, DIFF_HEADS, 2, DIFF_HEAD_DIM)
    q1, q2 = dq[..., 0, :].transpose(0, 2, 1, 3), dq[..., 1, :].transpose(0, 2, 1, 3)
    k1, k2 = dk_[..., 0, :].transpose(0, 2, 1, 3), dk_[..., 1, :].transpose(0, 2, 1, 3)
    dv = d_v.reshape(B, S, DIFF_HEADS, DIFF_V_DIM).transpose(0, 2, 1, 3)
    slopes = jnp.asarray(alibi_slopes(DIFF_HEADS))

    def alibi_bias(blk, q_pos, k_pos):
        dist = (q_pos[:, None] - k_pos[None, :]).astype(f32)
        return -(slopes[:, None, None] * dist[None])[None]

    a1 = causal_block_attention(q1, k1, dv, alibi_bias).astype(f32)
    a2 = causal_block_attention(q2, k2, dv, alibi_bias).astype(f32)
    lam_init = 0.8 - 0.6 * math.exp(-0.3 * layer_idx)
    lam = (jnp.exp(jnp.sum(diff_lq1.astype(f32) * diff_lk1.astype(f32)))
           - jnp.exp(jnp.sum(diff_lq2.astype(f32) * diff_lk2.astype(f32))) + lam_init)
    o_a = rms_norm(a1 - lam * a2, diff_subln) * (1.0 - lam_init)
    o_diff = o_a.transpose(0, 2, 1, 3).reshape(B, S, DIFF_V_WIDTH).astype(h.dtype)

    fq = f_q.reshape(B, S, FOX_HEADS, FOX_HEAD_DIM).transpose(0, 2, 1, 3)
    fk = f_k.reshape(B, S, FOX_HEADS, FOX_HEAD_DIM).transpose(0, 2, 1, 3)
    fv = f_v.reshape(B, S, FOX_HEADS, FOX_HEAD_DIM).transpose(0, 2, 1, 3)
    log_fg = jax.nn.log_sigmoid(f_f.astype(f32) + fox_b_f.astype(f32))
    F = jnp.cumsum(log_fg, axis=1).transpose(0, 2, 1)

    def fox_bias(blk, q_pos, k_pos):
        F_q = lax.dynamic_slice_in_dim(F, blk * Q_BLOCK, Q_BLOCK, axis=2)
        return F_q[..., :, None] - F[..., None, :]

    o_f = causal_block_attention(fq, fk, fv, fox_bias)
    o_fox = o_f.transpose(0, 2, 1, 3).reshape(B, S, FOX_WIDTH)

    x_c = jax.nn.silu(causal_depthwise_conv(m_x, mlstm_conv_w, mlstm_conv_b))
    xh = x_c.reshape(B, S, MLSTM_HEADS, MLSTM_HEAD_DIM)
    vh = m_x.reshape(B, S, MLSTM_HEADS, MLSTM_HEAD_DIM)
    mq = jnp.einsum('bshd,hde->bshe', xh, mlstm_wq)
    mk = jnp.einsum('bshd,hde->bshe', xh, mlstm_wk)
    mv = jnp.einsum('bshd,hde->bshe', vh, mlstm_wv)
    qkv_flat = jnp.concatenate([mq.reshape(B, S, MLSTM_WIDTH), mk.reshape(B, S, MLSTM_WIDTH),
                                mv.reshape(B, S, MLSTM_WIDTH)], axis=-1)
    if_pre = (qkv_flat @ mlstm_w_if + mlstm_b_if).astype(f32)
    log_i = if_pre[..., :MLSTM_HEADS].transpose(0, 2, 1)
    log_f = jax.nn.log_sigmoid(if_pre[..., MLSTM_HEADS:]).transpose(0, 2, 1)
    hm = mlstm_chunkwise(mq.transpose(0, 2, 1, 3), mk.transpose(0, 2, 1, 3),
                         mv.transpose(0, 2, 1, 3), log_i, log_f)
    mu = jnp.mean(hm, axis=-1, keepdims=True)
    var = jnp.mean(jnp.square(hm - mu), axis=-1, keepdims=True)
    hm = (hm - mu) * lax.rsqrt(var + LN_EPS)
    hm = hm.transpose(0, 2, 1, 3).reshape(B, S, MLSTM_WIDTH) * mlstm_norm.astype(f32)
    o_mlstm = ((hm.astype(h.dtype) + mlstm_skip * x_c) * jax.nn.silu(m_z))

    gates = jax.nn.sigmoid(g_pre.reshape(B, S, N_BRANCH, D_MODEL) + gate_bias)
    merged = (gates[..., 0, :] * (o_diff @ w_branch_diff)
              + gates[..., 1, :] * (o_fox @ w_branch_fox)
              + gates[..., 2, :] * (o_mlstm @ w_branch_mlstm))
    return merged @ w_out


def setup_inputs(seed: int = 0) -> dict:
    key = jax.random.key(seed)
    ks = iter(jax.random.split(key, 40))
    f32 = jnp.float32

    def w(shape, fan_in):
        return jax.random.normal(next(ks), shape, f32) * (fan_in ** -0.5)

    def gain(shape):
        return 1.0 + 0.02 * jax.random.normal(next(ks), shape, f32)

    def small(shape, s=0.01):
        return s * jax.random.normal(next(ks), shape, f32)

    L = DEPTH
    fox_b = jnp.tile(jnp.linspace(1.0, 4.0, FOX_HEADS, dtype=f32)[None], (L, 1)) + small((L, FOX_HEADS), 0.1)
    b_i = small((L, MLSTM_HEADS), 0.1)
    b_f = jnp.tile(jnp.linspace(3.0, 6.0, MLSTM_HEADS, dtype=f32)[None], (L, 1)) + small((L, MLSTM_HEADS), 0.1)
    return {
        "x": jax.random.normal(next(ks), (BATCH, SEQ, D_MODEL), f32),
        "ffn1_norm": gain((L, D_MODEL)),
        "ffn1_w_gate": w((L, D_MODEL, D_FF), D_MODEL),
        "ffn1_w_up": w((L, D_MODEL, D_FF), D_MODEL),
        "ffn1_w_down": w((L, D_FF, D_MODEL), D_FF),
        "mix_norm": gain((L, D_MODEL)),
        "w_in": w((L, D_MODEL, N_IN), D_MODEL),
        "gate_bias": small((L, N_BRANCH, D_MODEL)),
        "diff_lq1": small((L, DIFF_HEAD_DIM), 0.1),
        "diff_lk1": small((L, DIFF_HEAD_DIM), 0.1),
        "diff_lq2": small((L, DIFF_HEAD_DIM), 0.1),
        "diff_lk2": small((L, DIFF_HEAD_DIM), 0.1),
        "diff_subln": gain((L, DIFF_V_DIM)),
        "fox_b_f": fox_b,
        "mlstm_conv_w": w((L, MLSTM_CONV, MLSTM_WIDTH), MLSTM_CONV),
        "mlstm_conv_b": small((L, MLSTM_WIDTH)),
        "mlstm_wq": w((L, MLSTM_HEADS, MLSTM_HEAD_DIM, MLSTM_HEAD_DIM), MLSTM_HEAD_DIM),
        "mlstm_wk": w((L, MLSTM_HEADS, MLSTM_HEAD_DIM, MLSTM_HEAD_DIM), MLSTM_HEAD_DIM),
        "mlstm_wv": w((L, MLSTM_HEADS, MLSTM_HEAD_DIM, MLSTM_HEAD_DIM), MLSTM_HEAD_DIM),
        "mlstm_w_if": w((L, 3 * MLSTM_WIDTH, 2 * MLSTM_HEADS), 3 * MLSTM_WIDTH),
        "mlstm_b_if": jnp.concatenate([b_i, b_f], axis=-1),
        "mlstm_norm": gain((L, MLSTM_WIDTH)),
        "mlstm_skip": gain((L, MLSTM_WIDTH)),
        "w_branch_diff": w((L, DIFF_V_WIDTH, D_MODEL), DIFF_V_WIDTH),
        "w_branch_fox": w((L, FOX_WIDTH, D_MODEL), FOX_WIDTH),
        "w_branch_mlstm": w((L, MLSTM_WIDTH, D_MODEL), MLSTM_WIDTH),
        "w_out": w((L, D_MODEL, D_MODEL), D_MODEL),
        "ffn2_norm": gain((L, D_MODEL)),
        "ffn2_w_gate": w((L, D_MODEL, D_FF), D_MODEL),
        "ffn2_w_up": w((L, D_MODEL, D_FF), D_MODEL),
        "ffn2_w_down": w((L, D_FF, D_MODEL), D_FF),
        "final_norm": gain((D_MODEL,)),
    }


def reference(x, ffn1_norm, ffn1_w_gate, ffn1_w_up, ffn1_w_down,
              mix_norm, w_in, gate_bias,
              diff_lq1, diff_lk1, diff_lq2, diff_lk2, diff_subln,
              fox_b_f,
              mlstm_conv_w, mlstm_conv_b, mlstm_wq, mlstm_wk, mlstm_wv, mlstm_w_if, mlstm_b_if,
              mlstm_norm, mlstm_skip,
              w_branch_diff, w_branch_fox, w_branch_mlstm, w_out,
              ffn2_norm, ffn2_w_gate, ffn2_w_up, ffn2_w_down,
              final_norm):
    for l in range(DEPTH):
        x = x + 0.5 * swiglu(rms_norm(x, ffn1_norm[l]), ffn1_w_gate[l], ffn1_w_up[l], ffn1_w_down[l])
        x = x + hybrid_mixer(rms_norm(x, mix_norm[l]), l, w_in[l], gate_bias[l],
                             diff_lq1[l], diff_lk1[l], diff_lq2[l], diff_lk2[l], diff_subln[l],
                             fox_b_f[l],
                             mlstm_conv_w[l], mlstm_conv_b[l], mlstm_wq[l], mlstm_wk[l], mlstm_wv[l],
                             mlstm_w_if[l], mlstm_b_if[l], mlstm_norm[l], mlstm_skip[l],
                             w_branch_diff[l], w_branch_fox[l], w_branch_mlstm[l], w_out[l])
        x = x + 0.5 * swiglu(rms_norm(x, ffn2_norm[l]), ffn2_w_gate[l], ffn2_w_up[l], ffn2_w_down[l])
    return rms_norm(x, final_norm)
```

```python
import bisect
import numpy as np
import concourse.bass as bass
import concourse.mybir as mybir
from concourse.bass_utils import run_bass_kernel_spmd

F32 = mybir.dt.float32
BF16 = mybir.dt.bfloat16
AF = mybir.ActivationFunctionType
ALU = mybir.AluOpType
AX = mybir.AxisListType


class _Ev:
    __slots__ = ("eng", "idx", "val", "clock")

    def __init__(self, eng, idx, val, clock):
        self.eng, self.idx, self.val, self.clock = eng, idx, val, clock


class _Eng:
    def __init__(self, name, sem, self_sync):
        self.name, self.sem, self.self_sync = name, sem, self_sync
        self.ops = []
        self.count = 0
        self.sig_idx = []
        self.sig_val = []
        self.n_inst = 0
        self.inst_rec = []
        self.clock = {}
        self.last_compute = None


class _Slot:
    def __init__(self, name, sem):
        self.name, self.sem, self.total = name, sem, 0


class _Res:
    __slots__ = ("w", "rs")

    def __init__(self):
        self.w, self.rs = None, []


class Sched:
    def __init__(self, nc, sems):
        self.nc = nc
        self._sems = list(sems)
        self.engs = {}
        for name, ss in (("pe", False), ("act", True), ("dve", True), ("pool", True), ("sp", False)):
            self.engs[name] = _Eng(name, self._sems.pop(), ss)
        self.slots = {}
        self.resd = {}

    def slot(self, name):
        s = self.slots.get(name)
        if s is None:
            s = _Slot(name, self._sems.pop())
            self.slots[name] = s
        return s

    def res(self, key):
        r = self.resd.get(key)
        if r is None:
            r = _Res()
            self.resd[key] = r
        return r

    def _value(self, ev):
        if ev.val is not None:
            return ev.val
        E = self.engs[ev.eng]
        j = bisect.bisect_left(E.sig_idx, ev.idx)
        if j < len(E.sig_idx):
            return E.sig_val[j]
        E.count += 1
        rec = E.inst_rec[ev.idx]
        rec[2], rec[3] = E.sem, 1
        E.sig_idx.append(ev.idx)
        E.sig_val.append(E.count)
        ev.val = E.count
        return ev.val

    def _wait_for(self, E, evs):
        need = {}
        for ev in evs:
            if ev is None:
                continue
            if ev.eng == E.name and not E.self_sync:
                continue
            if ev.eng in self.slots:
                v = self.slots[ev.eng].total
            else:
                v = self._value(ev)
            if E.clock.get(ev.eng, 0) >= v:
                continue
            if need.get(ev.eng, (0, None))[0] < v:
                need[ev.eng] = (v, ev)
        for name, (v, ev) in need.items():
            if E.clock.get(name, 0) >= v:
                continue
            sem = self.slots[name].sem if name in self.slots else self.engs[name].sem
            E.ops.append(["w", sem, v])
            E.clock[name] = v
            for k, cv in ev.clock.items():
                if E.clock.get(k, 0) < cv:
                    E.clock[k] = cv

    def _deps(self, reads, writes):
        evs = []
        for k in reads:
            r = self.res(k)
            if r.w is not None:
                evs.append(r.w)
        for k in writes:
            r = self.res(k)
            if r.w is not None:
                evs.append(r.w)
            evs.extend(r.rs)
        return evs

    def _commit(self, ev, reads, writes):
        for k in reads:
            self.res(k).rs.append(ev)
        for k in writes:
            r = self.res(k)
            r.w, r.rs = ev, []

    def op(self, eng, fn, reads=(), writes=()):
        E = self.engs[eng]
        self._wait_for(E, self._deps(reads, writes))
        rec = ["i", fn, None, 0]
        E.ops.append(rec)
        E.inst_rec.append(rec)
        idx = E.n_inst
        E.n_inst += 1
        E.last_compute = idx
        clk = dict(E.clock)
        ev = _Ev(eng, idx, None, clk)
        self._commit(ev, reads, writes)
        return ev

    def dma(self, queue, slot, fn, reads=(), writes=()):
        E = self.engs[queue]
        S = self.slot(slot) if isinstance(slot, str) else slot
        self._wait_for(E, self._deps(reads, writes))
        S.total += 16
        rec = ["i", fn, S.sem, 16]
        E.ops.append(rec)
        E.inst_rec.append(rec)
        E.n_inst += 1
        ev = _Ev(S.name, -1, S.total, dict(E.clock))
        self._commit(ev, reads, writes)
        return ev

    def barrier(self):
        evs = []
        for E in self.engs.values():
            if E.last_compute is not None:
                ev = _Ev(E.name, E.last_compute, None, dict(E.clock))
                self._value(ev)
                evs.append(ev)
        for S in self.slots.values():
            if S.total:
                evs.append(_Ev(S.name, -1, S.total, {}))
        for E in self.engs.values():
            self._wait_for(E, evs)
        self.resd = {}

    def wait_all_dma(self, eng, slots):
        E = self.engs[eng]
        evs = [_Ev(self.slots[s].name, -1, self.slots[s].total, {}) for s in slots if self.slots[s].total]
        self._wait_for(E, evs)

    def emit(self, block):
        def replay(E):
            def run(e):
                for rec in E.ops:
                    if rec[0] == "w":
                        e.wait_ge(rec[1], rec[2])
                    else:
                        ins = rec[1](e)
                        if rec[2] is not None:
                            ins.then_inc(rec[2], rec[3])
            return run
        block.tensor(replay(self.engs["pe"]))
        block.scalar(replay(self.engs["act"]))
        block.vector(replay(self.engs["dve"]))
        block.gpsimd(replay(self.engs["pool"]))
        block.sync(replay(self.engs["sp"]))


D = 1024
S_LEN = 2048
NB = 8
DEPTH = 2
DFF = 2816
NFC = DFF // 128
NKC = D // 128
NT = S_LEN // 512
NT128 = S_LEN // 128
RMS_EPS = 1e-6
LN_EPS = 1e-5
N_IN = 7176
FFN_GROUPS = [(0, 6), (6, 12), (12, 17), (17, 22)]

W_X = 0
W_H = W_X + 16384
W_CONST = W_H + 8192
W_PT = W_CONST + 3072
W_W = W_PT + 1024
W_N = W_W + 9216
W_END = W_N + 12288
ARENA_WORDS = W_END


class K:
    def __init__(self, nc, S, arena, ps, dram):
        self.nc, self.S, self.arena, self.ps, self.d = nc, S, arena, ps, dram
        self.ps_rr = 0

    def f32(self, off, n):
        return self.arena[:, off:off + n]

    def bf(self, off, n):
        return self.arena[:, off:off + (n + 1) // 2].bitcast(BF16)

    def bank(self, b):
        return self.ps[:, b * 512:(b + 1) * 512]

    def X(self, c, t):
        o = W_X + c * S_LEN + t * 512
        return self.arena[:, o:o + 512]

    def H(self, c, t0=0, n=S_LEN):
        return self.bf(W_H + c * (S_LEN // 2), S_LEN)[:, t0:t0 + n]

    def PT(self, i):
        return self.bf(W_PT + i * 256, 512)


def build_consts(k):
    S, d = k.S, k.d
    o = W_CONST
    k.ident = k.f32(o, 128); o += 128
    k.ones_bf = k.bf(o, 128); o += 64
    k.tri_bf = k.bf(o, 128); o += 64
    k.normw = k.f32(o, 7 * 8); o += 56
    k.const_end = o
    S.dma("sp", "c0", lambda e: e.dma_start(out=k.ident, in_=d["ident"][:, :]), writes=["ident"])
    S.dma("pool", "c1", lambda e: e.dma_start(out=k.ones_bf, in_=d["ones"][:, :]), writes=["ones"])
    S.dma("pool", "c1", lambda e: e.dma_start(out=k.tri_bf, in_=d["tri"][:, :]), writes=["tri"])
    S.dma("sp", "c0", lambda e: e.dma_start(out=k.normw, in_=d["normw"][:, :]), writes=["normw"])


def load_x(k):
    S, d = k.S, k.d
    for c in range(NKC):
        o = W_X + c * S_LEN
        S.dma("sp", "xld", lambda e, c=c, o=o: e.dma_start(out=k.arena[:, o:o + S_LEN], in_=d["xT"][:, c, :]),
              writes=[("X", c, t) for t in range(NT)])


def rmsnorm(k, widx, out_mode):
    S = k.S
    R0 = W_N + 8192
    for t in range(NT):
        for c in range(NKC):
            pt = k.PT((t * NKC + c) % 4)
            key = ("PT", (t * NKC + c) % 4)
            S.op("act", lambda e, pt=pt, c=c, t=t: e.activation(out=pt, in_=k.X(c, t), func=AF.Square),
                 reads=[("X", c, t)], writes=[key])
            S.op("pe", lambda e, pt=pt, c=c, t=t: e.matmul(k.bank(t), k.ones_bf, pt, start=(c == 0), stop=(c == NKC - 1)),
                 reads=[key, "ones"], writes=[("ps", t)])
    for t in range(NT):
        R = k.f32(R0 + t * 512, 512)
        S.op("act", lambda e, R=R, t=t: e.activation(out=R, in_=k.bank(t), func=AF.Sqrt, bias=RMS_EPS, scale=1.0 / D),
             reads=[("ps", t)], writes=[("R", t)])
        S.op("dve", lambda e, R=R: e.reciprocal(R, R), reads=[("R", t)], writes=[("R", t)])
        for c in range(NKC):
            wcol = k.normw[:, widx * 8 + c:widx * 8 + c + 1]
            if out_mode == "H":
                S.op("dve", lambda e, R=R, c=c, t=t, wcol=wcol: e.scalar_tensor_tensor(
                    out=k.H(c, t * 512, 512), in0=k.X(c, t), scalar=wcol, in1=R, op0=ALU.mult, op1=ALU.mult),
                    reads=[("X", c, t), ("R", t), "normw"], writes=[("H", c, t)])
            else:
                S.op("dve", lambda e, R=R, c=c, t=t, wcol=wcol: e.scalar_tensor_tensor(
                    out=k.X(c, t), in0=k.X(c, t), scalar=wcol, in1=R, op0=ALU.mult, op1=ALU.mult),
                    reads=[("X", c, t), ("R", t), "normw"], writes=[("X", c, t)])


def ffn(k, l, which):
    S, d = k.S, k.d
    gu_d = d[f"ffn{which}_gu"]
    wd_d = d[f"ffn{which}_wd"]
    GU = [k.bf(W_W + i * 1024, 2048) for i in range(3)]
    WD = [k.bf(W_W + 3072 + i * 3072, 6 * 1024) for i in range(2)]
    A = [k.bf(W_N + i * 1024, 2048) for i in range(7)]
    SG = [k.f32(W_N + 7168 + i * 512, 512) for i in range(2)]

    def load_gu(c):
        s = c % 3
        S.dma("pool", f"gu{s}", lambda e: e.dma_start(out=GU[s], in_=gu_d[l, c, :, :]), writes=[("GU", s)])

    def load_wd(g):
        s = g % 2
        c0, c1 = FFN_GROUPS[g]
        for c in range(c0, c1):
            S.dma("pool", f"wd{s}", lambda e, c=c: e.dma_start(out=WD[s][:, (c - c0) * 1024:(c - c0 + 1) * 1024], in_=wd_d[l, c, :, :]),
                  writes=[("WD", s)])

    def gu_chunk(c):
        s = c % 3
        for t in range(NT):
            pr = (c * NT + t) % 3
            bg, bu = 2 * pr, 2 * pr + 1
            for g, b in ((0, bg), (1, bu)):
                for kc in range(NKC):
                    S.op("pe", lambda e, g=g, b=b, kc=kc, t=t: e.matmul(
                        k.bank(b), GU[s][:, (g * 8 + kc) * 128:(g * 8 + kc + 1) * 128], k.H(kc, t * 512, 512),
                        start=(kc == 0), stop=(kc == NKC - 1)),
                        reads=[("GU", s), ("H", kc, t)], writes=[("ps", b)])
            sg = SG[(c * NT + t) % 2]
            sgk = ("SG", (c * NT + t) % 2)
            S.op("act", lambda e, sg=sg, bg=bg: e.activation(out=sg, in_=k.bank(bg), func=AF.Silu),
                 reads=[("ps", bg)], writes=[sgk])
            S.op("dve", lambda e, sg=sg, bu=bu, t=t: e.tensor_tensor(
                out=A[c % 7][:, t * 512:(t + 1) * 512], in0=sg, in1=k.bank(bu), op=ALU.mult),
                reads=[sgk, ("ps", bu)], writes=[("A", c % 7, t)])

    dn_cnt = [0]

    def down(g):
        s = g % 2
        c0, c1 = FFN_GROUPS[g]
        for t in range(NT):
            for dd in range(NKC):
                b = 6 + dn_cnt[0] % 2
                dn_cnt[0] += 1
                for c in range(c0, c1):
                    S.op("pe", lambda e, b=b, c=c, dd=dd, t=t: e.matmul(
                        k.bank(b), WD[s][:, (c - c0) * 1024 + dd * 128:(c - c0) * 1024 + (dd + 1) * 128],
                        A[c % 7][:, t * 512:(t + 1) * 512], start=(c == c0), stop=(c == c1 - 1)),
                        reads=[("WD", s), ("A", c % 7, t)], writes=[("ps", b)])
                S.op("dve", lambda e, b=b, dd=dd, t=t: e.scalar_tensor_tensor(
                    out=k.X(dd, t), in0=k.bank(b), scalar=0.5, in1=k.X(dd, t), op0=ALU.mult, op1=ALU.add),
                    reads=[("ps", b), ("X", dd, t)], writes=[("X", dd, t)])

    for c in range(3):
        load_gu(c)
    load_wd(0)
    load_wd(1)
    grp_of = {}
    for g, (c0, c1) in enumerate(FFN_GROUPS):
        for c in range(c0, c1):
            grp_of[c] = g
    for c in range(NFC):
        gu_chunk(c)
        if c + 3 < NFC:
            load_gu(c + 3)
        g = grp_of[c]
        if c == FFN_GROUPS[g][0] and g > 0:
            down(g - 1)
            if g + 1 < len(FFN_GROUPS):
                load_wd(g + 1)
    down(len(FFN_GROUPS) - 1)


def store_out(k):
    S, d = k.S, k.d
    for c in range(NKC):
        o = W_X + c * S_LEN
        S.dma("sp", "ost", lambda e, c=c, o=o: e.dma_start(out=d["outT"][:, c, :], in_=k.arena[:, o:o + S_LEN]),
              reads=[("X", c, t) for t in range(NT)])
    S.wait_all_dma("sp", ["ost"])


U = 4096
W_SCR = W_END + 64
ARENA_WORDS = W_END + 2560
SB = W_SCR + 64
NEG_BIG = -30000.0
C_DQ, C_DK, C_DV = 0, 512, 1024
C_FQ, C_FK, C_FV, C_FF = 1536, 2048, 2560, 3072
C_MX, C_MZ, C_G = 3080, 3592, 4104


def DU(i):
    return W_X + i * U


def NU(i):
    return W_N + i * U


def fm(k, off, c, t0=0, n=S_LEN):
    return k.bf(off + c * (S_LEN // 2), S_LEN)[:, t0:t0 + n]


def next_bank(k):
    b = k.ps_rr % 8
    k.ps_rr += 1
    return b


def col(k):
    i = getattr(k, "_col_rr", 0)
    k._col_rr = i + 1
    i %= 48
    return k.arena[:, W_SCR + i:W_SCR + i + 1], ("col", i)


def evac(k, dst, src, src_key, dst_key, scale=None, func=None, eng=None):
    S = k.S
    if eng is None:
        k._ev_rr = getattr(k, "_ev_rr", 0) + 1
        eng = "act" if (func is not None or k._ev_rr % 2 == 0) else "dve"
    if eng == "act":
        f = func if func is not None else AF.Copy
        sc = 1.0 if scale is None else scale
        S.op("act", lambda e: e.activation(out=dst, in_=src, func=f, scale=sc), reads=[src_key], writes=[dst_key])
    else:
        if scale is None:
            S.op("dve", lambda e: e.tensor_copy(dst, src), reads=[src_key], writes=[dst_key])
        else:
            S.op("dve", lambda e: e.tensor_scalar(dst, src, scale, None, ALU.mult), reads=[src_key], writes=[dst_key])


def wcol_jobs(k, l, jobs):
    S, d = k.S, k.d

    def load(i):
        c0, nc_, _ = jobs[i]
        s = i % 2
        tile = k.bf(W_W + s * 2048, 4096).rearrange("p (k c) -> p k c", k=8)
        S.dma("pool", f"wc{s}", lambda e: e.dma_start(out=tile[:, :, 0:nc_], in_=d["w_in"][l, :, :, c0:c0 + nc_]),
              writes=[("WC", s)])
        return tile

    tiles = {}
    for i in range(min(2, len(jobs))):
        tiles[i] = load(i)
    for i in range(len(jobs)):
        jobs[i][2](tiles[i], ("WC", i % 2))
        if i + 2 < len(jobs):
            tiles[i + 2] = load(i + 2)


def h_fm(dst_off, scale=None, func=None):
    def mk(k, c_base, nchunks=4):
        def handler(W, wkey):
            S = k.S
            for m in range(nchunks):
                for t in range(NT):
                    b = next_bank(k)
                    for kc in range(NKC):
                        S.op("pe", lambda e, b=b, m=m, kc=kc, t=t: e.matmul(
                            k.bank(b), W[:, kc, m * 128:(m + 1) * 128], k.H(kc, t * 512, 512),
                            start=(kc == 0), stop=(kc == NKC - 1)),
                            reads=[wkey, ("H", kc, t)], writes=[("ps", b)])
                    evac(k, fm(k, dst_off, c_base + m, t * 512, 512), k.bank(b), ("ps", b),
                         ("fm", dst_off, c_base + m, t), scale=scale, func=func)
        return handler
    return mk


def h_tm(k, vbuf_fn, nh, dv):
    def handler(W, wkey):
        S = k.S
        for tt in range(NT128):
            b = next_bank(k)
            for kc in range(NKC):
                S.op("pe", lambda e, b=b, kc=kc, tt=tt: e.matmul(
                    k.bank(b), k.H(kc, tt * 128, 128), W[:, kc, 0:512],
                    start=(kc == 0), stop=(kc == NKC - 1)),
                    reads=[wkey, ("H", kc, tt // 4)], writes=[("ps", b)])
            dst = vbuf_fn(tt)[:, :, 0:dv]
            src = k.bank(b).rearrange("p (h e) -> p h e", h=nh)
            evac(k, dst, src, ("ps", b), ("V", tt))
    return handler


class AttnPass:
    def __init__(self, k, *, qf, kf, aqf, akf, bias_fn, vf, vkey, dv, mode, fin, sbanks, obanks, scale=1.0, augkey=None, pre=None):
        self.__dict__.update(locals())
        self.state = {}
        self.per_bank = 2 if dv == 128 else 4

    def blocks(self):
        return [(self, j, i) for j in range(4) for i in range(4 * j + 4)]

    def O(self, j, r):
        pb = self.per_bank
        ob = self.obanks[j % len(self.obanks)][r // pb]
        c0 = (r % pb) * (self.dv + 1)
        return self.k.bank(ob)[:, c0:c0 + self.dv + 1], ("ps", ob)

    def emit_s(self, j, i):
        k, S = self.k, self.k.S
        if self.pre is not None:
            pl = self.pre if isinstance(self.pre, list) else [self.pre]
            self.pre = None
            for f_ in pl:
                f_()
        r0 = max(0, i - 4 * j)
        n0 = r0 * 128
        ncol = 512 - n0
        diag = i >= 4 * j
        k._blk = getattr(k, "_blk", 0) + 1
        pti = k._blk % 4
        pt = k.PT(pti)
        ptk = ("PT", pti)
        if self.mode == "exp":
            sb = self.sbanks[k._blk % len(self.sbanks)]
            has_aug = self.akf is not None
            S.op("pe", lambda e: e.matmul(k.bank(sb)[:, n0:512], self.kf(i), self.qf(j * 512 + n0, ncol),
                                          start=True, stop=(not has_aug and not diag)),
                 reads=([self.augkey] if (self.augkey is not None and not has_aug) else []), writes=[("ps", sb)])
            if has_aug:
                S.op("pe", lambda e: e.matmul(k.bank(sb)[:, n0:512], self.akf(i), self.aqf(j, n0, ncol),
                                              start=False, stop=(not diag)),
                     reads=[self.augkey], writes=[("ps", sb)])
            if diag:
                S.op("pe", lambda e: e.matmul(k.bank(sb)[:, n0:n0 + 128], k.ident_bf, k.negtri_bf, start=False, stop=True),
                     reads=["identbf", "negtri"], writes=[("ps", sb)])
            bias = float(self.bias_fn(i, j)) if self.bias_fn is not None else 0.0
            S.op("act", lambda e: e.activation(out=pt[:, n0:512], in_=k.bank(sb)[:, n0:512], func=AF.Exp, bias=bias, scale=1.0),
                 reads=[("ps", sb)], writes=[ptk])
        else:
            pr = self.sbanks[k._blk % len(self.sbanks)]
            sb, eb = pr
            S.op("pe", lambda e: e.matmul(k.bank(sb)[:, n0:512], self.kf(i), self.qf(j * 512 + n0, ncol), start=True, stop=True),
                 reads=[], writes=[("ps", sb)])
            S.op("pe", lambda e: e.matmul(k.bank(eb)[:, n0:512], self.akf(i), self.aqf(j, n0, ncol), start=True, stop=(not diag)),
                 reads=[self.augkey], writes=[("ps", eb)])
            if diag:
                S.op("pe", lambda e: e.matmul(k.bank(eb)[:, n0:n0 + 128], k.ident_bf, k.negtri_bf, start=False, stop=True),
                     reads=["identbf", "negtri"], writes=[("ps", eb)])
            eti = k._blk % 2
            et = k.f32(SB + eti * 512, 512)
            S.op("act", lambda e: e.activation(out=et[:, n0:512], in_=k.bank(eb)[:, n0:512], func=AF.Exp),
                 reads=[("ps", eb)], writes=[("ET", eti)])
            sc = self.scale
            S.op("dve", lambda e: e.scalar_tensor_tensor(out=pt[:, n0:512], in0=k.bank(sb)[:, n0:512], scalar=sc,
                                                         in1=et[:, n0:512], op0=ALU.mult, op1=ALU.mult),
                 reads=[("ps", sb), ("ET", eti)], writes=[ptk])
        self.state[(j, i)] = (pt, ptk, r0)

    def emit_pv(self, j, i):
        k, S = self.k, self.k.S
        pt, ptk, r0 = self.state.pop((j, i))
        for r in range(r0, 4):
            o, okey = self.O(j, r)
            last = (i == 4 * j + r)
            S.op("pe", lambda e, r=r, o=o, last=last: e.matmul(o, pt[:, r * 128:(r + 1) * 128], self.vf(i), start=(i == 0 and r % self.per_bank == 0), stop=last, skip_group_check=True),
                 reads=[ptk, self.vkey], writes=[okey])
            if last:
                self.fin(j, r, o, okey)


def run_blocks(k, blocks, L=2):
    n = len(blocks)
    for idx in range(n + L):
        if idx < n:
            p, j, i = blocks[idx]
            p.emit_s(j, i)
        if idx >= L:
            p, j, i = blocks[idx - L]
            p.emit_pv(j, i)
        tick(k)
    flush_deferred(k)


def defer(k, n, fn):
    k._dq = getattr(k, "_dq", [])
    k._dq.append([n, fn])


def tick(k):
    dq = getattr(k, "_dq", [])
    k._dq = []
    keep = []
    for it in dq:
        it[0] -= 1
        if it[0] <= 0:
            it[1]()
        else:
            keep.append(it)
    k._dq = keep + k._dq


def flush_deferred(k):
    while getattr(k, "_dq", []):
        dq = k._dq
        k._dq = []
        for it in dq:
            it[1]()


def transpose_to_fm(k, src, src_key, dst, dst_key, post=None):
    S = k.S
    q = getattr(k, "_tq", 0)
    k._tq = q + 1
    tb = k.tbanks[q % len(k.tbanks)]
    pq = k.bank(tb)[:, 0:128]
    pkey = ("ps", tb)
    S.op("pe", lambda e: e.transpose(pq, src, k.ident), reads=[src_key, "ident"], writes=[pkey])
    if post is None:
        evac(k, dst, pq, pkey, dst_key)
    else:
        post(pq, pkey)


def softplus_neg(k, T, nh, banks, negb_col, key):
    S = k.S
    for t in range(NT):
        b = banks[t]
        S.op("act", lambda e, b=b, t=t: e.activation(out=T[0:nh, t * 512:(t + 1) * 512], in_=k.bank(b)[0:nh, :],
                                                    func=AF.Exp, bias=negb_col, scale=-1.0),
             reads=[("ps", b), "gcols"], writes=[key])
    S.op("act", lambda e: e.activation(out=T[0:nh, :], in_=T[0:nh, :], func=AF.Ln, bias=1.0, scale=1.0),
         reads=[key], writes=[key])


def build_aug(k, kvec, kkey, qvec, qkey, nh, spl_off, tag):
    S, d = k.S, k.d
    dk, dq = d["augk_" + tag], d["augq_" + tag]
    SPL = [k.bf(spl_off + i * 1024, 2048) for i in range(4)]
    ones = SPL[3]
    S.op("dve", lambda e: e.memset(ones[0:nh, :], 1.0), writes=[("SPL", 3)])
    for r in range(3):
        S.dma("sp", "augw", lambda e, r=r: e.dma_start(out=dk[0:nh, 3 + r, :], in_=ones[0:nh, :]), reads=[("SPL", 3)], writes=[("augd", tag)])
        S.dma("sp", "augw", lambda e, r=r: e.dma_start(out=dq[0:nh, r, :], in_=ones[0:nh, :]), reads=[("SPL", 3)], writes=[("augd", tag)])
    cnt = 0
    for vec, vkey, dram, r0 in ((kvec, kkey, dk, 0), (qvec, qkey, dq, 3)):
        for r in range(3):
            si = cnt % 3
            cnt += 1
            spl = SPL[si]
            S.op("dve", lambda e, spl=spl, vec=vec: e.tensor_copy(spl[0:nh, :], vec[0:nh, :]), reads=[vkey], writes=[("SPL", si)])
            if r < 2:
                S.op("dve", lambda e, spl=spl, vec=vec: e.tensor_tensor(out=vec[0:nh, :], in0=vec[0:nh, :], in1=spl[0:nh, :], op=ALU.subtract),
                     reads=[vkey, ("SPL", si)], writes=[vkey])
            S.dma("sp", "augw", lambda e, spl=spl, dram=dram, rr=r0 + r: e.dma_start(out=dram[0:nh, rr, :], in_=spl[0:nh, :]),
                  reads=[("SPL", si)], writes=[("augd", tag)])


def aug_views(k, slot):
    ak = k.bf(W_W + 5120 + slot * 2048, 2048)
    aq = k.bf(W_W + 5120 + slot * 2048 + 1024, 2048)
    akf = lambda i: ak[0:6, i * 128:(i + 1) * 128]
    aqf = lambda j, n0, n: aq[0:6, j * 512 + n0:j * 512 + n0 + n]
    return akf, aqf


def load_aug(k, tag, h, slot):
    S, d = k.S, k.d
    ak = k.bf(W_W + 5120 + slot * 2048, 2048)
    aq = k.bf(W_W + 5120 + slot * 2048 + 1024, 2048)
    S.dma("sp", f"augl{slot}", lambda e: e.dma_start(out=ak[0:6, :], in_=d["augk_" + tag][h, :, :]), reads=[("augd", tag)], writes=[("AUGS", slot)])
    S.dma("sp", f"augl{slot}", lambda e: e.dma_start(out=aq[0:6, :], in_=d["augq_" + tag][h, :, :]), reads=[("augd", tag)], writes=[("AUGS", slot)])


HB_OFF = [W_W + 0, W_W + 2048, W_W + 5120, W_W + 7168]


def hb_views(k, slot):
    return k.bf(HB_OFF[slot], 2048), k.bf(HB_OFF[slot] + 1024, 2048)


def hb_init(k):
    S = k.S
    for s_ in range(4):
        qb, kb = hb_views(k, s_)
        S.op("dve", lambda e, qb=qb: e.memset(qb, 0.0), writes=[("HB", s_)])
        S.op("dve", lambda e, kb=kb: e.memset(kb, 0.0), writes=[("HB", s_)])


def hb_build(k, slot, par, q_src, k_src, augq_ap, augk_ap, R, queue):
    S = k.S
    qb, kb = hb_views(k, slot)
    r0 = par * 64
    a0 = 64 if par == 0 else 0
    S.op("pool", lambda e: e.tensor_copy(qb[r0:r0 + 64, :], q_src[r0:r0 + 64, :]), writes=[("HB", slot)])
    S.op("pool", lambda e: e.tensor_copy(kb[r0:r0 + 64, :], k_src[r0:r0 + 64, :]), writes=[("HB", slot)])
    S.dma(queue, f"hb{queue}{slot}", lambda e: e.dma_start(out=qb[a0:a0 + R, :], in_=augq_ap), writes=[("HB", slot)])
    S.dma(queue, f"hb{queue}{slot}", lambda e: e.dma_start(out=kb[a0:a0 + R, :], in_=augk_ap), writes=[("HB", slot)])


def layer_consts(k, l):
    S, d = k.S, k.d
    o = k.const_end
    k.gb = k.f32(o, 24); o += 24
    k.convw = k.f32(o, 16); o += 16
    k.convb = k.f32(o, 4); o += 4
    k.skipc = k.f32(o, 4); o += 4
    k.gcols = k.f32(o, 8); o += 8
    k.lamt = k.f32(o, 256); o += 256
    k.lamc = k.f32(o, 8); o += 8
    k.subln = k.f32(o, 128); o += 128
    k.normB = k.f32(o, 512); o += 512
    k.AKd = k.bf(o, 4 * 128); o += 256
    k.AQd = k.bf(o, 4 * 512); o += 1024
    k.ident_bf = k.bf(o, 128); o += 64
    k.negtri_bf = k.bf(o, 128); o += 64
    assert o <= W_CONST + 3072, o
    S.dma("sp", "lc", lambda e: e.dma_start(out=k.gb, in_=d["gbcols"][l, :, :]), writes=["lconst"])
    S.dma("sp", "lc", lambda e: e.dma_start(out=k.convw, in_=d["convw"][l, :, :]), writes=["lconst"])
    S.dma("sp", "lc", lambda e: e.dma_start(out=k.convb, in_=d["convb"][l, :, :]), writes=["lconst"])
    S.dma("sp", "lc", lambda e: e.dma_start(out=k.skipc, in_=d["skipc"][l, :, :]), writes=["lconst"])
    S.dma("sp", "lc", lambda e: e.dma_start(out=k.gcols[0:8, :], in_=d["gcols"][l, :, :]), writes=["gcols"])
    S.dma("sp", "lc", lambda e: e.dma_start(out=k.lamt, in_=d["difflam"][l:l + 1, :].partition_broadcast(128)), writes=["lamt"])
    S.dma("sp", "lc", lambda e: e.dma_start(out=k.subln, in_=d["diff_subln"][l:l + 1, :].partition_broadcast(128)), writes=["subln"])
    S.dma("sp", "lc", lambda e: e.dma_start(out=k.normB, in_=d["mlstm_norm"][l:l + 1, :].partition_broadcast(128)), writes=["normB"])
    S.op("dve", lambda e: e.tensor_scalar(k.gcols[0:8, 4:5], k.gcols[0:8, 0:1], -1.0, None, ALU.mult), reads=["gcols"], writes=["gcols"])
    S.op("dve", lambda e: e.tensor_scalar(k.gcols[0:8, 5:6], k.gcols[0:8, 2:3], -1.0, None, ALU.mult), reads=["gcols"], writes=["gcols"])
    if l == 0:
        S.dma("pool", "lc2", lambda e: e.dma_start(out=k.AKd[0:3, :], in_=d["alibi_k"][:, :]), writes=["alibi"])
        S.dma("pool", "lc2", lambda e: e.dma_start(out=k.AQd[0:3, :], in_=d["alibi_q"][:, :]), writes=["alibi"])
        S.dma("pool", "lc2", lambda e: e.dma_start(out=k.ident_bf, in_=d["ident"][:, :]), writes=["identbf"])
        S.dma("pool", "lc2", lambda e: e.dma_start(out=k.negtri_bf, in_=d["negtri"][:, :]), writes=["negtri"])
    import math
    lam_init = 0.8 - 0.6 * math.exp(-0.3 * l)
    lt, lc = k.lamt, k.lamc
    S.op("dve", lambda e: e.tensor_tensor(out=lt[:, 0:64], in0=lt[:, 0:64], in1=lt[:, 64:128], op=ALU.mult), reads=["lamt"], writes=["lamt"])
    S.op("dve", lambda e: e.tensor_tensor(out=lt[:, 128:192], in0=lt[:, 128:192], in1=lt[:, 192:256], op=ALU.mult), reads=["lamt"], writes=["lamt"])
    S.op("dve", lambda e: e.reduce_sum(out=lc[:, 0:1], in_=lt[:, 0:64], axis=AX.X), reads=["lamt"], writes=["lamc"])
    S.op("dve", lambda e: e.reduce_sum(out=lc[:, 1:2], in_=lt[:, 128:192], axis=AX.X), reads=["lamt"], writes=["lamc"])
    S.op("act", lambda e: e.activation(out=lc[:, 2:4], in_=lc[:, 0:2], func=AF.Exp), reads=["lamc"], writes=["lamc"])
    S.op("dve", lambda e: e.tensor_tensor(out=lc[:, 4:5], in0=lc[:, 3:4], in1=lc[:, 2:3], op=ALU.subtract), reads=["lamc"], writes=["lamc"])
    S.op("dve", lambda e: e.tensor_scalar(lc[:, 5:6], lc[:, 4:5], -lam_init, None, ALU.add), reads=["lamc"], writes=["neglam"])
    S.op("dve", lambda e: e.tensor_scalar(k.subln, k.subln, 1.0 - lam_init, None, ALU.mult), reads=["subln"], writes=["subln"])
    k.neglam = lc[:, 5:6]


def diff_branch(k, l):
    S = k.S
    QT, KT, VO, OD = DU(0), DU(1), DU(2), NU(0)
    V = k.bf(VO, 16 * 4 * 129).rearrange("p (t h e) -> p t h e", t=16, h=4)
    S.op("dve", lambda e: e.memset(k.bf(VO, 16 * 4 * 129), 1.0), writes=[("V", "all")])
    S.barrier()
    wcol_jobs(k, l, [
        (C_DQ, 512, h_fm(QT)(k, 0)),
        (C_DK, 512, h_fm(KT, scale=0.125)(k, 0)),
        (C_DV, 512, h_tm(k, lambda tt: V[:, tt, :, :], 4, 128)),
    ])
    S.barrier()
    k.tbanks = [2, 3]
    slopes = [2.0 ** (-8.0 * (h + 1) / 4) for h in range(4)]
    A1 = [[k.f32(SB + (p * 4 + r) * 128, 128) for r in range(4)] for p in range(2)]
    TB = SB + 1024
    blocks = []
    pres = []
    first_pass = []
    hb_init(k)
    for h in range(4):
        passes = []
        for c in range(2):
            slot = c + 2 * (h % 2)
            qb, kb = hb_views(k, slot)

            def qf(t0, n, qb=qb):
                return qb[:, t0:t0 + n]

            def kf(i, kb=kb):
                return kb[:, i * 128:(i + 1) * 128]

            pres.append(lambda h=h, c=c, slot=slot: hb_build(k, slot, c, fm(k, QT, h), fm(k, KT, h),
                                                              k.d["alibi_qf"][h, :, :], k.d["alibi_kf"][h, :, :], 3, "pool"))

            def bias_fn(i, j, h=h):
                return slopes[h] * (128.0 * i - 512.0 * j)

            def vf(i, h=h):
                return V[:, i, h, :]

            if c == 0:
                def fin(j, r, o, okey, h=h):
                    rc, rk = col(k)
                    a1 = A1[j % 2][r]
                    S.op("dve", lambda e: e.reciprocal(rc, o[:, 128:129]), reads=[okey], writes=[rk])
                    S.op("dve", lambda e: e.tensor_scalar(a1, o[:, 0:128], rc, None, ALU.mult), reads=[okey, rk], writes=[("A1", j % 2, r)])
            else:
                def fin(j, r, o, okey, h=h):
                    rc, rk = col(k)
                    sc, sk = col(k)
                    k._fp = getattr(k, "_fp", 0) + 1
                    n = k._fp
                    tmp = k.f32(TB + (n % 5) * 128, 128)
                    sq = k.f32(TB + 640 + (n % 2) * 128, 128)
                    ot = k.f32(TB + 896 + (n % 4) * 128, 128)
                    tk_, sqk, otk = ("TMP", n % 5), ("SQ", n % 2), ("OT", n % 4)
                    a1 = A1[j % 2][r]
                    tt = 4 * j + r
                    S.op("dve", lambda e: e.reciprocal(rc, o[:, 128:129]), reads=[okey], writes=[rk])
                    S.op("dve", lambda e: e.tensor_scalar(tmp, o[:, 0:128], rc, None, ALU.mult), reads=[okey, rk], writes=[tk_])
                    S.op("dve", lambda e: e.scalar_tensor_tensor(out=tmp, in0=tmp, scalar=k.neglam, in1=a1, op0=ALU.mult, op1=ALU.add),
                         reads=[tk_, ("A1", j % 2, r), "neglam"], writes=[tk_])
                    S.op("dve", lambda e: e.tensor_tensor(out=sq, in0=tmp, in1=tmp, op=ALU.mult), reads=[tk_], writes=[sqk])
                    S.op("dve", lambda e: e.reduce_sum(out=sc, in_=sq, axis=AX.X), reads=[sqk], writes=[sk])

                    def stage_b():
                        S.op("act", lambda e: e.activation(out=sc, in_=sc, func=AF.Ln, bias=RMS_EPS, scale=1.0 / 128), reads=[sk], writes=[sk])
                        S.op("act", lambda e: e.activation(out=sc, in_=sc, func=AF.Exp, scale=-0.5), reads=[sk], writes=[sk])

                    def stage_c():
                        S.op("dve", lambda e: e.scalar_tensor_tensor(out=ot, in0=tmp, scalar=sc, in1=k.subln, op0=ALU.mult, op1=ALU.mult),
                             reads=[tk_, sk, "subln"], writes=[otk])
                        defer(k, 2, lambda: transpose_to_fm(k, ot, otk, fm(k, OD, h, tt * 128, 128), ("fm", OD, h, tt)))
                    defer(k, 2, stage_b)
                    defer(k, 3, stage_c)
            passes.append(AttnPass(k, qf=qf, kf=kf, aqf=None, akf=None, bias_fn=bias_fn, vf=vf, vkey=("V", "x"), dv=128,
                                   mode="exp", fin=fin, sbanks=[0, 1], obanks=[(4, 5)] if c == 0 else [(6, 7)], augkey=("HB", slot)))
        first_pass.append(passes[0])
        b0, b1 = passes[0].blocks(), passes[1].blocks()
        for j in range(4):
            blocks += [b for b in b0 if b[1] == j]
            blocks += [b for b in b1 if b[1] == j]
    for u in range(4):
        first_pass[u].pre = (pres[0:4] if u == 0 else (pres[2 * u + 2:2 * u + 4] if u < 3 else None))
    run_blocks(k, blocks)
    S.barrier()


def fox_branch(k, l):
    S, d = k.S, k.d
    QT, KT, VO, OF = DU(0), DU(1), DU(2), NU(2)
    V = k.bf(VO, 16 * 8 * 65).rearrange("p (t h e) -> p t h e", t=16, h=8)
    WFF = k.bf(W_W + 4096, 64).rearrange("p (k c) -> p k c", k=8)
    S.op("dve", lambda e: e.memset(k.bf(VO, 16 * 8 * 65), 1.0), writes=[("V", "all")])
    S.dma("pool", "wff", lambda e: e.dma_start(out=WFF, in_=d["w_in"][l, :, :, C_FF:C_FF + 8]), writes=["WFF"])
    S.barrier()
    wcol_jobs(k, l, [
        (C_FQ, 512, h_fm(QT)(k, 0)),
        (C_FK, 512, h_fm(KT, scale=0.125)(k, 0)),
        (C_FV, 512, h_tm(k, lambda tt: V[:, tt, :, :], 8, 64)),
    ])
    for t in range(NT):
        for kc in range(NKC):
            S.op("pe", lambda e, kc=kc, t=t: e.matmul(k.bank(t)[0:8, :], WFF[:, kc, :], k.H(kc, t * 512, 512),
                                                      start=(kc == 0), stop=(kc == NKC - 1)),
                 reads=["WFF", ("H", kc, t)], writes=[("ps", t)])
    S.barrier()
    T1 = k.f32(W_W, 2048)
    T2 = k.f32(W_W + 2048, 2048)
    ONES = k.f32(W_W + 7168, 2048)
    S.op("dve", lambda e: e.memset(ONES[0:8, :], 1.0), writes=["ONES"])
    softplus_neg(k, T1, 8, [0, 1, 2, 3], k.gcols[0:8, 4:5], "T1")
    S.op("dve", lambda e: e.tensor_tensor_scan(T2[0:8, :], ONES[0:8, :], T1[0:8, :], 0.0, ALU.mult, ALU.add), reads=["T1", "ONES"], writes=["T2"])
    S.op("dve", lambda e: e.tensor_scalar(T1[0:8, :], T2[0:8, :], -1.0, None, ALU.mult), reads=["T2"], writes=["T1"])
    build_aug(k, T2, "T2", T1, "T1", 8, NU(2), "fox")
    S.barrier()
    k.tbanks = [3, 6, 7]
    OTB = [[k.f32(SB + (p * 4 + r) * 128, 128) for r in range(4)] for p in range(2)]
    blocks = []
    pres = []
    first_pass = []
    hb_init(k)
    for pr in range(4):
        passes = []
        for c in range(2):
            hd = 2 * pr + c
            slot = c + 2 * (pr % 2)
            qb, kb = hb_views(k, slot)

            def qf(t0, n, qb=qb):
                return qb[:, t0:t0 + n]

            def kf(i, kb=kb):
                return kb[:, i * 128:(i + 1) * 128]

            pres.append(lambda pr=pr, c=c, slot=slot, hd=hd: hb_build(k, slot, c, fm(k, QT, pr), fm(k, KT, pr),
                                                                      k.d["augq_fox"][hd, :, :], k.d["augk_fox"][hd, :, :], 6, "sp"))

            def vf(i, hd=hd):
                return V[:, i, hd, :]

            def fin(j, r, o, okey, pr=pr, c=c):
                rc, rk = col(k)
                ot = OTB[j % 2][r]
                otk = ("OTB", j % 2, r)
                S.op("dve", lambda e: e.reciprocal(rc, o[:, 64:65]), reads=[okey], writes=[rk])
                S.op("dve", lambda e: e.tensor_scalar(ot[:, c * 64:(c + 1) * 64], o[:, 0:64], rc, None, ALU.mult), reads=[okey, rk], writes=[otk])
                if c == 1:
                    tt = 4 * j + r
                    defer(k, 3, lambda: transpose_to_fm(k, ot, otk, fm(k, OF, pr, tt * 128, 128), ("fm", OF, pr, tt)))
            passes.append(AttnPass(k, qf=qf, kf=kf, aqf=None, akf=None, bias_fn=None, vf=vf, vkey=("V", "x"), dv=64,
                                   mode="exp", fin=fin, sbanks=[0, 1, 2], obanks=[(4,)] if c == 0 else [(5,)], augkey=("HB", slot)))
        first_pass.append(passes[0])
        b0, b1 = passes[0].blocks(), passes[1].blocks()
        for j in range(4):
            blocks += [b for b in b0 if b[1] == j]
            blocks += [b for b in b1 if b[1] == j]
    for u in range(4):
        first_pass[u].pre = (pres[0:4] if u == 0 else (pres[2 * u + 2:2 * u + 4] if u < 3 else None))
    run_blocks(k, blocks)
    S.barrier()


def mlstm_branch(k, l):
    S, d = k.S, k.d
    QT, KT, VT, MX, XC, SZ, VA = DU(0), DU(1), DU(2), DU(3), NU(0), NU(1), NU(2)
    Vall = k.bf(VA, 16 * 4 * 129).rearrange("p (t h e) -> p t h e", t=16, h=4)
    WQKV = k.bf(W_W + 4096, 1536).rearrange("p (g h e) -> p g h e", g=3, h=4)
    WIF = k.bf(W_W + 4096 + 768, 96).rearrange("p (j o) -> p j o", j=12)
    S.op("dve", lambda e: e.memset(k.bf(VA, 16 * 4 * 129), 1.0), writes=[("V", "all")])
    S.dma("pool", "wqkv", lambda e: e.dma_start(out=k.bf(W_W + 4096, 1536), in_=d["wqkv"][l, :, :]), writes=["WQKV"])
    S.dma("pool", "wqkv", lambda e: e.dma_start(out=k.bf(W_W + 4096 + 768, 96), in_=d["wif"][l, :, :]), writes=["WIF"])
    S.barrier()
    wcol_jobs(k, l, [
        (C_MX, 512, h_fm(MX)(k, 0)),
        (C_MZ, 512, h_fm(SZ, func=AF.Silu)(k, 0)),
    ])
    ACC = k.f32(SB, 2048)
    for c in range(4):
        mx = fm(k, MX, c)
        mkeys = [("fm", MX, c, t) for t in range(NT)]
        w = lambda j, c=c: k.convw[:, j * 4 + c:j * 4 + c + 1]
        S.op("dve", lambda e, mx=mx, c=c, w=w: e.tensor_scalar(ACC, mx, w(3), k.convb[:, c:c + 1], ALU.mult, ALU.add),
             reads=mkeys + ["lconst"], writes=["ACC"])
        for sh in (1, 2, 3):
            S.op("dve", lambda e, mx=mx, sh=sh, w=w: e.scalar_tensor_tensor(
                out=ACC[:, sh:], in0=mx[:, 0:S_LEN - sh], scalar=w(3 - sh), in1=ACC[:, sh:], op0=ALU.mult, op1=ALU.add),
                reads=mkeys + ["ACC", "lconst"], writes=["ACC"])
        S.op("act", lambda e, c=c: e.activation(out=fm(k, XC, c), in_=ACC, func=AF.Silu), reads=["ACC"],
             writes=[("fm", XC, c, t) for t in range(NT)])
    for h in range(4):
        for t in range(NT):
            for g, src, dst in ((0, XC, QT), (1, XC, KT), (2, MX, VT)):
                b = next_bank(k)
                S.op("pe", lambda e, b=b, g=g, src=src, h=h, t=t: e.matmul(
                    k.bank(b), WQKV[:, g, h, :], fm(k, src, h, t * 512, 512), start=True, stop=True),
                    reads=["WQKV", ("fm", src, h, t)], writes=[("ps", b)])
                evac(k, fm(k, dst, h, t * 512, 512), k.bank(b), ("ps", b), ("fm", dst, h, t))
        for tt in range(NT128):
            b = next_bank(k)
            S.op("pe", lambda e, b=b, h=h, tt=tt: e.matmul(
                k.bank(b)[:, 0:128], fm(k, MX, h, tt * 128, 128), WQKV[:, 2, h, :], start=True, stop=True),
                reads=["WQKV", ("fm", MX, h, tt // 4)], writes=[("ps", b)])
            evac(k, Vall[:, tt, h, 0:128], k.bank(b)[:, 0:128], ("ps", b), ("V", tt, h))
    S.barrier()
    for t in range(NT):
        for g, bb in ((0, t), (1, 4 + t)):
            for jj in range(12):
                src = (QT, KT, VT)[jj // 4]
                S.op("pe", lambda e, g=g, bb=bb, jj=jj, src=src, t=t: e.matmul(
                    k.bank(bb)[0:4, :], WIF[:, jj, g * 4:(g + 1) * 4], fm(k, src, jj % 4, t * 512, 512),
                    start=(jj == 0), stop=(jj == 11)), reads=["WIF"], writes=[("ps", bb)])
    T1 = k.f32(W_W, 2048)
    T2 = k.f32(W_W + 2048, 2048)
    T3 = k.f32(W_W + 5120, 2048)
    ONES = k.f32(W_W + 7168, 2048)
    S.op("dve", lambda e: e.memset(ONES[0:4, :], 1.0), writes=["ONES"])
    softplus_neg(k, T1, 4, [4, 5, 6, 7], k.gcols[0:4, 5:6], "T1")
    S.op("dve", lambda e: e.tensor_tensor_scan(T2[0:4, :], ONES[0:4, :], T1[0:4, :], 0.0, ALU.mult, ALU.add), reads=["T1", "ONES"], writes=["T2"])
    for t in range(NT):
        S.op("dve", lambda e, t=t: e.tensor_scalar(T1[0:4, t * 512:(t + 1) * 512], k.bank(t)[0:4, :], k.gcols[0:4, 1:2], None, ALU.add),
             reads=[("ps", t), "gcols", "T2"], writes=["T1"])
    S.op("dve", lambda e: e.tensor_tensor(out=T1[0:4, :], in0=T1[0:4, :], in1=T2[0:4, :], op=ALU.add), reads=["T1", "T2"], writes=["T1"])
    S.op("dve", lambda e: e.tensor_tensor_scan(T3[0:4, :], ONES[0:4, :], T1[0:4, :], 0.0, ALU.mult, ALU.max), reads=["T1", "ONES"], writes=["T3"])
    S.op("dve", lambda e: e.tensor_tensor(out=T2[0:4, :], in0=T2[0:4, :], in1=T3[0:4, :], op=ALU.subtract), reads=["T2", "T3"], writes=["T2"])
    S.op("act", lambda e: e.activation(out=T2[0:4, :], in_=T2[0:4, :], func=AF.Exp), reads=["T2"], writes=["T2"])
    EM = k.f32(W_CONST + 2700, 64)
    for tt in range(NT128):
        S.op("pe", lambda e, tt=tt: e.transpose(k.bank(3)[:, tt * 4:(tt + 1) * 4], T2[0:4, tt * 128:(tt + 1) * 128], k.ident[0:4, 0:4]),
             reads=["T2", "ident"], writes=[("ps", 3)])
    S.op("dve", lambda e: e.tensor_copy(EM, k.bank(3)[:, 0:64]), reads=[("ps", 3)], writes=["EM"])
    S.op("dve", lambda e: e.tensor_scalar(T3[0:4, :], T3[0:4, :], -1.0, None, ALU.mult), reads=["T3"], writes=["T3"])
    build_aug(k, T1, "T1", T3, "T3", 4, DU(3), "ml")
    S.barrier()
    k.tbanks = [3, 6]
    TB = SB + 1024
    blocks = []
    for h in range(4):
        akf, aqf = aug_views(k, h % 2)

        def qf(t0, n, h=h):
            return fm(k, QT, h, t0, n)

        def kf(i, h=h):
            return fm(k, KT, h, i * 128, 128)

        def vf(i, h=h):
            return Vall[:, i, h, :]

        def fin(j, r, o, okey, h=h):
            tt = 4 * j + r
            c1, k1 = col(k)
            c2, k2 = col(k)
            k._fp = getattr(k, "_fp", 0) + 1
            n = k._fp
            HH = k.f32(TB + (n % 4) * 128, 128)
            SQ = k.f32(TB + 512, 128)
            TF = k.f32(TB + 640 + (n % 2) * 128, 128)
            HN = k.f32(TB + 896 + (n % 4) * 128, 128)
            hk, sk, nk, fk = ("HH", n % 4), ("SQ", 0), ("HN", n % 4), ("TF", n % 2)
            S.op("dve", lambda e: e.tensor_copy(c2, o[:, 128:129]), reads=[okey], writes=[k2])
            S.op("dve", lambda e: e.scalar_tensor_tensor(out=c1, in0=c2, scalar=-1.0, in1=c2, op0=ALU.mult, op1=ALU.max), reads=[k2], writes=[k1])
            S.op("dve", lambda e: e.tensor_tensor(out=c1, in0=c1, in1=EM[:, tt * 4 + h:tt * 4 + h + 1], op=ALU.max), reads=[k1, "EM"], writes=[k1])
            S.op("dve", lambda e: e.reciprocal(c1, c1), reads=[k1], writes=[k1])
            S.op("dve", lambda e: e.tensor_scalar(HH, o[:, 0:128], c1, None, ALU.mult), reads=[okey, k1], writes=[hk])
            S.op("dve", lambda e: e.reduce_sum(out=c2, in_=HH, axis=AX.X), reads=[hk], writes=[k2])
            S.op("dve", lambda e: e.tensor_scalar(c2, c2, -1.0 / 128, None, ALU.mult), reads=[k2], writes=[k2])
            S.op("dve", lambda e: e.tensor_scalar(HH, HH, c2, None, ALU.add), reads=[hk, k2], writes=[hk])
            S.op("dve", lambda e: e.tensor_tensor(out=SQ, in0=HH, in1=HH, op=ALU.mult), reads=[hk], writes=[sk])
            S.op("dve", lambda e: e.reduce_sum(out=c2, in_=SQ, axis=AX.X), reads=[sk], writes=[k2])

            def stage_b():
                S.op("act", lambda e: e.activation(out=c2, in_=c2, func=AF.Ln, bias=LN_EPS, scale=1.0 / 128), reads=[k2], writes=[k2])
                S.op("act", lambda e: e.activation(out=c2, in_=c2, func=AF.Exp, scale=-0.5), reads=[k2], writes=[k2])

            def post(pq, pkey):
                xc = fm(k, XC, h, tt * 128, 128)
                sz = fm(k, SZ, h, tt * 128, 128)
                S.op("dve", lambda e: e.scalar_tensor_tensor(out=TF, in0=xc, scalar=k.skipc[:, h:h + 1], in1=pq, op0=ALU.mult, op1=ALU.add),
                     reads=[pkey, "lconst"], writes=[fk])
                S.op("dve", lambda e: e.tensor_tensor(out=sz, in0=TF, in1=sz, op=ALU.mult), reads=[fk], writes=[("fm", SZ, h, tt)])

            def stage_c():
                S.op("dve", lambda e: e.scalar_tensor_tensor(out=HN, in0=HH, scalar=c2, in1=k.normB[:, h * 128:(h + 1) * 128], op0=ALU.mult, op1=ALU.mult),
                     reads=[hk, k2, "normB"], writes=[nk])
                defer(k, 2, lambda: transpose_to_fm(k, HN, nk, None, None, post=post))
            defer(k, 2, stage_b)
            defer(k, 3, stage_c)
        ps_ = AttnPass(k, qf=qf, kf=kf, aqf=aqf, akf=akf, bias_fn=None, vf=vf, vkey=("V", "x"), dv=128, mode="mul", fin=fin,
                       sbanks=[(0, 1), (2, 7)], obanks=[(4, 5)], scale=128.0 ** -0.5, augkey=("AUGS", h % 2),
                       pre=(lambda h=h: load_aug(k, "ml", h, h % 2)))
        blocks += ps_.blocks()
    run_blocks(k, blocks, L=1)
    S.barrier()


def merge_and_out(k, l):
    S, d = k.S, k.d
    OB = [NU(0), NU(2), NU(1)]
    MG = DU(0)
    GT = [k.f32(SB + i * 512, 512) for i in range(2)]
    ACC = k.f32(SB + 1024, 512)
    PB = k.f32(SB + 1536, 512)

    def load_mp(dd):
        s = dd % 2
        tile = k.bf(W_W + s * 2304, 4608)
        S.dma("pool", f"mp{s}", lambda e: e.dma_start(out=tile, in_=d["mpack"][l, dd, :, :]), writes=[("MP", s)])
        return tile

    tiles = {0: load_mp(0), 1: load_mp(1)}
    cnt = 0
    for dd in range(NKC):
        MP = tiles[dd]
        mkey = ("MP", dd % 2)
        for t in range(NT):
            for b in range(3):
                bg, bo = next_bank(k), next_bank(k)
                for kc in range(NKC):
                    S.op("pe", lambda e, bg=bg, b=b, kc=kc, t=t, MP=MP: e.matmul(
                        k.bank(bg), MP[:, b * 1536 + kc * 128:b * 1536 + (kc + 1) * 128], k.H(kc, t * 512, 512),
                        start=(kc == 0), stop=(kc == NKC - 1)), reads=[mkey, ("H", kc, t)], writes=[("ps", bg)])
                for kc in range(4):
                    S.op("pe", lambda e, bo=bo, b=b, kc=kc, t=t, MP=MP: e.matmul(
                        k.bank(bo), MP[:, b * 1536 + 1024 + kc * 128:b * 1536 + 1024 + (kc + 1) * 128], fm(k, OB[b], kc, t * 512, 512),
                        start=(kc == 0), stop=(kc == 3)), reads=[mkey], writes=[("ps", bo)])
                gt = GT[cnt % 2]
                gk = ("GT", cnt % 2)
                cnt += 1
                gbc = k.gb[:, b * 8 + dd:b * 8 + dd + 1]
                S.op("act", lambda e, gt=gt, bg=bg, gbc=gbc: e.activation(out=gt, in_=k.bank(bg), func=AF.Sigmoid, bias=gbc, scale=1.0),
                     reads=[("ps", bg), "lconst"], writes=[gk])
                if b == 0:
                    S.op("dve", lambda e, gt=gt, bo=bo: e.tensor_tensor(out=ACC, in0=gt, in1=k.bank(bo), op=ALU.mult),
                         reads=[gk, ("ps", bo)], writes=["MACC"])
                else:
                    S.op("dve", lambda e, gt=gt, bo=bo: e.tensor_tensor(out=PB, in0=gt, in1=k.bank(bo), op=ALU.mult),
                         reads=[gk, ("ps", bo)], writes=["MPB"])
                    dst = ACC if b == 1 else fm(k, MG, dd, t * 512, 512)
                    dkey = "MACC" if b == 1 else ("fm", MG, dd, t)
                    S.op("dve", lambda e, dst=dst: e.tensor_tensor(out=dst, in0=ACC, in1=PB, op=ALU.add),
                         reads=["MACC", "MPB"], writes=[dkey] + (["MACC"] if b == 2 else []))
        if dd + 2 < NKC:
            tiles[dd + 2] = load_mp(dd + 2)
    S.barrier()
    MN = NU(0)
    for i in range(4):
        S.op("dve" if i % 2 == 0 else "pool", lambda e, i=i: e.tensor_copy(k.bf(MN + i * 2048, 4096), k.bf(MG + i * 2048, 4096)),
             writes=[("MN", i)])
    S.barrier()
    XS = [k.f32(SB + i * 512, 512) for i in range(2)]

    WOT = [k.bf(W_W + dd * 512, 1024) for dd in range(NKC)]
    for dd in range(NKC):
        S.dma("pool", "wo", lambda e, dd=dd: e.dma_start(out=WOT[dd], in_=d["wout"][l, dd, :, :]), writes=[("WO", dd)])
    cnt = 0
    for t in range(NT):
        for dd in range(NKC):
            WO = WOT[dd]
            b = next_bank(k)
            xs = XS[cnt % 2]
            xk = ("XS", cnt % 2)
            cnt += 1
            S.dma("sp", f"xs{cnt % 2}", lambda e, xs=xs, dd=dd, t=t: e.dma_start(out=xs, in_=d["xspill"][:, dd, t * 512:(t + 1) * 512]),
                  writes=[xk])
            for kc in range(NKC):
                S.op("pe", lambda e, b=b, kc=kc, t=t, WO=WO: e.matmul(
                    k.bank(b), WO[:, kc * 128:(kc + 1) * 128], fm(k, MN, kc, t * 512, 512),
                    start=(kc == 0), stop=(kc == NKC - 1)), reads=[("WO", dd)], writes=[("ps", b)])
            S.op("dve", lambda e, b=b, xs=xs, dd=dd, t=t: e.tensor_tensor(out=k.X(dd, t), in0=k.bank(b), in1=xs, op=ALU.add),
                 reads=[("ps", b), xk], writes=[("X", dd, t)])


def spill_x(k):
    S, d = k.S, k.d
    for c in range(NKC):
        o = W_X + c * S_LEN
        S.dma("sp", "xsp", lambda e, c=c, o=o: e.dma_start(out=d["xspill"][:, c, :], in_=k.arena[:, o:o + S_LEN]),
              reads=[("X", c, t) for t in range(NT)], writes=["xspill"])


def mixer(k, l, branches=("ml", "diff", "fox")):
    S = k.S
    layer_consts(k, l)
    rmsnorm(k, l * 3 + 1, "H")
    spill_x(k)
    S.barrier()
    if "ml" in branches:
        mlstm_branch(k, l)
    if "diff" in branches:
        diff_branch(k, l)
    if "fox" in branches:
        fox_branch(k, l)
    merge_and_out(k, l)


def _chunk_rows(w):
    K_, N_ = w.shape
    return w.reshape(K_ // 128, 128, N_).transpose(1, 0, 2)


def _cols(v):
    return v.reshape(-1, 128).T


def prep_shared(inp):
    f = np.float32
    sh = {}
    for which in (1, 2):
        wg, wu, wd = inp[f"ffn{which}_w_gate"], inp[f"ffn{which}_w_up"], inp[f"ffn{which}_w_down"]
        gu = np.empty((DEPTH, NFC, 128, 2, NKC, 128), f)
        for l in range(DEPTH):
            for g, w in enumerate((wg[l], wu[l])):
                gu[l, :, :, g] = w.reshape(NKC, 128, NFC, 128).transpose(2, 1, 0, 3)
        sh[f"ffn{which}_gu"] = gu.reshape(DEPTH, NFC, 128, 2 * NKC * 128)
        sh[f"ffn{which}_wd"] = np.ascontiguousarray(wd.reshape(DEPTH, NFC, 128, D))
    vecs = []
    for l in range(DEPTH):
        vecs += [inp["ffn1_norm"][l], inp["mix_norm"][l], inp["ffn2_norm"][l]]
    vecs.append(inp["final_norm"])
    sh["normw"] = np.ascontiguousarray(np.stack([_cols(v) for v in vecs], axis=1).reshape(128, 56)).astype(f)
    sh["ident"] = np.eye(128, dtype=f)
    sh["ones"] = np.ones((128, 128), f)
    sh["tri"] = np.triu(np.ones((128, 128), f))
    sh["negtri"] = (np.tril(np.ones((128, 128), f), -1) * NEG_BIG).astype(f)
    w_in = inp["w_in"]
    sh["w_in"] = np.ascontiguousarray(np.stack([_chunk_rows(w_in[l]) for l in range(DEPTH)]))
    gb = np.empty((DEPTH, 128, 24), f)
    cw = np.empty((DEPTH, 128, 16), f)
    cb = np.empty((DEPTH, 128, 4), f)
    sk = np.empty((DEPTH, 128, 4), f)
    gc = np.zeros((DEPTH, 8, 8), f)
    for l in range(DEPTH):
        for b in range(3):
            gb[l, :, b * 8:(b + 1) * 8] = _cols(inp["gate_bias"][l, b])
        for j in range(4):
            cw[l, :, j * 4:(j + 1) * 4] = _cols(inp["mlstm_conv_w"][l, j])
        cb[l] = _cols(inp["mlstm_conv_b"][l])
        sk[l] = _cols(inp["mlstm_skip"][l])
        gc[l, :, 0] = inp["fox_b_f"][l]
        gc[l, 0:4, 1] = inp["mlstm_b_if"][l, 0:4]
        gc[l, 0:4, 2] = inp["mlstm_b_if"][l, 4:8]
    sh["gbcols"], sh["convw"], sh["convb"], sh["skipc"], sh["gcols"] = gb, cw, cb, sk, gc
    sh["difflam"] = np.ascontiguousarray(np.concatenate(
        [inp["diff_lq1"], inp["diff_lk1"], inp["diff_lq2"], inp["diff_lk2"]], axis=1)).astype(f)
    sh["diff_subln"] = np.ascontiguousarray(inp["diff_subln"]).astype(f)
    sh["mlstm_norm"] = np.ascontiguousarray(inp["mlstm_norm"]).astype(f)
    slopes = [2.0 ** (-8.0 * (h + 1) / 4) for h in range(4)]
    ak = np.zeros((3, 4, 128), f)
    aq = np.zeros((3, 4, 512), f)
    relq = np.arange(512)
    for h in range(4):
        ak[0, h] = slopes[h] * np.arange(128)
        ak[1, h] = 1.0
        ak[2, h] = 1.0
        aq[0, h] = 1.0
        aq[1, h] = -slopes[h] * (128 * (relq // 128))
        aq[2, h] = -slopes[h] * (relq % 128)
    sh["alibi_k"] = ak.reshape(3, 512)
    sh["alibi_q"] = aq.reshape(3, 2048)
    tpos = np.arange(S_LEN)
    akf = np.zeros((4, 3, S_LEN), f)
    aqf = np.zeros((4, 3, S_LEN), f)
    for h in range(4):
        akf[h, 0] = slopes[h] * (tpos % 128)
        akf[h, 1] = 1.0
        akf[h, 2] = 1.0
        aqf[h, 0] = 1.0
        aqf[h, 1] = -slopes[h] * (128 * ((tpos % 512) // 128))
        aqf[h, 2] = -slopes[h] * (tpos % 128)
    sh["alibi_kf"], sh["alibi_qf"] = akf, aqf
    wqkv = np.empty((DEPTH, 128, 3, 4, 128), f)
    for g, nm in enumerate(("mlstm_wq", "mlstm_wk", "mlstm_wv")):
        wqkv[:, :, g] = inp[nm].transpose(0, 2, 1, 3)
    sh["wqkv"] = wqkv.reshape(DEPTH, 128, 1536)
    sh["wif"] = np.ascontiguousarray(inp["mlstm_w_if"].reshape(DEPTH, 12, 128, 8).transpose(0, 2, 1, 3)).reshape(DEPTH, 128, 96)
    mp = np.empty((DEPTH, NKC, 128, 3, 1536), f)
    wbs = (inp["w_branch_diff"], inp["w_branch_fox"], inp["w_branch_mlstm"])
    for l in range(DEPTH):
        for b in range(3):
            g = w_in[l][:, C_G + b * D:C_G + (b + 1) * D]
            mp[l, :, :, b, 0:1024] = g.reshape(NKC, 128, NKC, 128).transpose(2, 1, 0, 3).reshape(NKC, 128, 1024)
            mp[l, :, :, b, 1024:1536] = wbs[b][l].reshape(4, 128, NKC, 128).transpose(2, 1, 0, 3).reshape(NKC, 128, 512)
    sh["mpack"] = mp.reshape(DEPTH, NKC, 128, 4608)
    wo = np.empty((DEPTH, NKC, 128, 1024), f)
    for l in range(DEPTH):
        wo[l] = inp["w_out"][l].reshape(NKC, 128, NKC, 128).transpose(2, 1, 0, 3).reshape(NKC, 128, 1024)
    sh["wout"] = wo
    return sh


def build_program(shapes, plan):
    from contextlib import ExitStack
    nc = bass.Bass("TRN2", target_bir_lowering=False)
    dram = {}
    for name, shp in shapes.items():
        dram[name] = nc.dram_tensor(name, list(shp), F32, kind="ExternalInput").ap()
    dram["outT"] = nc.dram_tensor("outT", [128, NKC, S_LEN], F32, kind="ExternalOutput").ap()
    dram["xspill"] = nc.dram_tensor("xspill", [128, NKC, S_LEN], F32, kind="Internal").ap()
    for nm in ("augk_fox", "augq_fox", "augk_ml", "augq_ml"):
        dram[nm] = nc.dram_tensor(nm, [8, 6, S_LEN], BF16, kind="Internal").ap()
    with ExitStack() as es:
        sems = [es.enter_context(nc.semaphore(f"s{i}")) for i in range(70)]
        S = Sched(nc, sems)
        arena = nc.alloc_sbuf_tensor("arena", [128, ARENA_WORDS], F32)
        ps = es.enter_context(nc.psum_tensor("ps", [128, 4096], F32))
        k = K(nc, S, arena, ps, dram)
        plan(k)
        with nc.Block() as block:
            S.emit(block)
    return nc


def full_plan(k):
    build_consts(k)
    load_x(k)
    for l in range(DEPTH):
        rmsnorm(k, l * 3 + 0, "H")
        ffn(k, l, 1)
        mixer(k, l)
        rmsnorm(k, l * 3 + 2, "H")
        k.S.barrier()
        ffn(k, l, 2)
    rmsnorm(k, 6, "X")
    store_out(k)


def run(inputs, plan=full_plan, trace=False, cores=NB):
    sh = prep_shared(inputs)
    x = np.asarray(inputs["x"], np.float32)
    in_maps = []
    for b in range(cores):
        m = dict(sh)
        m["xT"] = np.ascontiguousarray(x[b].T.reshape(NKC, 128, S_LEN).transpose(1, 0, 2))
        in_maps.append(m)
    shapes = {n: a.shape for n, a in in_maps[0].items()}
    nc = build_program(shapes, plan)
    res = run_bass_kernel_spmd(nc, in_maps, core_ids=list(range(cores)), trace=trace)
    out = np.empty((cores, S_LEN, D), np.float32)
    for b in range(cores):
        o = res.results[b]["outT"]
        out[b] = o.transpose(1, 0, 2).reshape(D, S_LEN).T
    return out, res


def kernel(**inputs):
    out, _ = run(inputs)
    return out
```

```python
import bisect
import numpy as np
import concourse.bass as bass
import concourse.mybir as mybir
from concourse.bass_utils import run_bass_kernel_spmd

F32 = mybir.dt.float32
BF16 = mybir.dt.bfloat16
AF = mybir.ActivationFunctionType
ALU = mybir.AluOpType
AX = mybir.AxisListType


class _Ev:
    __slots__ = ("eng", "idx", "val", "clock")

    def __init__(self, eng, idx, val, clock):
        self.eng, self.idx, self.val, self.clock = eng, idx, val, clock


class _Eng:
    def __init__(self, name, sem, self_sync):
        self.name, self.sem, self.self_sync = name, sem, self_sync
        self.ops = []
        self.count = 0
        self.sig_idx = []
        self.sig_val = []
        self.n_inst = 0
        self.inst_rec = []
        self.clock = {}
        self.last_compute = None


class _Slot:
    def __init__(self, name, sem):
        self.name, self.sem, self.total = name, sem, 0


class _Res:
    __slots__ = ("w", "rs")

    def __init__(self):
        self.w, self.rs = None, []


class Sched:
    def __init__(self, nc, sems):
        self.nc = nc
        self._sems = list(sems)
        self.engs = {}
        for name, ss in (("pe", False), ("act", True), ("dve", True), ("pool", True), ("sp", False)):
            self.engs[name] = _Eng(name, self._sems.pop(), ss)
        self.slots = {}
        self.resd = {}

    def slot(self, name):
        s = self.slots.get(name)
        if s is None:
            s = _Slot(name, self._sems.pop())
            self.slots[name] = s
        return s

    def res(self, key):
        r = self.resd.get(key)
        if r is None:
            r = _Res()
            self.resd[key] = r
        return r

    def _value(self, ev):
        if ev.val is not None:
            return ev.val
        E = self.engs[ev.eng]
        j = bisect.bisect_left(E.sig_idx, ev.idx)
        if j < len(E.sig_idx):
            return E.sig_val[j]
        E.count += 1
        rec = E.inst_rec[ev.idx]
        rec[2], rec[3] = E.sem, 1
        E.sig_idx.append(ev.idx)
        E.sig_val.append(E.count)
        ev.val = E.count
        return ev.val

    def _wait_for(self, E, evs):
        need = {}
        for ev in evs:
            if ev is None:
                continue
            if ev.eng == E.name and not E.self_sync:
                continue
            if ev.eng in self.slots:
                v = self.slots[ev.eng].total
            else:
                v = self._value(ev)
            if E.clock.get(ev.eng, 0) >= v:
                continue
            if need.get(ev.eng, (0, None))[0] < v:
                need[ev.eng] = (v, ev)
        for name, (v, ev) in need.items():
            if E.clock.get(name, 0) >= v:
                continue
            sem = self.slots[name].sem if name in self.slots else self.engs[name].sem
            E.ops.append(["w", sem, v])
            E.clock[name] = v
            for k, cv in ev.clock.items():
                if E.clock.get(k, 0) < cv:
                    E.clock[k] = cv

    def _deps(self, reads, writes):
        evs = []
        for k in reads:
            r = self.res(k)
            if r.w is not None:
                evs.append(r.w)
        for k in writes:
            r = self.res(k)
            if r.w is not None:
                evs.append(r.w)
            evs.extend(r.rs)
        return evs

    def _commit(self, ev, reads, writes):
        for k in reads:
            self.res(k).rs.append(ev)
        for k in writes:
            r = self.res(k)
            r.w, r.rs = ev, []

    def op(self, eng, fn, reads=(), writes=()):
        E = self.engs[eng]
        self._wait_for(E, self._deps(reads, writes))
        rec = ["i", fn, None, 0]
        E.ops.append(rec)
        E.inst_rec.append(rec)
        idx = E.n_inst
        E.n_inst += 1
        E.last_compute = idx
        clk = dict(E.clock)
        ev = _Ev(eng, idx, None, clk)
        self._commit(ev, reads, writes)
        return ev

    def dma(self, queue, slot, fn, reads=(), writes=()):
        E = self.engs[queue]
        S = self.slot(slot) if isinstance(slot, str) else slot
        self._wait_for(E, self._deps(reads, writes))
        S.total += 16
        rec = ["i", fn, S.sem, 16]
        E.ops.append(rec)
        E.inst_rec.append(rec)
        E.n_inst += 1
        ev = _Ev(S.name, -1, S.total, dict(E.clock))
        self._commit(ev, reads, writes)
        return ev

    def barrier(self):
        evs = []
        for E in self.engs.values():
            if E.last_compute is not None:
                ev = _Ev(E.name, E.last_compute, None, dict(E.clock))
                self._value(ev)
                evs.append(ev)
        for S in self.slots.values():
            if S.total:
                evs.append(_Ev(S.name, -1, S.total, {}))
        for E in self.engs.values():
            self._wait_for(E, evs)
        self.resd = {}

    def wait_all_dma(self, eng, slots):
        E = self.engs[eng]
        evs = [_Ev(self.slots[s].name, -1, self.slots[s].total, {}) for s in slots if self.slots[s].total]
        self._wait_for(E, evs)

    def emit(self, block):
        def replay(E):
            def run(e):
                for rec in E.ops:
                    if rec[0] == "w":
                        e.wait_ge(rec[1], rec[2])
                    else:
                        ins = rec[1](e)
                        if rec[2] is not None:
                            ins.then_inc(rec[2], rec[3])
            return run
        block.tensor(replay(self.engs["pe"]))
        block.scalar(replay(self.engs["act"]))
        block.vector(replay(self.engs["dve"]))
        block.gpsimd(replay(self.engs["pool"]))
        block.sync(replay(self.engs["sp"]))


D = 1024
S_LEN = 2048
NB = 8
DEPTH = 2
DFF = 2816
NFC = DFF // 128
NKC = D // 128
NT = S_LEN // 512
NT128 = S_LEN // 128
RMS_EPS = 1e-6
LN_EPS = 1e-5
N_IN = 7176
FFN_GROUPS = [(0, 6), (6, 12), (12, 17), (17, 22)]

W_X = 0
W_H = W_X + 16384
W_CONST = W_H + 8192
W_PT = W_CONST + 3072
W_W = W_PT + 1024
W_N = W_W + 9216
W_END = W_N + 12288
ARENA_WORDS = W_END


class K:
    def __init__(self, nc, S, arena, ps, dram):
        self.nc, self.S, self.arena, self.ps, self.d = nc, S, arena, ps, dram
        self.ps_rr = 0

    def f32(self, off, n):
        return self.arena[:, off:off + n]

    def bf(self, off, n):
        return self.arena[:, off:off + (n + 1) // 2].bitcast(BF16)

    def bank(self, b):
        return self.ps[:, b * 512:(b + 1) * 512]

    def X(self, c, t):
        o = W_X + c * S_LEN + t * 512
        return self.arena[:, o:o + 512]

    def H(self, c, t0=0, n=S_LEN):
        return self.bf(W_H + c * (S_LEN // 2), S_LEN)[:, t0:t0 + n]

    def PT(self, i):
        return self.bf(W_PT + i * 256, 512)


def build_consts(k):
    S, d = k.S, k.d
    o = W_CONST
    k.ident = k.f32(o, 128); o += 128
    k.ones_bf = k.bf(o, 128); o += 64
    k.tri_bf = k.bf(o, 128); o += 64
    k.normw = k.f32(o, 7 * 8); o += 56
    k.const_end = o
    S.dma("sp", "c0", lambda e: e.dma_start(out=k.ident, in_=d["ident"][:, :]), writes=["ident"])
    S.dma("pool", "c1", lambda e: e.dma_start(out=k.ones_bf, in_=d["ones"][:, :]), writes=["ones"])
    S.dma("pool", "c1", lambda e: e.dma_start(out=k.tri_bf, in_=d["tri"][:, :]), writes=["tri"])
    S.dma("sp", "c0", lambda e: e.dma_start(out=k.normw, in_=d["normw"][:, :]), writes=["normw"])


def load_x(k):
    S, d = k.S, k.d
    for c in range(NKC):
        o = W_X + c * S_LEN
        S.dma("sp", "xld", lambda e, c=c, o=o: e.dma_start(out=k.arena[:, o:o + S_LEN], in_=d["xT"][:, c, :]),
              writes=[("X", c, t) for t in range(NT)])


def rmsnorm(k, widx, out_mode):
    S = k.S
    R0 = W_N + 8192
    for t in range(NT):
        for c in range(NKC):
            pt = k.PT((t * NKC + c) % 4)
            key = ("PT", (t * NKC + c) % 4)
            S.op("act", lambda e, pt=pt, c=c, t=t: e.activation(out=pt, in_=k.X(c, t), func=AF.Square),
                 reads=[("X", c, t)], writes=[key])
            S.op("pe", lambda e, pt=pt, c=c, t=t: e.matmul(k.bank(t), k.ones_bf, pt, start=(c == 0), stop=(c == NKC - 1)),
                 reads=[key, "ones"], writes=[("ps", t)])
    for t in range(NT):
        R = k.f32(R0 + t * 512, 512)
        S.op("act", lambda e, R=R, t=t: e.activation(out=R, in_=k.bank(t), func=AF.Sqrt, bias=RMS_EPS, scale=1.0 / D),
             reads=[("ps", t)], writes=[("R", t)])
        S.op("dve", lambda e, R=R: e.reciprocal(R, R), reads=[("R", t)], writes=[("R", t)])
        for c in range(NKC):
            wcol = k.normw[:, widx * 8 + c:widx * 8 + c + 1]
            if out_mode == "H":
                S.op("dve", lambda e, R=R, c=c, t=t, wcol=wcol: e.scalar_tensor_tensor(
                    out=k.H(c, t * 512, 512), in0=k.X(c, t), scalar=wcol, in1=R, op0=ALU.mult, op1=ALU.mult),
                    reads=[("X", c, t), ("R", t), "normw"], writes=[("H", c, t)])
            else:
                S.op("dve", lambda e, R=R, c=c, t=t, wcol=wcol: e.scalar_tensor_tensor(
                    out=k.X(c, t), in0=k.X(c, t), scalar=wcol, in1=R, op0=ALU.mult, op1=ALU.mult),
                    reads=[("X", c, t), ("R", t), "normw"], writes=[("X", c, t)])


def ffn(k, l, which):
    S, d = k.S, k.d
    gu_d = d[f"ffn{which}_gu"]
    wd_d = d[f"ffn{which}_wd"]
    GU = [k.bf(W_W + i * 1024, 2048) for i in range(3)]
    WD = [k.bf(W_W + 3072 + i * 3072, 6 * 1024) for i in range(2)]
    A = [k.bf(W_N + i * 1024, 2048) for i in range(7)]
    SG = [k.f32(W_N + 7168 + i * 512, 512) for i in range(2)]

    def load_gu(c):
        s = c % 3
        S.dma("pool", f"gu{s}", lambda e: e.dma_start(out=GU[s], in_=gu_d[l, c, :, :]), writes=[("GU", s)])

    def load_wd(g):
        s = g % 2
        c0, c1 = FFN_GROUPS[g]
        for c in range(c0, c1):
            S.dma("pool", f"wd{s}", lambda e, c=c: e.dma_start(out=WD[s][:, (c - c0) * 1024:(c - c0 + 1) * 1024], in_=wd_d[l, c, :, :]),
                  writes=[("WD", s)])

    def gu_chunk(c):
        s = c % 3
        for t in range(NT):
            pr = (c * NT + t) % 3
            bg, bu = 2 * pr, 2 * pr + 1
            for g, b in ((0, bg), (1, bu)):
                for kc in range(NKC):
                    S.op("pe", lambda e, g=g, b=b, kc=kc, t=t: e.matmul(
                        k.bank(b), GU[s][:, (g * 8 + kc) * 128:(g * 8 + kc + 1) * 128], k.H(kc, t * 512, 512),
                        start=(kc == 0), stop=(kc == NKC - 1)),
                        reads=[("GU", s), ("H", kc, t)], writes=[("ps", b)])
            sg = SG[(c * NT + t) % 2]
            sgk = ("SG", (c * NT + t) % 2)
            S.op("act", lambda e, sg=sg, bg=bg: e.activation(out=sg, in_=k.bank(bg), func=AF.Silu),
                 reads=[("ps", bg)], writes=[sgk])
            S.op("dve", lambda e, sg=sg, bu=bu, t=t: e.tensor_tensor(
                out=A[c % 7][:, t * 512:(t + 1) * 512], in0=sg, in1=k.bank(bu), op=ALU.mult),
                reads=[sgk, ("ps", bu)], writes=[("A", c % 7, t)])

    dn_cnt = [0]

    def down(g):
        s = g % 2
        c0, c1 = FFN_GROUPS[g]
        for t in range(NT):
            for dd in range(NKC):
                b = 6 + dn_cnt[0] % 2
                dn_cnt[0] += 1
                for c in range(c0, c1):
                    S.op("pe", lambda e, b=b, c=c, dd=dd, t=t: e.matmul(
                        k.bank(b), WD[s][:, (c - c0) * 1024 + dd * 128:(c - c0) * 1024 + (dd + 1) * 128],
                        A[c % 7][:, t * 512:(t + 1) * 512], start=(c == c0), stop=(c == c1 - 1)),
                        reads=[("WD", s), ("A", c % 7, t)], writes=[("ps", b)])
                S.op("dve", lambda e, b=b, dd=dd, t=t: e.scalar_tensor_tensor(
                    out=k.X(dd, t), in0=k.bank(b), scalar=0.5, in1=k.X(dd, t), op0=ALU.mult, op1=ALU.add),
                    reads=[("ps", b), ("X", dd, t)], writes=[("X", dd, t)])

    for c in range(3):
        load_gu(c)
    load_wd(0)
    load_wd(1)
    grp_of = {}
    for g, (c0, c1) in enumerate(FFN_GROUPS):
        for c in range(c0, c1):
            grp_of[c] = g
    for c in range(NFC):
        gu_chunk(c)
        if c + 3 < NFC:
            load_gu(c + 3)
        g = grp_of[c]
        if c == FFN_GROUPS[g][0] and g > 0:
            down(g - 1)
            if g + 1 < len(FFN_GROUPS):
                load_wd(g + 1)
    down(len(FFN_GROUPS) - 1)


def store_out(k):
    S, d = k.S, k.d
    for c in range(NKC):
        o = W_X + c * S_LEN
        S.dma("sp", "ost", lambda e, c=c, o=o: e.dma_start(out=d["outT"][:, c, :], in_=k.arena[:, o:o + S_LEN]),
              reads=[("X", c, t) for t in range(NT)])
    S.wait_all_dma("sp", ["ost"])


U = 4096
W_SCR = W_END + 64
ARENA_WORDS = W_END + 2560
SB = W_SCR + 64
NEG_BIG = -30000.0
C_DQ, C_DK, C_DV = 0, 512, 1024
C_FQ, C_FK, C_FV, C_FF = 1536, 2048, 2560, 3072
C_MX, C_MZ, C_G = 3080, 3592, 4104


def DU(i):
    return W_X + i * U


def NU(i):
    return W_N + i * U


def fm(k, off, c, t0=0, n=S_LEN):
    return k.bf(off + c * (S_LEN // 2), S_LEN)[:, t0:t0 + n]


def next_bank(k):
    b = k.ps_rr % 8
    k.ps_rr += 1
    return b


def col(k):
    i = getattr(k, "_col_rr", 0)
    k._col_rr = i + 1
    i %= 48
    return k.arena[:, W_SCR + i:W_SCR + i + 1], ("col", i)


def evac(k, dst, src, src_key, dst_key, scale=None, func=None, eng=None):
    S = k.S
    if eng is None:
        k._ev_rr = getattr(k, "_ev_rr", 0) + 1
        eng = "act" if (func is not None or k._ev_rr % 2 == 0) else "dve"
    if eng == "act":
        f = func if func is not None else AF.Copy
        sc = 1.0 if scale is None else scale
        S.op("act", lambda e: e.activation(out=dst, in_=src, func=f, scale=sc), reads=[src_key], writes=[dst_key])
    else:
        if scale is None:
            S.op("dve", lambda e: e.tensor_copy(dst, src), reads=[src_key], writes=[dst_key])
        else:
            S.op("dve", lambda e: e.tensor_scalar(dst, src, scale, None, ALU.mult), reads=[src_key], writes=[dst_key])


def wcol_jobs(k, l, jobs):
    S, d = k.S, k.d

    def load(i):
        c0, nc_, _ = jobs[i]
        s = i % 2
        tile = k.bf(W_W + s * 2048, 4096).rearrange("p (k c) -> p k c", k=8)
        S.dma("pool", f"wc{s}", lambda e: e.dma_start(out=tile[:, :, 0:nc_], in_=d["w_in"][l, :, :, c0:c0 + nc_]),
              writes=[("WC", s)])
        return tile

    tiles = {}
    for i in range(min(2, len(jobs))):
        tiles[i] = load(i)
    for i in range(len(jobs)):
        jobs[i][2](tiles[i], ("WC", i % 2))
        if i + 2 < len(jobs):
            tiles[i + 2] = load(i + 2)


def h_fm(dst_off, scale=None, func=None):
    def mk(k, c_base, nchunks=4):
        def handler(W, wkey):
            S = k.S
            for m in range(nchunks):
                for t in range(NT):
                    b = next_bank(k)
                    for kc in range(NKC):
                        S.op("pe", lambda e, b=b, m=m, kc=kc, t=t: e.matmul(
                            k.bank(b), W[:, kc, m * 128:(m + 1) * 128], k.H(kc, t * 512, 512),
                            start=(kc == 0), stop=(kc == NKC - 1)),
                            reads=[wkey, ("H", kc, t)], writes=[("ps", b)])
                    evac(k, fm(k, dst_off, c_base + m, t * 512, 512), k.bank(b), ("ps", b),
                         ("fm", dst_off, c_base + m, t), scale=scale, func=func)
        return handler
    return mk


def h_tm(k, vbuf_fn, nh, dv):
    def handler(W, wkey):
        S = k.S
        for tt in range(NT128):
            b = next_bank(k)
            for kc in range(NKC):
                S.op("pe", lambda e, b=b, kc=kc, tt=tt: e.matmul(
                    k.bank(b), k.H(kc, tt * 128, 128), W[:, kc, 0:512],
                    start=(kc == 0), stop=(kc == NKC - 1)),
                    reads=[wkey, ("H", kc, tt // 4)], writes=[("ps", b)])
            dst = vbuf_fn(tt)[:, :, 0:dv]
            src = k.bank(b).rearrange("p (h e) -> p h e", h=nh)
            evac(k, dst, src, ("ps", b), ("V", tt))
    return handler


class AttnPass:
    def __init__(self, k, *, qf, kf, aqf, akf, bias_fn, vf, vkey, dv, mode, fin, sbanks, obanks, scale=1.0, augkey=None, pre=None, act_func=None, scale_fn=None, mask_mul=False):
        self.__dict__.update(locals())
        self.state = {}
        self.per_bank = 2 if dv == 128 else 4

    def blocks(self):
        return [(self, j, i) for j in range(4) for i in range(4 * j + 4)]

    def O(self, j, r):
        pb = self.per_bank
        ob = self.obanks[j % len(self.obanks)][r // pb]
        c0 = (r % pb) * (self.dv + 1)
        return self.k.bank(ob)[:, c0:c0 + self.dv + 1], ("ps", ob)

    def emit_s(self, j, i):
        k, S = self.k, self.k.S
        if self.pre is not None:
            pl = self.pre if isinstance(self.pre, list) else [self.pre]
            self.pre = None
            for f_ in pl:
                f_()
        r0 = max(0, i - 4 * j)
        n0 = r0 * 128
        ncol = 512 - n0
        diag = i >= 4 * j
        k._blk = getattr(k, "_blk", 0) + 1
        pti = k._blk % 4
        pt = k.PT(pti)
        ptk = ("PT", pti)
        if self.mode == "exp":
            sb = self.sbanks[k._blk % len(self.sbanks)]
            has_aug = self.akf is not None
            S.op("pe", lambda e: e.matmul(k.bank(sb)[:, n0:512], self.kf(i), self.qf(j * 512 + n0, ncol),
                                          start=True, stop=(not has_aug and (not diag or self.mask_mul))),
                 reads=([self.augkey] if (self.augkey is not None and not has_aug) else []), writes=[("ps", sb)])
            if has_aug:
                S.op("pe", lambda e: e.matmul(k.bank(sb)[:, n0:512], self.akf(i), self.aqf(j, n0, ncol),
                                              start=False, stop=(not diag)),
                     reads=[self.augkey], writes=[("ps", sb)])
            if diag and not self.mask_mul:
                S.op("pe", lambda e: e.matmul(k.bank(sb)[:, n0:n0 + 128], k.ident_bf, k.negtri_bf, start=False, stop=True),
                     reads=["identbf", "negtri"], writes=[("ps", sb)])
            bias = float(self.bias_fn(i, j)) if self.bias_fn is not None else 0.0
            if self.act_func is None:
                S.op("act", lambda e: e.activation(out=pt[:, n0:512], in_=k.bank(sb)[:, n0:512], func=AF.Exp, bias=bias, scale=1.0),
                     reads=[("ps", sb)], writes=[ptk])
            else:
                sc_ap = self.scale_fn(i, j)
                S.op("act", lambda e: e.activation(out=pt[:, n0:512], in_=k.bank(sb)[:, n0:512], func=self.act_func, scale=sc_ap),
                     reads=[("ps", sb), "UC"], writes=[ptk])
            if diag and self.mask_mul:
                S.op("pool", lambda e: e.tensor_tensor(out=pt[:, n0:n0 + 128], in0=pt[:, n0:n0 + 128], in1=k.tri_bf, op=ALU.mult),
                     reads=[ptk, "tri"], writes=[ptk])
        else:
            pr = self.sbanks[k._blk % len(self.sbanks)]
            sb, eb = pr
            S.op("pe", lambda e: e.matmul(k.bank(sb)[:, n0:512], self.kf(i), self.qf(j * 512 + n0, ncol), start=True, stop=True),
                 reads=[], writes=[("ps", sb)])
            S.op("pe", lambda e: e.matmul(k.bank(eb)[:, n0:512], self.akf(i), self.aqf(j, n0, ncol), start=True, stop=(not diag)),
                 reads=[self.augkey], writes=[("ps", eb)])
            if diag:
                S.op("pe", lambda e: e.matmul(k.bank(eb)[:, n0:n0 + 128], k.ident_bf, k.negtri_bf, start=False, stop=True),
                     reads=["identbf", "negtri"], writes=[("ps", eb)])
            eti = k._blk % 2
            et = k.f32(SB + eti * 512, 512)
            S.op("act", lambda e: e.activation(out=et[:, n0:512], in_=k.bank(eb)[:, n0:512], func=AF.Exp),
                 reads=[("ps", eb)], writes=[("ET", eti)])
            sc = self.scale
            S.op("dve", lambda e: e.scalar_tensor_tensor(out=pt[:, n0:512], in0=k.bank(sb)[:, n0:512], scalar=sc,
                                                         in1=et[:, n0:512], op0=ALU.mult, op1=ALU.mult),
                 reads=[("ps", sb), ("ET", eti)], writes=[ptk])
        self.state[(j, i)] = (pt, ptk, r0)

    def emit_pv(self, j, i):
        k, S = self.k, self.k.S
        pt, ptk, r0 = self.state.pop((j, i))
        for r in range(r0, 4):
            o, okey = self.O(j, r)
            last = (i == 4 * j + r)
            S.op("pe", lambda e, r=r, o=o, last=last: e.matmul(o, pt[:, r * 128:(r + 1) * 128], self.vf(i), start=(i == 0 and r % self.per_bank == 0), stop=last, skip_group_check=True),
                 reads=[ptk, self.vkey], writes=[okey])
            if last:
                self.fin(j, r, o, okey)


def run_blocks(k, blocks, L=2):
    n = len(blocks)
    for idx in range(n + L):
        if idx < n:
            p, j, i = blocks[idx]
            p.emit_s(j, i)
        if idx >= L:
            p, j, i = blocks[idx - L]
            p.emit_pv(j, i)
        tick(k)
    flush_deferred(k)


def defer(k, n, fn):
    k._dq = getattr(k, "_dq", [])
    k._dq.append([n, fn])


def tick(k):
    dq = getattr(k, "_dq", [])
    k._dq = []
    keep = []
    for it in dq:
        it[0] -= 1
        if it[0] <= 0:
            it[1]()
        else:
            keep.append(it)
    k._dq = keep + k._dq


def flush_deferred(k):
    while getattr(k, "_dq", []):
        dq = k._dq
        k._dq = []
        for it in dq:
            it[1]()


def transpose_to_fm(k, src, src_key, dst, dst_key, post=None):
    S = k.S
    q = getattr(k, "_tq", 0)
    k._tq = q + 1
    tb = k.tbanks[q % len(k.tbanks)]
    pq = k.bank(tb)[:, 0:128]
    pkey = ("ps", tb)
    S.op("pe", lambda e: e.transpose(pq, src, k.ident), reads=[src_key, "ident"], writes=[pkey])
    if post is None:
        evac(k, dst, pq, pkey, dst_key)
    else:
        post(pq, pkey)


def softplus_neg(k, T, nh, banks, negb_col, key):
    S = k.S
    for t in range(NT):
        b = banks[t]
        S.op("act", lambda e, b=b, t=t: e.activation(out=T[0:nh, t * 512:(t + 1) * 512], in_=k.bank(b)[0:nh, :],
                                                    func=AF.Exp, bias=negb_col, scale=-1.0),
             reads=[("ps", b), "gcols"], writes=[key])
    S.op("act", lambda e: e.activation(out=T[0:nh, :], in_=T[0:nh, :], func=AF.Ln, bias=1.0, scale=1.0),
         reads=[key], writes=[key])


def build_aug(k, kvec, kkey, qvec, qkey, nh, spl_off, tag):
    S, d = k.S, k.d
    dk, dq = d["augk_" + tag], d["augq_" + tag]
    SPL = [k.bf(spl_off + i * 1024, 2048) for i in range(4)]
    ones = SPL[3]
    S.op("dve", lambda e: e.memset(ones[0:nh, :], 1.0), writes=[("SPL", 3)])
    for r in range(3):
        S.dma("sp", "augw", lambda e, r=r: e.dma_start(out=dk[0:nh, 3 + r, :], in_=ones[0:nh, :]), reads=[("SPL", 3)], writes=[("augd", tag)])
        S.dma("sp", "augw", lambda e, r=r: e.dma_start(out=dq[0:nh, r, :], in_=ones[0:nh, :]), reads=[("SPL", 3)], writes=[("augd", tag)])
    cnt = 0
    for vec, vkey, dram, r0 in ((kvec, kkey, dk, 0), (qvec, qkey, dq, 3)):
        for r in range(3):
            si = cnt % 3
            cnt += 1
            spl = SPL[si]
            S.op("dve", lambda e, spl=spl, vec=vec: e.tensor_copy(spl[0:nh, :], vec[0:nh, :]), reads=[vkey], writes=[("SPL", si)])
            if r < 2:
                S.op("dve", lambda e, spl=spl, vec=vec: e.tensor_tensor(out=vec[0:nh, :], in0=vec[0:nh, :], in1=spl[0:nh, :], op=ALU.subtract),
                     reads=[vkey, ("SPL", si)], writes=[vkey])
            S.dma("sp", "augw", lambda e, spl=spl, dram=dram, rr=r0 + r: e.dma_start(out=dram[0:nh, rr, :], in_=spl[0:nh, :]),
                  reads=[("SPL", si)], writes=[("augd", tag)])


def aug_views(k, slot):
    ak = k.bf(W_W + 5120 + slot * 2048, 2048)
    aq = k.bf(W_W + 5120 + slot * 2048 + 1024, 2048)
    akf = lambda i: ak[0:6, i * 128:(i + 1) * 128]
    aqf = lambda j, n0, n: aq[0:6, j * 512 + n0:j * 512 + n0 + n]
    return akf, aqf


def load_aug(k, tag, h, slot):
    S, d = k.S, k.d
    ak = k.bf(W_W + 5120 + slot * 2048, 2048)
    aq = k.bf(W_W + 5120 + slot * 2048 + 1024, 2048)
    S.dma("sp", f"augl{slot}", lambda e: e.dma_start(out=ak[0:6, :], in_=d["augk_" + tag][h, :, :]), reads=[("augd", tag)], writes=[("AUGS", slot)])
    S.dma("sp", f"augl{slot}", lambda e: e.dma_start(out=aq[0:6, :], in_=d["augq_" + tag][h, :, :]), reads=[("augd", tag)], writes=[("AUGS", slot)])


HB_OFF = [W_W + 0, W_W + 2048, W_W + 5120, W_W + 7168]


def hb_views(k, slot):
    return k.bf(HB_OFF[slot], 2048), k.bf(HB_OFF[slot] + 1024, 2048)


def hb_init(k):
    S = k.S
    for s_ in range(4):
        qb, kb = hb_views(k, s_)
        S.op("dve", lambda e, qb=qb: e.memset(qb, 0.0), writes=[("HB", s_)])
        S.op("dve", lambda e, kb=kb: e.memset(kb, 0.0), writes=[("HB", s_)])


def hb_build(k, slot, par, q_src, k_src, augq_ap, augk_ap, R, queue):
    S = k.S
    qb, kb = hb_views(k, slot)
    r0 = par * 64
    a0 = 64 if par == 0 else 0
    S.op("pool", lambda e: e.tensor_copy(qb[r0:r0 + 64, :], q_src[r0:r0 + 64, :]), writes=[("HB", slot)])
    S.op("pool", lambda e: e.tensor_copy(kb[r0:r0 + 64, :], k_src[r0:r0 + 64, :]), writes=[("HB", slot)])
    S.dma(queue, f"hb{queue}{slot}", lambda e: e.dma_start(out=qb[a0:a0 + R, :], in_=augq_ap), writes=[("HB", slot)])
    S.dma(queue, f"hb{queue}{slot}", lambda e: e.dma_start(out=kb[a0:a0 + R, :], in_=augk_ap), writes=[("HB", slot)])


def layer_consts(k, l):
    S, d = k.S, k.d
    o = k.const_end
    k.gb = k.f32(o, 24); o += 24
    k.convw = k.f32(o, 16); o += 16
    k.convb = k.f32(o, 4); o += 4
    k.skipc = k.f32(o, 4); o += 4
    k.gcols = k.f32(o, 8); o += 8
    k.lamt = k.f32(o, 256); o += 256
    k.lamc = k.f32(o, 8); o += 8
    k.subln = k.f32(o, 128); o += 128
    k.normB = k.f32(o, 512); o += 512
    k.AKd = k.bf(o, 4 * 128); o += 256
    k.AQd = k.bf(o, 4 * 512); o += 1024
    k.ident_bf = k.bf(o, 128); o += 64
    k.negtri_bf = k.bf(o, 128); o += 64
    assert o <= W_CONST + 3072, o
    S.dma("sp", "lc", lambda e: e.dma_start(out=k.gb, in_=d["gbcols"][l, :, :]), writes=["lconst"])
    S.dma("sp", "lc", lambda e: e.dma_start(out=k.convw, in_=d["convw"][l, :, :]), writes=["lconst"])
    S.dma("sp", "lc", lambda e: e.dma_start(out=k.convb, in_=d["convb"][l, :, :]), writes=["lconst"])
    S.dma("sp", "lc", lambda e: e.dma_start(out=k.skipc, in_=d["skipc"][l, :, :]), writes=["lconst"])
    S.dma("sp", "lc", lambda e: e.dma_start(out=k.gcols[0:8, :], in_=d["gcols"][l, :, :]), writes=["gcols"])
    S.dma("sp", "lc", lambda e: e.dma_start(out=k.lamt, in_=d["difflam"][l:l + 1, :].partition_broadcast(128)), writes=["lamt"])
    S.dma("sp", "lc", lambda e: e.dma_start(out=k.subln, in_=d["diff_subln"][l:l + 1, :].partition_broadcast(128)), writes=["subln"])
    S.dma("sp", "lc", lambda e: e.dma_start(out=k.normB, in_=d["mlstm_norm"][l:l + 1, :].partition_broadcast(128)), writes=["normB"])
    S.op("dve", lambda e: e.tensor_scalar(k.gcols[0:8, 4:5], k.gcols[0:8, 0:1], -1.0, None, ALU.mult), reads=["gcols"], writes=["gcols"])
    S.op("dve", lambda e: e.tensor_scalar(k.gcols[0:8, 5:6], k.gcols[0:8, 2:3], -1.0, None, ALU.mult), reads=["gcols"], writes=["gcols"])
    if l == 0:
        S.dma("pool", "lc2", lambda e: e.dma_start(out=k.AKd[0:3, :], in_=d["alibi_k"][:, :]), writes=["alibi"])
        S.dma("pool", "lc2", lambda e: e.dma_start(out=k.AQd[0:3, :], in_=d["alibi_q"][:, :]), writes=["alibi"])
        S.dma("pool", "lc2", lambda e: e.dma_start(out=k.ident_bf, in_=d["ident"][:, :]), writes=["identbf"])
        S.dma("pool", "lc2", lambda e: e.dma_start(out=k.negtri_bf, in_=d["negtri"][:, :]), writes=["negtri"])
    import math
    lam_init = 0.8 - 0.6 * math.exp(-0.3 * l)
    lt, lc = k.lamt, k.lamc
    S.op("dve", lambda e: e.tensor_tensor(out=lt[:, 0:64], in0=lt[:, 0:64], in1=lt[:, 64:128], op=ALU.mult), reads=["lamt"], writes=["lamt"])
    S.op("dve", lambda e: e.tensor_tensor(out=lt[:, 128:192], in0=lt[:, 128:192], in1=lt[:, 192:256], op=ALU.mult), reads=["lamt"], writes=["lamt"])
    S.op("dve", lambda e: e.reduce_sum(out=lc[:, 0:1], in_=lt[:, 0:64], axis=AX.X), reads=["lamt"], writes=["lamc"])
    S.op("dve", lambda e: e.reduce_sum(out=lc[:, 1:2], in_=lt[:, 128:192], axis=AX.X), reads=["lamt"], writes=["lamc"])
    S.op("act", lambda e: e.activation(out=lc[:, 2:4], in_=lc[:, 0:2], func=AF.Exp), reads=["lamc"], writes=["lamc"])
    S.op("dve", lambda e: e.tensor_tensor(out=lc[:, 4:5], in0=lc[:, 3:4], in1=lc[:, 2:3], op=ALU.subtract), reads=["lamc"], writes=["lamc"])
    S.op("dve", lambda e: e.tensor_scalar(lc[:, 5:6], lc[:, 4:5], -lam_init, None, ALU.add), reads=["lamc"], writes=["neglam"])
    S.op("dve", lambda e: e.tensor_scalar(k.subln, k.subln, 1.0 - lam_init, None, ALU.mult), reads=["subln"], writes=["subln"])
    k.neglam = lc[:, 5:6]


def diff_branch(k, l):
    S = k.S
    QT, KT, VO, OD = DU(0), DU(1), DU(2), NU(0)
    V = k.bf(VO, 16 * 4 * 129).rearrange("p (t h e) -> p t h e", t=16, h=4)
    S.op("dve", lambda e: e.memset(k.bf(VO, 16 * 4 * 129), 1.0), writes=[("V", "all")])
    S.barrier()
    wcol_jobs(k, l, [
        (C_DQ, 512, h_fm(QT)(k, 0)),
        (C_DK, 512, h_fm(KT, scale=0.125)(k, 0)),
        (C_DV, 512, h_tm(k, lambda tt: V[:, tt, :, :], 4, 128)),
    ])
    S.barrier()
    k.tbanks = [2, 3]
    slopes = [2.0 ** (-8.0 * (h + 1) / 4) for h in range(4)]
    A1 = [[k.f32(SB + (p * 4 + r) * 128, 128) for r in range(4)] for p in range(2)]
    TB = SB + 1024
    blocks = []
    pres = []
    first_pass = []
    hb_init(k)
    for h in range(4):
        passes = []
        for c in range(2):
            slot = c + 2 * (h % 2)
            qb, kb = hb_views(k, slot)

            def qf(t0, n, qb=qb):
                return qb[:, t0:t0 + n]

            def kf(i, kb=kb):
                return kb[:, i * 128:(i + 1) * 128]

            pres.append(lambda h=h, c=c, slot=slot: hb_build(k, slot, c, fm(k, QT, h), fm(k, KT, h),
                                                              k.d["alibi_qf"][h, :, :], k.d["alibi_kf"][h, :, :], 3, "pool"))

            def bias_fn(i, j, h=h):
                return slopes[h] * (128.0 * i - 512.0 * j)

            def vf(i, h=h):
                return V[:, i, h, :]

            if c == 0:
                def fin(j, r, o, okey, h=h):
                    rc, rk = col(k)
                    a1 = A1[j % 2][r]
                    S.op("dve", lambda e: e.reciprocal(rc, o[:, 128:129]), reads=[okey], writes=[rk])
                    S.op("dve", lambda e: e.tensor_scalar(a1, o[:, 0:128], rc, None, ALU.mult), reads=[okey, rk], writes=[("A1", j % 2, r)])
            else:
                def fin(j, r, o, okey, h=h):
                    rc, rk = col(k)
                    sc, sk = col(k)
                    k._fp = getattr(k, "_fp", 0) + 1
                    n = k._fp
                    tmp = k.f32(TB + (n % 5) * 128, 128)
                    sq = k.f32(TB + 640 + (n % 2) * 128, 128)
                    ot = k.f32(TB + 896 + (n % 4) * 128, 128)
                    tk_, sqk, otk = ("TMP", n % 5), ("SQ", n % 2), ("OT", n % 4)
                    a1 = A1[j % 2][r]
                    tt = 4 * j + r
                    S.op("dve", lambda e: e.reciprocal(rc, o[:, 128:129]), reads=[okey], writes=[rk])
                    S.op("dve", lambda e: e.tensor_scalar(tmp, o[:, 0:128], rc, None, ALU.mult), reads=[okey, rk], writes=[tk_])
                    S.op("dve", lambda e: e.scalar_tensor_tensor(out=tmp, in0=tmp, scalar=k.neglam, in1=a1, op0=ALU.mult, op1=ALU.add),
                         reads=[tk_, ("A1", j % 2, r), "neglam"], writes=[tk_])
                    S.op("dve", lambda e: e.tensor_tensor(out=sq, in0=tmp, in1=tmp, op=ALU.mult), reads=[tk_], writes=[sqk])
                    S.op("dve", lambda e: e.reduce_sum(out=sc, in_=sq, axis=AX.X), reads=[sqk], writes=[sk])

                    def stage_b():
                        S.op("act", lambda e: e.activation(out=sc, in_=sc, func=AF.Ln, bias=RMS_EPS, scale=1.0 / 128), reads=[sk], writes=[sk])
                        S.op("act", lambda e: e.activation(out=sc, in_=sc, func=AF.Exp, scale=-0.5), reads=[sk], writes=[sk])

                    def stage_c():
                        S.op("dve", lambda e: e.scalar_tensor_tensor(out=ot, in0=tmp, scalar=sc, in1=k.subln, op0=ALU.mult, op1=ALU.mult),
                             reads=[tk_, sk, "subln"], writes=[otk])
                        defer(k, 2, lambda: transpose_to_fm(k, ot, otk, fm(k, OD, h, tt * 128, 128), ("fm", OD, h, tt)))
                    defer(k, 2, stage_b)
                    defer(k, 3, stage_c)
            passes.append(AttnPass(k, qf=qf, kf=kf, aqf=None, akf=None, bias_fn=bias_fn, vf=vf, vkey=("V", "x"), dv=128,
                                   mode="exp", fin=fin, sbanks=[0, 1], obanks=[(4, 5)] if c == 0 else [(6, 7)], augkey=("HB", slot)))
        first_pass.append(passes[0])
        b0, b1 = passes[0].blocks(), passes[1].blocks()
        for j in range(4):
            blocks += [b for b in b0 if b[1] == j]
            blocks += [b for b in b1 if b[1] == j]
    for u in range(4):
        first_pass[u].pre = (pres[0:4] if u == 0 else (pres[2 * u + 2:2 * u + 4] if u < 3 else None))
    run_blocks(k, blocks)
    S.barrier()


def fox_branch(k, l):
    S, d = k.S, k.d
    QT, KT, VO, OF = DU(0), DU(1), DU(2), NU(2)
    V = k.bf(VO, 16 * 8 * 65).rearrange("p (t h e) -> p t h e", t=16, h=8)
    WFF = k.bf(W_W + 4096, 64).rearrange("p (k c) -> p k c", k=8)
    S.op("dve", lambda e: e.memset(k.bf(VO, 16 * 8 * 65), 1.0), writes=[("V", "all")])
    S.dma("pool", "wff", lambda e: e.dma_start(out=WFF, in_=d["w_in"][l, :, :, C_FF:C_FF + 8]), writes=["WFF"])
    S.barrier()
    wcol_jobs(k, l, [
        (C_FQ, 512, h_fm(QT)(k, 0)),
        (C_FK, 512, h_fm(KT, scale=0.125)(k, 0)),
        (C_FV, 512, h_tm(k, lambda tt: V[:, tt, :, :], 8, 64)),
    ])
    for t in range(NT):
        for kc in range(NKC):
            S.op("pe", lambda e, kc=kc, t=t: e.matmul(k.bank(t)[0:8, :], WFF[:, kc, :], k.H(kc, t * 512, 512),
                                                      start=(kc == 0), stop=(kc == NKC - 1)),
                 reads=["WFF", ("H", kc, t)], writes=[("ps", t)])
    S.barrier()
    T1 = k.f32(W_W, 2048)
    T2 = k.f32(W_W + 2048, 2048)
    ONES = k.f32(W_W + 7168, 2048)
    S.op("dve", lambda e: e.memset(ONES[0:8, :], 1.0), writes=["ONES"])
    softplus_neg(k, T1, 8, [0, 1, 2, 3], k.gcols[0:8, 4:5], "T1")
    S.op("dve", lambda e: e.tensor_tensor_scan(T2[0:8, :], ONES[0:8, :], T1[0:8, :], 0.0, ALU.mult, ALU.add), reads=["T1", "ONES"], writes=["T2"])
    S.op("dve", lambda e: e.tensor_scalar(T1[0:8, :], T2[0:8, :], -1.0, None, ALU.mult), reads=["T2"], writes=["T1"])
    build_aug(k, T2, "T2", T1, "T1", 8, NU(2), "fox")
    S.barrier()
    k.tbanks = [3, 6, 7]
    OTB = [[k.f32(SB + (p * 4 + r) * 128, 128) for r in range(4)] for p in range(2)]
    blocks = []
    pres = []
    first_pass = []
    hb_init(k)
    for pr in range(4):
        passes = []
        for c in range(2):
            hd = 2 * pr + c
            slot = c + 2 * (pr % 2)
            qb, kb = hb_views(k, slot)

            def qf(t0, n, qb=qb):
                return qb[:, t0:t0 + n]

            def kf(i, kb=kb):
                return kb[:, i * 128:(i + 1) * 128]

            pres.append(lambda pr=pr, c=c, slot=slot, hd=hd: hb_build(k, slot, c, fm(k, QT, pr), fm(k, KT, pr),
                                                                      k.d["augq_fox"][hd, :, :], k.d["augk_fox"][hd, :, :], 6, "sp"))

            def vf(i, hd=hd):
                return V[:, i, hd, :]

            def fin(j, r, o, okey, pr=pr, c=c):
                rc, rk = col(k)
                ot = OTB[j % 2][r]
                otk = ("OTB", j % 2, r)
                S.op("dve", lambda e: e.reciprocal(rc, o[:, 64:65]), reads=[okey], writes=[rk])
                S.op("dve", lambda e: e.tensor_scalar(ot[:, c * 64:(c + 1) * 64], o[:, 0:64], rc, None, ALU.mult), reads=[okey, rk], writes=[otk])
                if c == 1:
                    tt = 4 * j + r
                    defer(k, 3, lambda: transpose_to_fm(k, ot, otk, fm(k, OF, pr, tt * 128, 128), ("fm", OF, pr, tt)))
            passes.append(AttnPass(k, qf=qf, kf=kf, aqf=None, akf=None, bias_fn=None, vf=vf, vkey=("V", "x"), dv=64,
                                   mode="exp", fin=fin, sbanks=[0, 1, 2], obanks=[(4,)] if c == 0 else [(5,)], augkey=("HB", slot)))
        first_pass.append(passes[0])
        b0, b1 = passes[0].blocks(), passes[1].blocks()
        for j in range(4):
            blocks += [b for b in b0 if b[1] == j]
            blocks += [b for b in b1 if b[1] == j]
    for u in range(4):
        first_pass[u].pre = (pres[0:4] if u == 0 else (pres[2 * u + 2:2 * u + 4] if u < 3 else None))
    run_blocks(k, blocks)
    S.barrier()


def mlstm_branch(k, l):
    S, d = k.S, k.d
    QT, KT, VT, MX, XC, SZ, VA = DU(0), DU(1), DU(2), DU(3), NU(0), NU(1), NU(2)
    Vall = k.bf(VA, 16 * 4 * 129).rearrange("p (t h e) -> p t h e", t=16, h=4)
    WQKV = k.bf(W_W + 4096, 1536).rearrange("p (g h e) -> p g h e", g=3, h=4)
    WIF = k.bf(W_W + 4096 + 768, 96).rearrange("p (j o) -> p j o", j=12)
    S.op("dve", lambda e: e.memset(k.bf(VA, 16 * 4 * 129), 1.0), writes=[("V", "all")])
    S.dma("pool", "wqkv", lambda e: e.dma_start(out=k.bf(W_W + 4096, 1536), in_=d["wqkv"][l, :, :]), writes=["WQKV"])
    S.dma("pool", "wqkv", lambda e: e.dma_start(out=k.bf(W_W + 4096 + 768, 96), in_=d["wif"][l, :, :]), writes=["WIF"])
    S.barrier()
    wcol_jobs(k, l, [
        (C_MX, 512, h_fm(MX)(k, 0)),
        (C_MZ, 512, h_fm(SZ, func=AF.Silu)(k, 0)),
    ])
    ACC = k.f32(SB, 2048)
    for c in range(4):
        mx = fm(k, MX, c)
        mkeys = [("fm", MX, c, t) for t in range(NT)]
        w = lambda j, c=c: k.convw[:, j * 4 + c:j * 4 + c + 1]
        S.op("dve", lambda e, mx=mx, c=c, w=w: e.tensor_scalar(ACC, mx, w(3), k.convb[:, c:c + 1], ALU.mult, ALU.add),
             reads=mkeys + ["lconst"], writes=["ACC"])
        for sh in (1, 2, 3):
            S.op("dve", lambda e, mx=mx, sh=sh, w=w: e.scalar_tensor_tensor(
                out=ACC[:, sh:], in0=mx[:, 0:S_LEN - sh], scalar=w(3 - sh), in1=ACC[:, sh:], op0=ALU.mult, op1=ALU.add),
                reads=mkeys + ["ACC", "lconst"], writes=["ACC"])
        S.op("act", lambda e, c=c: e.activation(out=fm(k, XC, c), in_=ACC, func=AF.Silu), reads=["ACC"],
             writes=[("fm", XC, c, t) for t in range(NT)])
    for h in range(4):
        for t in range(NT):
            for g, src, dst in ((0, XC, QT), (1, XC, KT), (2, MX, VT)):
                b = next_bank(k)
                S.op("pe", lambda e, b=b, g=g, src=src, h=h, t=t: e.matmul(
                    k.bank(b), WQKV[:, g, h, :], fm(k, src, h, t * 512, 512), start=True, stop=True),
                    reads=["WQKV", ("fm", src, h, t)], writes=[("ps", b)])
                evac(k, fm(k, dst, h, t * 512, 512), k.bank(b), ("ps", b), ("fm", dst, h, t))
        for tt in range(NT128):
            b = next_bank(k)
            S.op("pe", lambda e, b=b, h=h, tt=tt: e.matmul(
                k.bank(b)[:, 0:128], fm(k, MX, h, tt * 128, 128), WQKV[:, 2, h, :], start=True, stop=True),
                reads=["WQKV", ("fm", MX, h, tt // 4)], writes=[("ps", b)])
            evac(k, Vall[:, tt, h, 0:128], k.bank(b)[:, 0:128], ("ps", b), ("V", tt, h))
    S.barrier()
    for t in range(NT):
        for g, bb in ((0, t), (1, 4 + t)):
            for jj in range(12):
                src = (QT, KT, VT)[jj // 4]
                S.op("pe", lambda e, g=g, bb=bb, jj=jj, src=src, t=t: e.matmul(
                    k.bank(bb)[0:4, :], WIF[:, jj, g * 4:(g + 1) * 4], fm(k, src, jj % 4, t * 512, 512),
                    start=(jj == 0), stop=(jj == 11)), reads=["WIF"], writes=[("ps", bb)])
    T1 = k.f32(W_W, 2048)
    T2 = k.f32(W_W + 2048, 2048)
    T3 = k.f32(W_W + 5120, 2048)
    ONES = k.f32(W_W + 7168, 2048)
    S.op("dve", lambda e: e.memset(ONES[0:4, :], 1.0), writes=["ONES"])
    softplus_neg(k, T1, 4, [4, 5, 6, 7], k.gcols[0:4, 5:6], "T1")
    S.op("dve", lambda e: e.tensor_tensor_scan(T2[0:4, :], ONES[0:4, :], T1[0:4, :], 0.0, ALU.mult, ALU.add), reads=["T1", "ONES"], writes=["T2"])
    for t in range(NT):
        S.op("dve", lambda e, t=t: e.tensor_scalar(T1[0:4, t * 512:(t + 1) * 512], k.bank(t)[0:4, :], k.gcols[0:4, 1:2], None, ALU.add),
             reads=[("ps", t), "gcols", "T2"], writes=["T1"])
    S.op("dve", lambda e: e.tensor_tensor(out=T1[0:4, :], in0=T1[0:4, :], in1=T2[0:4, :], op=ALU.add), reads=["T1", "T2"], writes=["T1"])
    S.op("dve", lambda e: e.tensor_tensor_scan(T3[0:4, :], ONES[0:4, :], T1[0:4, :], 0.0, ALU.mult, ALU.max), reads=["T1", "ONES"], writes=["T3"])
    S.op("dve", lambda e: e.tensor_scalar(T3[0:4, :], T3[0:4, :], -1.0, None, ALU.mult), reads=["T3"], writes=["T3"])
    UR = ONES
    EM = k.f32(W_CONST + 2700, 64)
    UC = k.f32(W_CONST + 2780, 160)
    for j in range(NT):
        nb = T3[0:4, 512 * j - 1:512 * j] if j > 0 else 0.0
        S.op("act", lambda e, j=j, nb=nb: e.activation(out=T2[0:4, j * 512:(j + 1) * 512], in_=T2[0:4, j * 512:(j + 1) * 512],
                                                     func=AF.Exp, bias=nb, scale=1.0), reads=["T2", "T3"], writes=["T2"])
    for tt in range(NT128):
        S.op("pe", lambda e, tt=tt: e.transpose(k.bank(3)[:, tt * 4:(tt + 1) * 4], T2[0:4, tt * 128:(tt + 1) * 128], k.ident[0:4, 0:4]),
             reads=["T2", "ident"], writes=[("ps", 3)])
    S.op("dve", lambda e: e.tensor_copy(EM, k.bank(3)[:, 0:64]), reads=[("ps", 3)], writes=["EM"])
    for j in range(NT):
        nb = T3[0:4, 512 * j - 1:512 * j] if j > 0 else 0.0
        ncol = (j + 1) * 512
        S.op("act", lambda e, nb=nb, ncol=ncol: e.activation(out=UR[0:4, 0:ncol], in_=T1[0:4, 0:ncol], func=AF.Exp, bias=nb, scale=1.0),
             reads=["T1", "T3", "ONES"], writes=["ONES"])
        for i in range(4 * j + 4):
            idx = 2 * j * (j + 1) + i
            S.op("pe", lambda e, i=i, idx=idx: e.transpose(k.bank(2)[:, idx * 4:(idx + 1) * 4], UR[0:4, i * 128:(i + 1) * 128], k.ident[0:4, 0:4]),
                 reads=["ONES", "ident"], writes=[("ps", 2)])
    S.op("dve", lambda e: e.tensor_scalar(UC, k.bank(2)[:, 0:160], 128.0 ** -0.5, None, ALU.mult), reads=[("ps", 2)], writes=["UC"])
    S.barrier()
    k.tbanks = [3, 6, 7]
    TB = SB + 1024
    blocks = []
    for h in range(4):

        def qf(t0, n, h=h):
            return fm(k, QT, h, t0, n)

        def kf(i, h=h):
            return fm(k, KT, h, i * 128, 128)

        def vf(i, h=h):
            return Vall[:, i, h, :]

        def fin(j, r, o, okey, h=h):
            tt = 4 * j + r
            c1, k1 = col(k)
            c2, k2 = col(k)
            k._fp = getattr(k, "_fp", 0) + 1
            n = k._fp
            HH = k.f32(TB + (n % 4) * 128, 128)
            SQ = k.f32(TB + 512, 128)
            TF = k.f32(TB + 640 + (n % 2) * 128, 128)
            HN = k.f32(TB + 896 + (n % 4) * 128, 128)
            hk, sk, nk, fk = ("HH", n % 4), ("SQ", 0), ("HN", n % 4), ("TF", n % 2)
            S.op("dve", lambda e: e.tensor_copy(c2, o[:, 128:129]), reads=[okey], writes=[k2])
            S.op("dve", lambda e: e.scalar_tensor_tensor(out=c1, in0=c2, scalar=-1.0, in1=c2, op0=ALU.mult, op1=ALU.max), reads=[k2], writes=[k1])
            S.op("dve", lambda e: e.tensor_tensor(out=c1, in0=c1, in1=EM[:, tt * 4 + h:tt * 4 + h + 1], op=ALU.max), reads=[k1, "EM"], writes=[k1])
            S.op("dve", lambda e: e.reciprocal(c1, c1), reads=[k1], writes=[k1])
            S.op("dve", lambda e: e.tensor_scalar(HH, o[:, 0:128], c1, None, ALU.mult), reads=[okey, k1], writes=[hk])
            S.op("dve", lambda e: e.reduce_sum(out=c2, in_=HH, axis=AX.X), reads=[hk], writes=[k2])
            S.op("dve", lambda e: e.tensor_scalar(c2, c2, -1.0 / 128, None, ALU.mult), reads=[k2], writes=[k2])
            S.op("dve", lambda e: e.tensor_scalar(HH, HH, c2, None, ALU.add), reads=[hk, k2], writes=[hk])
            S.op("dve", lambda e: e.tensor_tensor(out=SQ, in0=HH, in1=HH, op=ALU.mult), reads=[hk], writes=[sk])
            S.op("dve", lambda e: e.reduce_sum(out=c2, in_=SQ, axis=AX.X), reads=[sk], writes=[k2])

            def stage_b():
                S.op("act", lambda e: e.activation(out=c2, in_=c2, func=AF.Ln, bias=LN_EPS, scale=1.0 / 128), reads=[k2], writes=[k2])
                S.op("act", lambda e: e.activation(out=c2, in_=c2, func=AF.Exp, scale=-0.5), reads=[k2], writes=[k2])

            def post(pq, pkey):
                xc = fm(k, XC, h, tt * 128, 128)
                sz = fm(k, SZ, h, tt * 128, 128)
                S.op("dve", lambda e: e.scalar_tensor_tensor(out=TF, in0=xc, scalar=k.skipc[:, h:h + 1], in1=pq, op0=ALU.mult, op1=ALU.add),
                     reads=[pkey, "lconst"], writes=[fk])
                S.op("dve", lambda e: e.tensor_tensor(out=sz, in0=TF, in1=sz, op=ALU.mult), reads=[fk], writes=[("fm", SZ, h, tt)])

            def stage_c():
                S.op("dve", lambda e: e.scalar_tensor_tensor(out=HN, in0=HH, scalar=c2, in1=k.normB[:, h * 128:(h + 1) * 128], op0=ALU.mult, op1=ALU.mult),
                     reads=[hk, k2, "normB"], writes=[nk])
                defer(k, 2, lambda: transpose_to_fm(k, HN, nk, None, None, post=post))
            defer(k, 2, stage_b)
            defer(k, 3, stage_c)
        def scale_fn(i, j, h=h):
            idx = 2 * j * (j + 1) + i
            return UC[:, idx * 4 + h:idx * 4 + h + 1]
        ps_ = AttnPass(k, qf=qf, kf=kf, aqf=None, akf=None, bias_fn=None, vf=vf, vkey=("V", "x"), dv=128, mode="exp", fin=fin,
                       sbanks=[0, 1, 2], obanks=[(4, 5)], act_func=AF.Copy, scale_fn=scale_fn, mask_mul=True)
        blocks += ps_.blocks()
    run_blocks(k, blocks, L=2)
    S.barrier()


def merge_and_out(k, l):
    S, d = k.S, k.d
    OB = [NU(0), NU(2), NU(1)]
    MG = DU(0)
    GT = [k.f32(SB + i * 512, 512) for i in range(2)]
    ACC = k.f32(SB + 1024, 512)
    PB = k.f32(SB + 1536, 512)

    def load_mp(dd):
        s = dd % 2
        tile = k.bf(W_W + s * 2304, 4608)
        S.dma("pool", f"mp{s}", lambda e: e.dma_start(out=tile, in_=d["mpack"][l, dd, :, :]), writes=[("MP", s)])
        return tile

    tiles = {0: load_mp(0), 1: load_mp(1)}
    cnt = 0
    for dd in range(NKC):
        MP = tiles[dd]
        mkey = ("MP", dd % 2)
        for t in range(NT):
            for b in range(3):
                bg, bo = next_bank(k), next_bank(k)
                for kc in range(NKC):
                    S.op("pe", lambda e, bg=bg, b=b, kc=kc, t=t, MP=MP: e.matmul(
                        k.bank(bg), MP[:, b * 1536 + kc * 128:b * 1536 + (kc + 1) * 128], k.H(kc, t * 512, 512),
                        start=(kc == 0), stop=(kc == NKC - 1)), reads=[mkey, ("H", kc, t)], writes=[("ps", bg)])
                for kc in range(4):
                    S.op("pe", lambda e, bo=bo, b=b, kc=kc, t=t, MP=MP: e.matmul(
                        k.bank(bo), MP[:, b * 1536 + 1024 + kc * 128:b * 1536 + 1024 + (kc + 1) * 128], fm(k, OB[b], kc, t * 512, 512),
                        start=(kc == 0), stop=(kc == 3)), reads=[mkey], writes=[("ps", bo)])
                gt = GT[cnt % 2]
                gk = ("GT", cnt % 2)
                cnt += 1
                gbc = k.gb[:, b * 8 + dd:b * 8 + dd + 1]
                S.op("act", lambda e, gt=gt, bg=bg, gbc=gbc: e.activation(out=gt, in_=k.bank(bg), func=AF.Sigmoid, bias=gbc, scale=1.0),
                     reads=[("ps", bg), "lconst"], writes=[gk])
                if b == 0:
                    S.op("dve", lambda e, gt=gt, bo=bo: e.tensor_tensor(out=ACC, in0=gt, in1=k.bank(bo), op=ALU.mult),
                         reads=[gk, ("ps", bo)], writes=["MACC"])
                else:
                    S.op("dve", lambda e, gt=gt, bo=bo: e.tensor_tensor(out=PB, in0=gt, in1=k.bank(bo), op=ALU.mult),
                         reads=[gk, ("ps", bo)], writes=["MPB"])
                    dst = ACC if b == 1 else fm(k, MG, dd, t * 512, 512)
                    dkey = "MACC" if b == 1 else ("fm", MG, dd, t)
                    S.op("dve", lambda e, dst=dst: e.tensor_tensor(out=dst, in0=ACC, in1=PB, op=ALU.add),
                         reads=["MACC", "MPB"], writes=[dkey] + (["MACC"] if b == 2 else []))
        if dd + 2 < NKC:
            tiles[dd + 2] = load_mp(dd + 2)
    S.barrier()
    MN = NU(0)
    for i in range(4):
        S.op("dve" if i % 2 == 0 else "pool", lambda e, i=i: e.tensor_copy(k.bf(MN + i * 2048, 4096), k.bf(MG + i * 2048, 4096)),
             writes=[("MN", i)])
    S.barrier()
    XS = [k.f32(SB + i * 512, 512) for i in range(2)]

    WOT = [k.bf(W_W + dd * 512, 1024) for dd in range(NKC)]
    for dd in range(NKC):
        S.dma("pool", "wo", lambda e, dd=dd: e.dma_start(out=WOT[dd], in_=d["wout"][l, dd, :, :]), writes=[("WO", dd)])
    cnt = 0
    for t in range(NT):
        for dd in range(NKC):
            WO = WOT[dd]
            b = next_bank(k)
            xs = XS[cnt % 2]
            xk = ("XS", cnt % 2)
            cnt += 1
            S.dma("sp", f"xs{cnt % 2}", lambda e, xs=xs, dd=dd, t=t: e.dma_start(out=xs, in_=d["xspill"][:, dd, t * 512:(t + 1) * 512]),
                  writes=[xk])
            for kc in range(NKC):
                S.op("pe", lambda e, b=b, kc=kc, t=t, WO=WO: e.matmul(
                    k.bank(b), WO[:, kc * 128:(kc + 1) * 128], fm(k, MN, kc, t * 512, 512),
                    start=(kc == 0), stop=(kc == NKC - 1)), reads=[("WO", dd)], writes=[("ps", b)])
            S.op("dve", lambda e, b=b, xs=xs, dd=dd, t=t: e.tensor_tensor(out=k.X(dd, t), in0=k.bank(b), in1=xs, op=ALU.add),
                 reads=[("ps", b), xk], writes=[("X", dd, t)])


def spill_x(k):
    S, d = k.S, k.d
    for c in range(NKC):
        o = W_X + c * S_LEN
        S.dma("sp", "xsp", lambda e, c=c, o=o: e.dma_start(out=d["xspill"][:, c, :], in_=k.arena[:, o:o + S_LEN]),
              reads=[("X", c, t) for t in range(NT)], writes=["xspill"])


def mixer(k, l, branches=("ml", "diff", "fox")):
    S = k.S
    layer_consts(k, l)
    rmsnorm(k, l * 3 + 1, "H")
    spill_x(k)
    S.barrier()
    if "ml" in branches:
        mlstm_branch(k, l)
    if "diff" in branches:
        diff_branch(k, l)
    if "fox" in branches:
        fox_branch(k, l)
    merge_and_out(k, l)


def _chunk_rows(w):
    K_, N_ = w.shape
    return w.reshape(K_ // 128, 128, N_).transpose(1, 0, 2)


def _cols(v):
    return v.reshape(-1, 128).T


def prep_shared(inp):
    f = np.float32
    sh = {}
    for which in (1, 2):
        wg, wu, wd = inp[f"ffn{which}_w_gate"], inp[f"ffn{which}_w_up"], inp[f"ffn{which}_w_down"]
        gu = np.empty((DEPTH, NFC, 128, 2, NKC, 128), f)
        for l in range(DEPTH):
            for g, w in enumerate((wg[l], wu[l])):
                gu[l, :, :, g] = w.reshape(NKC, 128, NFC, 128).transpose(2, 1, 0, 3)
        sh[f"ffn{which}_gu"] = gu.reshape(DEPTH, NFC, 128, 2 * NKC * 128)
        sh[f"ffn{which}_wd"] = np.ascontiguousarray(wd.reshape(DEPTH, NFC, 128, D))
    vecs = []
    for l in range(DEPTH):
        vecs += [inp["ffn1_norm"][l], inp["mix_norm"][l], inp["ffn2_norm"][l]]
    vecs.append(inp["final_norm"])
    sh["normw"] = np.ascontiguousarray(np.stack([_cols(v) for v in vecs], axis=1).reshape(128, 56)).astype(f)
    sh["ident"] = np.eye(128, dtype=f)
    sh["ones"] = np.ones((128, 128), f)
    sh["tri"] = np.triu(np.ones((128, 128), f))
    sh["negtri"] = (np.tril(np.ones((128, 128), f), -1) * NEG_BIG).astype(f)
    w_in = inp["w_in"]
    sh["w_in"] = np.ascontiguousarray(np.stack([_chunk_rows(w_in[l]) for l in range(DEPTH)]))
    gb = np.empty((DEPTH, 128, 24), f)
    cw = np.empty((DEPTH, 128, 16), f)
    cb = np.empty((DEPTH, 128, 4), f)
    sk = np.empty((DEPTH, 128, 4), f)
    gc = np.zeros((DEPTH, 8, 8), f)
    for l in range(DEPTH):
        for b in range(3):
            gb[l, :, b * 8:(b + 1) * 8] = _cols(inp["gate_bias"][l, b])
        for j in range(4):
            cw[l, :, j * 4:(j + 1) * 4] = _cols(inp["mlstm_conv_w"][l, j])
        cb[l] = _cols(inp["mlstm_conv_b"][l])
        sk[l] = _cols(inp["mlstm_skip"][l])
        gc[l, :, 0] = inp["fox_b_f"][l]
        gc[l, 0:4, 1] = inp["mlstm_b_if"][l, 0:4]
        gc[l, 0:4, 2] = inp["mlstm_b_if"][l, 4:8]
    sh["gbcols"], sh["convw"], sh["convb"], sh["skipc"], sh["gcols"] = gb, cw, cb, sk, gc
    sh["difflam"] = np.ascontiguousarray(np.concatenate(
        [inp["diff_lq1"], inp["diff_lk1"], inp["diff_lq2"], inp["diff_lk2"]], axis=1)).astype(f)
    sh["diff_subln"] = np.ascontiguousarray(inp["diff_subln"]).astype(f)
    sh["mlstm_norm"] = np.ascontiguousarray(inp["mlstm_norm"]).astype(f)
    slopes = [2.0 ** (-8.0 * (h + 1) / 4) for h in range(4)]
    ak = np.zeros((3, 4, 128), f)
    aq = np.zeros((3, 4, 512), f)
    relq = np.arange(512)
    for h in range(4):
        ak[0, h] = slopes[h] * np.arange(128)
        ak[1, h] = 1.0
        ak[2, h] = 1.0
        aq[0, h] = 1.0
        aq[1, h] = -slopes[h] * (128 * (relq // 128))
        aq[2, h] = -slopes[h] * (relq % 128)
    sh["alibi_k"] = ak.reshape(3, 512)
    sh["alibi_q"] = aq.reshape(3, 2048)
    tpos = np.arange(S_LEN)
    akf = np.zeros((4, 3, S_LEN), f)
    aqf = np.zeros((4, 3, S_LEN), f)
    for h in range(4):
        akf[h, 0] = slopes[h] * (tpos % 128)
        akf[h, 1] = 1.0
        akf[h, 2] = 1.0
        aqf[h, 0] = 1.0
        aqf[h, 1] = -slopes[h] * (128 * ((tpos % 512) // 128))
        aqf[h, 2] = -slopes[h] * (tpos % 128)
    sh["alibi_kf"], sh["alibi_qf"] = akf, aqf
    wqkv = np.empty((DEPTH, 128, 3, 4, 128), f)
    for g, nm in enumerate(("mlstm_wq", "mlstm_wk", "mlstm_wv")):
        wqkv[:, :, g] = inp[nm].transpose(0, 2, 1, 3)
    sh["wqkv"] = wqkv.reshape(DEPTH, 128, 1536)
    sh["wif"] = np.ascontiguousarray(inp["mlstm_w_if"].reshape(DEPTH, 12, 128, 8).transpose(0, 2, 1, 3)).reshape(DEPTH, 128, 96)
    mp = np.empty((DEPTH, NKC, 128, 3, 1536), f)
    wbs = (inp["w_branch_diff"], inp["w_branch_fox"], inp["w_branch_mlstm"])
    for l in range(DEPTH):
        for b in range(3):
            g = w_in[l][:, C_G + b * D:C_G + (b + 1) * D]
            mp[l, :, :, b, 0:1024] = g.reshape(NKC, 128, NKC, 128).transpose(2, 1, 0, 3).reshape(NKC, 128, 1024)
            mp[l, :, :, b, 1024:1536] = wbs[b][l].reshape(4, 128, NKC, 128).transpose(2, 1, 0, 3).reshape(NKC, 128, 512)
    sh["mpack"] = mp.reshape(DEPTH, NKC, 128, 4608)
    wo = np.empty((DEPTH, NKC, 128, 1024), f)
    for l in range(DEPTH):
        wo[l] = inp["w_out"][l].reshape(NKC, 128, NKC, 128).transpose(2, 1, 0, 3).reshape(NKC, 128, 1024)
    sh["wout"] = wo
    return sh


def build_program(shapes, plan):
    from contextlib import ExitStack
    nc = bass.Bass("TRN2", target_bir_lowering=False)
    dram = {}
    for name, shp in shapes.items():
        dram[name] = nc.dram_tensor(name, list(shp), F32, kind="ExternalInput").ap()
    dram["outT"] = nc.dram_tensor("outT", [128, NKC, S_LEN], F32, kind="ExternalOutput").ap()
    dram["xspill"] = nc.dram_tensor("xspill", [128, NKC, S_LEN], F32, kind="Internal").ap()
    for nm in ("augk_fox", "augq_fox", "augk_ml", "augq_ml"):
        dram[nm] = nc.dram_tensor(nm, [8, 6, S_LEN], BF16, kind="Internal").ap()
    with ExitStack() as es:
        sems = [es.enter_context(nc.semaphore(f"s{i}")) for i in range(70)]
        S = Sched(nc, sems)
        arena = nc.alloc_sbuf_tensor("arena", [128, ARENA_WORDS], F32)
        ps = es.enter_context(nc.psum_tensor("ps", [128, 4096], F32))
        k = K(nc, S, arena, ps, dram)
        plan(k)
        with nc.Block() as block:
            S.emit(block)
    return nc


def full_plan(k):
    build_consts(k)
    load_x(k)
    for l in range(DEPTH):
        rmsnorm(k, l * 3 + 0, "H")
        ffn(k, l, 1)
        mixer(k, l)
        rmsnorm(k, l * 3 + 2, "H")
        k.S.barrier()
        ffn(k, l, 2)
    rmsnorm(k, 6, "X")
    store_out(k)


def run(inputs, plan=full_plan, trace=False, cores=NB):
    sh = prep_shared(inputs)
    x = np.asarray(inputs["x"], np.float32)
    in_maps = []
    for b in range(cores):
        m = dict(sh)
        m["xT"] = np.ascontiguousarray(x[b].T.reshape(NKC, 128, S_LEN).transpose(1, 0, 2))
        in_maps.append(m)
    shapes = {n: a.shape for n, a in in_maps[0].items()}
    nc = build_program(shapes, plan)
    res = run_bass_kernel_spmd(nc, in_maps, core_ids=list(range(cores)), trace=trace)
    out = np.empty((cores, S_LEN, D), np.float32)
    for b in range(cores):
        o = res.results[b]["outT"]
        out[b] = o.transpose(1, 0, 2).reshape(D, S_LEN).T
    return out, res


def kernel(**inputs):
    out, _ = run(inputs)
    return out
```

```python
import bisect
import numpy as np
import concourse.bass as bass
import concourse.mybir as mybir
from concourse.bass_utils import run_bass_kernel_spmd

F32 = mybir.dt.float32
BF16 = mybir.dt.bfloat16
AF = mybir.ActivationFunctionType
ALU = mybir.AluOpType
AX = mybir.AxisListType


class _Ev:
    __slots__ = ("eng", "idx", "val", "clock")

    def __init__(self, eng, idx, val, clock):
        self.eng, self.idx, self.val, self.clock = eng, idx, val, clock


class _Eng:
    def __init__(self, name, sem, self_sync):
        self.name, self.sem, self.self_sync = name, sem, self_sync
        self.ops = []
        self.count = 0
        self.sig_idx = []
        self.sig_val = []
        self.n_inst = 0
        self.inst_rec = []
        self.clock = {}
        self.last_compute = None


class _Slot:
    def __init__(self, name, sem):
        self.name, self.sem, self.total = name, sem, 0


class _Res:
    __slots__ = ("w", "rs")

    def __init__(self):
        self.w, self.rs = None, []


class Sched:
    def __init__(self, nc, sems):
        self.nc = nc
        self._sems = list(sems)
        self.engs = {}
        for name, ss in (("pe", False), ("act", True), ("dve", True), ("pool", True), ("sp", False)):
            self.engs[name] = _Eng(name, self._sems.pop(), ss)
        self.slots = {}
        self.resd = {}

    def slot(self, name):
        s = self.slots.get(name)
        if s is None:
            s = _Slot(name, self._sems.pop())
            self.slots[name] = s
        return s

    def res(self, key):
        r = self.resd.get(key)
        if r is None:
            r = _Res()
            self.resd[key] = r
        return r

    def _value(self, ev):
        if ev.val is not None:
            return ev.val
        E = self.engs[ev.eng]
        j = bisect.bisect_left(E.sig_idx, ev.idx)
        if j < len(E.sig_idx):
            return E.sig_val[j]
        E.count += 1
        rec = E.inst_rec[ev.idx]
        rec[2], rec[3] = E.sem, 1
        E.sig_idx.append(ev.idx)
        E.sig_val.append(E.count)
        ev.val = E.count
        return ev.val

    def _wait_for(self, E, evs):
        need = {}
        for ev in evs:
            if ev is None:
                continue
            if ev.eng == E.name and not E.self_sync:
                continue
            if ev.eng in self.slots:
                v = self.slots[ev.eng].total
            else:
                v = self._value(ev)
            if E.clock.get(ev.eng, 0) >= v:
                continue
            if need.get(ev.eng, (0, None))[0] < v:
                need[ev.eng] = (v, ev)
        for name, (v, ev) in need.items():
            if E.clock.get(name, 0) >= v:
                continue
            sem = self.slots[name].sem if name in self.slots else self.engs[name].sem
            E.ops.append(["w", sem, v])
            E.clock[name] = v
            for k, cv in ev.clock.items():
                if E.clock.get(k, 0) < cv:
                    E.clock[k] = cv

    def _deps(self, reads, writes):
        evs = []
        for k in reads:
            r = self.res(k)
            if r.w is not None:
                evs.append(r.w)
        for k in writes:
            r = self.res(k)
            if r.w is not None:
                evs.append(r.w)
            evs.extend(r.rs)
        return evs

    def _commit(self, ev, reads, writes):
        for k in reads:
            self.res(k).rs.append(ev)
        for k in writes:
            r = self.res(k)
            r.w, r.rs = ev, []

    def op(self, eng, fn, reads=(), writes=()):
        E = self.engs[eng]
        self._wait_for(E, self._deps(reads, writes))
        rec = ["i", fn, None, 0]
        E.ops.append(rec)
        E.inst_rec.append(rec)
        idx = E.n_inst
        E.n_inst += 1
        E.last_compute = idx
        clk = dict(E.clock)
        ev = _Ev(eng, idx, None, clk)
        self._commit(ev, reads, writes)
        return ev

    def dma(self, queue, slot, fn, reads=(), writes=()):
        E = self.engs[queue]
        S = self.slot(slot) if isinstance(slot, str) else slot
        self._wait_for(E, self._deps(reads, writes))
        S.total += 16
        rec = ["i", fn, S.sem, 16]
        E.ops.append(rec)
        E.inst_rec.append(rec)
        E.n_inst += 1
        ev = _Ev(S.name, -1, S.total, dict(E.clock))
        self._commit(ev, reads, writes)
        return ev

    def barrier(self):
        evs = []
        for E in self.engs.values():
            if E.last_compute is not None:
                ev = _Ev(E.name, E.last_compute, None, dict(E.clock))
                self._value(ev)
                evs.append(ev)
        for S in self.slots.values():
            if S.total:
                evs.append(_Ev(S.name, -1, S.total, {}))
        for E in self.engs.values():
            self._wait_for(E, evs)
        self.resd = {}

    def wait_all_dma(self, eng, slots):
        E = self.engs[eng]
        evs = [_Ev(self.slots[s].name, -1, self.slots[s].total, {}) for s in slots if self.slots[s].total]
        self._wait_for(E, evs)

    def emit(self, block):
        def replay(E):
            def run(e):
                for rec in E.ops:
                    if rec[0] == "w":
                        e.wait_ge(rec[1], rec[2])
                    else:
                        ins = rec[1](e)
                        if rec[2] is not None:
                            ins.then_inc(rec[2], rec[3])
            return run
        block.tensor(replay(self.engs["pe"]))
        block.scalar(replay(self.engs["act"]))
        block.vector(replay(self.engs["dve"]))
        block.gpsimd(replay(self.engs["pool"]))
        block.sync(replay(self.engs["sp"]))


D = 1024
S_LEN = 2048
NB = 8
DEPTH = 2
DFF = 2816
NFC = DFF // 128
NKC = D // 128
NT = S_LEN // 512
NT128 = S_LEN // 128
RMS_EPS = 1e-6
LN_EPS = 1e-5
N_IN = 7176
FFN_GROUPS = [(0, 6), (6, 12), (12, 17), (17, 22)]

W_X = 0
W_H = W_X + 16384
W_CONST = W_H + 8192
W_PT = W_CONST + 3072
W_W = W_PT + 1024
W_N = W_W + 9216
W_END = W_N + 12288
ARENA_WORDS = W_END


class K:
    def __init__(self, nc, S, arena, ps, dram):
        self.nc, self.S, self.arena, self.ps, self.d = nc, S, arena, ps, dram
        self.ps_rr = 0

    def f32(self, off, n):
        return self.arena[:, off:off + n]

    def bf(self, off, n):
        return self.arena[:, off:off + (n + 1) // 2].bitcast(BF16)

    def bank(self, b):
        return self.ps[:, b * 512:(b + 1) * 512]

    def X(self, c, t):
        o = W_X + c * S_LEN + t * 512
        return self.arena[:, o:o + 512]

    def H(self, c, t0=0, n=S_LEN):
        return self.bf(W_H + c * (S_LEN // 2), S_LEN)[:, t0:t0 + n]

    def PT(self, i):
        return self.bf(W_PT + i * 256, 512)


def build_consts(k):
    S, d = k.S, k.d
    o = W_CONST
    k.ident = k.f32(o, 128); o += 128
    k.ones_bf = k.bf(o, 128); o += 64
    k.tri_bf = k.bf(o, 128); o += 64
    k.normw = k.f32(o, 7 * 8); o += 56
    k.const_end = o
    S.dma("sp", "c0", lambda e: e.dma_start(out=k.ident, in_=d["ident"][:, :]), writes=["ident"])
    S.dma("pool", "c1", lambda e: e.dma_start(out=k.ones_bf, in_=d["ones"][:, :]), writes=["ones"])
    S.dma("pool", "c1", lambda e: e.dma_start(out=k.tri_bf, in_=d["tri"][:, :]), writes=["tri"])
    S.dma("sp", "c0", lambda e: e.dma_start(out=k.normw, in_=d["normw"][:, :]), writes=["normw"])


def load_x(k):
    S, d = k.S, k.d
    for c in range(NKC):
        o = W_X + c * S_LEN
        S.dma("sp", "xld", lambda e, c=c, o=o: e.dma_start(out=k.arena[:, o:o + S_LEN], in_=d["xT"][:, c, :]),
              writes=[("X", c, t) for t in range(NT)])


def rmsnorm(k, widx, out_mode):
    S = k.S
    R0 = W_N + 8192
    for t in range(NT):
        for c in range(NKC):
            pt = k.PT((t * NKC + c) % 4)
            key = ("PT", (t * NKC + c) % 4)
            S.op("act", lambda e, pt=pt, c=c, t=t: e.activation(out=pt, in_=k.X(c, t), func=AF.Square),
                 reads=[("X", c, t)], writes=[key])
            S.op("pe", lambda e, pt=pt, c=c, t=t: e.matmul(k.bank(t), k.ones_bf, pt, start=(c == 0), stop=(c == NKC - 1)),
                 reads=[key, "ones"], writes=[("ps", t)])
    for t in range(NT):
        R = k.f32(R0 + t * 512, 512)
        S.op("act", lambda e, R=R, t=t: e.activation(out=R, in_=k.bank(t), func=AF.Sqrt, bias=RMS_EPS, scale=1.0 / D),
             reads=[("ps", t)], writes=[("R", t)])
        S.op("dve", lambda e, R=R: e.reciprocal(R, R), reads=[("R", t)], writes=[("R", t)])
        for c in range(NKC):
            wcol = k.normw[:, widx * 8 + c:widx * 8 + c + 1]
            if out_mode == "H":
                S.op("dve", lambda e, R=R, c=c, t=t, wcol=wcol: e.scalar_tensor_tensor(
                    out=k.H(c, t * 512, 512), in0=k.X(c, t), scalar=wcol, in1=R, op0=ALU.mult, op1=ALU.mult),
                    reads=[("X", c, t), ("R", t), "normw"], writes=[("H", c, t)])
            else:
                S.op("dve", lambda e, R=R, c=c, t=t, wcol=wcol: e.scalar_tensor_tensor(
                    out=k.X(c, t), in0=k.X(c, t), scalar=wcol, in1=R, op0=ALU.mult, op1=ALU.mult),
                    reads=[("X", c, t), ("R", t), "normw"], writes=[("X", c, t)])


def ffn(k, l, which):
    S, d = k.S, k.d
    gu_d = d[f"ffn{which}_gu"]
    wd_d = d[f"ffn{which}_wd"]
    GU = [k.bf(W_W + i * 1024, 2048) for i in range(3)]
    WD = [k.bf(W_W + 3072 + i * 3072, 6 * 1024) for i in range(2)]
    A = [k.bf(W_N + i * 1024, 2048) for i in range(7)]
    SG = [k.f32(W_N + 7168 + i * 512, 512) for i in range(2)]

    def load_gu(c):
        s = c % 3
        S.dma("pool", f"gu{s}", lambda e: e.dma_start(out=GU[s], in_=gu_d[l, c, :, :]), writes=[("GU", s)])

    def load_wd(g):
        s = g % 2
        c0, c1 = FFN_GROUPS[g]
        for c in range(c0, c1):
            S.dma("pool", f"wd{s}", lambda e, c=c: e.dma_start(out=WD[s][:, (c - c0) * 1024:(c - c0 + 1) * 1024], in_=wd_d[l, c, :, :]),
                  writes=[("WD", s)])

    def gu_chunk(c):
        s = c % 3
        for t in range(NT):
            pr = (c * NT + t) % 3
            bg, bu = 2 * pr, 2 * pr + 1
            for g, b in ((0, bg), (1, bu)):
                for kc in range(NKC):
                    S.op("pe", lambda e, g=g, b=b, kc=kc, t=t: e.matmul(
                        k.bank(b), GU[s][:, (g * 8 + kc) * 128:(g * 8 + kc + 1) * 128], k.H(kc, t * 512, 512),
                        start=(kc == 0), stop=(kc == NKC - 1)),
                        reads=[("GU", s), ("H", kc, t)], writes=[("ps", b)])
            sg = SG[(c * NT + t) % 2]
            sgk = ("SG", (c * NT + t) % 2)
            S.op("act", lambda e, sg=sg, bg=bg: e.activation(out=sg, in_=k.bank(bg), func=AF.Silu),
                 reads=[("ps", bg)], writes=[sgk])
            S.op("dve", lambda e, sg=sg, bu=bu, t=t: e.tensor_tensor(
                out=A[c % 7][:, t * 512:(t + 1) * 512], in0=sg, in1=k.bank(bu), op=ALU.mult),
                reads=[sgk, ("ps", bu)], writes=[("A", c % 7, t)])

    dn_cnt = [0]

    def down(g):
        s = g % 2
        c0, c1 = FFN_GROUPS[g]
        for t in range(NT):
            for dd in range(NKC):
                b = 6 + dn_cnt[0] % 2
                dn_cnt[0] += 1
                for c in range(c0, c1):
                    S.op("pe", lambda e, b=b, c=c, dd=dd, t=t: e.matmul(
                        k.bank(b), WD[s][:, (c - c0) * 1024 + dd * 128:(c - c0) * 1024 + (dd + 1) * 128],
                        A[c % 7][:, t * 512:(t + 1) * 512], start=(c == c0), stop=(c == c1 - 1)),
                        reads=[("WD", s), ("A", c % 7, t)], writes=[("ps", b)])
                S.op("dve", lambda e, b=b, dd=dd, t=t: e.scalar_tensor_tensor(
                    out=k.X(dd, t), in0=k.bank(b), scalar=0.5, in1=k.X(dd, t), op0=ALU.mult, op1=ALU.add),
                    reads=[("ps", b), ("X", dd, t)], writes=[("X", dd, t)])

    for c in range(3):
        load_gu(c)
    load_wd(0)
    load_wd(1)
    grp_of = {}
    for g, (c0, c1) in enumerate(FFN_GROUPS):
        for c in range(c0, c1):
            grp_of[c] = g
    for c in range(NFC):
        gu_chunk(c)
        if c + 3 < NFC:
            load_gu(c + 3)
        g = grp_of[c]
        if c == FFN_GROUPS[g][0] and g > 0:
            down(g - 1)
            if g + 1 < len(FFN_GROUPS):
                load_wd(g + 1)
    down(len(FFN_GROUPS) - 1)


def store_out(k):
    S, d = k.S, k.d
    for c in range(NKC):
        o = W_X + c * S_LEN
        S.dma("sp", "ost", lambda e, c=c, o=o: e.dma_start(out=d["outT"][:, c, :], in_=k.arena[:, o:o + S_LEN]),
              reads=[("X", c, t) for t in range(NT)])
    S.wait_all_dma("sp", ["ost"])


U = 4096
W_SCR = W_END + 64
ARENA_WORDS = W_END + 2560
SB = W_SCR + 64
NEG_BIG = -30000.0
C_DQ, C_DK, C_DV = 0, 512, 1024
C_FQ, C_FK, C_FV, C_FF = 1536, 2048, 2560, 3072
C_MX, C_MZ, C_G = 3080, 3592, 4104


def DU(i):
    return W_X + i * U


def NU(i):
    return W_N + i * U


def fm(k, off, c, t0=0, n=S_LEN):
    return k.bf(off + c * (S_LEN // 2), S_LEN)[:, t0:t0 + n]


def next_bank(k):
    b = k.ps_rr % 8
    k.ps_rr += 1
    return b


def col(k):
    i = getattr(k, "_col_rr", 0)
    k._col_rr = i + 1
    i %= 48
    return k.arena[:, W_SCR + i:W_SCR + i + 1], ("col", i)


def evac(k, dst, src, src_key, dst_key, scale=None, func=None, eng=None):
    S = k.S
    if eng is None:
        k._ev_rr = getattr(k, "_ev_rr", 0) + 1
        eng = "act" if (func is not None or k._ev_rr % 2 == 0) else "dve"
    if eng == "act":
        f = func if func is not None else AF.Copy
        sc = 1.0 if scale is None else scale
        S.op("act", lambda e: e.activation(out=dst, in_=src, func=f, scale=sc), reads=[src_key], writes=[dst_key])
    else:
        if scale is None:
            S.op("dve", lambda e: e.tensor_copy(dst, src), reads=[src_key], writes=[dst_key])
        else:
            S.op("dve", lambda e: e.tensor_scalar(dst, src, scale, None, ALU.mult), reads=[src_key], writes=[dst_key])


def wcol_jobs(k, l, jobs):
    S, d = k.S, k.d

    def load(i):
        c0, nc_, _ = jobs[i]
        s = i % 2
        tile = k.bf(W_W + s * 2048, 4096).rearrange("p (k c) -> p k c", k=8)
        S.dma("pool", f"wc{s}", lambda e: e.dma_start(out=tile[:, :, 0:nc_], in_=d["w_in"][l, :, :, c0:c0 + nc_]),
              writes=[("WC", s)])
        return tile

    tiles = {}
    for i in range(min(2, len(jobs))):
        tiles[i] = load(i)
    for i in range(len(jobs)):
        jobs[i][2](tiles[i], ("WC", i % 2))
        if i + 2 < len(jobs):
            tiles[i + 2] = load(i + 2)


def h_fm(dst_off, scale=None, func=None):
    def mk(k, c_base, nchunks=4):
        def handler(W, wkey):
            S = k.S
            for m in range(nchunks):
                for t in range(NT):
                    b = next_bank(k)
                    for kc in range(NKC):
                        S.op("pe", lambda e, b=b, m=m, kc=kc, t=t: e.matmul(
                            k.bank(b), W[:, kc, m * 128:(m + 1) * 128], k.H(kc, t * 512, 512),
                            start=(kc == 0), stop=(kc == NKC - 1)),
                            reads=[wkey, ("H", kc, t)], writes=[("ps", b)])
                    evac(k, fm(k, dst_off, c_base + m, t * 512, 512), k.bank(b), ("ps", b),
                         ("fm", dst_off, c_base + m, t), scale=scale, func=func)
        return handler
    return mk


def h_tm(k, vbuf_fn, nh, dv):
    def handler(W, wkey):
        S = k.S
        for tt in range(NT128):
            b = next_bank(k)
            for kc in range(NKC):
                S.op("pe", lambda e, b=b, kc=kc, tt=tt: e.matmul(
                    k.bank(b), k.H(kc, tt * 128, 128), W[:, kc, 0:512],
                    start=(kc == 0), stop=(kc == NKC - 1)),
                    reads=[wkey, ("H", kc, tt // 4)], writes=[("ps", b)])
            dst = vbuf_fn(tt)[:, :, 0:dv]
            src = k.bank(b).rearrange("p (h e) -> p h e", h=nh)
            evac(k, dst, src, ("ps", b), ("V", tt))
    return handler


class AttnPass:
    def __init__(self, k, *, qf, kf, aqf, akf, bias_fn, vf, vkey, dv, mode, fin, sbanks, obanks, scale=1.0, augkey=None, pre=None, act_func=None, scale_fn=None, mask_mul=False):
        self.__dict__.update(locals())
        self.state = {}
        self.per_bank = 2 if dv == 128 else 4

    def blocks(self):
        return [(self, j, i) for j in range(4) for i in range(4 * j + 4)]

    def O(self, j, r):
        pb = self.per_bank
        ob = self.obanks[j % len(self.obanks)][r // pb]
        c0 = (r % pb) * (self.dv + 1)
        return self.k.bank(ob)[:, c0:c0 + self.dv + 1], ("ps", ob)

    def emit_s(self, j, i):
        k, S = self.k, self.k.S
        if self.pre is not None:
            pl = self.pre if isinstance(self.pre, list) else [self.pre]
            self.pre = None
            for f_ in pl:
                f_()
        r0 = max(0, i - 4 * j)
        n0 = r0 * 128
        ncol = 512 - n0
        diag = i >= 4 * j
        k._blk = getattr(k, "_blk", 0) + 1
        pti = k._blk % 4
        pt = k.PT(pti)
        ptk = ("PT", pti)
        if self.mode == "exp":
            sb = self.sbanks[k._blk % len(self.sbanks)]
            has_aug = self.akf is not None
            S.op("pe", lambda e: e.matmul(k.bank(sb)[:, n0:512], self.kf(i), self.qf(j * 512 + n0, ncol),
                                          start=True, stop=(not has_aug and (not diag or self.mask_mul))),
                 reads=([self.augkey] if (self.augkey is not None and not has_aug) else []), writes=[("ps", sb)])
            if has_aug:
                S.op("pe", lambda e: e.matmul(k.bank(sb)[:, n0:512], self.akf(i), self.aqf(j, n0, ncol),
                                              start=False, stop=(not diag)),
                     reads=[self.augkey], writes=[("ps", sb)])
            if diag and not self.mask_mul:
                S.op("pe", lambda e: e.matmul(k.bank(sb)[:, n0:n0 + 128], k.ident_bf, k.negtri_bf, start=False, stop=True),
                     reads=["identbf", "negtri"], writes=[("ps", sb)])
            bias = float(self.bias_fn(i, j)) if self.bias_fn is not None else 0.0
            if self.act_func is None:
                S.op("act", lambda e: e.activation(out=pt[:, n0:512], in_=k.bank(sb)[:, n0:512], func=AF.Exp, bias=bias, scale=1.0),
                     reads=[("ps", sb)], writes=[ptk])
            else:
                sc_ap = self.scale_fn(i, j)
                S.op("act", lambda e: e.activation(out=pt[:, n0:512], in_=k.bank(sb)[:, n0:512], func=self.act_func, scale=sc_ap),
                     reads=[("ps", sb), "UC"], writes=[ptk])
            if diag and self.mask_mul:
                S.op("pool", lambda e: e.tensor_tensor(out=pt[:, n0:n0 + 128], in0=pt[:, n0:n0 + 128], in1=k.tri_bf, op=ALU.mult),
                     reads=[ptk, "tri"], writes=[ptk])
        else:
            pr = self.sbanks[k._blk % len(self.sbanks)]
            sb, eb = pr
            S.op("pe", lambda e: e.matmul(k.bank(sb)[:, n0:512], self.kf(i), self.qf(j * 512 + n0, ncol), start=True, stop=True),
                 reads=[], writes=[("ps", sb)])
            S.op("pe", lambda e: e.matmul(k.bank(eb)[:, n0:512], self.akf(i), self.aqf(j, n0, ncol), start=True, stop=(not diag)),
                 reads=[self.augkey], writes=[("ps", eb)])
            if diag:
                S.op("pe", lambda e: e.matmul(k.bank(eb)[:, n0:n0 + 128], k.ident_bf, k.negtri_bf, start=False, stop=True),
                     reads=["identbf", "negtri"], writes=[("ps", eb)])
            eti = k._blk % 2
            et = k.f32(SB + eti * 512, 512)
            S.op("act", lambda e: e.activation(out=et[:, n0:512], in_=k.bank(eb)[:, n0:512], func=AF.Exp),
                 reads=[("ps", eb)], writes=[("ET", eti)])
            sc = self.scale
            S.op("dve", lambda e: e.scalar_tensor_tensor(out=pt[:, n0:512], in0=k.bank(sb)[:, n0:512], scalar=sc,
                                                         in1=et[:, n0:512], op0=ALU.mult, op1=ALU.mult),
                 reads=[("ps", sb), ("ET", eti)], writes=[ptk])
        self.state[(j, i)] = (pt, ptk, r0)

    def emit_pv(self, j, i):
        k, S = self.k, self.k.S
        pt, ptk, r0 = self.state.pop((j, i))
        for r in range(r0, 4):
            o, okey = self.O(j, r)
            last = (i == 4 * j + r)
            S.op("pe", lambda e, r=r, o=o, last=last: e.matmul(o, pt[:, r * 128:(r + 1) * 128], self.vf(i), start=(i == 0 and r % self.per_bank == 0), stop=last, skip_group_check=True),
                 reads=[ptk, self.vkey], writes=[okey])
            if last:
                self.fin(j, r, o, okey)


def run_blocks(k, blocks, L=2):
    n = len(blocks)
    for idx in range(n + L):
        if idx < n:
            p, j, i = blocks[idx]
            p.emit_s(j, i)
        if idx >= L:
            p, j, i = blocks[idx - L]
            p.emit_pv(j, i)
        tick(k)
    flush_deferred(k)


def defer(k, n, fn):
    k._dq = getattr(k, "_dq", [])
    k._dq.append([n, fn])


def tick(k):
    dq = getattr(k, "_dq", [])
    k._dq = []
    keep = []
    for it in dq:
        it[0] -= 1
        if it[0] <= 0:
            it[1]()
        else:
            keep.append(it)
    k._dq = keep + k._dq


def flush_deferred(k):
    while getattr(k, "_dq", []):
        dq = k._dq
        k._dq = []
        for it in dq:
            it[1]()


def transpose_to_fm(k, src, src_key, dst, dst_key, post=None):
    S = k.S
    q = getattr(k, "_tq", 0)
    k._tq = q + 1
    tb = k.tbanks[q % len(k.tbanks)]
    pq = k.bank(tb)[:, 0:128]
    pkey = ("ps", tb)
    S.op("pe", lambda e: e.transpose(pq, src, k.ident), reads=[src_key, "ident"], writes=[pkey])
    if post is None:
        evac(k, dst, pq, pkey, dst_key)
    else:
        post(pq, pkey)


def softplus_neg(k, T, nh, banks, negb_col, key):
    S = k.S
    for t in range(NT):
        b = banks[t]
        S.op("act", lambda e, b=b, t=t: e.activation(out=T[0:nh, t * 512:(t + 1) * 512], in_=k.bank(b)[0:nh, :],
                                                    func=AF.Exp, bias=negb_col, scale=-1.0),
             reads=[("ps", b), "gcols"], writes=[key])
    S.op("act", lambda e: e.activation(out=T[0:nh, :], in_=T[0:nh, :], func=AF.Ln, bias=1.0, scale=1.0),
         reads=[key], writes=[key])


def build_aug(k, kvec, kkey, qvec, qkey, nh, spl_off, tag):
    S, d = k.S, k.d
    dk, dq = d["augk_" + tag], d["augq_" + tag]
    SPL = [k.bf(spl_off + i * 1024, 2048) for i in range(4)]
    ones = SPL[3]
    S.op("dve", lambda e: e.memset(ones[0:nh, :], 1.0), writes=[("SPL", 3)])
    for r in range(3):
        S.dma("sp", "augw", lambda e, r=r: e.dma_start(out=dk[0:nh, 3 + r, :], in_=ones[0:nh, :]), reads=[("SPL", 3)], writes=[("augd", tag)])
        S.dma("sp", "augw", lambda e, r=r: e.dma_start(out=dq[0:nh, r, :], in_=ones[0:nh, :]), reads=[("SPL", 3)], writes=[("augd", tag)])
    cnt = 0
    for vec, vkey, dram, r0 in ((kvec, kkey, dk, 0), (qvec, qkey, dq, 3)):
        for r in range(3):
            si = cnt % 3
            cnt += 1
            spl = SPL[si]
            S.op("dve", lambda e, spl=spl, vec=vec: e.tensor_copy(spl[0:nh, :], vec[0:nh, :]), reads=[vkey], writes=[("SPL", si)])
            if r < 2:
                S.op("dve", lambda e, spl=spl, vec=vec: e.tensor_tensor(out=vec[0:nh, :], in0=vec[0:nh, :], in1=spl[0:nh, :], op=ALU.subtract),
                     reads=[vkey, ("SPL", si)], writes=[vkey])
            S.dma("sp", "augw", lambda e, spl=spl, dram=dram, rr=r0 + r: e.dma_start(out=dram[0:nh, rr, :], in_=spl[0:nh, :]),
                  reads=[("SPL", si)], writes=[("augd", tag)])


def aug_views(k, slot):
    ak = k.bf(W_W + 5120 + slot * 2048, 2048)
    aq = k.bf(W_W + 5120 + slot * 2048 + 1024, 2048)
    akf = lambda i: ak[0:6, i * 128:(i + 1) * 128]
    aqf = lambda j, n0, n: aq[0:6, j * 512 + n0:j * 512 + n0 + n]
    return akf, aqf


def load_aug(k, tag, h, slot):
    S, d = k.S, k.d
    ak = k.bf(W_W + 5120 + slot * 2048, 2048)
    aq = k.bf(W_W + 5120 + slot * 2048 + 1024, 2048)
    S.dma("sp", f"augl{slot}", lambda e: e.dma_start(out=ak[0:6, :], in_=d["augk_" + tag][h, :, :]), reads=[("augd", tag)], writes=[("AUGS", slot)])
    S.dma("sp", f"augl{slot}", lambda e: e.dma_start(out=aq[0:6, :], in_=d["augq_" + tag][h, :, :]), reads=[("augd", tag)], writes=[("AUGS", slot)])


HB_OFF = [W_W + 0, W_W + 2048, W_W + 5120, W_W + 7168]


def hb_views(k, slot):
    return k.bf(HB_OFF[slot], 2048), k.bf(HB_OFF[slot] + 1024, 2048)


def hb_init(k):
    S = k.S
    for s_ in range(4):
        qb, kb = hb_views(k, s_)
        S.op("dve", lambda e, qb=qb: e.memset(qb, 0.0), writes=[("HB", s_)])
        S.op("dve", lambda e, kb=kb: e.memset(kb, 0.0), writes=[("HB", s_)])


def hb_build(k, slot, par, q_src, k_src, augq_ap, augk_ap, R, queue):
    S = k.S
    qb, kb = hb_views(k, slot)
    r0 = par * 64
    a0 = 64 if par == 0 else 0
    S.op("pool", lambda e: e.tensor_copy(qb[r0:r0 + 64, :], q_src[r0:r0 + 64, :]), writes=[("HB", slot)])
    S.op("pool", lambda e: e.tensor_copy(kb[r0:r0 + 64, :], k_src[r0:r0 + 64, :]), writes=[("HB", slot)])
    S.dma(queue, f"hb{queue}{slot}", lambda e: e.dma_start(out=qb[a0:a0 + R, :], in_=augq_ap), writes=[("HB", slot)])
    S.dma(queue, f"hb{queue}{slot}", lambda e: e.dma_start(out=kb[a0:a0 + R, :], in_=augk_ap), writes=[("HB", slot)])


def layer_consts(k, l):
    S, d = k.S, k.d
    o = k.const_end
    k.gb = k.f32(o, 24); o += 24
    k.convw = k.f32(o, 16); o += 16
    k.convb = k.f32(o, 4); o += 4
    k.skipc = k.f32(o, 4); o += 4
    k.normc = k.f32(o, 4); o += 4
    k.gcols = k.f32(o, 8); o += 8
    k.lamt = k.f32(o, 256); o += 256
    k.lamc = k.f32(o, 8); o += 8
    k.subln = k.f32(o, 128); o += 128
    k.normB = k.f32(o, 512); o += 512
    k.AKd = k.bf(o, 4 * 128); o += 256
    k.AQd = k.bf(o, 4 * 512); o += 1024
    k.ident_bf = k.bf(o, 128); o += 64
    k.negtri_bf = k.bf(o, 128); o += 64
    assert o <= W_CONST + 3072, o
    S.dma("sp", "lc", lambda e: e.dma_start(out=k.gb, in_=d["gbcols"][l, :, :]), writes=["lconst"])
    S.dma("sp", "lc", lambda e: e.dma_start(out=k.convw, in_=d["convw"][l, :, :]), writes=["lconst"])
    S.dma("sp", "lc", lambda e: e.dma_start(out=k.convb, in_=d["convb"][l, :, :]), writes=["lconst"])
    S.dma("sp", "lc", lambda e: e.dma_start(out=k.skipc, in_=d["skipc"][l, :, :]), writes=["lconst"])
    S.dma("sp", "lc", lambda e: e.dma_start(out=k.normc, in_=d["normc"][l, :, :]), writes=["lconst"])
    S.dma("sp", "lc", lambda e: e.dma_start(out=k.gcols[0:8, :], in_=d["gcols"][l, :, :]), writes=["gcols"])
    S.dma("sp", "lc", lambda e: e.dma_start(out=k.lamt, in_=d["difflam"][l:l + 1, :].partition_broadcast(128)), writes=["lamt"])
    S.dma("sp", "lc", lambda e: e.dma_start(out=k.subln, in_=d["diff_subln"][l:l + 1, :].partition_broadcast(128)), writes=["subln"])
    S.dma("sp", "lc", lambda e: e.dma_start(out=k.normB, in_=d["mlstm_norm"][l:l + 1, :].partition_broadcast(128)), writes=["normB"])
    S.op("dve", lambda e: e.tensor_scalar(k.gcols[0:8, 4:5], k.gcols[0:8, 0:1], -1.0, None, ALU.mult), reads=["gcols"], writes=["gcols"])
    S.op("dve", lambda e: e.tensor_scalar(k.gcols[0:8, 5:6], k.gcols[0:8, 2:3], -1.0, None, ALU.mult), reads=["gcols"], writes=["gcols"])
    if l == 0:
        S.dma("pool", "lc2", lambda e: e.dma_start(out=k.AKd[0:3, :], in_=d["alibi_k"][:, :]), writes=["alibi"])
        S.dma("pool", "lc2", lambda e: e.dma_start(out=k.AQd[0:3, :], in_=d["alibi_q"][:, :]), writes=["alibi"])
        S.dma("pool", "lc2", lambda e: e.dma_start(out=k.ident_bf, in_=d["ident"][:, :]), writes=["identbf"])
        S.dma("pool", "lc2", lambda e: e.dma_start(out=k.negtri_bf, in_=d["negtri"][:, :]), writes=["negtri"])
    import math
    lam_init = 0.8 - 0.6 * math.exp(-0.3 * l)
    lt, lc = k.lamt, k.lamc
    S.op("dve", lambda e: e.tensor_tensor(out=lt[:, 0:64], in0=lt[:, 0:64], in1=lt[:, 64:128], op=ALU.mult), reads=["lamt"], writes=["lamt"])
    S.op("dve", lambda e: e.tensor_tensor(out=lt[:, 128:192], in0=lt[:, 128:192], in1=lt[:, 192:256], op=ALU.mult), reads=["lamt"], writes=["lamt"])
    S.op("dve", lambda e: e.reduce_sum(out=lc[:, 0:1], in_=lt[:, 0:64], axis=AX.X), reads=["lamt"], writes=["lamc"])
    S.op("dve", lambda e: e.reduce_sum(out=lc[:, 1:2], in_=lt[:, 128:192], axis=AX.X), reads=["lamt"], writes=["lamc"])
    S.op("act", lambda e: e.activation(out=lc[:, 2:4], in_=lc[:, 0:2], func=AF.Exp), reads=["lamc"], writes=["lamc"])
    S.op("dve", lambda e: e.tensor_tensor(out=lc[:, 4:5], in0=lc[:, 3:4], in1=lc[:, 2:3], op=ALU.subtract), reads=["lamc"], writes=["lamc"])
    S.op("dve", lambda e: e.tensor_scalar(lc[:, 5:6], lc[:, 4:5], -lam_init, None, ALU.add), reads=["lamc"], writes=["neglam"])
    S.op("dve", lambda e: e.tensor_scalar(k.subln, k.subln, 1.0 - lam_init, None, ALU.mult), reads=["subln"], writes=["subln"])
    k.neglam = lc[:, 5:6]


def diff_branch(k, l):
    S = k.S
    QT, KT, VO, OD = DU(0), DU(1), DU(2), NU(0)
    V = k.bf(VO, 16 * 4 * 129).rearrange("p (t h e) -> p t h e", t=16, h=4)
    S.op("dve", lambda e: e.memset(k.bf(VO, 16 * 4 * 129), 1.0), writes=[("V", "all")])
    S.barrier()
    wcol_jobs(k, l, [
        (C_DQ, 512, h_fm(QT)(k, 0)),
        (C_DK, 512, h_fm(KT, scale=0.125)(k, 0)),
        (C_DV, 512, h_tm(k, lambda tt: V[:, tt, :, :], 4, 128)),
    ])
    S.barrier()
    k.tbanks = [2, 3]
    slopes = [2.0 ** (-8.0 * (h + 1) / 4) for h in range(4)]
    A1 = [[k.f32(SB + (p * 4 + r) * 128, 128) for r in range(4)] for p in range(2)]
    TB = SB + 1024
    blocks = []
    pres = []
    first_pass = []
    hb_init(k)
    for h in range(4):
        passes = []
        for c in range(2):
            slot = c + 2 * (h % 2)
            qb, kb = hb_views(k, slot)

            def qf(t0, n, qb=qb):
                return qb[:, t0:t0 + n]

            def kf(i, kb=kb):
                return kb[:, i * 128:(i + 1) * 128]

            pres.append(lambda h=h, c=c, slot=slot: hb_build(k, slot, c, fm(k, QT, h), fm(k, KT, h),
                                                              k.d["alibi_qf"][h, :, :], k.d["alibi_kf"][h, :, :], 3, "pool"))

            def bias_fn(i, j, h=h):
                return slopes[h] * (128.0 * i - 512.0 * j)

            def vf(i, h=h):
                return V[:, i, h, :]

            if c == 0:
                def fin(j, r, o, okey, h=h):
                    rc, rk = col(k)
                    a1 = A1[j % 2][r]
                    S.op("dve", lambda e: e.reciprocal(rc, o[:, 128:129]), reads=[okey], writes=[rk])
                    S.op("dve", lambda e: e.tensor_scalar(a1, o[:, 0:128], rc, None, ALU.mult), reads=[okey, rk], writes=[("A1", j % 2, r)])
            else:
                def fin(j, r, o, okey, h=h):
                    rc, rk = col(k)
                    sc, sk = col(k)
                    k._fp = getattr(k, "_fp", 0) + 1
                    n = k._fp
                    tmp = k.f32(TB + (n % 5) * 128, 128)
                    sq = k.f32(TB + 640 + (n % 2) * 128, 128)
                    ot = k.f32(TB + 896 + (n % 4) * 128, 128)
                    tk_, sqk, otk = ("TMP", n % 5), ("SQ", n % 2), ("OT", n % 4)
                    a1 = A1[j % 2][r]
                    tt = 4 * j + r
                    S.op("dve", lambda e: e.reciprocal(rc, o[:, 128:129]), reads=[okey], writes=[rk])
                    S.op("dve", lambda e: e.tensor_scalar(tmp, o[:, 0:128], rc, None, ALU.mult), reads=[okey, rk], writes=[tk_])
                    S.op("dve", lambda e: e.scalar_tensor_tensor(out=tmp, in0=tmp, scalar=k.neglam, in1=a1, op0=ALU.mult, op1=ALU.add),
                         reads=[tk_, ("A1", j % 2, r), "neglam"], writes=[tk_])
                    S.op("dve", lambda e: e.tensor_tensor(out=sq, in0=tmp, in1=tmp, op=ALU.mult), reads=[tk_], writes=[sqk])
                    S.op("dve", lambda e: e.reduce_sum(out=sc, in_=sq, axis=AX.X), reads=[sqk], writes=[sk])

                    def stage_b():
                        S.op("act", lambda e: e.activation(out=sc, in_=sc, func=AF.Ln, bias=RMS_EPS, scale=1.0 / 128), reads=[sk], writes=[sk])
                        S.op("act", lambda e: e.activation(out=sc, in_=sc, func=AF.Exp, scale=-0.5), reads=[sk], writes=[sk])

                    def stage_c():
                        S.op("dve", lambda e: e.scalar_tensor_tensor(out=ot, in0=tmp, scalar=sc, in1=k.subln, op0=ALU.mult, op1=ALU.mult),
                             reads=[tk_, sk, "subln"], writes=[otk])
                        defer(k, 2, lambda: transpose_to_fm(k, ot, otk, fm(k, OD, h, tt * 128, 128), ("fm", OD, h, tt)))
                    defer(k, 2, stage_b)
                    defer(k, 3, stage_c)
            passes.append(AttnPass(k, qf=qf, kf=kf, aqf=None, akf=None, bias_fn=bias_fn, vf=vf, vkey=("V", "x"), dv=128,
                                   mode="exp", fin=fin, sbanks=[0, 1], obanks=[(4, 5)] if c == 0 else [(6, 7)], augkey=("HB", slot)))
        first_pass.append(passes[0])
        b0, b1 = passes[0].blocks(), passes[1].blocks()
        for j in range(4):
            blocks += [b for b in b0 if b[1] == j]
            blocks += [b for b in b1 if b[1] == j]
    for u in range(4):
        first_pass[u].pre = (pres[0:4] if u == 0 else (pres[2 * u + 2:2 * u + 4] if u < 3 else None))
    run_blocks(k, blocks)
    S.barrier()


def fox_branch(k, l):
    S, d = k.S, k.d
    QT, KT, VO, OF = DU(0), DU(1), DU(2), NU(2)
    V = k.bf(VO, 16 * 8 * 65).rearrange("p (t h e) -> p t h e", t=16, h=8)
    WFF = k.bf(W_W + 4096, 64).rearrange("p (k c) -> p k c", k=8)
    S.op("dve", lambda e: e.memset(k.bf(VO, 16 * 8 * 65), 1.0), writes=[("V", "all")])
    S.dma("pool", "wff", lambda e: e.dma_start(out=WFF, in_=d["w_in"][l, :, :, C_FF:C_FF + 8]), writes=["WFF"])
    S.barrier()
    wcol_jobs(k, l, [
        (C_FQ, 512, h_fm(QT)(k, 0)),
        (C_FK, 512, h_fm(KT, scale=0.125)(k, 0)),
        (C_FV, 512, h_tm(k, lambda tt: V[:, tt, :, :], 8, 64)),
    ])
    for t in range(NT):
        for kc in range(NKC):
            S.op("pe", lambda e, kc=kc, t=t: e.matmul(k.bank(t)[0:8, :], WFF[:, kc, :], k.H(kc, t * 512, 512),
                                                      start=(kc == 0), stop=(kc == NKC - 1)),
                 reads=["WFF", ("H", kc, t)], writes=[("ps", t)])
    S.barrier()
    T1 = k.f32(W_W, 2048)
    T2 = k.f32(W_W + 2048, 2048)
    ONES = k.f32(W_W + 7168, 2048)
    S.op("dve", lambda e: e.memset(ONES[0:8, :], 1.0), writes=["ONES"])
    softplus_neg(k, T1, 8, [0, 1, 2, 3], k.gcols[0:8, 4:5], "T1")
    S.op("dve", lambda e: e.tensor_tensor_scan(T2[0:8, :], ONES[0:8, :], T1[0:8, :], 0.0, ALU.mult, ALU.add), reads=["T1", "ONES"], writes=["T2"])
    S.op("dve", lambda e: e.tensor_scalar(T1[0:8, :], T2[0:8, :], -1.0, None, ALU.mult), reads=["T2"], writes=["T1"])
    build_aug(k, T2, "T2", T1, "T1", 8, NU(2), "fox")
    S.barrier()
    k.tbanks = [3, 6, 7]
    OTB = [[k.f32(SB + (p * 4 + r) * 128, 128) for r in range(4)] for p in range(2)]
    blocks = []
    pres = []
    first_pass = []
    hb_init(k)
    for pr in range(4):
        passes = []
        for c in range(2):
            hd = 2 * pr + c
            slot = c + 2 * (pr % 2)
            qb, kb = hb_views(k, slot)

            def qf(t0, n, qb=qb):
                return qb[:, t0:t0 + n]

            def kf(i, kb=kb):
                return kb[:, i * 128:(i + 1) * 128]

            pres.append(lambda pr=pr, c=c, slot=slot, hd=hd: hb_build(k, slot, c, fm(k, QT, pr), fm(k, KT, pr),
                                                                      k.d["augq_fox"][hd, :, :], k.d["augk_fox"][hd, :, :], 6, "sp"))

            def vf(i, hd=hd):
                return V[:, i, hd, :]

            def fin(j, r, o, okey, pr=pr, c=c):
                rc, rk = col(k)
                ot = OTB[j % 2][r]
                otk = ("OTB", j % 2, r)
                S.op("dve", lambda e: e.reciprocal(rc, o[:, 64:65]), reads=[okey], writes=[rk])
                S.op("dve", lambda e: e.tensor_scalar(ot[:, c * 64:(c + 1) * 64], o[:, 0:64], rc, None, ALU.mult), reads=[okey, rk], writes=[otk])
                if c == 1:
                    tt = 4 * j + r
                    defer(k, 3, lambda: transpose_to_fm(k, ot, otk, fm(k, OF, pr, tt * 128, 128), ("fm", OF, pr, tt)))
            passes.append(AttnPass(k, qf=qf, kf=kf, aqf=None, akf=None, bias_fn=None, vf=vf, vkey=("V", "x"), dv=64,
                                   mode="exp", fin=fin, sbanks=[0, 1, 2], obanks=[(4,)] if c == 0 else [(5,)], augkey=("HB", slot)))
        first_pass.append(passes[0])
        b0, b1 = passes[0].blocks(), passes[1].blocks()
        for j in range(4):
            blocks += [b for b in b0 if b[1] == j]
            blocks += [b for b in b1 if b[1] == j]
    for u in range(4):
        first_pass[u].pre = (pres[0:4] if u == 0 else (pres[2 * u + 2:2 * u + 4] if u < 3 else None))
    run_blocks(k, blocks)
    S.barrier()


def mlstm_branch(k, l):
    S, d = k.S, k.d
    QT, KT, VT, MX, XC, SZ, VA = DU(0), DU(1), DU(2), DU(3), NU(0), NU(1), NU(2)
    Vall = k.bf(VA, 16 * 4 * 129).rearrange("p (t h e) -> p t h e", t=16, h=4)
    WQKV = k.bf(W_W + 4096, 1536).rearrange("p (g h e) -> p g h e", g=3, h=4)
    WIF = k.bf(W_W + 4096 + 768, 96).rearrange("p (j o) -> p j o", j=12)
    S.op("dve", lambda e: e.memset(k.bf(VA, 16 * 4 * 129), 1.0), writes=[("V", "all")])
    S.dma("pool", "wqkv", lambda e: e.dma_start(out=k.bf(W_W + 4096, 1536), in_=d["wqkv"][l, :, :]), writes=["WQKV"])
    S.dma("pool", "wqkv", lambda e: e.dma_start(out=k.bf(W_W + 4096 + 768, 96), in_=d["wif"][l, :, :]), writes=["WIF"])
    S.barrier()
    wcol_jobs(k, l, [
        (C_MX, 512, h_fm(MX)(k, 0)),
        (C_MZ, 512, h_fm(SZ, func=AF.Silu)(k, 0)),
    ])
    ACC = k.f32(SB, 2048)
    for c in range(4):
        mx = fm(k, MX, c)
        mkeys = [("fm", MX, c, t) for t in range(NT)]
        w = lambda j, c=c: k.convw[:, j * 4 + c:j * 4 + c + 1]
        S.op("dve", lambda e, mx=mx, c=c, w=w: e.tensor_scalar(ACC, mx, w(3), k.convb[:, c:c + 1], ALU.mult, ALU.add),
             reads=mkeys + ["lconst"], writes=["ACC"])
        for sh in (1, 2, 3):
            S.op("dve", lambda e, mx=mx, sh=sh, w=w: e.scalar_tensor_tensor(
                out=ACC[:, sh:], in0=mx[:, 0:S_LEN - sh], scalar=w(3 - sh), in1=ACC[:, sh:], op0=ALU.mult, op1=ALU.add),
                reads=mkeys + ["ACC", "lconst"], writes=["ACC"])
        S.op("act", lambda e, c=c: e.activation(out=fm(k, XC, c), in_=ACC, func=AF.Silu), reads=["ACC"],
             writes=[("fm", XC, c, t) for t in range(NT)])
    for h in range(4):
        for t in range(NT):
            for g, src, dst in ((0, XC, QT), (1, XC, KT), (2, MX, VT)):
                b = next_bank(k)
                S.op("pe", lambda e, b=b, g=g, src=src, h=h, t=t: e.matmul(
                    k.bank(b), WQKV[:, g, h, :], fm(k, src, h, t * 512, 512), start=True, stop=True),
                    reads=["WQKV", ("fm", src, h, t)], writes=[("ps", b)])
                evac(k, fm(k, dst, h, t * 512, 512), k.bank(b), ("ps", b), ("fm", dst, h, t))
        for tt in range(NT128):
            b = next_bank(k)
            S.op("pe", lambda e, b=b, h=h, tt=tt: e.matmul(
                k.bank(b)[:, 0:128], fm(k, MX, h, tt * 128, 128), WQKV[:, 2, h, :], start=True, stop=True),
                reads=["WQKV", ("fm", MX, h, tt // 4)], writes=[("ps", b)])
            evac(k, Vall[:, tt, h, 0:128], k.bank(b)[:, 0:128], ("ps", b), ("V", tt, h))
    for h in range(4):
        S.op("dve", lambda e, h=h: e.tensor_scalar(fm(k, XC, h), fm(k, XC, h), k.skipc[:, h:h + 1], None, ALU.mult),
             reads=[("fm", XC, h, t) for t in range(NT)] + ["lconst"], writes=[("fm", XC, h, t) for t in range(NT)])
    S.barrier()
    for t in range(NT):
        for g, bb in ((0, t), (1, 4 + t)):
            for jj in range(12):
                src = (QT, KT, VT)[jj // 4]
                S.op("pe", lambda e, g=g, bb=bb, jj=jj, src=src, t=t: e.matmul(
                    k.bank(bb)[0:4, :], WIF[:, jj, g * 4:(g + 1) * 4], fm(k, src, jj % 4, t * 512, 512),
                    start=(jj == 0), stop=(jj == 11)), reads=["WIF"], writes=[("ps", bb)])
    T1 = k.f32(W_W, 2048)
    T2 = k.f32(W_W + 2048, 2048)
    T3 = k.f32(W_W + 5120, 2048)
    ONES = k.f32(W_W + 7168, 2048)
    S.op("dve", lambda e: e.memset(ONES[0:4, :], 1.0), writes=["ONES"])
    softplus_neg(k, T1, 4, [4, 5, 6, 7], k.gcols[0:4, 5:6], "T1")
    S.op("dve", lambda e: e.tensor_tensor_scan(T2[0:4, :], ONES[0:4, :], T1[0:4, :], 0.0, ALU.mult, ALU.add), reads=["T1", "ONES"], writes=["T2"])
    for t in range(NT):
        S.op("dve", lambda e, t=t: e.tensor_scalar(T1[0:4, t * 512:(t + 1) * 512], k.bank(t)[0:4, :], k.gcols[0:4, 1:2], None, ALU.add),
             reads=[("ps", t), "gcols", "T2"], writes=["T1"])
    S.op("dve", lambda e: e.tensor_tensor(out=T1[0:4, :], in0=T1[0:4, :], in1=T2[0:4, :], op=ALU.add), reads=["T1", "T2"], writes=["T1"])
    S.op("dve", lambda e: e.tensor_tensor_scan(T3[0:4, :], ONES[0:4, :], T1[0:4, :], 0.0, ALU.mult, ALU.max), reads=["T1", "ONES"], writes=["T3"])
    S.op("dve", lambda e: e.tensor_scalar(T3[0:4, :], T3[0:4, :], -1.0, None, ALU.mult), reads=["T3"], writes=["T3"])
    UR = ONES
    EM = k.f32(W_CONST + 2700, 64)
    UC = k.f32(W_CONST + 2780, 160)
    for j in range(NT):
        nb = T3[0:4, 512 * j - 1:512 * j] if j > 0 else 0.0
        S.op("act", lambda e, j=j, nb=nb: e.activation(out=T2[0:4, j * 512:(j + 1) * 512], in_=T2[0:4, j * 512:(j + 1) * 512],
                                                     func=AF.Exp, bias=nb, scale=1.0), reads=["T2", "T3"], writes=["T2"])
    for tt in range(NT128):
        S.op("pe", lambda e, tt=tt: e.transpose(k.bank(3)[:, tt * 4:(tt + 1) * 4], T2[0:4, tt * 128:(tt + 1) * 128], k.ident[0:4, 0:4]),
             reads=["T2", "ident"], writes=[("ps", 3)])
    S.op("dve", lambda e: e.tensor_copy(EM, k.bank(3)[:, 0:64]), reads=[("ps", 3)], writes=["EM"])
    for j in range(NT):
        nb = T3[0:4, 512 * j - 1:512 * j] if j > 0 else 0.0
        ncol = (j + 1) * 512
        S.op("act", lambda e, nb=nb, ncol=ncol: e.activation(out=UR[0:4, 0:ncol], in_=T1[0:4, 0:ncol], func=AF.Exp, bias=nb, scale=1.0),
             reads=["T1", "T3", "ONES"], writes=["ONES"])
        for i in range(4 * j + 4):
            idx = 2 * j * (j + 1) + i
            S.op("pe", lambda e, i=i, idx=idx: e.transpose(k.bank(2)[:, idx * 4:(idx + 1) * 4], UR[0:4, i * 128:(i + 1) * 128], k.ident[0:4, 0:4]),
                 reads=["ONES", "ident"], writes=[("ps", 2)])
    S.op("dve", lambda e: e.tensor_scalar(UC, k.bank(2)[:, 0:160], 128.0 ** -0.5, None, ALU.mult), reads=[("ps", 2)], writes=["UC"])
    S.barrier()
    k.tbanks = [3, 6, 7]
    TB = SB + 1024
    blocks = []
    for h in range(4):

        def qf(t0, n, h=h):
            return fm(k, QT, h, t0, n)

        def kf(i, h=h):
            return fm(k, KT, h, i * 128, 128)

        def vf(i, h=h):
            return Vall[:, i, h, :]

        def fin(j, r, o, okey, h=h):
            tt = 4 * j + r
            c1, k1 = col(k)
            c2, k2 = col(k)
            c3, k3 = col(k)
            k._fp = getattr(k, "_fp", 0) + 1
            n = k._fp
            NUM = k.f32(TB + (n % 4) * 128, 128)
            TF = k.f32(TB + 640 + (n % 2) * 128, 128)
            HN = k.f32(TB + 896 + (n % 4) * 128, 128)
            ST6 = k.f32(TB + 512 + (n % 8) * 8, 6)
            MV = k.f32(TB + 576 + (n % 8) * 4, 2)
            hk, nk, fk, stk, mvk = ("HH", n % 4), ("HN", n % 4), ("TF", n % 2), ("ST6", n % 8), ("MV", n % 8)
            den = o[:, 128:129]
            S.op("dve", lambda e: e.tensor_tensor(out=c1, in0=den, in1=EM[:, tt * 4 + h:tt * 4 + h + 1], op=ALU.max), reads=[okey, "EM"], writes=[k1])
            S.op("dve", lambda e: e.scalar_tensor_tensor(out=c1, in0=den, scalar=-1.0, in1=c1, op0=ALU.mult, op1=ALU.max), reads=[okey, k1], writes=[k1])
            S.op("dve", lambda e: e.reciprocal(c1, c1), reads=[k1], writes=[k1])
            S.op("dve", lambda e: e.tensor_scalar(NUM, o[:, 0:128], c1, None, ALU.mult), reads=[okey, k1], writes=[hk])
            S.op("dve", lambda e: e.bn_stats(ST6, NUM), reads=[hk], writes=[stk])
            S.op("dve", lambda e: e.bn_aggr(MV, ST6), reads=[stk], writes=[mvk])

            def stage_b():
                S.op("act", lambda e: e.activation(out=c2, in_=MV[:, 1:2], func=AF.Ln, bias=LN_EPS, scale=1.0), reads=[mvk], writes=[k2])
                S.op("act", lambda e: e.activation(out=c2, in_=c2, func=AF.Exp, scale=-0.5), reads=[k2], writes=[k2])

            def post(pq, pkey):
                xs = fm(k, XC, h, tt * 128, 128)
                sz = fm(k, SZ, h, tt * 128, 128)
                S.op("dve", lambda e: e.scalar_tensor_tensor(out=TF, in0=pq, scalar=k.normc[:, h:h + 1], in1=xs, op0=ALU.mult, op1=ALU.add),
                     reads=[pkey, "lconst"], writes=[fk])
                S.op("dve", lambda e: e.tensor_tensor(out=sz, in0=TF, in1=sz, op=ALU.mult), reads=[fk], writes=[("fm", SZ, h, tt)])

            def stage_c():
                S.op("dve", lambda e: e.tensor_scalar(HN, NUM, MV[:, 0:1], c2, ALU.subtract, ALU.mult), reads=[hk, mvk, k2], writes=[nk])
                defer(k, 2, lambda: transpose_to_fm(k, HN, nk, None, None, post=post))
            defer(k, 2, stage_b)
            defer(k, 3, stage_c)

        def scale_fn(i, j, h=h):
            idx = 2 * j * (j + 1) + i
            return UC[:, idx * 4 + h:idx * 4 + h + 1]
        ps_ = AttnPass(k, qf=qf, kf=kf, aqf=None, akf=None, bias_fn=None, vf=vf, vkey=("V", "x"), dv=128, mode="exp", fin=fin,
                       sbanks=[0, 1, 2], obanks=[(4, 5)], act_func=AF.Copy, scale_fn=scale_fn, mask_mul=True)
        blocks += ps_.blocks()
    run_blocks(k, blocks, L=2)
    S.barrier()


def merge_and_out(k, l):
    S, d = k.S, k.d
    OB = [NU(0), NU(2), NU(1)]
    MG = DU(0)
    GT = [k.f32(SB + i * 512, 512) for i in range(2)]
    ACC = k.f32(SB + 1024, 512)
    PB = k.f32(SB + 1536, 512)

    def load_mp(dd):
        s = dd % 2
        tile = k.bf(W_W + s * 2304, 4608)
        S.dma("pool", f"mp{s}", lambda e: e.dma_start(out=tile, in_=d["mpack"][l, dd, :, :]), writes=[("MP", s)])
        return tile

    tiles = {0: load_mp(0), 1: load_mp(1)}
    cnt = 0
    for dd in range(NKC):
        MP = tiles[dd]
        mkey = ("MP", dd % 2)
        for t in range(NT):
            for b in range(3):
                bg, bo = next_bank(k), next_bank(k)
                for kc in range(NKC):
                    S.op("pe", lambda e, bg=bg, b=b, kc=kc, t=t, MP=MP: e.matmul(
                        k.bank(bg), MP[:, b * 1536 + kc * 128:b * 1536 + (kc + 1) * 128], k.H(kc, t * 512, 512),
                        start=(kc == 0), stop=(kc == NKC - 1)), reads=[mkey, ("H", kc, t)], writes=[("ps", bg)])
                for kc in range(4):
                    S.op("pe", lambda e, bo=bo, b=b, kc=kc, t=t, MP=MP: e.matmul(
                        k.bank(bo), MP[:, b * 1536 + 1024 + kc * 128:b * 1536 + 1024 + (kc + 1) * 128], fm(k, OB[b], kc, t * 512, 512),
                        start=(kc == 0), stop=(kc == 3)), reads=[mkey], writes=[("ps", bo)])
                gt = GT[cnt % 2]
                gk = ("GT", cnt % 2)
                cnt += 1
                gbc = k.gb[:, b * 8 + dd:b * 8 + dd + 1]
                S.op("act", lambda e, gt=gt, bg=bg, gbc=gbc: e.activation(out=gt, in_=k.bank(bg), func=AF.Sigmoid, bias=gbc, scale=1.0),
                     reads=[("ps", bg), "lconst"], writes=[gk])
                if b == 0:
                    S.op("dve", lambda e, gt=gt, bo=bo: e.tensor_tensor(out=ACC, in0=gt, in1=k.bank(bo), op=ALU.mult),
                         reads=[gk, ("ps", bo)], writes=["MACC"])
                else:
                    S.op("dve", lambda e, gt=gt, bo=bo: e.tensor_tensor(out=PB, in0=gt, in1=k.bank(bo), op=ALU.mult),
                         reads=[gk, ("ps", bo)], writes=["MPB"])
                    dst = ACC if b == 1 else fm(k, MG, dd, t * 512, 512)
                    dkey = "MACC" if b == 1 else ("fm", MG, dd, t)
                    S.op("dve", lambda e, dst=dst: e.tensor_tensor(out=dst, in0=ACC, in1=PB, op=ALU.add),
                         reads=["MACC", "MPB"], writes=[dkey] + (["MACC"] if b == 2 else []))
        if dd + 2 < NKC:
            tiles[dd + 2] = load_mp(dd + 2)
    S.barrier()
    MN = NU(0)
    for i in range(4):
        S.op("dve" if i % 2 == 0 else "pool", lambda e, i=i: e.tensor_copy(k.bf(MN + i * 2048, 4096), k.bf(MG + i * 2048, 4096)),
             writes=[("MN", i)])
    S.barrier()
    XS = [k.f32(SB + i * 512, 512) for i in range(2)]

    WOT = [k.bf(W_W + dd * 512, 1024) for dd in range(NKC)]
    for dd in range(NKC):
        S.dma("pool", "wo", lambda e, dd=dd: e.dma_start(out=WOT[dd], in_=d["wout"][l, dd, :, :]), writes=[("WO", dd)])
    cnt = 0
    for t in range(NT):
        for dd in range(NKC):
            WO = WOT[dd]
            b = next_bank(k)
            xs = XS[cnt % 2]
            xk = ("XS", cnt % 2)
            cnt += 1
            S.dma("sp", f"xs{cnt % 2}", lambda e, xs=xs, dd=dd, t=t: e.dma_start(out=xs, in_=d["xspill"][:, dd, t * 512:(t + 1) * 512]),
                  writes=[xk])
            for kc in range(NKC):
                S.op("pe", lambda e, b=b, kc=kc, t=t, WO=WO: e.matmul(
                    k.bank(b), WO[:, kc * 128:(kc + 1) * 128], fm(k, MN, kc, t * 512, 512),
                    start=(kc == 0), stop=(kc == NKC - 1)), reads=[("WO", dd)], writes=[("ps", b)])
            S.op("dve", lambda e, b=b, xs=xs, dd=dd, t=t: e.tensor_tensor(out=k.X(dd, t), in0=k.bank(b), in1=xs, op=ALU.add),
                 reads=[("ps", b), xk], writes=[("X", dd, t)])


def spill_x(k):
    S, d = k.S, k.d
    for c in range(NKC):
        o = W_X + c * S_LEN
        S.dma("sp", "xsp", lambda e, c=c, o=o: e.dma_start(out=d["xspill"][:, c, :], in_=k.arena[:, o:o + S_LEN]),
              reads=[("X", c, t) for t in range(NT)], writes=["xspill"])


def mixer(k, l, branches=("ml", "diff", "fox")):
    S = k.S
    layer_consts(k, l)
    rmsnorm(k, l * 3 + 1, "H")
    spill_x(k)
    S.barrier()
    if "ml" in branches:
        mlstm_branch(k, l)
    if "diff" in branches:
        diff_branch(k, l)
    if "fox" in branches:
        fox_branch(k, l)
    merge_and_out(k, l)


def _chunk_rows(w):
    K_, N_ = w.shape
    return w.reshape(K_ // 128, 128, N_).transpose(1, 0, 2)


def _cols(v):
    return v.reshape(-1, 128).T


def prep_shared(inp):
    f = np.float32
    sh = {}
    for which in (1, 2):
        wg, wu, wd = inp[f"ffn{which}_w_gate"], inp[f"ffn{which}_w_up"], inp[f"ffn{which}_w_down"]
        gu = np.empty((DEPTH, NFC, 128, 2, NKC, 128), f)
        for l in range(DEPTH):
            for g, w in enumerate((wg[l], wu[l])):
                gu[l, :, :, g] = w.reshape(NKC, 128, NFC, 128).transpose(2, 1, 0, 3)
        sh[f"ffn{which}_gu"] = gu.reshape(DEPTH, NFC, 128, 2 * NKC * 128)
        sh[f"ffn{which}_wd"] = np.ascontiguousarray(wd.reshape(DEPTH, NFC, 128, D))
    vecs = []
    for l in range(DEPTH):
        vecs += [inp["ffn1_norm"][l], inp["mix_norm"][l], inp["ffn2_norm"][l]]
    vecs.append(inp["final_norm"])
    sh["normw"] = np.ascontiguousarray(np.stack([_cols(v) for v in vecs], axis=1).reshape(128, 56)).astype(f)
    sh["ident"] = np.eye(128, dtype=f)
    sh["ones"] = np.ones((128, 128), f)
    sh["tri"] = np.triu(np.ones((128, 128), f))
    sh["negtri"] = (np.tril(np.ones((128, 128), f), -1) * NEG_BIG).astype(f)
    w_in = inp["w_in"]
    sh["w_in"] = np.ascontiguousarray(np.stack([_chunk_rows(w_in[l]) for l in range(DEPTH)]))
    gb = np.empty((DEPTH, 128, 24), f)
    cw = np.empty((DEPTH, 128, 16), f)
    cb = np.empty((DEPTH, 128, 4), f)
    sk = np.empty((DEPTH, 128, 4), f)
    nmc = np.empty((DEPTH, 128, 4), f)
    gc = np.zeros((DEPTH, 8, 8), f)
    for l in range(DEPTH):
        for b in range(3):
            gb[l, :, b * 8:(b + 1) * 8] = _cols(inp["gate_bias"][l, b])
        for j in range(4):
            cw[l, :, j * 4:(j + 1) * 4] = _cols(inp["mlstm_conv_w"][l, j])
        cb[l] = _cols(inp["mlstm_conv_b"][l])
        sk[l] = _cols(inp["mlstm_skip"][l])
        nmc[l] = _cols(inp["mlstm_norm"][l])
        gc[l, :, 0] = inp["fox_b_f"][l]
        gc[l, 0:4, 1] = inp["mlstm_b_if"][l, 0:4]
        gc[l, 0:4, 2] = inp["mlstm_b_if"][l, 4:8]
    sh["gbcols"], sh["convw"], sh["convb"], sh["skipc"], sh["gcols"] = gb, cw, cb, sk, gc
    sh["normc"] = nmc
    sh["difflam"] = np.ascontiguousarray(np.concatenate(
        [inp["diff_lq1"], inp["diff_lk1"], inp["diff_lq2"], inp["diff_lk2"]], axis=1)).astype(f)
    sh["diff_subln"] = np.ascontiguousarray(inp["diff_subln"]).astype(f)
    sh["mlstm_norm"] = np.ascontiguousarray(inp["mlstm_norm"]).astype(f)
    slopes = [2.0 ** (-8.0 * (h + 1) / 4) for h in range(4)]
    ak = np.zeros((3, 4, 128), f)
    aq = np.zeros((3, 4, 512), f)
    relq = np.arange(512)
    for h in range(4):
        ak[0, h] = slopes[h] * np.arange(128)
        ak[1, h] = 1.0
        ak[2, h] = 1.0
        aq[0, h] = 1.0
        aq[1, h] = -slopes[h] * (128 * (relq // 128))
        aq[2, h] = -slopes[h] * (relq % 128)
    sh["alibi_k"] = ak.reshape(3, 512)
    sh["alibi_q"] = aq.reshape(3, 2048)
    tpos = np.arange(S_LEN)
    akf = np.zeros((4, 3, S_LEN), f)
    aqf = np.zeros((4, 3, S_LEN), f)
    for h in range(4):
        akf[h, 0] = slopes[h] * (tpos % 128)
        akf[h, 1] = 1.0
        akf[h, 2] = 1.0
        aqf[h, 0] = 1.0
        aqf[h, 1] = -slopes[h] * (128 * ((tpos % 512) // 128))
        aqf[h, 2] = -slopes[h] * (tpos % 128)
    sh["alibi_kf"], sh["alibi_qf"] = akf, aqf
    wqkv = np.empty((DEPTH, 128, 3, 4, 128), f)
    for g, nm in enumerate(("mlstm_wq", "mlstm_wk", "mlstm_wv")):
        wqkv[:, :, g] = inp[nm].transpose(0, 2, 1, 3)
    sh["wqkv"] = wqkv.reshape(DEPTH, 128, 1536)
    sh["wif"] = np.ascontiguousarray(inp["mlstm_w_if"].reshape(DEPTH, 12, 128, 8).transpose(0, 2, 1, 3)).reshape(DEPTH, 128, 96)
    mp = np.empty((DEPTH, NKC, 128, 3, 1536), f)
    wbs = (inp["w_branch_diff"], inp["w_branch_fox"], inp["w_branch_mlstm"])
    for l in range(DEPTH):
        for b in range(3):
            g = w_in[l][:, C_G + b * D:C_G + (b + 1) * D]
            mp[l, :, :, b, 0:1024] = g.reshape(NKC, 128, NKC, 128).transpose(2, 1, 0, 3).reshape(NKC, 128, 1024)
            mp[l, :, :, b, 1024:1536] = wbs[b][l].reshape(4, 128, NKC, 128).transpose(2, 1, 0, 3).reshape(NKC, 128, 512)
    sh["mpack"] = mp.reshape(DEPTH, NKC, 128, 4608)
    wo = np.empty((DEPTH, NKC, 128, 1024), f)
    for l in range(DEPTH):
        wo[l] = inp["w_out"][l].reshape(NKC, 128, NKC, 128).transpose(2, 1, 0, 3).reshape(NKC, 128, 1024)
    sh["wout"] = wo
    return sh


def build_program(shapes, plan):
    from contextlib import ExitStack
    nc = bass.Bass("TRN2", target_bir_lowering=False)
    dram = {}
    for name, shp in shapes.items():
        dram[name] = nc.dram_tensor(name, list(shp), F32, kind="ExternalInput").ap()
    dram["outT"] = nc.dram_tensor("outT", [128, NKC, S_LEN], F32, kind="ExternalOutput").ap()
    dram["xspill"] = nc.dram_tensor("xspill", [128, NKC, S_LEN], F32, kind="Internal").ap()
    for nm in ("augk_fox", "augq_fox", "augk_ml", "augq_ml"):
        dram[nm] = nc.dram_tensor(nm, [8, 6, S_LEN], BF16, kind="Internal").ap()
    with ExitStack() as es:
        sems = [es.enter_context(nc.semaphore(f"s{i}")) for i in range(70)]
        S = Sched(nc, sems)
        arena = nc.alloc_sbuf_tensor("arena", [128, ARENA_WORDS], F32)
        ps = es.enter_context(nc.psum_tensor("ps", [128, 4096], F32))
        k = K(nc, S, arena, ps, dram)
        plan(k)
        with nc.Block() as block:
            S.emit(block)
    return nc


def full_plan(k):
    build_consts(k)
    load_x(k)
    for l in range(DEPTH):
        rmsnorm(k, l * 3 + 0, "H")
        ffn(k, l, 1)
        mixer(k, l)
        rmsnorm(k, l * 3 + 2, "H")
        k.S.barrier()
        ffn(k, l, 2)
    rmsnorm(k, 6, "X")
    store_out(k)


def run(inputs, plan=full_plan, trace=False, cores=NB):
    sh = prep_shared(inputs)
    x = np.asarray(inputs["x"], np.float32)
    in_maps = []
    for b in range(cores):
        m = dict(sh)
        m["xT"] = np.ascontiguousarray(x[b].T.reshape(NKC, 128, S_LEN).transpose(1, 0, 2))
        in_maps.append(m)
    shapes = {n: a.shape for n, a in in_maps[0].items()}
    nc = build_program(shapes, plan)
    res = run_bass_kernel_spmd(nc, in_maps, core_ids=list(range(cores)), trace=trace)
    out = np.empty((cores, S_LEN, D), np.float32)
    for b in range(cores):
        o = res.results[b]["outT"]
        out[b] = o.transpose(1, 0, 2).reshape(D, S_LEN).T
    return out, res


def kernel(**inputs):
    out, _ = run(inputs)
    return out
```

```python
import bisect
import numpy as np
import concourse.bass as bass
import concourse.mybir as mybir
from concourse.bass_utils import run_bass_kernel_spmd

F32 = mybir.dt.float32
BF16 = mybir.dt.bfloat16
AF = mybir.ActivationFunctionType
ALU = mybir.AluOpType
AX = mybir.AxisListType


class _Ev:
    __slots__ = ("eng", "idx", "val", "clock")

    def __init__(self, eng, idx, val, clock):
        self.eng, self.idx, self.val, self.clock = eng, idx, val, clock


class _Eng:
    def __init__(self, name, sem, self_sync):
        self.name, self.sem, self.self_sync = name, sem, self_sync
        self.ops = []
        self.count = 0
        self.sig_idx = []
        self.sig_val = []
        self.n_inst = 0
        self.inst_rec = []
        self.clock = {}
        self.last_compute = None


class _Slot:
    def __init__(self, name, sem):
        self.name, self.sem, self.total = name, sem, 0


class _Res:
    __slots__ = ("w", "rs")

    def __init__(self):
        self.w, self.rs = None, []


class Sched:
    def __init__(self, nc, sems):
        self.nc = nc
        self._sems = list(sems)
        self.engs = {}
        for name, ss in (("pe", False), ("act", True), ("dve", True), ("pool", True), ("sp", False)):
            self.engs[name] = _Eng(name, self._sems.pop(), ss)
        self.slots = {}
        self.resd = {}

    def slot(self, name):
        s = self.slots.get(name)
        if s is None:
            s = _Slot(name, self._sems.pop())
            self.slots[name] = s
        return s

    def res(self, key):
        r = self.resd.get(key)
        if r is None:
            r = _Res()
            self.resd[key] = r
        return r

    def _value(self, ev):
        if ev.val is not None:
            return ev.val
        E = self.engs[ev.eng]
        j = bisect.bisect_left(E.sig_idx, ev.idx)
        if j < len(E.sig_idx):
            return E.sig_val[j]
        E.count += 1
        rec = E.inst_rec[ev.idx]
        rec[2], rec[3] = E.sem, 1
        E.sig_idx.append(ev.idx)
        E.sig_val.append(E.count)
        ev.val = E.count
        return ev.val

    def _wait_for(self, E, evs):
        need = {}
        for ev in evs:
            if ev is None:
                continue
            if ev.eng == E.name and not E.self_sync:
                continue
            if ev.eng in self.slots:
                v = self.slots[ev.eng].total
            else:
                v = self._value(ev)
            if E.clock.get(ev.eng, 0) >= v:
                continue
            if need.get(ev.eng, (0, None))[0] < v:
                need[ev.eng] = (v, ev)
        for name, (v, ev) in need.items():
            if E.clock.get(name, 0) >= v:
                continue
            sem = self.slots[name].sem if name in self.slots else self.engs[name].sem
            E.ops.append(["w", sem, v])
            E.clock[name] = v
            for k, cv in ev.clock.items():
                if E.clock.get(k, 0) < cv:
                    E.clock[k] = cv

    def _deps(self, reads, writes):
        evs = []
        for k in reads:
            r = self.res(k)
            if r.w is not None:
                evs.append(r.w)
        for k in writes:
            r = self.res(k)
            if r.w is not None:
                evs.append(r.w)
            evs.extend(r.rs)
        return evs

    def _commit(self, ev, reads, writes):
        for k in reads:
            self.res(k).rs.append(ev)
        for k in writes:
            r = self.res(k)
            r.w, r.rs = ev, []

    def op(self, eng, fn, reads=(), writes=()):
        E = self.engs[eng]
        self._wait_for(E, self._deps(reads, writes))
        rec = ["i", fn, None, 0]
        E.ops.append(rec)
        E.inst_rec.append(rec)
        idx = E.n_inst
        E.n_inst += 1
        E.last_compute = idx
        clk = dict(E.clock)
        ev = _Ev(eng, idx, None, clk)
        self._commit(ev, reads, writes)
        return ev

    def dma(self, queue, slot, fn, reads=(), writes=()):
        E = self.engs[queue]
        S = self.slot(slot) if isinstance(slot, str) else slot
        self._wait_for(E, self._deps(reads, writes))
        S.total += 16
        rec = ["i", fn, S.sem, 16]
        E.ops.append(rec)
        E.inst_rec.append(rec)
        E.n_inst += 1
        ev = _Ev(S.name, -1, S.total, dict(E.clock))
        self._commit(ev, reads, writes)
        return ev

    def barrier(self):
        evs = []
        for E in self.engs.values():
            if E.last_compute is not None:
                ev = _Ev(E.name, E.last_compute, None, dict(E.clock))
                self._value(ev)
                evs.append(ev)
        for S in self.slots.values():
            if S.total:
                evs.append(_Ev(S.name, -1, S.total, {}))
        for E in self.engs.values():
            self._wait_for(E, evs)
        self.resd = {}

    def wait_all_dma(self, eng, slots):
        E = self.engs[eng]
        evs = [_Ev(self.slots[s].name, -1, self.slots[s].total, {}) for s in slots if self.slots[s].total]
        self._wait_for(E, evs)

    def emit(self, block):
        def replay(E):
            def run(e):
                for rec in E.ops:
                    if rec[0] == "w":
                        e.wait_ge(rec[1], rec[2])
                    else:
                        ins = rec[1](e)
                        if rec[2] is not None:
                            ins.then_inc(rec[2], rec[3])
            return run
        block.tensor(replay(self.engs["pe"]))
        block.scalar(replay(self.engs["act"]))
        block.vector(replay(self.engs["dve"]))
        block.gpsimd(replay(self.engs["pool"]))
        block.sync(replay(self.engs["sp"]))


D = 1024
S_LEN = 2048
NB = 8
DEPTH = 2
DFF = 2816
NFC = DFF // 128
NKC = D // 128
NT = S_LEN // 512
NT128 = S_LEN // 128
RMS_EPS = 1e-6
LN_EPS = 1e-5
N_IN = 7176
FFN_GROUPS = [(0, 6), (6, 12), (12, 17), (17, 22)]

W_X = 0
W_H = W_X + 16384
W_CONST = W_H + 8192
W_PT = W_CONST + 3072
W_W = W_PT + 1024
W_N = W_W + 9216
W_END = W_N + 12288
ARENA_WORDS = W_END


class K:
    def __init__(self, nc, S, arena, ps, dram):
        self.nc, self.S, self.arena, self.ps, self.d = nc, S, arena, ps, dram
        self.ps_rr = 0

    def f32(self, off, n):
        return self.arena[:, off:off + n]

    def bf(self, off, n):
        return self.arena[:, off:off + (n + 1) // 2].bitcast(BF16)

    def bank(self, b):
        return self.ps[:, b * 512:(b + 1) * 512]

    def X(self, c, t):
        o = W_X + c * S_LEN + t * 512
        return self.arena[:, o:o + 512]

    def H(self, c, t0=0, n=S_LEN):
        return self.bf(W_H + c * (S_LEN // 2), S_LEN)[:, t0:t0 + n]

    def PT(self, i):
        return self.bf(W_PT + i * 256, 512)


def build_consts(k):
    S, d = k.S, k.d
    o = W_CONST
    k.ident = k.f32(o, 128); o += 128
    k.ones_bf = k.bf(o, 128); o += 64
    k.tri_bf = k.bf(o, 128); o += 64
    k.normw = k.f32(o, 7 * 8); o += 56
    k.const_end = o
    S.dma("sp", "c0", lambda e: e.dma_start(out=k.ident, in_=d["ident"][:, :]), writes=["ident"])
    S.dma("pool", "c1", lambda e: e.dma_start(out=k.ones_bf, in_=d["ones"][:, :]), writes=["ones"])
    S.dma("pool", "c1", lambda e: e.dma_start(out=k.tri_bf, in_=d["tri"][:, :]), writes=["tri"])
    S.dma("sp", "c0", lambda e: e.dma_start(out=k.normw, in_=d["normw"][:, :]), writes=["normw"])


def load_x(k):
    S, d = k.S, k.d
    for c in range(NKC):
        o = W_X + c * S_LEN
        S.dma("sp", "xld", lambda e, c=c, o=o: e.dma_start(out=k.arena[:, o:o + S_LEN], in_=d["xT"][:, c, :]),
              writes=[("X", c, t) for t in range(NT)])


def rmsnorm(k, widx, out_mode):
    S = k.S
    R0 = W_N + 8192
    for t in range(NT):
        for c in range(NKC):
            pt = k.PT((t * NKC + c) % 4)
            key = ("PT", (t * NKC + c) % 4)
            S.op("act", lambda e, pt=pt, c=c, t=t: e.activation(out=pt, in_=k.X(c, t), func=AF.Square),
                 reads=[("X", c, t)], writes=[key])
            S.op("pe", lambda e, pt=pt, c=c, t=t: e.matmul(k.bank(t), k.ones_bf, pt, start=(c == 0), stop=(c == NKC - 1)),
                 reads=[key, "ones"], writes=[("ps", t)])
    for t in range(NT):
        R = k.f32(R0 + t * 512, 512)
        S.op("act", lambda e, R=R, t=t: e.activation(out=R, in_=k.bank(t), func=AF.Sqrt, bias=RMS_EPS, scale=1.0 / D),
             reads=[("ps", t)], writes=[("R", t)])
        S.op("dve", lambda e, R=R: e.reciprocal(R, R), reads=[("R", t)], writes=[("R", t)])
        for c in range(NKC):
            wcol = k.normw[:, widx * 8 + c:widx * 8 + c + 1]
            if out_mode == "H":
                S.op("dve", lambda e, R=R, c=c, t=t, wcol=wcol: e.scalar_tensor_tensor(
                    out=k.H(c, t * 512, 512), in0=k.X(c, t), scalar=wcol, in1=R, op0=ALU.mult, op1=ALU.mult),
                    reads=[("X", c, t), ("R", t), "normw"], writes=[("H", c, t)])
            else:
                S.op("dve", lambda e, R=R, c=c, t=t, wcol=wcol: e.scalar_tensor_tensor(
                    out=k.X(c, t), in0=k.X(c, t), scalar=wcol, in1=R, op0=ALU.mult, op1=ALU.mult),
                    reads=[("X", c, t), ("R", t), "normw"], writes=[("X", c, t)])


def ffn(k, l, which):
    S, d = k.S, k.d
    gu_d = d[f"ffn{which}_gu"]
    wd_d = d[f"ffn{which}_wd"]
    GU = [k.bf(W_W + i * 1024, 2048) for i in range(3)]
    WD = [k.bf(W_W + 3072 + i * 3072, 6 * 1024) for i in range(2)]
    A = [k.bf(W_N + i * 1024, 2048) for i in range(7)]
    SG = [k.f32(W_N + 7168 + i * 512, 512) for i in range(2)]

    def load_gu(c):
        s = c % 3
        S.dma("pool", f"gu{s}", lambda e: e.dma_start(out=GU[s], in_=gu_d[l, c, :, :]), writes=[("GU", s)])

    def load_wd(g):
        s = g % 2
        c0, c1 = FFN_GROUPS[g]
        for c in range(c0, c1):
            S.dma("pool", f"wd{s}", lambda e, c=c: e.dma_start(out=WD[s][:, (c - c0) * 1024:(c - c0 + 1) * 1024], in_=wd_d[l, c, :, :]),
                  writes=[("WD", s)])

    def gu_chunk(c):
        s = c % 3
        for t in range(NT):
            pr = (c * NT + t) % 3
            bg, bu = 2 * pr, 2 * pr + 1
            for g, b in ((0, bg), (1, bu)):
                for kc in range(NKC):
                    S.op("pe", lambda e, g=g, b=b, kc=kc, t=t: e.matmul(
                        k.bank(b), GU[s][:, (g * 8 + kc) * 128:(g * 8 + kc + 1) * 128], k.H(kc, t * 512, 512),
                        start=(kc == 0), stop=(kc == NKC - 1)),
                        reads=[("GU", s), ("H", kc, t)], writes=[("ps", b)])
            sg = SG[(c * NT + t) % 2]
            sgk = ("SG", (c * NT + t) % 2)
            S.op("act", lambda e, sg=sg, bg=bg: e.activation(out=sg, in_=k.bank(bg), func=AF.Silu),
                 reads=[("ps", bg)], writes=[sgk])
            S.op("dve", lambda e, sg=sg, bu=bu, t=t: e.tensor_tensor(
                out=A[c % 7][:, t * 512:(t + 1) * 512], in0=sg, in1=k.bank(bu), op=ALU.mult),
                reads=[sgk, ("ps", bu)], writes=[("A", c % 7, t)])

    dn_cnt = [0]

    def down(g):
        s = g % 2
        c0, c1 = FFN_GROUPS[g]
        for t in range(NT):
            for dd in range(NKC):
                b = 6 + dn_cnt[0] % 2
                dn_cnt[0] += 1
                for c in range(c0, c1):
                    S.op("pe", lambda e, b=b, c=c, dd=dd, t=t: e.matmul(
                        k.bank(b), WD[s][:, (c - c0) * 1024 + dd * 128:(c - c0) * 1024 + (dd + 1) * 128],
                        A[c % 7][:, t * 512:(t + 1) * 512], start=(c == c0), stop=(c == c1 - 1)),
                        reads=[("WD", s), ("A", c % 7, t)], writes=[("ps", b)])
                S.op("dve", lambda e, b=b, dd=dd, t=t: e.scalar_tensor_tensor(
                    out=k.X(dd, t), in0=k.bank(b), scalar=0.5, in1=k.X(dd, t), op0=ALU.mult, op1=ALU.add),
                    reads=[("ps", b), ("X", dd, t)], writes=[("X", dd, t)])

    for c in range(3):
        load_gu(c)
    load_wd(0)
    load_wd(1)
    grp_of = {}
    for g, (c0, c1) in enumerate(FFN_GROUPS):
        for c in range(c0, c1):
            grp_of[c] = g
    for c in range(NFC):
        gu_chunk(c)
        if c + 3 < NFC:
            load_gu(c + 3)
        g = grp_of[c]
        if c == FFN_GROUPS[g][0] and g > 0:
            down(g - 1)
            if g + 1 < len(FFN_GROUPS):
                load_wd(g + 1)
    down(len(FFN_GROUPS) - 1)


def store_out(k):
    S, d = k.S, k.d
    for c in range(NKC):
        o = W_X + c * S_LEN
        S.dma("sp", "ost", lambda e, c=c, o=o: e.dma_start(out=d["outT"][:, c, :], in_=k.arena[:, o:o + S_LEN]),
              reads=[("X", c, t) for t in range(NT)])
    S.wait_all_dma("sp", ["ost"])


U = 4096
W_SCR = W_END + 64
ARENA_WORDS = W_END + 2560
SB = W_SCR + 64
NEG_BIG = -30000.0
C_DQ, C_DK, C_DV = 0, 512, 1024
C_FQ, C_FK, C_FV, C_FF = 1536, 2048, 2560, 3072
C_MX, C_MZ, C_G = 3080, 3592, 4104


def DU(i):
    return W_X + i * U


def NU(i):
    return W_N + i * U


def fm(k, off, c, t0=0, n=S_LEN):
    return k.bf(off + c * (S_LEN // 2), S_LEN)[:, t0:t0 + n]


def next_bank(k):
    b = k.ps_rr % 8
    k.ps_rr += 1
    return b


def col(k):
    i = getattr(k, "_col_rr", 0)
    k._col_rr = i + 1
    i %= 48
    return k.arena[:, W_SCR + i:W_SCR + i + 1], ("col", i)


def evac(k, dst, src, src_key, dst_key, scale=None, func=None, eng=None):
    S = k.S
    if eng is None:
        k._ev_rr = getattr(k, "_ev_rr", 0) + 1
        eng = "act" if (func is not None or k._ev_rr % 2 == 0) else "dve"
    if eng == "act":
        f = func if func is not None else AF.Copy
        sc = 1.0 if scale is None else scale
        S.op("act", lambda e: e.activation(out=dst, in_=src, func=f, scale=sc), reads=[src_key], writes=[dst_key])
    else:
        if scale is None:
            S.op("dve", lambda e: e.tensor_copy(dst, src), reads=[src_key], writes=[dst_key])
        else:
            S.op("dve", lambda e: e.tensor_scalar(dst, src, scale, None, ALU.mult), reads=[src_key], writes=[dst_key])


def wcol_jobs(k, l, jobs):
    S, d = k.S, k.d

    def load(i):
        c0, nc_, _ = jobs[i]
        s = i % 2
        tile = k.bf(W_W + s * 2048, 4096).rearrange("p (k c) -> p k c", k=8)
        S.dma("pool", f"wc{s}", lambda e: e.dma_start(out=tile[:, :, 0:nc_], in_=d["w_in"][l, :, :, c0:c0 + nc_]),
              writes=[("WC", s)])
        return tile

    tiles = {}
    for i in range(min(2, len(jobs))):
        tiles[i] = load(i)
    for i in range(len(jobs)):
        jobs[i][2](tiles[i], ("WC", i % 2))
        if i + 2 < len(jobs):
            tiles[i + 2] = load(i + 2)


def h_fm(dst_off, scale=None, func=None):
    def mk(k, c_base, nchunks=4):
        def handler(W, wkey):
            S = k.S
            for m in range(nchunks):
                for t in range(NT):
                    b = next_bank(k)
                    for kc in range(NKC):
                        S.op("pe", lambda e, b=b, m=m, kc=kc, t=t: e.matmul(
                            k.bank(b), W[:, kc, m * 128:(m + 1) * 128], k.H(kc, t * 512, 512),
                            start=(kc == 0), stop=(kc == NKC - 1)),
                            reads=[wkey, ("H", kc, t)], writes=[("ps", b)])
                    evac(k, fm(k, dst_off, c_base + m, t * 512, 512), k.bank(b), ("ps", b),
                         ("fm", dst_off, c_base + m, t), scale=scale, func=func)
        return handler
    return mk


def h_tm(k, vbuf_fn, nh, dv):
    def handler(W, wkey):
        S = k.S
        for tt in range(NT128):
            b = next_bank(k)
            for kc in range(NKC):
                S.op("pe", lambda e, b=b, kc=kc, tt=tt: e.matmul(
                    k.bank(b), k.H(kc, tt * 128, 128), W[:, kc, 0:512],
                    start=(kc == 0), stop=(kc == NKC - 1)),
                    reads=[wkey, ("H", kc, tt // 4)], writes=[("ps", b)])
            dst = vbuf_fn(tt)[:, :, 0:dv]
            src = k.bank(b).rearrange("p (h e) -> p h e", h=nh)
            evac(k, dst, src, ("ps", b), ("V", tt))
    return handler


class AttnPass:
    def __init__(self, k, *, qf, kf, aqf, akf, bias_fn, vf, vkey, dv, mode, fin, sbanks, obanks, scale=1.0, augkey=None, pre=None, act_func=None, scale_fn=None, mask_mul=False):
        self.__dict__.update(locals())
        self.state = {}
        self.per_bank = 2 if dv == 128 else 4

    def blocks(self):
        return [(self, j, i) for j in range(4) for i in range(4 * j + 4)]

    def O(self, j, r):
        pb = self.per_bank
        ob = self.obanks[j % len(self.obanks)][r // pb]
        c0 = (r % pb) * (self.dv + 1)
        return self.k.bank(ob)[:, c0:c0 + self.dv + 1], ("ps", ob)

    def emit_s(self, j, i):
        k, S = self.k, self.k.S
        if self.pre is not None:
            pl = self.pre if isinstance(self.pre, list) else [self.pre]
            self.pre = None
            for f_ in pl:
                f_()
        r0 = max(0, i - 4 * j)
        n0 = r0 * 128
        ncol = 512 - n0
        diag = i >= 4 * j
        k._blk = getattr(k, "_blk", 0) + 1
        pti = k._blk % 4
        pt = k.PT(pti)
        ptk = ("PT", pti)
        if self.mode == "exp":
            sb = self.sbanks[k._blk % len(self.sbanks)]
            has_aug = self.akf is not None
            S.op("pe", lambda e: e.matmul(k.bank(sb)[:, n0:512], self.kf(i), self.qf(j * 512 + n0, ncol),
                                          start=True, stop=(not has_aug and (not diag or self.mask_mul))),
                 reads=([self.augkey] if (self.augkey is not None and not has_aug) else []), writes=[("ps", sb)])
            if has_aug:
                S.op("pe", lambda e: e.matmul(k.bank(sb)[:, n0:512], self.akf(i), self.aqf(j, n0, ncol),
                                              start=False, stop=(not diag)),
                     reads=[self.augkey], writes=[("ps", sb)])
            if diag and not self.mask_mul:
                S.op("pe", lambda e: e.matmul(k.bank(sb)[:, n0:n0 + 128], k.ident_bf, k.negtri_bf, start=False, stop=True),
                     reads=["identbf", "negtri"], writes=[("ps", sb)])
            bias = float(self.bias_fn(i, j)) if self.bias_fn is not None else 0.0
            if self.act_func is None:
                S.op("act", lambda e: e.activation(out=pt[:, n0:512], in_=k.bank(sb)[:, n0:512], func=AF.Exp, bias=bias, scale=1.0),
                     reads=[("ps", sb)], writes=[ptk])
            else:
                sc_ap = self.scale_fn(i, j)
                S.op("act", lambda e: e.activation(out=pt[:, n0:512], in_=k.bank(sb)[:, n0:512], func=self.act_func, scale=sc_ap),
                     reads=[("ps", sb), "UC"], writes=[ptk])
            if diag and self.mask_mul:
                S.op("pool", lambda e: e.tensor_tensor(out=pt[:, n0:n0 + 128], in0=pt[:, n0:n0 + 128], in1=k.tri_bf, op=ALU.mult),
                     reads=[ptk, "tri"], writes=[ptk])
        else:
            pr = self.sbanks[k._blk % len(self.sbanks)]
            sb, eb = pr
            S.op("pe", lambda e: e.matmul(k.bank(sb)[:, n0:512], self.kf(i), self.qf(j * 512 + n0, ncol), start=True, stop=True),
                 reads=[], writes=[("ps", sb)])
            S.op("pe", lambda e: e.matmul(k.bank(eb)[:, n0:512], self.akf(i), self.aqf(j, n0, ncol), start=True, stop=(not diag)),
                 reads=[self.augkey], writes=[("ps", eb)])
            if diag:
                S.op("pe", lambda e: e.matmul(k.bank(eb)[:, n0:n0 + 128], k.ident_bf, k.negtri_bf, start=False, stop=True),
                     reads=["identbf", "negtri"], writes=[("ps", eb)])
            eti = k._blk % 2
            et = k.f32(SB + eti * 512, 512)
            S.op("act", lambda e: e.activation(out=et[:, n0:512], in_=k.bank(eb)[:, n0:512], func=AF.Exp),
                 reads=[("ps", eb)], writes=[("ET", eti)])
            sc = self.scale
            S.op("dve", lambda e: e.scalar_tensor_tensor(out=pt[:, n0:512], in0=k.bank(sb)[:, n0:512], scalar=sc,
                                                         in1=et[:, n0:512], op0=ALU.mult, op1=ALU.mult),
                 reads=[("ps", sb), ("ET", eti)], writes=[ptk])
        self.state[(j, i)] = (pt, ptk, r0)

    def emit_pv(self, j, i):
        k, S = self.k, self.k.S
        pt, ptk, r0 = self.state.pop((j, i))
        for r in range(r0, 4):
            o, okey = self.O(j, r)
            last = (i == 4 * j + r)
            S.op("pe", lambda e, r=r, o=o, last=last: e.matmul(o, pt[:, r * 128:(r + 1) * 128], self.vf(i), start=(i == 0 and r % self.per_bank == 0), stop=last, skip_group_check=True),
                 reads=[ptk, self.vkey], writes=[okey])
            if last:
                self.fin(j, r, o, okey)


def run_blocks(k, blocks, L=2):
    n = len(blocks)
    for idx in range(n + L):
        if idx < n:
            p, j, i = blocks[idx]
            p.emit_s(j, i)
        if idx >= L:
            p, j, i = blocks[idx - L]
            p.emit_pv(j, i)
        tick(k)
    flush_deferred(k)


def defer(k, n, fn):
    k._dq = getattr(k, "_dq", [])
    k._dq.append([n, fn])


def tick(k):
    dq = getattr(k, "_dq", [])
    k._dq = []
    keep = []
    for it in dq:
        it[0] -= 1
        if it[0] <= 0:
            it[1]()
        else:
            keep.append(it)
    k._dq = keep + k._dq


def flush_deferred(k):
    while getattr(k, "_dq", []):
        dq = k._dq
        k._dq = []
        for it in dq:
            it[1]()


def transpose_to_fm(k, src, src_key, dst, dst_key, post=None):
    S = k.S
    q = getattr(k, "_tq", 0)
    k._tq = q + 1
    tb = k.tbanks[q % len(k.tbanks)]
    pq = k.bank(tb)[:, 0:128]
    pkey = ("ps", tb)
    S.op("pe", lambda e: e.transpose(pq, src, k.ident), reads=[src_key, "ident"], writes=[pkey])
    if post is None:
        evac(k, dst, pq, pkey, dst_key)
    else:
        post(pq, pkey)


def softplus_neg(k, T, nh, banks, negb_col, key):
    S = k.S
    for t in range(NT):
        b = banks[t]
        S.op("act", lambda e, b=b, t=t: e.activation(out=T[0:nh, t * 512:(t + 1) * 512], in_=k.bank(b)[0:nh, :],
                                                    func=AF.Exp, bias=negb_col, scale=-1.0),
             reads=[("ps", b), "gcols"], writes=[key])
    S.op("act", lambda e: e.activation(out=T[0:nh, :], in_=T[0:nh, :], func=AF.Ln, bias=1.0, scale=1.0),
         reads=[key], writes=[key])


def build_aug(k, kvec, kkey, qvec, qkey, nh, spl_off, tag):
    S, d = k.S, k.d
    dk, dq = d["augk_" + tag], d["augq_" + tag]
    SPL = [k.bf(spl_off + i * 1024, 2048) for i in range(4)]
    ones = SPL[3]
    S.op("dve", lambda e: e.memset(ones[0:nh, :], 1.0), writes=[("SPL", 3)])
    for r in range(3):
        S.dma("sp", "augw", lambda e, r=r: e.dma_start(out=dk[0:nh, 3 + r, :], in_=ones[0:nh, :]), reads=[("SPL", 3)], writes=[("augd", tag)])
        S.dma("sp", "augw", lambda e, r=r: e.dma_start(out=dq[0:nh, r, :], in_=ones[0:nh, :]), reads=[("SPL", 3)], writes=[("augd", tag)])
    cnt = 0
    for vec, vkey, dram, r0 in ((kvec, kkey, dk, 0), (qvec, qkey, dq, 3)):
        for r in range(3):
            si = cnt % 3
            cnt += 1
            spl = SPL[si]
            S.op("dve", lambda e, spl=spl, vec=vec: e.tensor_copy(spl[0:nh, :], vec[0:nh, :]), reads=[vkey], writes=[("SPL", si)])
            if r < 2:
                S.op("dve", lambda e, spl=spl, vec=vec: e.tensor_tensor(out=vec[0:nh, :], in0=vec[0:nh, :], in1=spl[0:nh, :], op=ALU.subtract),
                     reads=[vkey, ("SPL", si)], writes=[vkey])
            S.dma("sp", "augw", lambda e, spl=spl, dram=dram, rr=r0 + r: e.dma_start(out=dram[0:nh, rr, :], in_=spl[0:nh, :]),
                  reads=[("SPL", si)], writes=[("augd", tag)])


def aug_views(k, slot):
    ak = k.bf(W_W + 5120 + slot * 2048, 2048)
    aq = k.bf(W_W + 5120 + slot * 2048 + 1024, 2048)
    akf = lambda i: ak[0:6, i * 128:(i + 1) * 128]
    aqf = lambda j, n0, n: aq[0:6, j * 512 + n0:j * 512 + n0 + n]
    return akf, aqf


def load_aug(k, tag, h, slot):
    S, d = k.S, k.d
    ak = k.bf(W_W + 5120 + slot * 2048, 2048)
    aq = k.bf(W_W + 5120 + slot * 2048 + 1024, 2048)
    S.dma("sp", f"augl{slot}", lambda e: e.dma_start(out=ak[0:6, :], in_=d["augk_" + tag][h, :, :]), reads=[("augd", tag)], writes=[("AUGS", slot)])
    S.dma("sp", f"augl{slot}", lambda e: e.dma_start(out=aq[0:6, :], in_=d["augq_" + tag][h, :, :]), reads=[("augd", tag)], writes=[("AUGS", slot)])


HB_OFF = [W_W + 0, W_W + 2048, W_W + 5120, W_W + 7168]


def hb_views(k, slot):
    return k.bf(HB_OFF[slot], 2048), k.bf(HB_OFF[slot] + 1024, 2048)


def hb_init(k):
    S = k.S
    for s_ in range(4):
        qb, kb = hb_views(k, s_)
        S.op("dve", lambda e, qb=qb: e.memset(qb, 0.0), writes=[("HB", s_)])
        S.op("dve", lambda e, kb=kb: e.memset(kb, 0.0), writes=[("HB", s_)])


def hb_build(k, slot, par, q_src, k_src, augq_ap, augk_ap, R, queue):
    S = k.S
    qb, kb = hb_views(k, slot)
    r0 = par * 64
    a0 = 64 if par == 0 else 0
    S.op("pool", lambda e: e.tensor_copy(qb[r0:r0 + 64, :], q_src[r0:r0 + 64, :]), writes=[("HB", slot)])
    S.op("pool", lambda e: e.tensor_copy(kb[r0:r0 + 64, :], k_src[r0:r0 + 64, :]), writes=[("HB", slot)])
    S.dma(queue, f"hb{queue}{slot}", lambda e: e.dma_start(out=qb[a0:a0 + R, :], in_=augq_ap), writes=[("HB", slot)])
    S.dma(queue, f"hb{queue}{slot}", lambda e: e.dma_start(out=kb[a0:a0 + R, :], in_=augk_ap), writes=[("HB", slot)])


def layer_consts(k, l):
    S, d = k.S, k.d
    o = k.const_end
    k.gb = k.f32(o, 24); o += 24
    k.convw = k.f32(o, 16); o += 16
    k.convb = k.f32(o, 4); o += 4
    k.skipc = k.f32(o, 4); o += 4
    k.normc = k.f32(o, 4); o += 4
    k.gcols = k.f32(o, 8); o += 8
    k.lamt = k.f32(o, 256); o += 256
    k.lamc = k.f32(o, 8); o += 8
    k.subln = k.f32(o, 128); o += 128
    k.normB = k.f32(o, 512); o += 512
    k.AKd = k.bf(o, 4 * 128); o += 256
    k.AQd = k.bf(o, 4 * 512); o += 1024
    k.ident_bf = k.bf(o, 128); o += 64
    k.negtri_bf = k.bf(o, 128); o += 64
    assert o <= W_CONST + 3072, o
    S.dma("sp", "lc", lambda e: e.dma_start(out=k.gb, in_=d["gbcols"][l, :, :]), writes=["lconst"])
    S.dma("sp", "lc", lambda e: e.dma_start(out=k.convw, in_=d["convw"][l, :, :]), writes=["lconst"])
    S.dma("sp", "lc", lambda e: e.dma_start(out=k.convb, in_=d["convb"][l, :, :]), writes=["lconst"])
    S.dma("sp", "lc", lambda e: e.dma_start(out=k.skipc, in_=d["skipc"][l, :, :]), writes=["lconst"])
    S.dma("sp", "lc", lambda e: e.dma_start(out=k.normc, in_=d["normc"][l, :, :]), writes=["lconst"])
    S.dma("sp", "lc", lambda e: e.dma_start(out=k.gcols[0:8, :], in_=d["gcols"][l, :, :]), writes=["gcols"])
    S.dma("sp", "lc", lambda e: e.dma_start(out=k.lamt, in_=d["difflam"][l:l + 1, :].partition_broadcast(128)), writes=["lamt"])
    S.dma("sp", "lc", lambda e: e.dma_start(out=k.subln, in_=d["diff_subln"][l:l + 1, :].partition_broadcast(128)), writes=["subln"])
    S.dma("sp", "lc", lambda e: e.dma_start(out=k.normB, in_=d["mlstm_norm"][l:l + 1, :].partition_broadcast(128)), writes=["normB"])
    S.op("dve", lambda e: e.tensor_scalar(k.gcols[0:8, 4:5], k.gcols[0:8, 0:1], -1.0, None, ALU.mult), reads=["gcols"], writes=["gcols"])
    S.op("dve", lambda e: e.tensor_scalar(k.gcols[0:8, 5:6], k.gcols[0:8, 2:3], -1.0, None, ALU.mult), reads=["gcols"], writes=["gcols"])
    if l == 0:
        S.dma("pool", "lc2", lambda e: e.dma_start(out=k.AKd[0:3, :], in_=d["alibi_k"][:, :]), writes=["alibi"])
        S.dma("pool", "lc2", lambda e: e.dma_start(out=k.AQd[0:3, :], in_=d["alibi_q"][:, :]), writes=["alibi"])
        S.dma("pool", "lc2", lambda e: e.dma_start(out=k.ident_bf, in_=d["ident"][:, :]), writes=["identbf"])
        S.dma("pool", "lc2", lambda e: e.dma_start(out=k.negtri_bf, in_=d["negtri"][:, :]), writes=["negtri"])
    import math
    lam_init = 0.8 - 0.6 * math.exp(-0.3 * l)
    lt, lc = k.lamt, k.lamc
    S.op("dve", lambda e: e.tensor_tensor(out=lt[:, 0:64], in0=lt[:, 0:64], in1=lt[:, 64:128], op=ALU.mult), reads=["lamt"], writes=["lamt"])
    S.op("dve", lambda e: e.tensor_tensor(out=lt[:, 128:192], in0=lt[:, 128:192], in1=lt[:, 192:256], op=ALU.mult), reads=["lamt"], writes=["lamt"])
    S.op("dve", lambda e: e.reduce_sum(out=lc[:, 0:1], in_=lt[:, 0:64], axis=AX.X), reads=["lamt"], writes=["lamc"])
    S.op("dve", lambda e: e.reduce_sum(out=lc[:, 1:2], in_=lt[:, 128:192], axis=AX.X), reads=["lamt"], writes=["lamc"])
    S.op("act", lambda e: e.activation(out=lc[:, 2:4], in_=lc[:, 0:2], func=AF.Exp), reads=["lamc"], writes=["lamc"])
    S.op("dve", lambda e: e.tensor_tensor(out=lc[:, 4:5], in0=lc[:, 3:4], in1=lc[:, 2:3], op=ALU.subtract), reads=["lamc"], writes=["lamc"])
    S.op("dve", lambda e: e.tensor_scalar(lc[:, 5:6], lc[:, 4:5], -lam_init, None, ALU.add), reads=["lamc"], writes=["neglam"])
    S.op("dve", lambda e: e.tensor_scalar(k.subln, k.subln, 1.0 - lam_init, None, ALU.mult), reads=["subln"], writes=["subln"])
    k.neglam = lc[:, 5:6]


def diff_branch(k, l):
    S = k.S
    QT, KT, VO, OD = DU(0), DU(1), DU(2), NU(0)
    V = k.bf(VO, 16 * 4 * 129).rearrange("p (t h e) -> p t h e", t=16, h=4)
    S.op("dve", lambda e: e.memset(k.bf(VO, 16 * 4 * 129), 1.0), writes=[("V", "all")])
    S.barrier()
    wcol_jobs(k, l, [
        (C_DQ, 512, h_fm(QT)(k, 0)),
        (C_DK, 512, h_fm(KT, scale=0.125)(k, 0)),
        (C_DV, 512, h_tm(k, lambda tt: V[:, tt, :, :], 4, 128)),
    ])
    S.barrier()
    k.tbanks = [3]
    slopes = [2.0 ** (-8.0 * (h + 1) / 4) for h in range(4)]
    A1 = [[k.f32(SB + (p * 4 + r) * 128, 128) for r in range(4)] for p in range(2)]
    TB = SB + 1024
    blocks = []
    pres = []
    first_pass = []
    hb_init(k)
    for h in range(4):
        passes = []
        for c in range(2):
            slot = c + 2 * (h % 2)
            qb, kb = hb_views(k, slot)

            def qf(t0, n, qb=qb):
                return qb[:, t0:t0 + n]

            def kf(i, kb=kb):
                return kb[:, i * 128:(i + 1) * 128]

            pres.append(lambda h=h, c=c, slot=slot: hb_build(k, slot, c, fm(k, QT, h), fm(k, KT, h),
                                                              k.d["alibi_qf"][h, :, :], k.d["alibi_kf"][h, :, :], 3, "pool"))

            def bias_fn(i, j, h=h):
                return slopes[h] * (128.0 * i - 512.0 * j)

            def vf(i, h=h):
                return V[:, i, h, :]

            if c == 0:
                def fin(j, r, o, okey, h=h):
                    rc, rk = col(k)
                    a1 = A1[j % 2][r]
                    S.op("dve", lambda e: e.reciprocal(rc, o[:, 128:129]), reads=[okey], writes=[rk])
                    S.op("dve", lambda e: e.tensor_scalar(a1, o[:, 0:128], rc, None, ALU.mult), reads=[okey, rk], writes=[("A1", j % 2, r)])
            else:
                def fin(j, r, o, okey, h=h):
                    rc, rk = col(k)
                    sc, sk = col(k)
                    k._fp = getattr(k, "_fp", 0) + 1
                    n = k._fp
                    tmp = k.f32(TB + (n % 5) * 128, 128)
                    sq = k.f32(TB + 640 + (n % 2) * 128, 128)
                    ot = k.f32(TB + 896 + (n % 4) * 128, 128)
                    tk_, sqk, otk = ("TMP", n % 5), ("SQ", n % 2), ("OT", n % 4)
                    a1 = A1[j % 2][r]
                    tt = 4 * j + r
                    S.op("dve", lambda e: e.reciprocal(rc, o[:, 128:129]), reads=[okey], writes=[rk])
                    S.op("dve", lambda e: e.tensor_scalar(tmp, o[:, 0:128], rc, None, ALU.mult), reads=[okey, rk], writes=[tk_])
                    S.op("dve", lambda e: e.scalar_tensor_tensor(out=tmp, in0=tmp, scalar=k.neglam, in1=a1, op0=ALU.mult, op1=ALU.add),
                         reads=[tk_, ("A1", j % 2, r), "neglam"], writes=[tk_])
                    S.op("dve", lambda e: e.tensor_tensor(out=sq, in0=tmp, in1=tmp, op=ALU.mult), reads=[tk_], writes=[sqk])
                    S.op("dve", lambda e: e.reduce_sum(out=sc, in_=sq, axis=AX.X), reads=[sqk], writes=[sk])

                    def stage_b():
                        S.op("act", lambda e: e.activation(out=sc, in_=sc, func=AF.Ln, bias=RMS_EPS, scale=1.0 / 128), reads=[sk], writes=[sk])
                        S.op("act", lambda e: e.activation(out=sc, in_=sc, func=AF.Exp, scale=-0.5), reads=[sk], writes=[sk])

                    def stage_c():
                        S.op("dve", lambda e: e.scalar_tensor_tensor(out=ot, in0=tmp, scalar=sc, in1=k.subln, op0=ALU.mult, op1=ALU.mult),
                             reads=[tk_, sk, "subln"], writes=[otk])
                        defer(k, 2, lambda: transpose_to_fm(k, ot, otk, fm(k, OD, h, tt * 128, 128), ("fm", OD, h, tt)))
                    defer(k, 2, stage_b)
                    defer(k, 3, stage_c)
            passes.append(AttnPass(k, qf=qf, kf=kf, aqf=None, akf=None, bias_fn=bias_fn, vf=vf, vkey=("V", "x"), dv=128,
                                   mode="exp", fin=fin, sbanks=[0, 1, 2], obanks=[(4, 5)] if c == 0 else [(6, 7)], augkey=("HB", slot)))
        first_pass.append(passes[0])
        b0, b1 = passes[0].blocks(), passes[1].blocks()
        for j in range(4):
            blocks += [b for b in b0 if b[1] == j]
            blocks += [b for b in b1 if b[1] == j]
    for u in range(4):
        first_pass[u].pre = (pres[0:4] if u == 0 else (pres[2 * u + 2:2 * u + 4] if u < 3 else None))
    run_blocks(k, blocks)
    S.barrier()


def fox_branch(k, l):
    S, d = k.S, k.d
    QT, KT, VO, OF = DU(0), DU(1), DU(2), NU(2)
    V = k.bf(VO, 16 * 8 * 65).rearrange("p (t h e) -> p t h e", t=16, h=8)
    WFF = k.bf(W_W + 4096, 64).rearrange("p (k c) -> p k c", k=8)
    S.op("dve", lambda e: e.memset(k.bf(VO, 16 * 8 * 65), 1.0), writes=[("V", "all")])
    S.dma("pool", "wff", lambda e: e.dma_start(out=WFF, in_=d["w_in"][l, :, :, C_FF:C_FF + 8]), writes=["WFF"])
    S.barrier()
    wcol_jobs(k, l, [
        (C_FQ, 512, h_fm(QT)(k, 0)),
        (C_FK, 512, h_fm(KT, scale=0.125)(k, 0)),
        (C_FV, 512, h_tm(k, lambda tt: V[:, tt, :, :], 8, 64)),
    ])
    for t in range(NT):
        for kc in range(NKC):
            S.op("pe", lambda e, kc=kc, t=t: e.matmul(k.bank(t)[0:8, :], WFF[:, kc, :], k.H(kc, t * 512, 512),
                                                      start=(kc == 0), stop=(kc == NKC - 1)),
                 reads=["WFF", ("H", kc, t)], writes=[("ps", t)])
    S.barrier()
    T1 = k.f32(W_W, 2048)
    T2 = k.f32(W_W + 2048, 2048)
    ONES = k.f32(W_W + 7168, 2048)
    S.op("dve", lambda e: e.memset(ONES[0:8, :], 1.0), writes=["ONES"])
    softplus_neg(k, T1, 8, [0, 1, 2, 3], k.gcols[0:8, 4:5], "T1")
    S.op("dve", lambda e: e.tensor_tensor_scan(T2[0:8, :], ONES[0:8, :], T1[0:8, :], 0.0, ALU.mult, ALU.add), reads=["T1", "ONES"], writes=["T2"])
    S.op("dve", lambda e: e.tensor_scalar(T1[0:8, :], T2[0:8, :], -1.0, None, ALU.mult), reads=["T2"], writes=["T1"])
    build_aug(k, T2, "T2", T1, "T1", 8, NU(2), "fox")
    S.barrier()
    k.tbanks = [3, 7]
    OTB = [[k.f32(SB + (p * 4 + r) * 128, 128) for r in range(4)] for p in range(2)]
    blocks = []
    pres = []
    first_pass = []
    hb_init(k)
    for pr in range(4):
        passes = []
        for c in range(2):
            hd = 2 * pr + c
            slot = c + 2 * (pr % 2)
            qb, kb = hb_views(k, slot)

            def qf(t0, n, qb=qb):
                return qb[:, t0:t0 + n]

            def kf(i, kb=kb):
                return kb[:, i * 128:(i + 1) * 128]

            pres.append(lambda pr=pr, c=c, slot=slot, hd=hd: hb_build(k, slot, c, fm(k, QT, pr), fm(k, KT, pr),
                                                                      k.d["augq_fox"][hd, :, :], k.d["augk_fox"][hd, :, :], 6, "sp"))

            def vf(i, hd=hd):
                return V[:, i, hd, :]

            def fin(j, r, o, okey, pr=pr, c=c):
                rc, rk = col(k)
                ot = OTB[j % 2][r]
                otk = ("OTB", j % 2, r)
                S.op("dve", lambda e: e.reciprocal(rc, o[:, 64:65]), reads=[okey], writes=[rk])
                S.op("dve", lambda e: e.tensor_scalar(ot[:, c * 64:(c + 1) * 64], o[:, 0:64], rc, None, ALU.mult), reads=[okey, rk], writes=[otk])
                if c == 1:
                    tt = 4 * j + r
                    defer(k, 3, lambda: transpose_to_fm(k, ot, otk, fm(k, OF, pr, tt * 128, 128), ("fm", OF, pr, tt)))
            passes.append(AttnPass(k, qf=qf, kf=kf, aqf=None, akf=None, bias_fn=None, vf=vf, vkey=("V", "x"), dv=64,
                                   mode="exp", fin=fin, sbanks=[0, 1, 2, 6], obanks=[(4,)] if c == 0 else [(5,)], augkey=("HB", slot)))
        first_pass.append(passes[0])
        b0, b1 = passes[0].blocks(), passes[1].blocks()
        for j in range(4):
            blocks += [b for b in b0 if b[1] == j]
            blocks += [b for b in b1 if b[1] == j]
    for u in range(4):
        first_pass[u].pre = (pres[0:4] if u == 0 else (pres[2 * u + 2:2 * u + 4] if u < 3 else None))
    run_blocks(k, blocks, L=3)
    S.barrier()


def mlstm_branch(k, l):
    S, d = k.S, k.d
    QT, KT, VT, MX, XC, SZ, VA = DU(0), DU(1), DU(2), DU(3), NU(0), NU(1), NU(2)
    Vall = k.bf(VA, 16 * 4 * 129).rearrange("p (t h e) -> p t h e", t=16, h=4)
    WQKV = k.bf(W_W + 4096, 1536).rearrange("p (g h e) -> p g h e", g=3, h=4)
    WIF = k.bf(W_W + 4096 + 768, 96).rearrange("p (j o) -> p j o", j=12)
    S.op("dve", lambda e: e.memset(k.bf(VA, 16 * 4 * 129), 1.0), writes=[("V", "all")])
    S.dma("pool", "wqkv", lambda e: e.dma_start(out=k.bf(W_W + 4096, 1536), in_=d["wqkv"][l, :, :]), writes=["WQKV"])
    S.dma("pool", "wqkv", lambda e: e.dma_start(out=k.bf(W_W + 4096 + 768, 96), in_=d["wif"][l, :, :]), writes=["WIF"])
    S.barrier()
    wcol_jobs(k, l, [
        (C_MX, 512, h_fm(MX)(k, 0)),
        (C_MZ, 512, h_fm(SZ, func=AF.Silu)(k, 0)),
    ])
    ACC = k.f32(SB, 2048)
    for c in range(4):
        mx = fm(k, MX, c)
        mkeys = [("fm", MX, c, t) for t in range(NT)]
        w = lambda j, c=c: k.convw[:, j * 4 + c:j * 4 + c + 1]
        S.op("dve", lambda e, mx=mx, c=c, w=w: e.tensor_scalar(ACC, mx, w(3), k.convb[:, c:c + 1], ALU.mult, ALU.add),
             reads=mkeys + ["lconst"], writes=["ACC"])
        for sh in (1, 2, 3):
            S.op("dve", lambda e, mx=mx, sh=sh, w=w: e.scalar_tensor_tensor(
                out=ACC[:, sh:], in0=mx[:, 0:S_LEN - sh], scalar=w(3 - sh), in1=ACC[:, sh:], op0=ALU.mult, op1=ALU.add),
                reads=mkeys + ["ACC", "lconst"], writes=["ACC"])
        S.op("act", lambda e, c=c: e.activation(out=fm(k, XC, c), in_=ACC, func=AF.Silu), reads=["ACC"],
             writes=[("fm", XC, c, t) for t in range(NT)])
    for h in range(4):
        for t in range(NT):
            for g, src, dst in ((0, XC, QT), (1, XC, KT), (2, MX, VT)):
                b = next_bank(k)
                S.op("pe", lambda e, b=b, g=g, src=src, h=h, t=t: e.matmul(
                    k.bank(b), WQKV[:, g, h, :], fm(k, src, h, t * 512, 512), start=True, stop=True),
                    reads=["WQKV", ("fm", src, h, t)], writes=[("ps", b)])
                evac(k, fm(k, dst, h, t * 512, 512), k.bank(b), ("ps", b), ("fm", dst, h, t))
        for tt in range(NT128):
            b = next_bank(k)
            S.op("pe", lambda e, b=b, h=h, tt=tt: e.matmul(
                k.bank(b)[:, 0:128], fm(k, MX, h, tt * 128, 128), WQKV[:, 2, h, :], start=True, stop=True),
                reads=["WQKV", ("fm", MX, h, tt // 4)], writes=[("ps", b)])
            evac(k, Vall[:, tt, h, 0:128], k.bank(b)[:, 0:128], ("ps", b), ("V", tt, h))
    for h in range(4):
        S.op("dve", lambda e, h=h: e.tensor_scalar(fm(k, XC, h), fm(k, XC, h), k.skipc[:, h:h + 1], None, ALU.mult),
             reads=[("fm", XC, h, t) for t in range(NT)] + ["lconst"], writes=[("fm", XC, h, t) for t in range(NT)])
    S.barrier()
    for t in range(NT):
        for g, bb in ((0, t), (1, 4 + t)):
            for jj in range(12):
                src = (QT, KT, VT)[jj // 4]
                S.op("pe", lambda e, g=g, bb=bb, jj=jj, src=src, t=t: e.matmul(
                    k.bank(bb)[0:4, :], WIF[:, jj, g * 4:(g + 1) * 4], fm(k, src, jj % 4, t * 512, 512),
                    start=(jj == 0), stop=(jj == 11)), reads=["WIF"], writes=[("ps", bb)])
    T1 = k.f32(W_W, 2048)
    T2 = k.f32(W_W + 2048, 2048)
    T3 = k.f32(W_W + 5120, 2048)
    ONES = k.f32(W_W + 7168, 2048)
    S.op("dve", lambda e: e.memset(ONES[0:4, :], 1.0), writes=["ONES"])
    softplus_neg(k, T1, 4, [4, 5, 6, 7], k.gcols[0:4, 5:6], "T1")
    S.op("dve", lambda e: e.tensor_tensor_scan(T2[0:4, :], ONES[0:4, :], T1[0:4, :], 0.0, ALU.mult, ALU.add), reads=["T1", "ONES"], writes=["T2"])
    for t in range(NT):
        S.op("dve", lambda e, t=t: e.tensor_scalar(T1[0:4, t * 512:(t + 1) * 512], k.bank(t)[0:4, :], k.gcols[0:4, 1:2], None, ALU.add),
             reads=[("ps", t), "gcols", "T2"], writes=["T1"])
    S.op("dve", lambda e: e.tensor_tensor(out=T1[0:4, :], in0=T1[0:4, :], in1=T2[0:4, :], op=ALU.add), reads=["T1", "T2"], writes=["T1"])
    S.op("dve", lambda e: e.tensor_tensor_scan(T3[0:4, :], ONES[0:4, :], T1[0:4, :], 0.0, ALU.mult, ALU.max), reads=["T1", "ONES"], writes=["T3"])
    S.op("dve", lambda e: e.tensor_scalar(T3[0:4, :], T3[0:4, :], -1.0, None, ALU.mult), reads=["T3"], writes=["T3"])
    UR = ONES
    EM = k.f32(W_CONST + 2700, 64)
    UC = k.f32(W_CONST + 2780, 160)
    for j in range(NT):
        nb = T3[0:4, 512 * j - 1:512 * j] if j > 0 else 0.0
        S.op("act", lambda e, j=j, nb=nb: e.activation(out=T2[0:4, j * 512:(j + 1) * 512], in_=T2[0:4, j * 512:(j + 1) * 512],
                                                     func=AF.Exp, bias=nb, scale=1.0), reads=["T2", "T3"], writes=["T2"])
    for tt in range(NT128):
        S.op("pe", lambda e, tt=tt: e.transpose(k.bank(3)[:, tt * 4:(tt + 1) * 4], T2[0:4, tt * 128:(tt + 1) * 128], k.ident[0:4, 0:4]),
             reads=["T2", "ident"], writes=[("ps", 3)])
    S.op("dve", lambda e: e.tensor_copy(EM, k.bank(3)[:, 0:64]), reads=[("ps", 3)], writes=["EM"])
    for j in range(NT):
        nb = T3[0:4, 512 * j - 1:512 * j] if j > 0 else 0.0
        ncol = (j + 1) * 512
        S.op("act", lambda e, nb=nb, ncol=ncol: e.activation(out=UR[0:4, 0:ncol], in_=T1[0:4, 0:ncol], func=AF.Exp, bias=nb, scale=1.0),
             reads=["T1", "T3", "ONES"], writes=["ONES"])
        for i in range(4 * j + 4):
            idx = 2 * j * (j + 1) + i
            S.op("pe", lambda e, i=i, idx=idx: e.transpose(k.bank(2)[:, idx * 4:(idx + 1) * 4], UR[0:4, i * 128:(i + 1) * 128], k.ident[0:4, 0:4]),
                 reads=["ONES", "ident"], writes=[("ps", 2)])
    S.op("dve", lambda e: e.tensor_scalar(UC, k.bank(2)[:, 0:160], 128.0 ** -0.5, None, ALU.mult), reads=[("ps", 2)], writes=["UC"])
    S.barrier()
    k.tbanks = [3, 7]
    TB = SB + 1024
    blocks = []
    for h in range(4):

        def qf(t0, n, h=h):
            return fm(k, QT, h, t0, n)

        def kf(i, h=h):
            return fm(k, KT, h, i * 128, 128)

        def vf(i, h=h):
            return Vall[:, i, h, :]

        def fin(j, r, o, okey, h=h):
            tt = 4 * j + r
            c1, k1 = col(k)
            c2, k2 = col(k)
            c3, k3 = col(k)
            k._fp = getattr(k, "_fp", 0) + 1
            n = k._fp
            NUM = k.f32(TB + (n % 4) * 128, 128)
            TF = k.f32(TB + 640 + (n % 2) * 128, 128)
            HN = k.f32(TB + 896 + (n % 4) * 128, 128)
            ST6 = k.f32(TB + 512 + (n % 8) * 8, 6)
            MV = k.f32(TB + 576 + (n % 8) * 4, 2)
            hk, nk, fk, stk, mvk = ("HH", n % 4), ("HN", n % 4), ("TF", n % 2), ("ST6", n % 8), ("MV", n % 8)
            den = o[:, 128:129]
            S.op("dve", lambda e: e.tensor_tensor(out=c1, in0=den, in1=EM[:, tt * 4 + h:tt * 4 + h + 1], op=ALU.max), reads=[okey, "EM"], writes=[k1])
            S.op("dve", lambda e: e.scalar_tensor_tensor(out=c1, in0=den, scalar=-1.0, in1=c1, op0=ALU.mult, op1=ALU.max), reads=[okey, k1], writes=[k1])
            S.op("dve", lambda e: e.reciprocal(c1, c1), reads=[k1], writes=[k1])
            S.op("dve", lambda e: e.tensor_scalar(NUM, o[:, 0:128], c1, None, ALU.mult), reads=[okey, k1], writes=[hk])
            S.op("dve", lambda e: e.bn_stats(ST6, NUM), reads=[hk], writes=[stk])
            S.op("dve", lambda e: e.bn_aggr(MV, ST6), reads=[stk], writes=[mvk])

            def stage_b():
                S.op("act", lambda e: e.activation(out=c2, in_=MV[:, 1:2], func=AF.Ln, bias=LN_EPS, scale=1.0), reads=[mvk], writes=[k2])
                S.op("act", lambda e: e.activation(out=c2, in_=c2, func=AF.Exp, scale=-0.5), reads=[k2], writes=[k2])

            def post(pq, pkey):
                xs = fm(k, XC, h, tt * 128, 128)
                sz = fm(k, SZ, h, tt * 128, 128)
                S.op("dve", lambda e: e.scalar_tensor_tensor(out=TF, in0=pq, scalar=k.normc[:, h:h + 1], in1=xs, op0=ALU.mult, op1=ALU.add),
                     reads=[pkey, "lconst"], writes=[fk])
                S.op("dve", lambda e: e.tensor_tensor(out=sz, in0=TF, in1=sz, op=ALU.mult), reads=[fk], writes=[("fm", SZ, h, tt)])

            def stage_c():
                S.op("dve", lambda e: e.tensor_scalar(HN, NUM, MV[:, 0:1], c2, ALU.subtract, ALU.mult), reads=[hk, mvk, k2], writes=[nk])
                defer(k, 2, lambda: transpose_to_fm(k, HN, nk, None, None, post=post))
            defer(k, 2, stage_b)
            defer(k, 3, stage_c)

        def scale_fn(i, j, h=h):
            idx = 2 * j * (j + 1) + i
            return UC[:, idx * 4 + h:idx * 4 + h + 1]
        ps_ = AttnPass(k, qf=qf, kf=kf, aqf=None, akf=None, bias_fn=None, vf=vf, vkey=("V", "x"), dv=128, mode="exp", fin=fin,
                       sbanks=[0, 1, 2, 6], obanks=[(4, 5)], act_func=AF.Copy, scale_fn=scale_fn, mask_mul=True)
        blocks += ps_.blocks()
    run_blocks(k, blocks, L=3)
    S.barrier()


def merge_and_out(k, l):
    S, d = k.S, k.d
    OB = [NU(0), NU(2), NU(1)]
    MG = DU(0)
    GT = [k.f32(SB + i * 512, 512) for i in range(2)]
    ACC = k.f32(SB + 1024, 512)
    PB = k.f32(SB + 1536, 512)

    def load_mp(dd):
        s = dd % 2
        tile = k.bf(W_W + s * 2304, 4608)
        S.dma("pool", f"mp{s}", lambda e: e.dma_start(out=tile, in_=d["mpack"][l, dd, :, :]), writes=[("MP", s)])
        return tile

    tiles = {0: load_mp(0), 1: load_mp(1)}
    cnt = 0
    for dd in range(NKC):
        MP = tiles[dd]
        mkey = ("MP", dd % 2)
        for t in range(NT):
            for b in range(3):
                bg, bo = next_bank(k), next_bank(k)
                for kc in range(NKC):
                    S.op("pe", lambda e, bg=bg, b=b, kc=kc, t=t, MP=MP: e.matmul(
                        k.bank(bg), MP[:, b * 1536 + kc * 128:b * 1536 + (kc + 1) * 128], k.H(kc, t * 512, 512),
                        start=(kc == 0), stop=(kc == NKC - 1)), reads=[mkey, ("H", kc, t)], writes=[("ps", bg)])
                for kc in range(4):
                    S.op("pe", lambda e, bo=bo, b=b, kc=kc, t=t, MP=MP: e.matmul(
                        k.bank(bo), MP[:, b * 1536 + 1024 + kc * 128:b * 1536 + 1024 + (kc + 1) * 128], fm(k, OB[b], kc, t * 512, 512),
                        start=(kc == 0), stop=(kc == 3)), reads=[mkey], writes=[("ps", bo)])
                gt = GT[cnt % 2]
                gk = ("GT", cnt % 2)
                cnt += 1
                gbc = k.gb[:, b * 8 + dd:b * 8 + dd + 1]
                S.op("act", lambda e, gt=gt, bg=bg, gbc=gbc: e.activation(out=gt, in_=k.bank(bg), func=AF.Sigmoid, bias=gbc, scale=1.0),
                     reads=[("ps", bg), "lconst"], writes=[gk])
                if b == 0:
                    S.op("dve", lambda e, gt=gt, bo=bo: e.tensor_tensor(out=ACC, in0=gt, in1=k.bank(bo), op=ALU.mult),
                         reads=[gk, ("ps", bo)], writes=["MACC"])
                else:
                    S.op("dve", lambda e, gt=gt, bo=bo: e.tensor_tensor(out=PB, in0=gt, in1=k.bank(bo), op=ALU.mult),
                         reads=[gk, ("ps", bo)], writes=["MPB"])
                    dst = ACC if b == 1 else fm(k, MG, dd, t * 512, 512)
                    dkey = "MACC" if b == 1 else ("fm", MG, dd, t)
                    S.op("dve", lambda e, dst=dst: e.tensor_tensor(out=dst, in0=ACC, in1=PB, op=ALU.add),
                         reads=["MACC", "MPB"], writes=[dkey] + (["MACC"] if b == 2 else []))
        if dd + 2 < NKC:
            tiles[dd + 2] = load_mp(dd + 2)
    S.barrier()
    MN = NU(0)
    for i in range(4):
        S.op("dve" if i % 2 == 0 else "pool", lambda e, i=i: e.tensor_copy(k.bf(MN + i * 2048, 4096), k.bf(MG + i * 2048, 4096)),
             writes=[("MN", i)])
    S.barrier()
    XS = [k.f32(SB + i * 512, 512) for i in range(2)]

    WOT = [k.bf(W_W + dd * 512, 1024) for dd in range(NKC)]
    for dd in range(NKC):
        S.dma("pool", "wo", lambda e, dd=dd: e.dma_start(out=WOT[dd], in_=d["wout"][l, dd, :, :]), writes=[("WO", dd)])
    cnt = 0
    for t in range(NT):
        for dd in range(NKC):
            WO = WOT[dd]
            b = next_bank(k)
            xs = XS[cnt % 2]
            xk = ("XS", cnt % 2)
            cnt += 1
            S.dma("sp", f"xs{cnt % 2}", lambda e, xs=xs, dd=dd, t=t: e.dma_start(out=xs, in_=d["xspill"][:, dd, t * 512:(t + 1) * 512]),
                  writes=[xk])
            for kc in range(NKC):
                S.op("pe", lambda e, b=b, kc=kc, t=t, WO=WO: e.matmul(
                    k.bank(b), WO[:, kc * 128:(kc + 1) * 128], fm(k, MN, kc, t * 512, 512),
                    start=(kc == 0), stop=(kc == NKC - 1)), reads=[("WO", dd)], writes=[("ps", b)])
            S.op("dve", lambda e, b=b, xs=xs, dd=dd, t=t: e.tensor_tensor(out=k.X(dd, t), in0=k.bank(b), in1=xs, op=ALU.add),
                 reads=[("ps", b), xk], writes=[("X", dd, t)])


def spill_x(k):
    S, d = k.S, k.d
    for c in range(NKC):
        o = W_X + c * S_LEN
        S.dma("sp", "xsp", lambda e, c=c, o=o: e.dma_start(out=d["xspill"][:, c, :], in_=k.arena[:, o:o + S_LEN]),
              reads=[("X", c, t) for t in range(NT)], writes=["xspill"])


def mixer(k, l, branches=("ml", "diff", "fox")):
    S = k.S
    layer_consts(k, l)
    rmsnorm(k, l * 3 + 1, "H")
    spill_x(k)
    S.barrier()
    if "ml" in branches:
        mlstm_branch(k, l)
    if "diff" in branches:
        diff_branch(k, l)
    if "fox" in branches:
        fox_branch(k, l)
    merge_and_out(k, l)


def _chunk_rows(w):
    K_, N_ = w.shape
    return w.reshape(K_ // 128, 128, N_).transpose(1, 0, 2)


def _cols(v):
    return v.reshape(-1, 128).T


def prep_shared(inp):
    f = np.float32
    sh = {}
    for which in (1, 2):
        wg, wu, wd = inp[f"ffn{which}_w_gate"], inp[f"ffn{which}_w_up"], inp[f"ffn{which}_w_down"]
        gu = np.empty((DEPTH, NFC, 128, 2, NKC, 128), f)
        for l in range(DEPTH):
            for g, w in enumerate((wg[l], wu[l])):
                gu[l, :, :, g] = w.reshape(NKC, 128, NFC, 128).transpose(2, 1, 0, 3)
        sh[f"ffn{which}_gu"] = gu.reshape(DEPTH, NFC, 128, 2 * NKC * 128)
        sh[f"ffn{which}_wd"] = np.ascontiguousarray(wd.reshape(DEPTH, NFC, 128, D))
    vecs = []
    for l in range(DEPTH):
        vecs += [inp["ffn1_norm"][l], inp["mix_norm"][l], inp["ffn2_norm"][l]]
    vecs.append(inp["final_norm"])
    sh["normw"] = np.ascontiguousarray(np.stack([_cols(v) for v in vecs], axis=1).reshape(128, 56)).astype(f)
    sh["ident"] = np.eye(128, dtype=f)
    sh["ones"] = np.ones((128, 128), f)
    sh["tri"] = np.triu(np.ones((128, 128), f))
    sh["negtri"] = (np.tril(np.ones((128, 128), f), -1) * NEG_BIG).astype(f)
    w_in = inp["w_in"]
    sh["w_in"] = np.ascontiguousarray(np.stack([_chunk_rows(w_in[l]) for l in range(DEPTH)]))
    gb = np.empty((DEPTH, 128, 24), f)
    cw = np.empty((DEPTH, 128, 16), f)
    cb = np.empty((DEPTH, 128, 4), f)
    sk = np.empty((DEPTH, 128, 4), f)
    nmc = np.empty((DEPTH, 128, 4), f)
    gc = np.zeros((DEPTH, 8, 8), f)
    for l in range(DEPTH):
        for b in range(3):
            gb[l, :, b * 8:(b + 1) * 8] = _cols(inp["gate_bias"][l, b])
        for j in range(4):
            cw[l, :, j * 4:(j + 1) * 4] = _cols(inp["mlstm_conv_w"][l, j])
        cb[l] = _cols(inp["mlstm_conv_b"][l])
        sk[l] = _cols(inp["mlstm_skip"][l])
        nmc[l] = _cols(inp["mlstm_norm"][l])
        gc[l, :, 0] = inp["fox_b_f"][l]
        gc[l, 0:4, 1] = inp["mlstm_b_if"][l, 0:4]
        gc[l, 0:4, 2] = inp["mlstm_b_if"][l, 4:8]
    sh["gbcols"], sh["convw"], sh["convb"], sh["skipc"], sh["gcols"] = gb, cw, cb, sk, gc
    sh["normc"] = nmc
    sh["difflam"] = np.ascontiguousarray(np.concatenate(
        [inp["diff_lq1"], inp["diff_lk1"], inp["diff_lq2"], inp["diff_lk2"]], axis=1)).astype(f)
    sh["diff_subln"] = np.ascontiguousarray(inp["diff_subln"]).astype(f)
    sh["mlstm_norm"] = np.ascontiguousarray(inp["mlstm_norm"]).astype(f)
    slopes = [2.0 ** (-8.0 * (h + 1) / 4) for h in range(4)]
    ak = np.zeros((3, 4, 128), f)
    aq = np.zeros((3, 4, 512), f)
    relq = np.arange(512)
    for h in range(4):
        ak[0, h] = slopes[h] * np.arange(128)
        ak[1, h] = 1.0
        ak[2, h] = 1.0
        aq[0, h] = 1.0
        aq[1, h] = -slopes[h] * (128 * (relq // 128))
        aq[2, h] = -slopes[h] * (relq % 128)
    sh["alibi_k"] = ak.reshape(3, 512)
    sh["alibi_q"] = aq.reshape(3, 2048)
    tpos = np.arange(S_LEN)
    akf = np.zeros((4, 3, S_LEN), f)
    aqf = np.zeros((4, 3, S_LEN), f)
    for h in range(4):
        akf[h, 0] = slopes[h] * (tpos % 128)
        akf[h, 1] = 1.0
        akf[h, 2] = 1.0
        aqf[h, 0] = 1.0
        aqf[h, 1] = -slopes[h] * (128 * ((tpos % 512) // 128))
        aqf[h, 2] = -slopes[h] * (tpos % 128)
    sh["alibi_kf"], sh["alibi_qf"] = akf, aqf
    wqkv = np.empty((DEPTH, 128, 3, 4, 128), f)
    for g, nm in enumerate(("mlstm_wq", "mlstm_wk", "mlstm_wv")):
        wqkv[:, :, g] = inp[nm].transpose(0, 2, 1, 3)
    sh["wqkv"] = wqkv.reshape(DEPTH, 128, 1536)
    sh["wif"] = np.ascontiguousarray(inp["mlstm_w_if"].reshape(DEPTH, 12, 128, 8).transpose(0, 2, 1, 3)).reshape(DEPTH, 128, 96)
    mp = np.empty((DEPTH, NKC, 128, 3, 1536), f)
    wbs = (inp["w_branch_diff"], inp["w_branch_fox"], inp["w_branch_mlstm"])
    for l in range(DEPTH):
        for b in range(3):
            g = w_in[l][:, C_G + b * D:C_G + (b + 1) * D]
            mp[l, :, :, b, 0:1024] = g.reshape(NKC, 128, NKC, 128).transpose(2, 1, 0, 3).reshape(NKC, 128, 1024)
            mp[l, :, :, b, 1024:1536] = wbs[b][l].reshape(4, 128, NKC, 128).transpose(2, 1, 0, 3).reshape(NKC, 128, 512)
    sh["mpack"] = mp.reshape(DEPTH, NKC, 128, 4608)
    wo = np.empty((DEPTH, NKC, 128, 1024), f)
    for l in range(DEPTH):
        wo[l] = inp["w_out"][l].reshape(NKC, 128, NKC, 128).transpose(2, 1, 0, 3).reshape(NKC, 128, 1024)
    sh["wout"] = wo
    return sh


def build_program(shapes, plan):
    from contextlib import ExitStack
    nc = bass.Bass("TRN2", target_bir_lowering=False)
    dram = {}
    for name, shp in shapes.items():
        dram[name] = nc.dram_tensor(name, list(shp), F32, kind="ExternalInput").ap()
    dram["outT"] = nc.dram_tensor("outT", [128, NKC, S_LEN], F32, kind="ExternalOutput").ap()
    dram["xspill"] = nc.dram_tensor("xspill", [128, NKC, S_LEN], F32, kind="Internal").ap()
    for nm in ("augk_fox", "augq_fox", "augk_ml", "augq_ml"):
        dram[nm] = nc.dram_tensor(nm, [8, 6, S_LEN], BF16, kind="Internal").ap()
    with ExitStack() as es:
        sems = [es.enter_context(nc.semaphore(f"s{i}")) for i in range(70)]
        S = Sched(nc, sems)
        arena = nc.alloc_sbuf_tensor("arena", [128, ARENA_WORDS], F32)
        ps = es.enter_context(nc.psum_tensor("ps", [128, 4096], F32))
        k = K(nc, S, arena, ps, dram)
        plan(k)
        with nc.Block() as block:
            S.emit(block)
    return nc


def full_plan(k):
    build_consts(k)
    load_x(k)
    for l in range(DEPTH):
        rmsnorm(k, l * 3 + 0, "H")
        ffn(k, l, 1)
        mixer(k, l)
        rmsnorm(k, l * 3 + 2, "H")
        k.S.barrier()
        ffn(k, l, 2)
    rmsnorm(k, 6, "X")
    store_out(k)


def run(inputs, plan=full_plan, trace=False, cores=NB):
    sh = prep_shared(inputs)
    x = np.asarray(inputs["x"], np.float32)
    in_maps = []
    for b in range(cores):
        m = dict(sh)
        m["xT"] = np.ascontiguousarray(x[b].T.reshape(NKC, 128, S_LEN).transpose(1, 0, 2))
        in_maps.append(m)
    shapes = {n: a.shape for n, a in in_maps[0].items()}
    nc = build_program(shapes, plan)
    res = run_bass_kernel_spmd(nc, in_maps, core_ids=list(range(cores)), trace=trace)
    out = np.empty((cores, S_LEN, D), np.float32)
    for b in range(cores):
        o = res.results[b]["outT"]
        out[b] = o.transpose(1, 0, 2).reshape(D, S_LEN).T
    return out, res


def kernel(**inputs):
    out, _ = run(inputs)
    return out
```

```python
import bisect
import numpy as np
import concourse.bass as bass
import concourse.mybir as mybir
from concourse.bass_utils import run_bass_kernel_spmd

F32 = mybir.dt.float32
BF16 = mybir.dt.bfloat16
AF = mybir.ActivationFunctionType
ALU = mybir.AluOpType
AX = mybir.AxisListType


class _Ev:
    __slots__ = ("eng", "idx", "val", "clock")

    def __init__(self, eng, idx, val, clock):
        self.eng, self.idx, self.val, self.clock = eng, idx, val, clock


class _Eng:
    def __init__(self, name, sem, self_sync):
        self.name, self.sem, self.self_sync = name, sem, self_sync
        self.ops = []
        self.count = 0
        self.sig_idx = []
        self.sig_val = []
        self.n_inst = 0
        self.inst_rec = []
        self.clock = {}
        self.last_compute = None


class _Slot:
    def __init__(self, name, sem):
        self.name, self.sem, self.total = name, sem, 0


class _Res:
    __slots__ = ("w", "rs")

    def __init__(self):
        self.w, self.rs = None, []


class Sched:
    def __init__(self, nc, sems):
        self.nc = nc
        self._sems = list(sems)
        self.engs = {}
        for name, ss in (("pe", False), ("act", True), ("dve", True), ("pool", True), ("sp", False)):
            self.engs[name] = _Eng(name, self._sems.pop(), ss)
        self.slots = {}
        self.resd = {}

    def slot(self, name):
        s = self.slots.get(name)
        if s is None:
            s = _Slot(name, self._sems.pop())
            self.slots[name] = s
        return s

    def res(self, key):
        r = self.resd.get(key)
        if r is None:
            r = _Res()
            self.resd[key] = r
        return r

    def _value(self, ev):
        if ev.val is not None:
            return ev.val
        E = self.engs[ev.eng]
        j = bisect.bisect_left(E.sig_idx, ev.idx)
        if j < len(E.sig_idx):
            return E.sig_val[j]
        E.count += 1
        rec = E.inst_rec[ev.idx]
        rec[2], rec[3] = E.sem, 1
        E.sig_idx.append(ev.idx)
        E.sig_val.append(E.count)
        ev.val = E.count
        return ev.val

    def _wait_for(self, E, evs):
        need = {}
        for ev in evs:
            if ev is None:
                continue
            if ev.eng == E.name and not E.self_sync:
                continue
            if ev.eng in self.slots:
                v = self.slots[ev.eng].total
            else:
                v = self._value(ev)
            if E.clock.get(ev.eng, 0) >= v:
                continue
            if need.get(ev.eng, (0, None))[0] < v:
                need[ev.eng] = (v, ev)
        for name, (v, ev) in need.items():
            if E.clock.get(name, 0) >= v:
                continue
            sem = self.slots[name].sem if name in self.slots else self.engs[name].sem
            E.ops.append(["w", sem, v])
            E.clock[name] = v
            for k, cv in ev.clock.items():
                if E.clock.get(k, 0) < cv:
                    E.clock[k] = cv

    def _deps(self, reads, writes):
        evs = []
        for k in reads:
            r = self.res(k)
            if r.w is not None:
                evs.append(r.w)
        for k in writes:
            r = self.res(k)
            if r.w is not None:
                evs.append(r.w)
            evs.extend(r.rs)
        return evs

    def _commit(self, ev, reads, writes):
        for k in reads:
            self.res(k).rs.append(ev)
        for k in writes:
            r = self.res(k)
            r.w, r.rs = ev, []

    def op(self, eng, fn, reads=(), writes=()):
        E = self.engs[eng]
        self._wait_for(E, self._deps(reads, writes))
        rec = ["i", fn, None, 0]
        E.ops.append(rec)
        E.inst_rec.append(rec)
        idx = E.n_inst
        E.n_inst += 1
        E.last_compute = idx
        clk = dict(E.clock)
        ev = _Ev(eng, idx, None, clk)
        self._commit(ev, reads, writes)
        return ev

    def dma(self, queue, slot, fn, reads=(), writes=()):
        E = self.engs[queue]
        S = self.slot(slot) if isinstance(slot, str) else slot
        self._wait_for(E, self._deps(reads, writes))
        S.total += 16
        rec = ["i", fn, S.sem, 16]
        E.ops.append(rec)
        E.inst_rec.append(rec)
        E.n_inst += 1
        ev = _Ev(S.name, -1, S.total, dict(E.clock))
        self._commit(ev, reads, writes)
        return ev

    def barrier(self):
        evs = []
        for E in self.engs.values():
            if E.last_compute is not None:
                ev = _Ev(E.name, E.last_compute, None, dict(E.clock))
                self._value(ev)
                evs.append(ev)
        for S in self.slots.values():
            if S.total:
                evs.append(_Ev(S.name, -1, S.total, {}))
        for E in self.engs.values():
            self._wait_for(E, evs)
        self.resd = {}

    def wait_all_dma(self, eng, slots):
        E = self.engs[eng]
        evs = [_Ev(self.slots[s].name, -1, self.slots[s].total, {}) for s in slots if self.slots[s].total]
        self._wait_for(E, evs)

    def emit(self, block):
        def replay(E):
            def run(e):
                for rec in E.ops:
                    if rec[0] == "w":
                        e.wait_ge(rec[1], rec[2])
                    else:
                        ins = rec[1](e)
                        if rec[2] is not None:
                            ins.then_inc(rec[2], rec[3])
            return run
        block.tensor(replay(self.engs["pe"]))
        block.scalar(replay(self.engs["act"]))
        block.vector(replay(self.engs["dve"]))
        block.gpsimd(replay(self.engs["pool"]))
        block.sync(replay(self.engs["sp"]))


D = 1024
S_LEN = 2048
NB = 8
DEPTH = 2
DFF = 2816
NFC = DFF // 128
NKC = D // 128
NT = S_LEN // 512
NT128 = S_LEN // 128
RMS_EPS = 1e-6
LN_EPS = 1e-5
N_IN = 7176
FFN_GROUPS = [(0, 6), (6, 12), (12, 17), (17, 22)]

W_X = 0
W_H = W_X + 16384
W_CONST = W_H + 8192
W_PT = W_CONST + 3072
W_W = W_PT + 1024
W_N = W_W + 9216
W_END = W_N + 12288
ARENA_WORDS = W_END


class K:
    def __init__(self, nc, S, arena, ps, dram):
        self.nc, self.S, self.arena, self.ps, self.d = nc, S, arena, ps, dram
        self.ps_rr = 0

    def f32(self, off, n):
        return self.arena[:, off:off + n]

    def bf(self, off, n):
        return self.arena[:, off:off + (n + 1) // 2].bitcast(BF16)

    def bank(self, b):
        return self.ps[:, b * 512:(b + 1) * 512]

    def X(self, c, t):
        o = W_X + c * S_LEN + t * 512
        return self.arena[:, o:o + 512]

    def H(self, c, t0=0, n=S_LEN):
        return self.bf(W_H + c * (S_LEN // 2), S_LEN)[:, t0:t0 + n]

    def PT(self, i):
        return self.bf(W_PT + i * 256, 512)


def build_consts(k):
    S, d = k.S, k.d
    o = W_CONST
    k.ident = k.f32(o, 128); o += 128
    k.ones_bf = k.bf(o, 128); o += 64
    k.tri_bf = k.bf(o, 128); o += 64
    k.normw = k.f32(o, 7 * 8); o += 56
    k.const_end = o
    S.dma("sp", "c0", lambda e: e.dma_start(out=k.ident, in_=d["ident"][:, :]), writes=["ident"])
    S.dma("pool", "c1", lambda e: e.dma_start(out=k.ones_bf, in_=d["ones"][:, :]), writes=["ones"])
    S.dma("pool", "c1", lambda e: e.dma_start(out=k.tri_bf, in_=d["tri"][:, :]), writes=["tri"])
    S.dma("sp", "c0", lambda e: e.dma_start(out=k.normw, in_=d["normw"][:, :]), writes=["normw"])


def load_x(k):
    S, d = k.S, k.d
    for c in range(NKC):
        o = W_X + c * S_LEN
        S.dma("sp", "xld", lambda e, c=c, o=o: e.dma_start(out=k.arena[:, o:o + S_LEN], in_=d["xT"][:, c, :]),
              writes=[("X", c, t) for t in range(NT)])


def rmsnorm(k, widx, out_mode):
    S = k.S
    R0 = W_N + 8192
    for t in range(NT):
        for c in range(NKC):
            pt = k.PT((t * NKC + c) % 4)
            key = ("PT", (t * NKC + c) % 4)
            S.op("act", lambda e, pt=pt, c=c, t=t: e.activation(out=pt, in_=k.X(c, t), func=AF.Square),
                 reads=[("X", c, t)], writes=[key])
            S.op("pe", lambda e, pt=pt, c=c, t=t: e.matmul(k.bank(t), k.ones_bf, pt, start=(c == 0), stop=(c == NKC - 1)),
                 reads=[key, "ones"], writes=[("ps", t)])
    for t in range(NT):
        R = k.f32(R0 + t * 512, 512)
        S.op("act", lambda e, R=R, t=t: e.activation(out=R, in_=k.bank(t), func=AF.Sqrt, bias=RMS_EPS, scale=1.0 / D),
             reads=[("ps", t)], writes=[("R", t)])
        S.op("dve", lambda e, R=R: e.reciprocal(R, R), reads=[("R", t)], writes=[("R", t)])
        for c in range(NKC):
            wcol = k.normw[:, widx * 8 + c:widx * 8 + c + 1]
            if out_mode == "H":
                S.op("dve", lambda e, R=R, c=c, t=t, wcol=wcol: e.scalar_tensor_tensor(
                    out=k.H(c, t * 512, 512), in0=k.X(c, t), scalar=wcol, in1=R, op0=ALU.mult, op1=ALU.mult),
                    reads=[("X", c, t), ("R", t), "normw"], writes=[("H", c, t)])
            else:
                S.op("dve", lambda e, R=R, c=c, t=t, wcol=wcol: e.scalar_tensor_tensor(
                    out=k.X(c, t), in0=k.X(c, t), scalar=wcol, in1=R, op0=ALU.mult, op1=ALU.mult),
                    reads=[("X", c, t), ("R", t), "normw"], writes=[("X", c, t)])


def ffn(k, l, which):
    S, d = k.S, k.d
    gu_d = d[f"ffn{which}_gu"]
    wd_d = d[f"ffn{which}_wd"]
    GU = [k.bf(W_W + i * 1024, 2048) for i in range(3)]
    WD = [k.bf(W_W + 3072 + i * 3072, 6 * 1024) for i in range(2)]
    A = [k.bf(W_N + i * 1024, 2048) for i in range(7)]
    SG = [k.f32(W_N + 7168 + i * 512, 512) for i in range(2)]

    def load_gu(c):
        s = c % 3
        S.dma("pool", f"gu{s}", lambda e: e.dma_start(out=GU[s], in_=gu_d[l, c, :, :]), writes=[("GU", s)])

    def load_wd(g):
        s = g % 2
        c0, c1 = FFN_GROUPS[g]
        for c in range(c0, c1):
            S.dma("pool", f"wd{s}", lambda e, c=c: e.dma_start(out=WD[s][:, (c - c0) * 1024:(c - c0 + 1) * 1024], in_=wd_d[l, c, :, :]),
                  writes=[("WD", s)])

    def gu_chunk(c):
        s = c % 3
        for t in range(NT):
            pr = (c * NT + t) % 3
            bg, bu = 2 * pr, 2 * pr + 1
            for g, b in ((0, bg), (1, bu)):
                for kc in range(NKC):
                    S.op("pe", lambda e, g=g, b=b, kc=kc, t=t: e.matmul(
                        k.bank(b), GU[s][:, (g * 8 + kc) * 128:(g * 8 + kc + 1) * 128], k.H(kc, t * 512, 512),
                        start=(kc == 0), stop=(kc == NKC - 1)),
                        reads=[("GU", s), ("H", kc, t)], writes=[("ps", b)])
            sg = SG[(c * NT + t) % 2]
            sgk = ("SG", (c * NT + t) % 2)
            S.op("act", lambda e, sg=sg, bg=bg: e.activation(out=sg, in_=k.bank(bg), func=AF.Silu),
                 reads=[("ps", bg)], writes=[sgk])
            S.op("dve", lambda e, sg=sg, bu=bu, t=t: e.tensor_tensor(
                out=A[c % 7][:, t * 512:(t + 1) * 512], in0=sg, in1=k.bank(bu), op=ALU.mult),
                reads=[sgk, ("ps", bu)], writes=[("A", c % 7, t)])

    dn_cnt = [0]

    def down(g):
        s = g % 2
        c0, c1 = FFN_GROUPS[g]
        for t in range(NT):
            for dd in range(NKC):
                b = 6 + dn_cnt[0] % 2
                dn_cnt[0] += 1
                for c in range(c0, c1):
                    S.op("pe", lambda e, b=b, c=c, dd=dd, t=t: e.matmul(
                        k.bank(b), WD[s][:, (c - c0) * 1024 + dd * 128:(c - c0) * 1024 + (dd + 1) * 128],
                        A[c % 7][:, t * 512:(t + 1) * 512], start=(c == c0), stop=(c == c1 - 1)),
                        reads=[("WD", s), ("A", c % 7, t)], writes=[("ps", b)])
                S.op("dve", lambda e, b=b, dd=dd, t=t: e.scalar_tensor_tensor(
                    out=k.X(dd, t), in0=k.bank(b), scalar=0.5, in1=k.X(dd, t), op0=ALU.mult, op1=ALU.add),
                    reads=[("ps", b), ("X", dd, t)], writes=[("X", dd, t)])

    for c in range(3):
        load_gu(c)
    load_wd(0)
    load_wd(1)
    grp_of = {}
    for g, (c0, c1) in enumerate(FFN_GROUPS):
        for c in range(c0, c1):
            grp_of[c] = g
    for c in range(NFC):
        gu_chunk(c)
        if c + 3 < NFC:
            load_gu(c + 3)
        g = grp_of[c]
        if c == FFN_GROUPS[g][0] and g > 0:
            down(g - 1)
            if g + 1 < len(FFN_GROUPS):
                load_wd(g + 1)
    down(len(FFN_GROUPS) - 1)


def store_out(k):
    S, d = k.S, k.d
    for c in range(NKC):
        o = W_X + c * S_LEN
        S.dma("sp", "ost", lambda e, c=c, o=o: e.dma_start(out=d["outT"][:, c, :], in_=k.arena[:, o:o + S_LEN]),
              reads=[("X", c, t) for t in range(NT)])
    S.wait_all_dma("sp", ["ost"])


U = 4096
W_SCR = W_END + 64
ARENA_WORDS = W_END + 2560
SB = W_SCR + 64
NEG_BIG = -30000.0
WARM_FILL = 0
C_DQ, C_DK, C_DV = 0, 512, 1024
C_FQ, C_FK, C_FV, C_FF = 1536, 2048, 2560, 3072
C_MX, C_MZ, C_G = 3080, 3592, 4104


def DU(i):
    return W_X + i * U


def NU(i):
    return W_N + i * U


def fm(k, off, c, t0=0, n=S_LEN):
    return k.bf(off + c * (S_LEN // 2), S_LEN)[:, t0:t0 + n]


def next_bank(k):
    b = k.ps_rr % 8
    k.ps_rr += 1
    return b


def col(k):
    i = getattr(k, "_col_rr", 0)
    k._col_rr = i + 1
    i %= 48
    return k.arena[:, W_SCR + i:W_SCR + i + 1], ("col", i)


def evac(k, dst, src, src_key, dst_key, scale=None, func=None, eng=None):
    S = k.S
    if eng is None:
        k._ev_rr = getattr(k, "_ev_rr", 0) + 1
        eng = "act" if (func is not None or k._ev_rr % 2 == 0) else "dve"
    if eng == "act":
        f = func if func is not None else AF.Copy
        sc = 1.0 if scale is None else scale
        S.op("act", lambda e: e.activation(out=dst, in_=src, func=f, scale=sc), reads=[src_key], writes=[dst_key])
    else:
        if scale is None:
            S.op("dve", lambda e: e.tensor_copy(dst, src), reads=[src_key], writes=[dst_key])
        else:
            S.op("dve", lambda e: e.tensor_scalar(dst, src, scale, None, ALU.mult), reads=[src_key], writes=[dst_key])


def wcol_jobs(k, l, jobs):
    S, d = k.S, k.d

    def load(i):
        c0, nc_, _ = jobs[i]
        s = i % 2
        tile = k.bf(W_W + s * 2048, 4096).rearrange("p (k c) -> p k c", k=8)
        S.dma("pool", f"wc{s}", lambda e: e.dma_start(out=tile[:, :, 0:nc_], in_=d["w_in"][l, :, :, c0:c0 + nc_]),
              writes=[("WC", s)])
        return tile

    tiles = {}
    for i in range(min(2, len(jobs))):
        tiles[i] = load(i)
    for i in range(len(jobs)):
        jobs[i][2](tiles[i], ("WC", i % 2))
        if i + 2 < len(jobs):
            tiles[i + 2] = load(i + 2)


def h_fm(dst_off, scale=None, func=None):
    def mk(k, c_base, nchunks=4):
        def handler(W, wkey):
            S = k.S
            for m in range(nchunks):
                for t in range(NT):
                    b = next_bank(k)
                    for kc in range(NKC):
                        S.op("pe", lambda e, b=b, m=m, kc=kc, t=t: e.matmul(
                            k.bank(b), W[:, kc, m * 128:(m + 1) * 128], k.H(kc, t * 512, 512),
                            start=(kc == 0), stop=(kc == NKC - 1)),
                            reads=[wkey, ("H", kc, t)], writes=[("ps", b)])
                    evac(k, fm(k, dst_off, c_base + m, t * 512, 512), k.bank(b), ("ps", b),
                         ("fm", dst_off, c_base + m, t), scale=scale, func=func)
        return handler
    return mk


def h_tm(k, vbuf_fn, nh, dv):
    def handler(W, wkey):
        S = k.S
        for tt in range(NT128):
            b = next_bank(k)
            for kc in range(NKC):
                S.op("pe", lambda e, b=b, kc=kc, tt=tt: e.matmul(
                    k.bank(b), k.H(kc, tt * 128, 128), W[:, kc, 0:512],
                    start=(kc == 0), stop=(kc == NKC - 1)),
                    reads=[wkey, ("H", kc, tt // 4)], writes=[("ps", b)])
            dst = vbuf_fn(tt)[:, :, 0:dv]
            src = k.bank(b).rearrange("p (h e) -> p h e", h=nh)
            evac(k, dst, src, ("ps", b), ("V", tt))
    return handler


class AttnPass:
    def __init__(self, k, *, qf, kf, aqf, akf, bias_fn, vf, vkey, dv, mode, fin, sbanks, obanks, scale=1.0, augkey=None, pre=None, act_func=None, scale_fn=None, mask_mul=False):
        self.__dict__.update(locals())
        self.state = {}
        self.per_bank = 2 if dv == 128 else 4

    def blocks(self):
        return [(self, j, i) for j in range(4) for i in range(4 * j + 4)]

    def O(self, j, r):
        pb = self.per_bank
        ob = self.obanks[j % len(self.obanks)][r // pb]
        c0 = (r % pb) * (self.dv + 1)
        return self.k.bank(ob)[:, c0:c0 + self.dv + 1], ("ps", ob)

    def emit_s(self, j, i):
        k, S = self.k, self.k.S
        if self.pre is not None:
            pl = self.pre if isinstance(self.pre, list) else [self.pre]
            self.pre = None
            for f_ in pl:
                f_()
        r0 = max(0, i - 4 * j)
        n0 = r0 * 128
        ncol = 512 - n0
        diag = i >= 4 * j
        k._blk = getattr(k, "_blk", 0) + 1
        pti = k._blk % 4
        pt = k.PT(pti)
        ptk = ("PT", pti)
        if self.mode == "exp":
            sb = self.sbanks[k._blk % len(self.sbanks)]
            has_aug = self.akf is not None
            for rep in range(1 + WARM_FILL):
                real = (rep == WARM_FILL)
                S.op("pe", lambda e, real=real: e.matmul(k.bank(sb)[:, n0:512], self.kf(i), self.qf(j * 512 + n0, ncol),
                                                         start=True, stop=((not has_aug and (not diag or self.mask_mul)) or not real)),
                     reads=([self.augkey] if (self.augkey is not None and not has_aug) else []), writes=[("ps", sb)])
            if has_aug:
                S.op("pe", lambda e: e.matmul(k.bank(sb)[:, n0:512], self.akf(i), self.aqf(j, n0, ncol),
                                              start=False, stop=(not diag)),
                     reads=[self.augkey], writes=[("ps", sb)])
            if diag and not self.mask_mul:
                S.op("pe", lambda e: e.matmul(k.bank(sb)[:, n0:n0 + 128], k.ident_bf, k.negtri_bf, start=False, stop=True),
                     reads=["identbf", "negtri"], writes=[("ps", sb)])
            bias = float(self.bias_fn(i, j)) if self.bias_fn is not None else 0.0
            if self.act_func is None:
                S.op("act", lambda e: e.activation(out=pt[:, n0:512], in_=k.bank(sb)[:, n0:512], func=AF.Exp, bias=bias, scale=1.0),
                     reads=[("ps", sb)], writes=[ptk])
            else:
                sc_ap = self.scale_fn(i, j)
                S.op("act", lambda e: e.activation(out=pt[:, n0:512], in_=k.bank(sb)[:, n0:512], func=self.act_func, scale=sc_ap),
                     reads=[("ps", sb), "UC"], writes=[ptk])
            if diag and self.mask_mul:
                S.op("pool", lambda e: e.tensor_tensor(out=pt[:, n0:n0 + 128], in0=pt[:, n0:n0 + 128], in1=k.tri_bf, op=ALU.mult),
                     reads=[ptk, "tri"], writes=[ptk])
        else:
            pr = self.sbanks[k._blk % len(self.sbanks)]
            sb, eb = pr
            S.op("pe", lambda e: e.matmul(k.bank(sb)[:, n0:512], self.kf(i), self.qf(j * 512 + n0, ncol), start=True, stop=True),
                 reads=[], writes=[("ps", sb)])
            S.op("pe", lambda e: e.matmul(k.bank(eb)[:, n0:512], self.akf(i), self.aqf(j, n0, ncol), start=True, stop=(not diag)),
                 reads=[self.augkey], writes=[("ps", eb)])
            if diag:
                S.op("pe", lambda e: e.matmul(k.bank(eb)[:, n0:n0 + 128], k.ident_bf, k.negtri_bf, start=False, stop=True),
                     reads=["identbf", "negtri"], writes=[("ps", eb)])
            eti = k._blk % 2
            et = k.f32(SB + eti * 512, 512)
            S.op("act", lambda e: e.activation(out=et[:, n0:512], in_=k.bank(eb)[:, n0:512], func=AF.Exp),
                 reads=[("ps", eb)], writes=[("ET", eti)])
            sc = self.scale
            S.op("dve", lambda e: e.scalar_tensor_tensor(out=pt[:, n0:512], in0=k.bank(sb)[:, n0:512], scalar=sc,
                                                         in1=et[:, n0:512], op0=ALU.mult, op1=ALU.mult),
                 reads=[("ps", sb), ("ET", eti)], writes=[ptk])
        self.state[(j, i)] = (pt, ptk, r0)

    def emit_pv(self, j, i):
        k, S = self.k, self.k.S
        pt, ptk, r0 = self.state.pop((j, i))
        for r in range(r0, 4):
            o, okey = self.O(j, r)
            last = (i == 4 * j + r)
            S.op("pe", lambda e, r=r, o=o, last=last: e.matmul(o, pt[:, r * 128:(r + 1) * 128], self.vf(i), start=(i == 0 and r % self.per_bank == 0), stop=last, skip_group_check=True),
                 reads=[ptk, self.vkey], writes=[okey])
            if last:
                self.fin(j, r, o, okey)


def run_blocks(k, blocks, L=2):
    n = len(blocks)
    for idx in range(n + L):
        if idx < n:
            p, j, i = blocks[idx]
            p.emit_s(j, i)
        if idx >= L:
            p, j, i = blocks[idx - L]
            p.emit_pv(j, i)
        tick(k)
    flush_deferred(k)


def defer(k, n, fn):
    k._dq = getattr(k, "_dq", [])
    k._dq.append([n, fn])


def tick(k):
    dq = getattr(k, "_dq", [])
    k._dq = []
    keep = []
    for it in dq:
        it[0] -= 1
        if it[0] <= 0:
            it[1]()
        else:
            keep.append(it)
    k._dq = keep + k._dq


def flush_deferred(k):
    while getattr(k, "_dq", []):
        dq = k._dq
        k._dq = []
        for it in dq:
            it[1]()


def transpose_to_fm(k, src, src_key, dst, dst_key, post=None):
    S = k.S
    q = getattr(k, "_tq", 0)
    k._tq = q + 1
    tb = k.tbanks[q % len(k.tbanks)]
    pq = k.bank(tb)[:, 0:128]
    pkey = ("ps", tb)
    S.op("pe", lambda e: e.transpose(pq, src, k.ident), reads=[src_key, "ident"], writes=[pkey])
    if post is None:
        evac(k, dst, pq, pkey, dst_key)
    else:
        post(pq, pkey)


def softplus_neg(k, T, nh, banks, negb_col, key):
    S = k.S
    for t in range(NT):
        b = banks[t]
        S.op("act", lambda e, b=b, t=t: e.activation(out=T[0:nh, t * 512:(t + 1) * 512], in_=k.bank(b)[0:nh, :],
                                                    func=AF.Exp, bias=negb_col, scale=-1.0),
             reads=[("ps", b), "gcols"], writes=[key])
    S.op("act", lambda e: e.activation(out=T[0:nh, :], in_=T[0:nh, :], func=AF.Ln, bias=1.0, scale=1.0),
         reads=[key], writes=[key])


def build_aug(k, kvec, kkey, qvec, qkey, nh, spl_off, tag):
    S, d = k.S, k.d
    dk, dq = d["augk_" + tag], d["augq_" + tag]
    SPL = [k.bf(spl_off + i * 1024, 2048) for i in range(4)]
    ones = SPL[3]
    S.op("dve", lambda e: e.memset(ones[0:nh, :], 1.0), writes=[("SPL", 3)])
    for r in range(3):
        S.dma("sp", "augw", lambda e, r=r: e.dma_start(out=dk[0:nh, 3 + r, :], in_=ones[0:nh, :]), reads=[("SPL", 3)], writes=[("augd", tag)])
        S.dma("sp", "augw", lambda e, r=r: e.dma_start(out=dq[0:nh, r, :], in_=ones[0:nh, :]), reads=[("SPL", 3)], writes=[("augd", tag)])
    cnt = 0
    for vec, vkey, dram, r0 in ((kvec, kkey, dk, 0), (qvec, qkey, dq, 3)):
        for r in range(3):
            si = cnt % 3
            cnt += 1
            spl = SPL[si]
            S.op("dve", lambda e, spl=spl, vec=vec: e.tensor_copy(spl[0:nh, :], vec[0:nh, :]), reads=[vkey], writes=[("SPL", si)])
            if r < 2:
                S.op("dve", lambda e, spl=spl, vec=vec: e.tensor_tensor(out=vec[0:nh, :], in0=vec[0:nh, :], in1=spl[0:nh, :], op=ALU.subtract),
                     reads=[vkey, ("SPL", si)], writes=[vkey])
            S.dma("sp", "augw", lambda e, spl=spl, dram=dram, rr=r0 + r: e.dma_start(out=dram[0:nh, rr, :], in_=spl[0:nh, :]),
                  reads=[("SPL", si)], writes=[("augd", tag)])


def aug_views(k, slot):
    ak = k.bf(W_W + 5120 + slot * 2048, 2048)
    aq = k.bf(W_W + 5120 + slot * 2048 + 1024, 2048)
    akf = lambda i: ak[0:6, i * 128:(i + 1) * 128]
    aqf = lambda j, n0, n: aq[0:6, j * 512 + n0:j * 512 + n0 + n]
    return akf, aqf


def load_aug(k, tag, h, slot):
    S, d = k.S, k.d
    ak = k.bf(W_W + 5120 + slot * 2048, 2048)
    aq = k.bf(W_W + 5120 + slot * 2048 + 1024, 2048)
    S.dma("sp", f"augl{slot}", lambda e: e.dma_start(out=ak[0:6, :], in_=d["augk_" + tag][h, :, :]), reads=[("augd", tag)], writes=[("AUGS", slot)])
    S.dma("sp", f"augl{slot}", lambda e: e.dma_start(out=aq[0:6, :], in_=d["augq_" + tag][h, :, :]), reads=[("augd", tag)], writes=[("AUGS", slot)])


HB_OFF = [W_W + 0, W_W + 2048, W_W + 5120, W_W + 7168]


def hb_views(k, slot):
    return k.bf(HB_OFF[slot], 2048), k.bf(HB_OFF[slot] + 1024, 2048)


def hb_init(k):
    S = k.S
    for s_ in range(4):
        qb, kb = hb_views(k, s_)
        S.op("dve", lambda e, qb=qb: e.memset(qb, 0.0), writes=[("HB", s_)])
        S.op("dve", lambda e, kb=kb: e.memset(kb, 0.0), writes=[("HB", s_)])


def hb_build(k, slot, par, q_src, k_src, augq_ap, augk_ap, R, queue):
    S = k.S
    qb, kb = hb_views(k, slot)
    r0 = par * 64
    a0 = 64 if par == 0 else 0
    S.dma("sp", f"hbsp{slot}", lambda e: e.dma_start(out=qb[r0:r0 + 64, :], in_=q_src[r0:r0 + 64, :]), writes=[("HB", slot)])
    S.dma("sp", f"hbsp{slot}", lambda e: e.dma_start(out=kb[r0:r0 + 64, :], in_=k_src[r0:r0 + 64, :]), writes=[("HB", slot)])
    S.dma(queue, f"hb{queue}{slot}", lambda e: e.dma_start(out=qb[a0:a0 + R, :], in_=augq_ap), writes=[("HB", slot)])
    S.dma(queue, f"hb{queue}{slot}", lambda e: e.dma_start(out=kb[a0:a0 + R, :], in_=augk_ap), writes=[("HB", slot)])


def layer_consts(k, l):
    S, d = k.S, k.d
    o = k.const_end
    k.gb = k.f32(o, 24); o += 24
    k.convw = k.f32(o, 16); o += 16
    k.convb = k.f32(o, 4); o += 4
    k.skipc = k.f32(o, 4); o += 4
    k.normc = k.f32(o, 4); o += 4
    k.gcols = k.f32(o, 8); o += 8
    k.lamt = k.f32(o, 256); o += 256
    k.lamc = k.f32(o, 8); o += 8
    k.subln = k.f32(o, 128); o += 128
    k.normB = k.f32(o, 512); o += 512
    k.AKd = k.bf(o, 4 * 128); o += 256
    k.AQd = k.bf(o, 4 * 512); o += 1024
    k.ident_bf = k.bf(o, 128); o += 64
    k.negtri_bf = k.bf(o, 128); o += 64
    assert o <= W_CONST + 3072, o
    S.dma("sp", "lc", lambda e: e.dma_start(out=k.gb, in_=d["gbcols"][l, :, :]), writes=["lconst"])
    S.dma("sp", "lc", lambda e: e.dma_start(out=k.convw, in_=d["convw"][l, :, :]), writes=["lconst"])
    S.dma("sp", "lc", lambda e: e.dma_start(out=k.convb, in_=d["convb"][l, :, :]), writes=["lconst"])
    S.dma("sp", "lc", lambda e: e.dma_start(out=k.skipc, in_=d["skipc"][l, :, :]), writes=["lconst"])
    S.dma("sp", "lc", lambda e: e.dma_start(out=k.normc, in_=d["normc"][l, :, :]), writes=["lconst"])
    S.dma("sp", "lc", lambda e: e.dma_start(out=k.gcols[0:8, :], in_=d["gcols"][l, :, :]), writes=["gcols"])
    S.dma("sp", "lc", lambda e: e.dma_start(out=k.lamt, in_=d["difflam"][l:l + 1, :].partition_broadcast(128)), writes=["lamt"])
    S.dma("sp", "lc", lambda e: e.dma_start(out=k.subln, in_=d["diff_subln"][l:l + 1, :].partition_broadcast(128)), writes=["subln"])
    S.dma("sp", "lc", lambda e: e.dma_start(out=k.normB, in_=d["mlstm_norm"][l:l + 1, :].partition_broadcast(128)), writes=["normB"])
    S.op("dve", lambda e: e.tensor_scalar(k.gcols[0:8, 4:5], k.gcols[0:8, 0:1], -1.0, None, ALU.mult), reads=["gcols"], writes=["gcols"])
    S.op("dve", lambda e: e.tensor_scalar(k.gcols[0:8, 5:6], k.gcols[0:8, 2:3], -1.0, None, ALU.mult), reads=["gcols"], writes=["gcols"])
    if l == 0:
        S.dma("pool", "lc2", lambda e: e.dma_start(out=k.AKd[0:3, :], in_=d["alibi_k"][:, :]), writes=["alibi"])
        S.dma("pool", "lc2", lambda e: e.dma_start(out=k.AQd[0:3, :], in_=d["alibi_q"][:, :]), writes=["alibi"])
        S.dma("pool", "lc2", lambda e: e.dma_start(out=k.ident_bf, in_=d["ident"][:, :]), writes=["identbf"])
        S.dma("pool", "lc2", lambda e: e.dma_start(out=k.negtri_bf, in_=d["negtri"][:, :]), writes=["negtri"])
    import math
    lam_init = 0.8 - 0.6 * math.exp(-0.3 * l)
    lt, lc = k.lamt, k.lamc
    S.op("dve", lambda e: e.tensor_tensor(out=lt[:, 0:64], in0=lt[:, 0:64], in1=lt[:, 64:128], op=ALU.mult), reads=["lamt"], writes=["lamt"])
    S.op("dve", lambda e: e.tensor_tensor(out=lt[:, 128:192], in0=lt[:, 128:192], in1=lt[:, 192:256], op=ALU.mult), reads=["lamt"], writes=["lamt"])
    S.op("dve", lambda e: e.reduce_sum(out=lc[:, 0:1], in_=lt[:, 0:64], axis=AX.X), reads=["lamt"], writes=["lamc"])
    S.op("dve", lambda e: e.reduce_sum(out=lc[:, 1:2], in_=lt[:, 128:192], axis=AX.X), reads=["lamt"], writes=["lamc"])
    S.op("act", lambda e: e.activation(out=lc[:, 2:4], in_=lc[:, 0:2], func=AF.Exp), reads=["lamc"], writes=["lamc"])
    S.op("dve", lambda e: e.tensor_tensor(out=lc[:, 4:5], in0=lc[:, 3:4], in1=lc[:, 2:3], op=ALU.subtract), reads=["lamc"], writes=["lamc"])
    S.op("dve", lambda e: e.tensor_scalar(lc[:, 5:6], lc[:, 4:5], -lam_init, None, ALU.add), reads=["lamc"], writes=["neglam"])
    S.op("dve", lambda e: e.tensor_scalar(k.subln, k.subln, 1.0 - lam_init, None, ALU.mult), reads=["subln"], writes=["subln"])
    k.neglam = lc[:, 5:6]


def diff_branch(k, l):
    S = k.S
    QT, KT, VO, OD = DU(0), DU(1), DU(2), NU(0)
    V = k.bf(VO, 16 * 4 * 129).rearrange("p (t h e) -> p t h e", t=16, h=4)
    S.op("dve", lambda e: e.memset(k.bf(VO, 16 * 4 * 129), 1.0), writes=[("V", "all")])
    S.barrier()
    wcol_jobs(k, l, [
        (C_DQ, 512, h_fm(QT)(k, 0)),
        (C_DK, 512, h_fm(KT, scale=0.125)(k, 0)),
        (C_DV, 512, h_tm(k, lambda tt: V[:, tt, :, :], 4, 128)),
    ])
    S.barrier()
    k.tbanks = [3]
    slopes = [2.0 ** (-8.0 * (h + 1) / 4) for h in range(4)]
    A1 = [[k.f32(SB + (p * 4 + r) * 128, 128) for r in range(4)] for p in range(2)]
    TB = SB + 1024
    blocks = []
    pres = []
    first_pass = []
    hb_init(k)
    for h in range(4):
        passes = []
        for c in range(2):
            slot = c + 2 * (h % 2)
            qb, kb = hb_views(k, slot)

            def qf(t0, n, qb=qb):
                return qb[:, t0:t0 + n]

            def kf(i, kb=kb):
                return kb[:, i * 128:(i + 1) * 128]

            pres.append(lambda h=h, c=c, slot=slot: hb_build(k, slot, c, fm(k, QT, h), fm(k, KT, h),
                                                              k.d["alibi_qf"][h, :, :], k.d["alibi_kf"][h, :, :], 3, "pool"))

            def bias_fn(i, j, h=h):
                return slopes[h] * (128.0 * i - 512.0 * j)

            def vf(i, h=h):
                return V[:, i, h, :]

            if c == 0:
                def fin(j, r, o, okey, h=h):
                    rc, rk = col(k)
                    a1 = A1[j % 2][r]
                    S.op("dve", lambda e: e.reciprocal(rc, o[:, 128:129]), reads=[okey], writes=[rk])
                    S.op("dve", lambda e: e.tensor_scalar(a1, o[:, 0:128], rc, None, ALU.mult), reads=[okey, rk], writes=[("A1", j % 2, r)])
            else:
                def fin(j, r, o, okey, h=h):
                    rc, rk = col(k)
                    sc, sk = col(k)
                    k._fp = getattr(k, "_fp", 0) + 1
                    n = k._fp
                    tmp = k.f32(TB + (n % 5) * 128, 128)
                    sq = k.f32(TB + 640 + (n % 2) * 128, 128)
                    ot = k.f32(TB + 896 + (n % 4) * 128, 128)
                    tk_, sqk, otk = ("TMP", n % 5), ("SQ", n % 2), ("OT", n % 4)
                    a1 = A1[j % 2][r]
                    tt = 4 * j + r
                    S.op("dve", lambda e: e.reciprocal(rc, o[:, 128:129]), reads=[okey], writes=[rk])
                    S.op("dve", lambda e: e.tensor_scalar(tmp, o[:, 0:128], rc, None, ALU.mult), reads=[okey, rk], writes=[tk_])
                    S.op("dve", lambda e: e.scalar_tensor_tensor(out=tmp, in0=tmp, scalar=k.neglam, in1=a1, op0=ALU.mult, op1=ALU.add),
                         reads=[tk_, ("A1", j % 2, r), "neglam"], writes=[tk_])
                    S.op("dve", lambda e: e.tensor_tensor(out=sq, in0=tmp, in1=tmp, op=ALU.mult), reads=[tk_], writes=[sqk])
                    S.op("dve", lambda e: e.reduce_sum(out=sc, in_=sq, axis=AX.X), reads=[sqk], writes=[sk])

                    def stage_b():
                        S.op("act", lambda e: e.activation(out=sc, in_=sc, func=AF.Ln, bias=RMS_EPS, scale=1.0 / 128), reads=[sk], writes=[sk])
                        S.op("act", lambda e: e.activation(out=sc, in_=sc, func=AF.Exp, scale=-0.5), reads=[sk], writes=[sk])

                    def stage_c():
                        S.op("dve", lambda e: e.scalar_tensor_tensor(out=ot, in0=tmp, scalar=sc, in1=k.subln, op0=ALU.mult, op1=ALU.mult),
                             reads=[tk_, sk, "subln"], writes=[otk])
                        defer(k, 2, lambda: transpose_to_fm(k, ot, otk, fm(k, OD, h, tt * 128, 128), ("fm", OD, h, tt)))
                    defer(k, 2, stage_b)
                    defer(k, 3, stage_c)
            passes.append(AttnPass(k, qf=qf, kf=kf, aqf=None, akf=None, bias_fn=bias_fn, vf=vf, vkey=("V", "x"), dv=128,
                                   mode="exp", fin=fin, sbanks=[0, 1, 2], obanks=[(4, 5)] if c == 0 else [(6, 7)], augkey=("HB", slot)))
        first_pass.append(passes[0])
        b0, b1 = passes[0].blocks(), passes[1].blocks()
        for j in range(4):
            blocks += [b for b in b0 if b[1] == j]
            blocks += [b for b in b1 if b[1] == j]
    for u in range(4):
        first_pass[u].pre = (pres[0:4] if u == 0 else (pres[2 * u + 2:2 * u + 4] if u < 3 else None))
    run_blocks(k, blocks)
    S.barrier()


def fox_branch(k, l):
    S, d = k.S, k.d
    QT, KT, VO, OF = DU(0), DU(1), DU(2), NU(2)
    V = k.bf(VO, 16 * 8 * 65).rearrange("p (t h e) -> p t h e", t=16, h=8)
    WFF = k.bf(W_W + 4096, 64).rearrange("p (k c) -> p k c", k=8)
    S.op("dve", lambda e: e.memset(k.bf(VO, 16 * 8 * 65), 1.0), writes=[("V", "all")])
    S.dma("pool", "wff", lambda e: e.dma_start(out=WFF, in_=d["w_in"][l, :, :, C_FF:C_FF + 8]), writes=["WFF"])
    S.barrier()
    wcol_jobs(k, l, [
        (C_FQ, 512, h_fm(QT)(k, 0)),
        (C_FK, 512, h_fm(KT, scale=0.125)(k, 0)),
        (C_FV, 512, h_tm(k, lambda tt: V[:, tt, :, :], 8, 64)),
    ])
    for t in range(NT):
        for kc in range(NKC):
            S.op("pe", lambda e, kc=kc, t=t: e.matmul(k.bank(t)[0:8, :], WFF[:, kc, :], k.H(kc, t * 512, 512),
                                                      start=(kc == 0), stop=(kc == NKC - 1)),
                 reads=["WFF", ("H", kc, t)], writes=[("ps", t)])
    S.barrier()
    T1 = k.f32(W_W, 2048)
    T2 = k.f32(W_W + 2048, 2048)
    ONES = k.f32(W_W + 7168, 2048)
    S.op("dve", lambda e: e.memset(ONES[0:8, :], 1.0), writes=["ONES"])
    softplus_neg(k, T1, 8, [0, 1, 2, 3], k.gcols[0:8, 4:5], "T1")
    S.op("dve", lambda e: e.tensor_tensor_scan(T2[0:8, :], ONES[0:8, :], T1[0:8, :], 0.0, ALU.mult, ALU.add), reads=["T1", "ONES"], writes=["T2"])
    S.op("dve", lambda e: e.tensor_scalar(T1[0:8, :], T2[0:8, :], -1.0, None, ALU.mult), reads=["T2"], writes=["T1"])
    build_aug(k, T2, "T2", T1, "T1", 8, NU(2), "fox")
    S.barrier()
    k.tbanks = [3, 7]
    OTB = [[k.f32(SB + (p * 4 + r) * 128, 128) for r in range(4)] for p in range(2)]
    blocks = []
    pres = []
    first_pass = []
    hb_init(k)
    for pr in range(4):
        passes = []
        for c in range(2):
            hd = 2 * pr + c
            slot = c + 2 * (pr % 2)
            qb, kb = hb_views(k, slot)

            def qf(t0, n, qb=qb):
                return qb[:, t0:t0 + n]

            def kf(i, kb=kb):
                return kb[:, i * 128:(i + 1) * 128]

            pres.append(lambda pr=pr, c=c, slot=slot, hd=hd: hb_build(k, slot, c, fm(k, QT, pr), fm(k, KT, pr),
                                                                      k.d["augq_fox"][hd, :, :], k.d["augk_fox"][hd, :, :], 6, "sp"))

            def vf(i, hd=hd):
                return V[:, i, hd, :]

            def fin(j, r, o, okey, pr=pr, c=c):
                rc, rk = col(k)
                ot = OTB[j % 2][r]
                otk = ("OTB", j % 2, r)
                S.op("dve", lambda e: e.reciprocal(rc, o[:, 64:65]), reads=[okey], writes=[rk])
                S.op("dve", lambda e: e.tensor_scalar(ot[:, c * 64:(c + 1) * 64], o[:, 0:64], rc, None, ALU.mult), reads=[okey, rk], writes=[otk])
                if c == 1:
                    tt = 4 * j + r
                    defer(k, 3, lambda: transpose_to_fm(k, ot, otk, fm(k, OF, pr, tt * 128, 128), ("fm", OF, pr, tt)))
            passes.append(AttnPass(k, qf=qf, kf=kf, aqf=None, akf=None, bias_fn=None, vf=vf, vkey=("V", "x"), dv=64,
                                   mode="exp", fin=fin, sbanks=[0, 1, 2, 6], obanks=[(4,)] if c == 0 else [(5,)], augkey=("HB", slot)))
        first_pass.append(passes[0])
        b0, b1 = passes[0].blocks(), passes[1].blocks()
        for j in range(4):
            blocks += [b for b in b0 if b[1] == j]
            blocks += [b for b in b1 if b[1] == j]
    for u in range(4):
        first_pass[u].pre = (pres[0:4] if u == 0 else (pres[2 * u + 2:2 * u + 4] if u < 3 else None))
    run_blocks(k, blocks, L=3)
    S.barrier()


def mlstm_branch(k, l):
    S, d = k.S, k.d
    QT, KT, VT, MX, XC, SZ, VA = DU(0), DU(1), DU(2), DU(3), NU(0), NU(1), NU(2)
    Vall = k.bf(VA, 16 * 4 * 129).rearrange("p (t h e) -> p t h e", t=16, h=4)
    WQKV = k.bf(W_W + 4096, 1536).rearrange("p (g h e) -> p g h e", g=3, h=4)
    WIF = k.bf(W_W + 4096 + 768, 96).rearrange("p (j o) -> p j o", j=12)
    S.op("dve", lambda e: e.memset(k.bf(VA, 16 * 4 * 129), 1.0), writes=[("V", "all")])
    S.dma("pool", "wqkv", lambda e: e.dma_start(out=k.bf(W_W + 4096, 1536), in_=d["wqkv"][l, :, :]), writes=["WQKV"])
    S.dma("pool", "wqkv", lambda e: e.dma_start(out=k.bf(W_W + 4096 + 768, 96), in_=d["wif"][l, :, :]), writes=["WIF"])
    S.barrier()
    wcol_jobs(k, l, [
        (C_MX, 512, h_fm(MX)(k, 0)),
        (C_MZ, 512, h_fm(SZ, func=AF.Silu)(k, 0)),
    ])
    ACC = k.f32(SB, 2048)
    for c in range(4):
        mx = fm(k, MX, c)
        mkeys = [("fm", MX, c, t) for t in range(NT)]
        w = lambda j, c=c: k.convw[:, j * 4 + c:j * 4 + c + 1]
        S.op("dve", lambda e, mx=mx, c=c, w=w: e.tensor_scalar(ACC, mx, w(3), k.convb[:, c:c + 1], ALU.mult, ALU.add),
             reads=mkeys + ["lconst"], writes=["ACC"])
        for sh in (1, 2, 3):
            S.op("dve", lambda e, mx=mx, sh=sh, w=w: e.scalar_tensor_tensor(
                out=ACC[:, sh:], in0=mx[:, 0:S_LEN - sh], scalar=w(3 - sh), in1=ACC[:, sh:], op0=ALU.mult, op1=ALU.add),
                reads=mkeys + ["ACC", "lconst"], writes=["ACC"])
        S.op("act", lambda e, c=c: e.activation(out=fm(k, XC, c), in_=ACC, func=AF.Silu), reads=["ACC"],
             writes=[("fm", XC, c, t) for t in range(NT)])
    for h in range(4):
        for t in range(NT):
            for g, src, dst in ((0, XC, QT), (1, XC, KT), (2, MX, VT)):
                b = next_bank(k)
                S.op("pe", lambda e, b=b, g=g, src=src, h=h, t=t: e.matmul(
                    k.bank(b), WQKV[:, g, h, :], fm(k, src, h, t * 512, 512), start=True, stop=True),
                    reads=["WQKV", ("fm", src, h, t)], writes=[("ps", b)])
                evac(k, fm(k, dst, h, t * 512, 512), k.bank(b), ("ps", b), ("fm", dst, h, t))
        for tt in range(NT128):
            b = next_bank(k)
            S.op("pe", lambda e, b=b, h=h, tt=tt: e.matmul(
                k.bank(b)[:, 0:128], fm(k, MX, h, tt * 128, 128), WQKV[:, 2, h, :], start=True, stop=True),
                reads=["WQKV", ("fm", MX, h, tt // 4)], writes=[("ps", b)])
            evac(k, Vall[:, tt, h, 0:128], k.bank(b)[:, 0:128], ("ps", b), ("V", tt, h))
    for h in range(4):
        S.op("dve", lambda e, h=h: e.tensor_scalar(fm(k, XC, h), fm(k, XC, h), k.skipc[:, h:h + 1], None, ALU.mult),
             reads=[("fm", XC, h, t) for t in range(NT)] + ["lconst"], writes=[("fm", XC, h, t) for t in range(NT)])
    S.barrier()
    for t in range(NT):
        for g, bb in ((0, t), (1, 4 + t)):
            for jj in range(12):
                src = (QT, KT, VT)[jj // 4]
                S.op("pe", lambda e, g=g, bb=bb, jj=jj, src=src, t=t: e.matmul(
                    k.bank(bb)[0:4, :], WIF[:, jj, g * 4:(g + 1) * 4], fm(k, src, jj % 4, t * 512, 512),
                    start=(jj == 0), stop=(jj == 11)), reads=["WIF"], writes=[("ps", bb)])
    T1 = k.f32(W_W, 2048)
    T2 = k.f32(W_W + 2048, 2048)
    T3 = k.f32(W_W + 5120, 2048)
    ONES = k.f32(W_W + 7168, 2048)
    S.op("dve", lambda e: e.memset(ONES[0:4, :], 1.0), writes=["ONES"])
    softplus_neg(k, T1, 4, [4, 5, 6, 7], k.gcols[0:4, 5:6], "T1")
    S.op("dve", lambda e: e.tensor_tensor_scan(T2[0:4, :], ONES[0:4, :], T1[0:4, :], 0.0, ALU.mult, ALU.add), reads=["T1", "ONES"], writes=["T2"])
    for t in range(NT):
        S.op("dve", lambda e, t=t: e.tensor_scalar(T1[0:4, t * 512:(t + 1) * 512], k.bank(t)[0:4, :], k.gcols[0:4, 1:2], None, ALU.add),
             reads=[("ps", t), "gcols", "T2"], writes=["T1"])
    S.op("dve", lambda e: e.tensor_tensor(out=T1[0:4, :], in0=T1[0:4, :], in1=T2[0:4, :], op=ALU.add), reads=["T1", "T2"], writes=["T1"])
    S.op("dve", lambda e: e.tensor_tensor_scan(T3[0:4, :], ONES[0:4, :], T1[0:4, :], 0.0, ALU.mult, ALU.max), reads=["T1", "ONES"], writes=["T3"])
    S.op("dve", lambda e: e.tensor_scalar(T3[0:4, :], T3[0:4, :], -1.0, None, ALU.mult), reads=["T3"], writes=["T3"])
    UR = ONES
    EM = k.f32(W_CONST + 2700, 64)
    UC = k.f32(W_CONST + 2780, 160)
    for j in range(NT):
        nb = T3[0:4, 512 * j - 1:512 * j] if j > 0 else 0.0
        S.op("act", lambda e, j=j, nb=nb: e.activation(out=T2[0:4, j * 512:(j + 1) * 512], in_=T2[0:4, j * 512:(j + 1) * 512],
                                                     func=AF.Exp, bias=nb, scale=1.0), reads=["T2", "T3"], writes=["T2"])
    for tt in range(NT128):
        S.op("pe", lambda e, tt=tt: e.transpose(k.bank(3)[:, tt * 4:(tt + 1) * 4], T2[0:4, tt * 128:(tt + 1) * 128], k.ident[0:4, 0:4]),
             reads=["T2", "ident"], writes=[("ps", 3)])
    S.op("dve", lambda e: e.tensor_copy(EM, k.bank(3)[:, 0:64]), reads=[("ps", 3)], writes=["EM"])
    for j in range(NT):
        nb = T3[0:4, 512 * j - 1:512 * j] if j > 0 else 0.0
        ncol = (j + 1) * 512
        S.op("act", lambda e, nb=nb, ncol=ncol: e.activation(out=UR[0:4, 0:ncol], in_=T1[0:4, 0:ncol], func=AF.Exp, bias=nb, scale=1.0),
             reads=["T1", "T3", "ONES"], writes=["ONES"])
        for i in range(4 * j + 4):
            idx = 2 * j * (j + 1) + i
            S.op("pe", lambda e, i=i, idx=idx: e.transpose(k.bank(2)[:, idx * 4:(idx + 1) * 4], UR[0:4, i * 128:(i + 1) * 128], k.ident[0:4, 0:4]),
                 reads=["ONES", "ident"], writes=[("ps", 2)])
    S.op("dve", lambda e: e.tensor_scalar(UC, k.bank(2)[:, 0:160], 128.0 ** -0.5, None, ALU.mult), reads=[("ps", 2)], writes=["UC"])
    S.barrier()
    k.tbanks = [3, 7]
    TB = SB + 1024
    blocks = []
    for h in range(4):

        def qf(t0, n, h=h):
            return fm(k, QT, h, t0, n)

        def kf(i, h=h):
            return fm(k, KT, h, i * 128, 128)

        def vf(i, h=h):
            return Vall[:, i, h, :]

        def fin(j, r, o, okey, h=h):
            tt = 4 * j + r
            c1, k1 = col(k)
            c2, k2 = col(k)
            c3, k3 = col(k)
            k._fp = getattr(k, "_fp", 0) + 1
            n = k._fp
            NUM = k.f32(TB + (n % 4) * 128, 128)
            TF = k.f32(TB + 640 + (n % 2) * 128, 128)
            HN = k.f32(TB + 896 + (n % 4) * 128, 128)
            ST6 = k.f32(TB + 512 + (n % 8) * 8, 6)
            MV = k.f32(TB + 576 + (n % 8) * 4, 2)
            hk, nk, fk, stk, mvk = ("HH", n % 4), ("HN", n % 4), ("TF", n % 2), ("ST6", n % 8), ("MV", n % 8)
            den = o[:, 128:129]
            S.op("dve", lambda e: e.tensor_tensor(out=c1, in0=den, in1=EM[:, tt * 4 + h:tt * 4 + h + 1], op=ALU.max), reads=[okey, "EM"], writes=[k1])
            S.op("dve", lambda e: e.scalar_tensor_tensor(out=c1, in0=den, scalar=-1.0, in1=c1, op0=ALU.mult, op1=ALU.max), reads=[okey, k1], writes=[k1])
            S.op("dve", lambda e: e.reciprocal(c1, c1), reads=[k1], writes=[k1])
            S.op("dve", lambda e: e.tensor_scalar(NUM, o[:, 0:128], c1, None, ALU.mult), reads=[okey, k1], writes=[hk])
            S.op("dve", lambda e: e.bn_stats(ST6, NUM), reads=[hk], writes=[stk])
            S.op("dve", lambda e: e.bn_aggr(MV, ST6), reads=[stk], writes=[mvk])

            def stage_b():
                S.op("act", lambda e: e.activation(out=c2, in_=MV[:, 1:2], func=AF.Ln, bias=LN_EPS, scale=1.0), reads=[mvk], writes=[k2])
                S.op("act", lambda e: e.activation(out=c2, in_=c2, func=AF.Exp, scale=-0.5), reads=[k2], writes=[k2])

            def post(pq, pkey):
                xs = fm(k, XC, h, tt * 128, 128)
                sz = fm(k, SZ, h, tt * 128, 128)
                S.op("dve", lambda e: e.scalar_tensor_tensor(out=TF, in0=pq, scalar=k.normc[:, h:h + 1], in1=xs, op0=ALU.mult, op1=ALU.add),
                     reads=[pkey, "lconst"], writes=[fk])
                S.op("dve", lambda e: e.tensor_tensor(out=sz, in0=TF, in1=sz, op=ALU.mult), reads=[fk], writes=[("fm", SZ, h, tt)])

            def stage_c():
                S.op("dve", lambda e: e.tensor_scalar(HN, NUM, MV[:, 0:1], c2, ALU.subtract, ALU.mult), reads=[hk, mvk, k2], writes=[nk])
                defer(k, 2, lambda: transpose_to_fm(k, HN, nk, None, None, post=post))
            defer(k, 2, stage_b)
            defer(k, 3, stage_c)

        def scale_fn(i, j, h=h):
            idx = 2 * j * (j + 1) + i
            return UC[:, idx * 4 + h:idx * 4 + h + 1]
        ps_ = AttnPass(k, qf=qf, kf=kf, aqf=None, akf=None, bias_fn=None, vf=vf, vkey=("V", "x"), dv=128, mode="exp", fin=fin,
                       sbanks=[0, 1, 2, 6], obanks=[(4, 5)], act_func=AF.Copy, scale_fn=scale_fn, mask_mul=True)
        blocks += ps_.blocks()
    run_blocks(k, blocks, L=3)
    S.barrier()


def merge_and_out(k, l):
    S, d = k.S, k.d
    OB = [NU(0), NU(2), NU(1)]
    MG = DU(0)
    GT = [k.f32(SB + i * 512, 512) for i in range(2)]
    ACC = k.f32(SB + 1024, 512)
    PB = k.f32(SB + 1536, 512)

    def load_mp(dd):
        s = dd % 2
        tile = k.bf(W_W + s * 2304, 4608)
        S.dma("pool", f"mp{s}", lambda e: e.dma_start(out=tile, in_=d["mpack"][l, dd, :, :]), writes=[("MP", s)])
        return tile

    tiles = {0: load_mp(0), 1: load_mp(1)}
    cnt = 0
    for dd in range(NKC):
        MP = tiles[dd]
        mkey = ("MP", dd % 2)
        for t in range(NT):
            for b in range(3):
                bg, bo = next_bank(k), next_bank(k)
                for kc in range(NKC):
                    S.op("pe", lambda e, bg=bg, b=b, kc=kc, t=t, MP=MP: e.matmul(
                        k.bank(bg), MP[:, b * 1536 + kc * 128:b * 1536 + (kc + 1) * 128], k.H(kc, t * 512, 512),
                        start=(kc == 0), stop=(kc == NKC - 1)), reads=[mkey, ("H", kc, t)], writes=[("ps", bg)])
                for kc in range(4):
                    S.op("pe", lambda e, bo=bo, b=b, kc=kc, t=t, MP=MP: e.matmul(
                        k.bank(bo), MP[:, b * 1536 + 1024 + kc * 128:b * 1536 + 1024 + (kc + 1) * 128], fm(k, OB[b], kc, t * 512, 512),
                        start=(kc == 0), stop=(kc == 3)), reads=[mkey], writes=[("ps", bo)])
                gt = GT[cnt % 2]
                gk = ("GT", cnt % 2)
                cnt += 1
                gbc = k.gb[:, b * 8 + dd:b * 8 + dd + 1]
                S.op("act", lambda e, gt=gt, bg=bg, gbc=gbc: e.activation(out=gt, in_=k.bank(bg), func=AF.Sigmoid, bias=gbc, scale=1.0),
                     reads=[("ps", bg), "lconst"], writes=[gk])
                if b == 0:
                    S.op("dve", lambda e, gt=gt, bo=bo: e.tensor_tensor(out=ACC, in0=gt, in1=k.bank(bo), op=ALU.mult),
                         reads=[gk, ("ps", bo)], writes=["MACC"])
                else:
                    S.op("dve", lambda e, gt=gt, bo=bo: e.tensor_tensor(out=PB, in0=gt, in1=k.bank(bo), op=ALU.mult),
                         reads=[gk, ("ps", bo)], writes=["MPB"])
                    dst = ACC if b == 1 else fm(k, MG, dd, t * 512, 512)
                    dkey = "MACC" if b == 1 else ("fm", MG, dd, t)
                    S.op("dve", lambda e, dst=dst: e.tensor_tensor(out=dst, in0=ACC, in1=PB, op=ALU.add),
                         reads=["MACC", "MPB"], writes=[dkey] + (["MACC"] if b == 2 else []))
        if dd + 2 < NKC:
            tiles[dd + 2] = load_mp(dd + 2)
    S.barrier()
    MN = NU(0)
    for i in range(4):
        S.op("dve" if i % 2 == 0 else "pool", lambda e, i=i: e.tensor_copy(k.bf(MN + i * 2048, 4096), k.bf(MG + i * 2048, 4096)),
             writes=[("MN", i)])
    S.barrier()
    XS = [k.f32(SB + i * 512, 512) for i in range(2)]

    WOT = [k.bf(W_W + dd * 512, 1024) for dd in range(NKC)]
    for dd in range(NKC):
        S.dma("pool", "wo", lambda e, dd=dd: e.dma_start(out=WOT[dd], in_=d["wout"][l, dd, :, :]), writes=[("WO", dd)])
    cnt = 0
    for t in range(NT):
        for dd in range(NKC):
            WO = WOT[dd]
            b = next_bank(k)
            xs = XS[cnt % 2]
            xk = ("XS", cnt % 2)
            cnt += 1
            S.dma("sp", f"xs{cnt % 2}", lambda e, xs=xs, dd=dd, t=t: e.dma_start(out=xs, in_=d["xspill"][:, dd, t * 512:(t + 1) * 512]),
                  writes=[xk])
            for kc in range(NKC):
                S.op("pe", lambda e, b=b, kc=kc, t=t, WO=WO: e.matmul(
                    k.bank(b), WO[:, kc * 128:(kc + 1) * 128], fm(k, MN, kc, t * 512, 512),
                    start=(kc == 0), stop=(kc == NKC - 1)), reads=[("WO", dd)], writes=[("ps", b)])
            S.op("dve", lambda e, b=b, xs=xs, dd=dd, t=t: e.tensor_tensor(out=k.X(dd, t), in0=k.bank(b), in1=xs, op=ALU.add),
                 reads=[("ps", b), xk], writes=[("X", dd, t)])


def spill_x(k):
    S, d = k.S, k.d
    for c in range(NKC):
        o = W_X + c * S_LEN
        S.dma("sp", "xsp", lambda e, c=c, o=o: e.dma_start(out=d["xspill"][:, c, :], in_=k.arena[:, o:o + S_LEN]),
              reads=[("X", c, t) for t in range(NT)], writes=["xspill"])


def mixer(k, l, branches=("ml", "diff", "fox")):
    S = k.S
    layer_consts(k, l)
    rmsnorm(k, l * 3 + 1, "H")
    spill_x(k)
    S.barrier()
    if "ml" in branches:
        mlstm_branch(k, l)
    if "diff" in branches:
        diff_branch(k, l)
    if "fox" in branches:
        fox_branch(k, l)
    merge_and_out(k, l)


def _chunk_rows(w):
    K_, N_ = w.shape
    return w.reshape(K_ // 128, 128, N_).transpose(1, 0, 2)


def _cols(v):
    return v.reshape(-1, 128).T


def prep_shared(inp):
    f = np.float32
    sh = {}
    for which in (1, 2):
        wg, wu, wd = inp[f"ffn{which}_w_gate"], inp[f"ffn{which}_w_up"], inp[f"ffn{which}_w_down"]
        gu = np.empty((DEPTH, NFC, 128, 2, NKC, 128), f)
        for l in range(DEPTH):
            for g, w in enumerate((wg[l], wu[l])):
                gu[l, :, :, g] = w.reshape(NKC, 128, NFC, 128).transpose(2, 1, 0, 3)
        sh[f"ffn{which}_gu"] = gu.reshape(DEPTH, NFC, 128, 2 * NKC * 128)
        sh[f"ffn{which}_wd"] = np.ascontiguousarray(wd.reshape(DEPTH, NFC, 128, D))
    vecs = []
    for l in range(DEPTH):
        vecs += [inp["ffn1_norm"][l], inp["mix_norm"][l], inp["ffn2_norm"][l]]
    vecs.append(inp["final_norm"])
    sh["normw"] = np.ascontiguousarray(np.stack([_cols(v) for v in vecs], axis=1).reshape(128, 56)).astype(f)
    sh["ident"] = np.eye(128, dtype=f)
    sh["ones"] = np.ones((128, 128), f)
    sh["tri"] = np.triu(np.ones((128, 128), f))
    sh["negtri"] = (np.tril(np.ones((128, 128), f), -1) * NEG_BIG).astype(f)
    w_in = inp["w_in"]
    sh["w_in"] = np.ascontiguousarray(np.stack([_chunk_rows(w_in[l]) for l in range(DEPTH)]))
    gb = np.empty((DEPTH, 128, 24), f)
    cw = np.empty((DEPTH, 128, 16), f)
    cb = np.empty((DEPTH, 128, 4), f)
    sk = np.empty((DEPTH, 128, 4), f)
    nmc = np.empty((DEPTH, 128, 4), f)
    gc = np.zeros((DEPTH, 8, 8), f)
    for l in range(DEPTH):
        for b in range(3):
            gb[l, :, b * 8:(b + 1) * 8] = _cols(inp["gate_bias"][l, b])
        for j in range(4):
            cw[l, :, j * 4:(j + 1) * 4] = _cols(inp["mlstm_conv_w"][l, j])
        cb[l] = _cols(inp["mlstm_conv_b"][l])
        sk[l] = _cols(inp["mlstm_skip"][l])
        nmc[l] = _cols(inp["mlstm_norm"][l])
        gc[l, :, 0] = inp["fox_b_f"][l]
        gc[l, 0:4, 1] = inp["mlstm_b_if"][l, 0:4]
        gc[l, 0:4, 2] = inp["mlstm_b_if"][l, 4:8]
    sh["gbcols"], sh["convw"], sh["convb"], sh["skipc"], sh["gcols"] = gb, cw, cb, sk, gc
    sh["normc"] = nmc
    sh["difflam"] = np.ascontiguousarray(np.concatenate(
        [inp["diff_lq1"], inp["diff_lk1"], inp["diff_lq2"], inp["diff_lk2"]], axis=1)).astype(f)
    sh["diff_subln"] = np.ascontiguousarray(inp["diff_subln"]).astype(f)
    sh["mlstm_norm"] = np.ascontiguousarray(inp["mlstm_norm"]).astype(f)
    slopes = [2.0 ** (-8.0 * (h + 1) / 4) for h in range(4)]
    ak = np.zeros((3, 4, 128), f)
    aq = np.zeros((3, 4, 512), f)
    relq = np.arange(512)
    for h in range(4):
        ak[0, h] = slopes[h] * np.arange(128)
        ak[1, h] = 1.0
        ak[2, h] = 1.0
        aq[0, h] = 1.0
        aq[1, h] = -slopes[h] * (128 * (relq // 128))
        aq[2, h] = -slopes[h] * (relq % 128)
    sh["alibi_k"] = ak.reshape(3, 512)
    sh["alibi_q"] = aq.reshape(3, 2048)
    tpos = np.arange(S_LEN)
    akf = np.zeros((4, 3, S_LEN), f)
    aqf = np.zeros((4, 3, S_LEN), f)
    for h in range(4):
        akf[h, 0] = slopes[h] * (tpos % 128)
        akf[h, 1] = 1.0
        akf[h, 2] = 1.0
        aqf[h, 0] = 1.0
        aqf[h, 1] = -slopes[h] * (128 * ((tpos % 512) // 128))
        aqf[h, 2] = -slopes[h] * (tpos % 128)
    sh["alibi_kf"], sh["alibi_qf"] = akf, aqf
    wqkv = np.empty((DEPTH, 128, 3, 4, 128), f)
    for g, nm in enumerate(("mlstm_wq", "mlstm_wk", "mlstm_wv")):
        wqkv[:, :, g] = inp[nm].transpose(0, 2, 1, 3)
    sh["wqkv"] = wqkv.reshape(DEPTH, 128, 1536)
    sh["wif"] = np.ascontiguousarray(inp["mlstm_w_if"].reshape(DEPTH, 12, 128, 8).transpose(0, 2, 1, 3)).reshape(DEPTH, 128, 96)
    mp = np.empty((DEPTH, NKC, 128, 3, 1536), f)
    wbs = (inp["w_branch_diff"], inp["w_branch_fox"], inp["w_branch_mlstm"])
    for l in range(DEPTH):
        for b in range(3):
            g = w_in[l][:, C_G + b * D:C_G + (b + 1) * D]
            mp[l, :, :, b, 0:1024] = g.reshape(NKC, 128, NKC, 128).transpose(2, 1, 0, 3).reshape(NKC, 128, 1024)
            mp[l, :, :, b, 1024:1536] = wbs[b][l].reshape(4, 128, NKC, 128).transpose(2, 1, 0, 3).reshape(NKC, 128, 512)
    sh["mpack"] = mp.reshape(DEPTH, NKC, 128, 4608)
    wo = np.empty((DEPTH, NKC, 128, 1024), f)
    for l in range(DEPTH):
        wo[l] = inp["w_out"][l].reshape(NKC, 128, NKC, 128).transpose(2, 1, 0, 3).reshape(NKC, 128, 1024)
    sh["wout"] = wo
    return sh


def build_program(shapes, plan):
    from contextlib import ExitStack
    nc = bass.Bass("TRN2", target_bir_lowering=False)
    dram = {}
    for name, shp in shapes.items():
        dram[name] = nc.dram_tensor(name, list(shp), F32, kind="ExternalInput").ap()
    dram["outT"] = nc.dram_tensor("outT", [128, NKC, S_LEN], F32, kind="ExternalOutput").ap()
    dram["xspill"] = nc.dram_tensor("xspill", [128, NKC, S_LEN], F32, kind="Internal").ap()
    for nm in ("augk_fox", "augq_fox", "augk_ml", "augq_ml"):
        dram[nm] = nc.dram_tensor(nm, [8, 6, S_LEN], BF16, kind="Internal").ap()
    with ExitStack() as es:
        sems = [es.enter_context(nc.semaphore(f"s{i}")) for i in range(70)]
        S = Sched(nc, sems)
        arena = nc.alloc_sbuf_tensor("arena", [128, ARENA_WORDS], F32)
        ps = es.enter_context(nc.psum_tensor("ps", [128, 4096], F32))
        k = K(nc, S, arena, ps, dram)
        plan(k)
        with nc.Block() as block:
            S.emit(block)
    return nc


def full_plan(k):
    build_consts(k)
    load_x(k)
    for l in range(DEPTH):
        rmsnorm(k, l * 3 + 0, "H")
        ffn(k, l, 1)
        mixer(k, l)
        rmsnorm(k, l * 3 + 2, "H")
        k.S.barrier()
        ffn(k, l, 2)
    rmsnorm(k, 6, "X")
    store_out(k)


def run(inputs, plan=full_plan, trace=False, cores=NB):
    sh = prep_shared(inputs)
    x = np.asarray(inputs["x"], np.float32)
    in_maps = []
    for b in range(cores):
        m = dict(sh)
        m["xT"] = np.ascontiguousarray(x[b].T.reshape(NKC, 128, S_LEN).transpose(1, 0, 2))
        in_maps.append(m)
    shapes = {n: a.shape for n, a in in_maps[0].items()}
    nc = build_program(shapes, plan)
    res = run_bass_kernel_spmd(nc, in_maps, core_ids=list(range(cores)), trace=trace)
    out = np.empty((cores, S_LEN, D), np.float32)
    for b in range(cores):
        o = res.results[b]["outT"]
        out[b] = o.transpose(1, 0, 2).reshape(D, S_LEN).T
    return out, res


def kernel(**inputs):
    out, _ = run(inputs)
    return out
```

```python
import bisect
import numpy as np
import concourse.bass as bass
import concourse.mybir as mybir
from concourse.bass_utils import run_bass_kernel_spmd

F32 = mybir.dt.float32
BF16 = mybir.dt.bfloat16
AF = mybir.ActivationFunctionType
ALU = mybir.AluOpType
AX = mybir.AxisListType


class _Ev:
    __slots__ = ("eng", "idx", "val", "clock")

    def __init__(self, eng, idx, val, clock):
        self.eng, self.idx, self.val, self.clock = eng, idx, val, clock


class _Eng:
    def __init__(self, name, sem, self_sync):
        self.name, self.sem, self.self_sync = name, sem, self_sync
        self.ops = []
        self.count = 0
        self.sig_idx = []
        self.sig_val = []
        self.n_inst = 0
        self.inst_rec = []
        self.clock = {}
        self.last_compute = None


class _Slot:
    def __init__(self, name, sem):
        self.name, self.sem, self.total = name, sem, 0


class _Res:
    __slots__ = ("w", "rs")

    def __init__(self):
        self.w, self.rs = None, []


class Sched:
    def __init__(self, nc, sems):
        self.nc = nc
        self._sems = list(sems)
        self.engs = {}
        for name, ss in (("pe", False), ("act", True), ("dve", True), ("pool", True), ("sp", False)):
            self.engs[name] = _Eng(name, self._sems.pop(), ss)
        self.slots = {}
        self.resd = {}

    def slot(self, name):
        s = self.slots.get(name)
        if s is None:
            s = _Slot(name, self._sems.pop())
            self.slots[name] = s
        return s

    def res(self, key):
        r = self.resd.get(key)
        if r is None:
            r = _Res()
            self.resd[key] = r
        return r

    def _value(self, ev):
        if ev.val is not None:
            return ev.val
        E = self.engs[ev.eng]
        j = bisect.bisect_left(E.sig_idx, ev.idx)
        if j < len(E.sig_idx):
            return E.sig_val[j]
        E.count += 1
        rec = E.inst_rec[ev.idx]
        rec[2], rec[3] = E.sem, 1
        E.sig_idx.append(ev.idx)
        E.sig_val.append(E.count)
        ev.val = E.count
        return ev.val

    def _wait_for(self, E, evs):
        need = {}
        for ev in evs:
            if ev is None:
                continue
            if ev.eng == E.name and not E.self_sync:
                continue
            if ev.eng in self.slots:
                v = self.slots[ev.eng].total
            else:
                v = self._value(ev)
            if E.clock.get(ev.eng, 0) >= v:
                continue
            if need.get(ev.eng, (0, None))[0] < v:
                need[ev.eng] = (v, ev)
        for name, (v, ev) in need.items():
            if E.clock.get(name, 0) >= v:
                continue
            sem = self.slots[name].sem if name in self.slots else self.engs[name].sem
            E.ops.append(["w", sem, v])
            E.clock[name] = v
            for k, cv in ev.clock.items():
                if E.clock.get(k, 0) < cv:
                    E.clock[k] = cv

    def _deps(self, reads, writes):
        evs = []
        for k in reads:
            r = self.res(k)
            if r.w is not None:
                evs.append(r.w)
        for k in writes:
            r = self.res(k)
            if r.w is not None:
                evs.append(r.w)
            evs.extend(r.rs)
        return evs

    def _commit(self, ev, reads, writes):
        for k in reads:
            self.res(k).rs.append(ev)
        for k in writes:
            r = self.res(k)
            r.w, r.rs = ev, []

    def op(self, eng, fn, reads=(), writes=()):
        E = self.engs[eng]
        self._wait_for(E, self._deps(reads, writes))
        rec = ["i", fn, None, 0]
        E.ops.append(rec)
        E.inst_rec.append(rec)
        idx = E.n_inst
        E.n_inst += 1
        E.last_compute = idx
        clk = dict(E.clock)
        ev = _Ev(eng, idx, None, clk)
        self._commit(ev, reads, writes)
        return ev

    def dma(self, queue, slot, fn, reads=(), writes=()):
        E = self.engs[queue]
        S = self.slot(slot) if isinstance(slot, str) else slot
        self._wait_for(E, self._deps(reads, writes))
        S.total += 16
        rec = ["i", fn, S.sem, 16]
        E.ops.append(rec)
        E.inst_rec.append(rec)
        E.n_inst += 1
        ev = _Ev(S.name, -1, S.total, dict(E.clock))
        self._commit(ev, reads, writes)
        return ev

    def barrier(self):
        evs = []
        for E in self.engs.values():
            if E.last_compute is not None:
                ev = _Ev(E.name, E.last_compute, None, dict(E.clock))
                self._value(ev)
                evs.append(ev)
        for S in self.slots.values():
            if S.total:
                evs.append(_Ev(S.name, -1, S.total, {}))
        for E in self.engs.values():
            self._wait_for(E, evs)
        self.resd = {}

    def wait_all_dma(self, eng, slots):
        E = self.engs[eng]
        evs = [_Ev(self.slots[s].name, -1, self.slots[s].total, {}) for s in slots if self.slots[s].total]
        self._wait_for(E, evs)

    def emit(self, block):
        def replay(E):
            def run(e):
                for rec in E.ops:
                    if rec[0] == "w":
                        e.wait_ge(rec[1], rec[2])
                    else:
                        ins = rec[1](e)
                        if rec[2] is not None:
                            ins.then_inc(rec[2], rec[3])
            return run
        block.tensor(replay(self.engs["pe"]))
        block.scalar(replay(self.engs["act"]))
        block.vector(replay(self.engs["dve"]))
        block.gpsimd(replay(self.engs["pool"]))
        block.sync(replay(self.engs["sp"]))


D = 1024
S_LEN = 2048
NB = 8
DEPTH = 2
DFF = 2816
NFC = DFF // 128
NKC = D // 128
NT = S_LEN // 512
NT128 = S_LEN // 128
RMS_EPS = 1e-6
LN_EPS = 1e-5
N_IN = 7176
FFN_GROUPS = [(0, 6), (6, 12), (12, 17), (17, 22)]

W_X = 0
W_H = W_X + 16384
W_CONST = W_H + 8192
W_PT = W_CONST + 3072
W_W = W_PT + 1024
W_N = W_W + 9216
W_END = W_N + 12288
ARENA_WORDS = W_END


class K:
    def __init__(self, nc, S, arena, ps, dram):
        self.nc, self.S, self.arena, self.ps, self.d = nc, S, arena, ps, dram
        self.ps_rr = 0

    def f32(self, off, n):
        return self.arena[:, off:off + n]

    def bf(self, off, n):
        return self.arena[:, off:off + (n + 1) // 2].bitcast(BF16)

    def bank(self, b):
        return self.ps[:, b * 512:(b + 1) * 512]

    def X(self, c, t):
        o = W_X + c * S_LEN + t * 512
        return self.arena[:, o:o + 512]

    def H(self, c, t0=0, n=S_LEN):
        return self.bf(W_H + c * (S_LEN // 2), S_LEN)[:, t0:t0 + n]

    def PT(self, i):
        return self.bf(W_PT + i * 256, 512)


def build_consts(k):
    S, d = k.S, k.d
    o = W_CONST
    k.ident = k.f32(o, 128); o += 128
    k.ones_bf = k.bf(o, 128); o += 64
    k.tri_bf = k.bf(o, 128); o += 64
    k.normw = k.f32(o, 7 * 8); o += 56
    k.const_end = o
    S.dma("sp", "c0", lambda e: e.dma_start(out=k.ident, in_=d["ident"][:, :]), writes=["ident"])
    S.dma("pool", "c1", lambda e: e.dma_start(out=k.ones_bf, in_=d["ones"][:, :]), writes=["ones"])
    S.dma("pool", "c1", lambda e: e.dma_start(out=k.tri_bf, in_=d["tri"][:, :]), writes=["tri"])
    S.dma("sp", "c0", lambda e: e.dma_start(out=k.normw, in_=d["normw"][:, :]), writes=["normw"])


def load_x(k):
    S, d = k.S, k.d
    for c in range(NKC):
        o = W_X + c * S_LEN
        S.dma("sp", "xld", lambda e, c=c, o=o: e.dma_start(out=k.arena[:, o:o + S_LEN], in_=d["xT"][:, c, :]),
              writes=[("X", c, t) for t in range(NT)])


def rmsnorm(k, widx, out_mode):
    S = k.S
    R0 = W_N + 8192
    for t in range(NT):
        for c in range(NKC):
            pt = k.PT((t * NKC + c) % 4)
            key = ("PT", (t * NKC + c) % 4)
            if c % 2 == 0:
                S.op("act", lambda e, pt=pt, c=c, t=t: e.activation(out=pt, in_=k.X(c, t), func=AF.Square),
                     reads=[("X", c, t)], writes=[key])
            else:
                S.op("dve", lambda e, pt=pt, c=c, t=t: e.tensor_tensor(out=pt, in0=k.X(c, t), in1=k.X(c, t), op=ALU.mult),
                     reads=[("X", c, t)], writes=[key])
            S.op("pe", lambda e, pt=pt, c=c, t=t: e.matmul(k.bank(t), k.ones_bf, pt, start=(c == 0), stop=(c == NKC - 1)),
                 reads=[key, "ones"], writes=[("ps", t)])
    for t in range(NT):
        R = k.f32(R0 + t * 512, 512)
        S.op("act", lambda e, R=R, t=t: e.activation(out=R, in_=k.bank(t), func=AF.Ln, bias=RMS_EPS, scale=1.0 / D),
             reads=[("ps", t)], writes=[("R", t)])
        S.op("act", lambda e, R=R: e.activation(out=R, in_=R, func=AF.Exp, scale=-0.5), reads=[("R", t)], writes=[("R", t)])
        for c in range(NKC):
            wcol = k.normw[:, widx * 8 + c:widx * 8 + c + 1]
            if out_mode == "H":
                S.op("dve", lambda e, R=R, c=c, t=t, wcol=wcol: e.scalar_tensor_tensor(
                    out=k.H(c, t * 512, 512), in0=k.X(c, t), scalar=wcol, in1=R, op0=ALU.mult, op1=ALU.mult),
                    reads=[("X", c, t), ("R", t), "normw"], writes=[("H", c, t)])
            else:
                S.op("dve", lambda e, R=R, c=c, t=t, wcol=wcol: e.scalar_tensor_tensor(
                    out=k.X(c, t), in0=k.X(c, t), scalar=wcol, in1=R, op0=ALU.mult, op1=ALU.mult),
                    reads=[("X", c, t), ("R", t), "normw"], writes=[("X", c, t)])


def ffn(k, l, which):
    S, d = k.S, k.d
    gu_d = d[f"ffn{which}_gu"]
    wd_d = d[f"ffn{which}_wd"]
    GU = [k.bf(W_W + i * 1024, 2048) for i in range(3)]
    WD = [k.bf(W_W + 3072 + i * 3072, 6 * 1024) for i in range(2)]
    A = [k.bf(W_N + i * 1024, 2048) for i in range(7)]
    SG = [k.f32(W_N + 7168 + i * 512, 512) for i in range(2)]

    def load_gu(c):
        s = c % 3
        S.dma("pool", f"gu{s}", lambda e: e.dma_start(out=GU[s], in_=gu_d[l, c, :, :]), writes=[("GU", s)])

    def load_wd(g):
        s = g % 2
        c0, c1 = FFN_GROUPS[g]
        for c in range(c0, c1):
            S.dma("pool", f"wd{s}", lambda e, c=c: e.dma_start(out=WD[s][:, (c - c0) * 1024:(c - c0 + 1) * 1024], in_=wd_d[l, c, :, :]),
                  writes=[("WD", s)])

    def gu_chunk(c):
        s = c % 3
        for t in range(NT):
            pr = (c * NT + t) % 3
            bg, bu = 2 * pr, 2 * pr + 1
            for g, b in ((0, bg), (1, bu)):
                for kc in range(NKC):
                    S.op("pe", lambda e, g=g, b=b, kc=kc, t=t: e.matmul(
                        k.bank(b), GU[s][:, (g * 8 + kc) * 128:(g * 8 + kc + 1) * 128], k.H(kc, t * 512, 512),
                        start=(kc == 0), stop=(kc == NKC - 1)),
                        reads=[("GU", s), ("H", kc, t)], writes=[("ps", b)])
            sg = SG[(c * NT + t) % 2]
            sgk = ("SG", (c * NT + t) % 2)
            S.op("act", lambda e, sg=sg, bg=bg: e.activation(out=sg, in_=k.bank(bg), func=AF.Silu),
                 reads=[("ps", bg)], writes=[sgk])
            S.op("dve", lambda e, sg=sg, bu=bu, t=t: e.tensor_tensor(
                out=A[c % 7][:, t * 512:(t + 1) * 512], in0=sg, in1=k.bank(bu), op=ALU.mult),
                reads=[sgk, ("ps", bu)], writes=[("A", c % 7, t)])

    dn_cnt = [0]

    def down(g):
        s = g % 2
        c0, c1 = FFN_GROUPS[g]
        for t in range(NT):
            for dd in range(NKC):
                b = 6 + dn_cnt[0] % 2
                dn_cnt[0] += 1
                for c in range(c0, c1):
                    S.op("pe", lambda e, b=b, c=c, dd=dd, t=t: e.matmul(
                        k.bank(b), WD[s][:, (c - c0) * 1024 + dd * 128:(c - c0) * 1024 + (dd + 1) * 128],
                        A[c % 7][:, t * 512:(t + 1) * 512], start=(c == c0), stop=(c == c1 - 1)),
                        reads=[("WD", s), ("A", c % 7, t)], writes=[("ps", b)])
                S.op("dve", lambda e, b=b, dd=dd, t=t: e.scalar_tensor_tensor(
                    out=k.X(dd, t), in0=k.bank(b), scalar=0.5, in1=k.X(dd, t), op0=ALU.mult, op1=ALU.add),
                    reads=[("ps", b), ("X", dd, t)], writes=[("X", dd, t)])

    for c in range(3):
        load_gu(c)
    load_wd(0)
    load_wd(1)
    grp_of = {}
    for g, (c0, c1) in enumerate(FFN_GROUPS):
        for c in range(c0, c1):
            grp_of[c] = g
    for c in range(NFC):
        gu_chunk(c)
        if c + 3 < NFC:
            load_gu(c + 3)
        g = grp_of[c]
        if c == FFN_GROUPS[g][0] and g > 0:
            down(g - 1)
            if g + 1 < len(FFN_GROUPS):
                load_wd(g + 1)
    down(len(FFN_GROUPS) - 1)


def store_out(k):
    S, d = k.S, k.d
    for c in range(NKC):
        o = W_X + c * S_LEN
        S.dma("sp", "ost", lambda e, c=c, o=o: e.dma_start(out=d["outT"][:, c, :], in_=k.arena[:, o:o + S_LEN]),
              reads=[("X", c, t) for t in range(NT)])
    S.wait_all_dma("sp", ["ost"])


U = 4096
W_SCR = W_END + 64
ARENA_WORDS = W_END + 2560
SB = W_SCR + 64
NEG_BIG = -30000.0
WARM_FILL = 0
C_DQ, C_DK, C_DV = 0, 512, 1024
C_FQ, C_FK, C_FV, C_FF = 1536, 2048, 2560, 3072
C_MX, C_MZ, C_G = 3080, 3592, 4104


def DU(i):
    return W_X + i * U


def NU(i):
    return W_N + i * U


def fm(k, off, c, t0=0, n=S_LEN):
    return k.bf(off + c * (S_LEN // 2), S_LEN)[:, t0:t0 + n]


def next_bank(k):
    b = k.ps_rr % 8
    k.ps_rr += 1
    return b


def col(k):
    i = getattr(k, "_col_rr", 0)
    k._col_rr = i + 1
    i %= 48
    return k.arena[:, W_SCR + i:W_SCR + i + 1], ("col", i)


def evac(k, dst, src, src_key, dst_key, scale=None, func=None, eng=None):
    S = k.S
    if eng is None:
        k._ev_rr = getattr(k, "_ev_rr", 0) + 1
        eng = "act" if (func is not None or k._ev_rr % 2 == 0) else "dve"
    if eng == "act":
        f = func if func is not None else AF.Copy
        sc = 1.0 if scale is None else scale
        S.op("act", lambda e: e.activation(out=dst, in_=src, func=f, scale=sc), reads=[src_key], writes=[dst_key])
    else:
        if scale is None:
            S.op("dve", lambda e: e.tensor_copy(dst, src), reads=[src_key], writes=[dst_key])
        else:
            S.op("dve", lambda e: e.tensor_scalar(dst, src, scale, None, ALU.mult), reads=[src_key], writes=[dst_key])


def wcol_jobs(k, l, jobs):
    S, d = k.S, k.d

    def load(i):
        c0, nc_, _ = jobs[i]
        s = i % 2
        tile = k.bf(W_W + s * 2048, 4096).rearrange("p (k c) -> p k c", k=8)
        S.dma("pool", f"wc{s}", lambda e: e.dma_start(out=tile[:, :, 0:nc_], in_=d["w_in"][l, :, :, c0:c0 + nc_]),
              writes=[("WC", s)])
        return tile

    tiles = {}
    for i in range(min(2, len(jobs))):
        tiles[i] = load(i)
    for i in range(len(jobs)):
        jobs[i][2](tiles[i], ("WC", i % 2))
        if i + 2 < len(jobs):
            tiles[i + 2] = load(i + 2)


def h_fm(dst_off, scale=None, func=None):
    def mk(k, c_base, nchunks=4):
        def handler(W, wkey):
            S = k.S
            for m in range(nchunks):
                for t in range(NT):
                    b = next_bank(k)
                    for kc in range(NKC):
                        S.op("pe", lambda e, b=b, m=m, kc=kc, t=t: e.matmul(
                            k.bank(b), W[:, kc, m * 128:(m + 1) * 128], k.H(kc, t * 512, 512),
                            start=(kc == 0), stop=(kc == NKC - 1)),
                            reads=[wkey, ("H", kc, t)], writes=[("ps", b)])
                    evac(k, fm(k, dst_off, c_base + m, t * 512, 512), k.bank(b), ("ps", b),
                         ("fm", dst_off, c_base + m, t), scale=scale, func=func)
        return handler
    return mk


def h_tm(k, vbuf_fn, nh, dv):
    def handler(W, wkey):
        S = k.S
        for tt in range(NT128):
            b = next_bank(k)
            for kc in range(NKC):
                S.op("pe", lambda e, b=b, kc=kc, tt=tt: e.matmul(
                    k.bank(b), k.H(kc, tt * 128, 128), W[:, kc, 0:512],
                    start=(kc == 0), stop=(kc == NKC - 1)),
                    reads=[wkey, ("H", kc, tt // 4)], writes=[("ps", b)])
            dst = vbuf_fn(tt)[:, :, 0:dv]
            src = k.bank(b).rearrange("p (h e) -> p h e", h=nh)
            evac(k, dst, src, ("ps", b), ("V", tt))
    return handler


class AttnPass:
    def __init__(self, k, *, qf, kf, aqf, akf, bias_fn, vf, vkey, dv, mode, fin, sbanks, obanks, scale=1.0, augkey=None, pre=None, act_func=None, scale_fn=None, mask_mul=False):
        self.__dict__.update(locals())
        self.state = {}
        self.per_bank = 2 if dv == 128 else 4

    def blocks(self):
        return [(self, j, i) for j in range(4) for i in range(4 * j + 4)]

    def O(self, j, r):
        pb = self.per_bank
        ob = self.obanks[j % len(self.obanks)][r // pb]
        c0 = (r % pb) * (self.dv + 1)
        return self.k.bank(ob)[:, c0:c0 + self.dv + 1], ("ps", ob)

    def emit_s(self, j, i):
        k, S = self.k, self.k.S
        if self.pre is not None:
            pl = self.pre if isinstance(self.pre, list) else [self.pre]
            self.pre = None
            for f_ in pl:
                f_()
        r0 = max(0, i - 4 * j)
        n0 = r0 * 128
        ncol = 512 - n0
        diag = i >= 4 * j
        k._blk = getattr(k, "_blk", 0) + 1
        pti = k._blk % 4
        pt = k.PT(pti)
        ptk = ("PT", pti)
        if self.mode == "exp":
            sb = self.sbanks[k._blk % len(self.sbanks)]
            has_aug = self.akf is not None
            for rep in range(1 + WARM_FILL):
                real = (rep == WARM_FILL)
                S.op("pe", lambda e, real=real: e.matmul(k.bank(sb)[:, n0:512], self.kf(i), self.qf(j * 512 + n0, ncol),
                                                         start=True, stop=((not has_aug and (not diag or self.mask_mul)) or not real)),
                     reads=([self.augkey] if (self.augkey is not None and not has_aug) else []), writes=[("ps", sb)])
            if has_aug:
                S.op("pe", lambda e: e.matmul(k.bank(sb)[:, n0:512], self.akf(i), self.aqf(j, n0, ncol),
                                              start=False, stop=(not diag)),
                     reads=[self.augkey], writes=[("ps", sb)])
            if diag and not self.mask_mul:
                S.op("pe", lambda e: e.matmul(k.bank(sb)[:, n0:n0 + 128], k.ident_bf, k.negtri_bf, start=False, stop=True),
                     reads=["identbf", "negtri"], writes=[("ps", sb)])
            bias = float(self.bias_fn(i, j)) if self.bias_fn is not None else 0.0
            if self.act_func is None:
                S.op("act", lambda e: e.activation(out=pt[:, n0:512], in_=k.bank(sb)[:, n0:512], func=AF.Exp, bias=bias, scale=1.0),
                     reads=[("ps", sb)], writes=[ptk])
            else:
                sc_ap = self.scale_fn(i, j)
                S.op("act", lambda e: e.activation(out=pt[:, n0:512], in_=k.bank(sb)[:, n0:512], func=self.act_func, scale=sc_ap),
                     reads=[("ps", sb), "UC"], writes=[ptk])
            if diag and self.mask_mul:
                S.op("pool", lambda e: e.tensor_tensor(out=pt[:, n0:n0 + 128], in0=pt[:, n0:n0 + 128], in1=k.tri_bf, op=ALU.mult),
                     reads=[ptk, "tri"], writes=[ptk])
        else:
            pr = self.sbanks[k._blk % len(self.sbanks)]
            sb, eb = pr
            S.op("pe", lambda e: e.matmul(k.bank(sb)[:, n0:512], self.kf(i), self.qf(j * 512 + n0, ncol), start=True, stop=True),
                 reads=[], writes=[("ps", sb)])
            S.op("pe", lambda e: e.matmul(k.bank(eb)[:, n0:512], self.akf(i), self.aqf(j, n0, ncol), start=True, stop=(not diag)),
                 reads=[self.augkey], writes=[("ps", eb)])
            if diag:
                S.op("pe", lambda e: e.matmul(k.bank(eb)[:, n0:n0 + 128], k.ident_bf, k.negtri_bf, start=False, stop=True),
                     reads=["identbf", "negtri"], writes=[("ps", eb)])
            eti = k._blk % 2
            et = k.f32(SB + eti * 512, 512)
            S.op("act", lambda e: e.activation(out=et[:, n0:512], in_=k.bank(eb)[:, n0:512], func=AF.Exp),
                 reads=[("ps", eb)], writes=[("ET", eti)])
            sc = self.scale
            S.op("dve", lambda e: e.scalar_tensor_tensor(out=pt[:, n0:512], in0=k.bank(sb)[:, n0:512], scalar=sc,
                                                         in1=et[:, n0:512], op0=ALU.mult, op1=ALU.mult),
                 reads=[("ps", sb), ("ET", eti)], writes=[ptk])
        self.state[(j, i)] = (pt, ptk, r0)

    def emit_pv(self, j, i):
        k, S = self.k, self.k.S
        pt, ptk, r0 = self.state.pop((j, i))
        for r in range(r0, 4):
            o, okey = self.O(j, r)
            last = (i == 4 * j + r)
            S.op("pe", lambda e, r=r, o=o, last=last: e.matmul(o, pt[:, r * 128:(r + 1) * 128], self.vf(i), start=(i == 0 and r % self.per_bank == 0), stop=last, skip_group_check=True),
                 reads=[ptk, self.vkey], writes=[okey])
            if last:
                self.fin(j, r, o, okey)


def run_blocks(k, blocks, L=2):
    n = len(blocks)
    for idx in range(n + L):
        if idx < n:
            p, j, i = blocks[idx]
            p.emit_s(j, i)
        if idx >= L:
            p, j, i = blocks[idx - L]
            p.emit_pv(j, i)
        tick(k)
    flush_deferred(k)


def defer(k, n, fn):
    k._dq = getattr(k, "_dq", [])
    k._dq.append([n, fn])


def tick(k):
    dq = getattr(k, "_dq", [])
    k._dq = []
    keep = []
    for it in dq:
        it[0] -= 1
        if it[0] <= 0:
            it[1]()
        else:
            keep.append(it)
    k._dq = keep + k._dq


def flush_deferred(k):
    while getattr(k, "_dq", []):
        dq = k._dq
        k._dq = []
        for it in dq:
            it[1]()


def transpose_to_fm(k, src, src_key, dst, dst_key, post=None):
    S = k.S
    q = getattr(k, "_tq", 0)
    k._tq = q + 1
    tb = k.tbanks[q % len(k.tbanks)]
    pq = k.bank(tb)[:, 0:128]
    pkey = ("ps", tb)
    S.op("pe", lambda e: e.transpose(pq, src, k.ident), reads=[src_key, "ident"], writes=[pkey])
    if post is None:
        evac(k, dst, pq, pkey, dst_key)
    else:
        post(pq, pkey)


def softplus_neg(k, T, nh, banks, negb_col, key):
    S = k.S
    for t in range(NT):
        b = banks[t]
        S.op("act", lambda e, b=b, t=t: e.activation(out=T[0:nh, t * 512:(t + 1) * 512], in_=k.bank(b)[0:nh, :],
                                                    func=AF.Exp, bias=negb_col, scale=-1.0),
             reads=[("ps", b), "gcols"], writes=[key])
    S.op("act", lambda e: e.activation(out=T[0:nh, :], in_=T[0:nh, :], func=AF.Ln, bias=1.0, scale=1.0),
         reads=[key], writes=[key])


def build_aug(k, kvec, kkey, qvec, qkey, nh, spl_off, tag):
    S, d = k.S, k.d
    dk, dq = d["augk_" + tag], d["augq_" + tag]
    SPL = [k.bf(spl_off + i * 1024, 2048) for i in range(4)]
    ones = SPL[3]
    S.op("dve", lambda e: e.memset(ones[0:nh, :], 1.0), writes=[("SPL", 3)])
    for r in range(3):
        S.dma("sp", "augw", lambda e, r=r: e.dma_start(out=dk[0:nh, 3 + r, :], in_=ones[0:nh, :]), reads=[("SPL", 3)], writes=[("augd", tag)])
        S.dma("sp", "augw", lambda e, r=r: e.dma_start(out=dq[0:nh, r, :], in_=ones[0:nh, :]), reads=[("SPL", 3)], writes=[("augd", tag)])
    cnt = 0
    for vec, vkey, dram, r0 in ((kvec, kkey, dk, 0), (qvec, qkey, dq, 3)):
        for r in range(3):
            si = cnt % 3
            cnt += 1
            spl = SPL[si]
            S.op("dve", lambda e, spl=spl, vec=vec: e.tensor_copy(spl[0:nh, :], vec[0:nh, :]), reads=[vkey], writes=[("SPL", si)])
            if r < 2:
                S.op("dve", lambda e, spl=spl, vec=vec: e.tensor_tensor(out=vec[0:nh, :], in0=vec[0:nh, :], in1=spl[0:nh, :], op=ALU.subtract),
                     reads=[vkey, ("SPL", si)], writes=[vkey])
            S.dma("sp", "augw", lambda e, spl=spl, dram=dram, rr=r0 + r: e.dma_start(out=dram[0:nh, rr, :], in_=spl[0:nh, :]),
                  reads=[("SPL", si)], writes=[("augd", tag)])


def aug_views(k, slot):
    ak = k.bf(W_W + 5120 + slot * 2048, 2048)
    aq = k.bf(W_W + 5120 + slot * 2048 + 1024, 2048)
    akf = lambda i: ak[0:6, i * 128:(i + 1) * 128]
    aqf = lambda j, n0, n: aq[0:6, j * 512 + n0:j * 512 + n0 + n]
    return akf, aqf


def load_aug(k, tag, h, slot):
    S, d = k.S, k.d
    ak = k.bf(W_W + 5120 + slot * 2048, 2048)
    aq = k.bf(W_W + 5120 + slot * 2048 + 1024, 2048)
    S.dma("sp", f"augl{slot}", lambda e: e.dma_start(out=ak[0:6, :], in_=d["augk_" + tag][h, :, :]), reads=[("augd", tag)], writes=[("AUGS", slot)])
    S.dma("sp", f"augl{slot}", lambda e: e.dma_start(out=aq[0:6, :], in_=d["augq_" + tag][h, :, :]), reads=[("augd", tag)], writes=[("AUGS", slot)])


HB_OFF = [W_W + 0, W_W + 2048, W_W + 5120, W_W + 7168]


def hb_views(k, slot):
    return k.bf(HB_OFF[slot], 2048), k.bf(HB_OFF[slot] + 1024, 2048)


def hb_init(k):
    S = k.S
    for s_ in range(4):
        qb, kb = hb_views(k, s_)
        S.op("act", lambda e, qb=qb: e.memzero(qb), writes=[("HB", s_)])
        S.op("act", lambda e, kb=kb: e.memzero(kb), writes=[("HB", s_)])


def hb_build(k, slot, par, q_src, k_src, augq_ap, augk_ap, R, queue):
    S = k.S
    qb, kb = hb_views(k, slot)
    r0 = par * 64
    a0 = 64 if par == 0 else 0
    S.dma("sp", f"hbsp{slot}", lambda e: e.dma_start(out=qb[r0:r0 + 64, :], in_=q_src[r0:r0 + 64, :]), writes=[("HB", slot)])
    S.dma("sp", f"hbsp{slot}", lambda e: e.dma_start(out=kb[r0:r0 + 64, :], in_=k_src[r0:r0 + 64, :]), writes=[("HB", slot)])
    S.dma(queue, f"hb{queue}{slot}", lambda e: e.dma_start(out=qb[a0:a0 + R, :], in_=augq_ap), writes=[("HB", slot)])
    S.dma(queue, f"hb{queue}{slot}", lambda e: e.dma_start(out=kb[a0:a0 + R, :], in_=augk_ap), writes=[("HB", slot)])


def layer_consts(k, l):
    S, d = k.S, k.d
    o = k.const_end
    k.gb = k.f32(o, 24); o += 24
    k.convw = k.f32(o, 16); o += 16
    k.convb = k.f32(o, 4); o += 4
    k.skipc = k.f32(o, 4); o += 4
    k.normc = k.f32(o, 4); o += 4
    k.gcols = k.f32(o, 8); o += 8
    k.lamt = k.f32(o, 256); o += 256
    k.lamc = k.f32(o, 8); o += 8
    k.subln = k.f32(o, 128); o += 128
    k.normB = k.f32(o, 512); o += 512
    k.AKd = k.bf(o, 4 * 128); o += 256
    k.AQd = k.bf(o, 4 * 512); o += 1024
    k.ident_bf = k.bf(o, 128); o += 64
    k.negtri_bf = k.bf(o, 128); o += 64
    assert o <= W_CONST + 3072, o
    S.dma("sp", "lc", lambda e: e.dma_start(out=k.gb, in_=d["gbcols"][l, :, :]), writes=["lconst"])
    S.dma("sp", "lc", lambda e: e.dma_start(out=k.convw, in_=d["convw"][l, :, :]), writes=["lconst"])
    S.dma("sp", "lc", lambda e: e.dma_start(out=k.convb, in_=d["convb"][l, :, :]), writes=["lconst"])
    S.dma("sp", "lc", lambda e: e.dma_start(out=k.skipc, in_=d["skipc"][l, :, :]), writes=["lconst"])
    S.dma("sp", "lc", lambda e: e.dma_start(out=k.normc, in_=d["normc"][l, :, :]), writes=["lconst"])
    S.dma("sp", "lc", lambda e: e.dma_start(out=k.gcols[0:8, :], in_=d["gcols"][l, :, :]), writes=["gcols"])
    S.dma("sp", "lc", lambda e: e.dma_start(out=k.lamt, in_=d["difflam"][l:l + 1, :].partition_broadcast(128)), writes=["lamt"])
    S.dma("sp", "lc", lambda e: e.dma_start(out=k.subln, in_=d["diff_subln"][l:l + 1, :].partition_broadcast(128)), writes=["subln"])
    S.dma("sp", "lc", lambda e: e.dma_start(out=k.normB, in_=d["mlstm_norm"][l:l + 1, :].partition_broadcast(128)), writes=["normB"])
    S.op("dve", lambda e: e.tensor_scalar(k.gcols[0:8, 4:5], k.gcols[0:8, 0:1], -1.0, None, ALU.mult), reads=["gcols"], writes=["gcols"])
    S.op("dve", lambda e: e.tensor_scalar(k.gcols[0:8, 5:6], k.gcols[0:8, 2:3], -1.0, None, ALU.mult), reads=["gcols"], writes=["gcols"])
    if l == 0:
        S.dma("pool", "lc2", lambda e: e.dma_start(out=k.AKd[0:3, :], in_=d["alibi_k"][:, :]), writes=["alibi"])
        S.dma("pool", "lc2", lambda e: e.dma_start(out=k.AQd[0:3, :], in_=d["alibi_q"][:, :]), writes=["alibi"])
        S.dma("pool", "lc2", lambda e: e.dma_start(out=k.ident_bf, in_=d["ident"][:, :]), writes=["identbf"])
        S.dma("pool", "lc2", lambda e: e.dma_start(out=k.negtri_bf, in_=d["negtri"][:, :]), writes=["negtri"])
    import math
    lam_init = 0.8 - 0.6 * math.exp(-0.3 * l)
    lt, lc = k.lamt, k.lamc
    S.op("dve", lambda e: e.tensor_tensor(out=lt[:, 0:64], in0=lt[:, 0:64], in1=lt[:, 64:128], op=ALU.mult), reads=["lamt"], writes=["lamt"])
    S.op("dve", lambda e: e.tensor_tensor(out=lt[:, 128:192], in0=lt[:, 128:192], in1=lt[:, 192:256], op=ALU.mult), reads=["lamt"], writes=["lamt"])
    S.op("dve", lambda e: e.reduce_sum(out=lc[:, 0:1], in_=lt[:, 0:64], axis=AX.X), reads=["lamt"], writes=["lamc"])
    S.op("dve", lambda e: e.reduce_sum(out=lc[:, 1:2], in_=lt[:, 128:192], axis=AX.X), reads=["lamt"], writes=["lamc"])
    S.op("act", lambda e: e.activation(out=lc[:, 2:4], in_=lc[:, 0:2], func=AF.Exp), reads=["lamc"], writes=["lamc"])
    S.op("dve", lambda e: e.tensor_tensor(out=lc[:, 4:5], in0=lc[:, 3:4], in1=lc[:, 2:3], op=ALU.subtract), reads=["lamc"], writes=["lamc"])
    S.op("dve", lambda e: e.tensor_scalar(lc[:, 5:6], lc[:, 4:5], -lam_init, None, ALU.add), reads=["lamc"], writes=["neglam"])
    S.op("dve", lambda e: e.tensor_scalar(k.subln, k.subln, 1.0 - lam_init, None, ALU.mult), reads=["subln"], writes=["subln"])
    k.neglam = lc[:, 5:6]


def diff_branch(k, l):
    S = k.S
    QT, KT, VO, OD = DU(0), DU(1), DU(2), NU(0)
    V = k.bf(VO, 16 * 4 * 129).rearrange("p (t h e) -> p t h e", t=16, h=4)
    S.op("dve", lambda e: e.memset(k.bf(VO, 16 * 4 * 129), 1.0), writes=[("V", "all")])
    S.barrier()
    wcol_jobs(k, l, [
        (C_DQ, 512, h_fm(QT)(k, 0)),
        (C_DK, 512, h_fm(KT, scale=0.125)(k, 0)),
        (C_DV, 512, h_tm(k, lambda tt: V[:, tt, :, :], 4, 128)),
    ])
    S.barrier()
    k.tbanks = [3]
    slopes = [2.0 ** (-8.0 * (h + 1) / 4) for h in range(4)]
    A1 = [[k.f32(SB + (p * 4 + r) * 128, 128) for r in range(4)] for p in range(2)]
    TB = SB + 1024
    blocks = []
    pres = []
    first_pass = []
    hb_init(k)
    for h in range(4):
        passes = []
        for c in range(2):
            slot = c + 2 * (h % 2)
            qb, kb = hb_views(k, slot)

            def qf(t0, n, qb=qb):
                return qb[:, t0:t0 + n]

            def kf(i, kb=kb):
                return kb[:, i * 128:(i + 1) * 128]

            pres.append(lambda h=h, c=c, slot=slot: hb_build(k, slot, c, fm(k, QT, h), fm(k, KT, h),
                                                              k.d["alibi_qf"][h, :, :], k.d["alibi_kf"][h, :, :], 3, "pool"))

            def bias_fn(i, j, h=h):
                return slopes[h] * (128.0 * i - 512.0 * j)

            def vf(i, h=h):
                return V[:, i, h, :]

            if c == 0:
                def fin(j, r, o, okey, h=h):
                    rc, rk = col(k)
                    a1 = A1[j % 2][r]
                    S.op("dve", lambda e: e.reciprocal(rc, o[:, 128:129]), reads=[okey], writes=[rk])
                    S.op("dve", lambda e: e.tensor_scalar(a1, o[:, 0:128], rc, None, ALU.mult), reads=[okey, rk], writes=[("A1", j % 2, r)])
            else:
                def fin(j, r, o, okey, h=h):
                    rc, rk = col(k)
                    sc, sk = col(k)
                    k._fp = getattr(k, "_fp", 0) + 1
                    n = k._fp
                    tmp = k.f32(TB + (n % 5) * 128, 128)
                    sq = k.f32(TB + 640 + (n % 2) * 128, 128)
                    ot = k.f32(TB + 896 + (n % 4) * 128, 128)
                    tk_, sqk, otk = ("TMP", n % 5), ("SQ", n % 2), ("OT", n % 4)
                    a1 = A1[j % 2][r]
                    tt = 4 * j + r
                    S.op("dve", lambda e: e.reciprocal(rc, o[:, 128:129]), reads=[okey], writes=[rk])
                    S.op("dve", lambda e: e.tensor_scalar(tmp, o[:, 0:128], rc, None, ALU.mult), reads=[okey, rk], writes=[tk_])
                    S.op("dve", lambda e: e.scalar_tensor_tensor(out=tmp, in0=tmp, scalar=k.neglam, in1=a1, op0=ALU.mult, op1=ALU.add),
                         reads=[tk_, ("A1", j % 2, r), "neglam"], writes=[tk_])
                    S.op("dve", lambda e: e.tensor_tensor(out=sq, in0=tmp, in1=tmp, op=ALU.mult), reads=[tk_], writes=[sqk])
                    S.op("dve", lambda e: e.reduce_sum(out=sc, in_=sq, axis=AX.X), reads=[sqk], writes=[sk])

                    def stage_b():
                        S.op("act", lambda e: e.activation(out=sc, in_=sc, func=AF.Ln, bias=RMS_EPS, scale=1.0 / 128), reads=[sk], writes=[sk])
                        S.op("act", lambda e: e.activation(out=sc, in_=sc, func=AF.Exp, scale=-0.5), reads=[sk], writes=[sk])

                    def stage_c():
                        S.op("dve", lambda e: e.scalar_tensor_tensor(out=ot, in0=tmp, scalar=sc, in1=k.subln, op0=ALU.mult, op1=ALU.mult),
                             reads=[tk_, sk, "subln"], writes=[otk])
                        defer(k, 2, lambda: transpose_to_fm(k, ot, otk, fm(k, OD, h, tt * 128, 128), ("fm", OD, h, tt)))
                    defer(k, 2, stage_b)
                    defer(k, 3, stage_c)
            passes.append(AttnPass(k, qf=qf, kf=kf, aqf=None, akf=None, bias_fn=bias_fn, vf=vf, vkey=("V", "x"), dv=128,
                                   mode="exp", fin=fin, sbanks=[0, 1, 2], obanks=[(4, 5)] if c == 0 else [(6, 7)], augkey=("HB", slot)))
        first_pass.append(passes[0])
        b0, b1 = passes[0].blocks(), passes[1].blocks()
        for j in range(4):
            blocks += [b for b in b0 if b[1] == j]
            blocks += [b for b in b1 if b[1] == j]
    for u in range(4):
        first_pass[u].pre = (pres[0:4] if u == 0 else (pres[2 * u + 2:2 * u + 4] if u < 3 else None))
    run_blocks(k, blocks)
    S.barrier()


def fox_branch(k, l):
    S, d = k.S, k.d
    QT, KT, VO, OF = DU(0), DU(1), DU(2), NU(2)
    V = k.bf(VO, 16 * 8 * 65).rearrange("p (t h e) -> p t h e", t=16, h=8)
    WFF = k.bf(W_W + 4096, 64).rearrange("p (k c) -> p k c", k=8)
    S.op("dve", lambda e: e.memset(k.bf(VO, 16 * 8 * 65), 1.0), writes=[("V", "all")])
    S.dma("pool", "wff", lambda e: e.dma_start(out=WFF, in_=d["w_in"][l, :, :, C_FF:C_FF + 8]), writes=["WFF"])
    S.barrier()
    wcol_jobs(k, l, [
        (C_FQ, 512, h_fm(QT)(k, 0)),
        (C_FK, 512, h_fm(KT, scale=0.125)(k, 0)),
        (C_FV, 512, h_tm(k, lambda tt: V[:, tt, :, :], 8, 64)),
    ])
    for t in range(NT):
        for kc in range(NKC):
            S.op("pe", lambda e, kc=kc, t=t: e.matmul(k.bank(t)[0:8, :], WFF[:, kc, :], k.H(kc, t * 512, 512),
                                                      start=(kc == 0), stop=(kc == NKC - 1)),
                 reads=["WFF", ("H", kc, t)], writes=[("ps", t)])
    S.barrier()
    T1 = k.f32(W_W, 2048)
    T2 = k.f32(W_W + 2048, 2048)
    ONES = k.f32(W_W + 7168, 2048)
    S.op("dve", lambda e: e.memset(ONES[0:8, :], 1.0), writes=["ONES"])
    softplus_neg(k, T1, 8, [0, 1, 2, 3], k.gcols[0:8, 4:5], "T1")
    S.op("dve", lambda e: e.tensor_tensor_scan(T2[0:8, :], ONES[0:8, :], T1[0:8, :], 0.0, ALU.mult, ALU.add), reads=["T1", "ONES"], writes=["T2"])
    S.op("dve", lambda e: e.tensor_scalar(T1[0:8, :], T2[0:8, :], -1.0, None, ALU.mult), reads=["T2"], writes=["T1"])
    build_aug(k, T2, "T2", T1, "T1", 8, NU(2), "fox")
    S.barrier()
    k.tbanks = [3, 7]
    OTB = [[k.f32(SB + (p * 4 + r) * 128, 128) for r in range(4)] for p in range(2)]
    blocks = []
    pres = []
    first_pass = []
    hb_init(k)
    for pr in range(4):
        passes = []
        for c in range(2):
            hd = 2 * pr + c
            slot = c + 2 * (pr % 2)
            qb, kb = hb_views(k, slot)

            def qf(t0, n, qb=qb):
                return qb[:, t0:t0 + n]

            def kf(i, kb=kb):
                return kb[:, i * 128:(i + 1) * 128]

            pres.append(lambda pr=pr, c=c, slot=slot, hd=hd: hb_build(k, slot, c, fm(k, QT, pr), fm(k, KT, pr),
                                                                      k.d["augq_fox"][hd, :, :], k.d["augk_fox"][hd, :, :], 6, "sp"))

            def vf(i, hd=hd):
                return V[:, i, hd, :]

            def fin(j, r, o, okey, pr=pr, c=c):
                rc, rk = col(k)
                ot = OTB[j % 2][r]
                otk = ("OTB", j % 2, r)
                S.op("dve", lambda e: e.reciprocal(rc, o[:, 64:65]), reads=[okey], writes=[rk])
                S.op("dve", lambda e: e.tensor_scalar(ot[:, c * 64:(c + 1) * 64], o[:, 0:64], rc, None, ALU.mult), reads=[okey, rk], writes=[otk])
                if c == 1:
                    tt = 4 * j + r
                    defer(k, 3, lambda: transpose_to_fm(k, ot, otk, fm(k, OF, pr, tt * 128, 128), ("fm", OF, pr, tt)))
            passes.append(AttnPass(k, qf=qf, kf=kf, aqf=None, akf=None, bias_fn=None, vf=vf, vkey=("V", "x"), dv=64,
                                   mode="exp", fin=fin, sbanks=[0, 1, 2, 6], obanks=[(4,)] if c == 0 else [(5,)], augkey=("HB", slot)))
        first_pass.append(passes[0])
        b0, b1 = passes[0].blocks(), passes[1].blocks()
        for j in range(4):
            blocks += [b for b in b0 if b[1] == j]
            blocks += [b for b in b1 if b[1] == j]
    for u in range(4):
        first_pass[u].pre = (pres[0:4] if u == 0 else (pres[2 * u + 2:2 * u + 4] if u < 3 else None))
    run_blocks(k, blocks, L=3)
    S.barrier()


def mlstm_branch(k, l):
    S, d = k.S, k.d
    QT, KT, VT, MX, XC, SZ, VA = DU(0), DU(1), DU(2), DU(3), NU(0), NU(1), NU(2)
    Vall = k.bf(VA, 16 * 4 * 129).rearrange("p (t h e) -> p t h e", t=16, h=4)
    WQKV = k.bf(W_W + 4096, 1536).rearrange("p (g h e) -> p g h e", g=3, h=4)
    WIF = k.bf(W_W + 4096 + 768, 96).rearrange("p (j o) -> p j o", j=12)
    S.op("dve", lambda e: e.memset(k.bf(VA, 16 * 4 * 129), 1.0), writes=[("V", "all")])
    S.dma("pool", "wqkv", lambda e: e.dma_start(out=k.bf(W_W + 4096, 1536), in_=d["wqkv"][l, :, :]), writes=["WQKV"])
    S.dma("pool", "wqkv", lambda e: e.dma_start(out=k.bf(W_W + 4096 + 768, 96), in_=d["wif"][l, :, :]), writes=["WIF"])
    S.barrier()
    wcol_jobs(k, l, [
        (C_MX, 512, h_fm(MX)(k, 0)),
        (C_MZ, 512, h_fm(SZ, func=AF.Silu)(k, 0)),
    ])
    ACC = k.f32(SB, 2048)
    for c in range(4):
        mx = fm(k, MX, c)
        mkeys = [("fm", MX, c, t) for t in range(NT)]
        w = lambda j, c=c: k.convw[:, j * 4 + c:j * 4 + c + 1]
        S.op("dve", lambda e, mx=mx, c=c, w=w: e.tensor_scalar(ACC, mx, w(3), k.convb[:, c:c + 1], ALU.mult, ALU.add),
             reads=mkeys + ["lconst"], writes=["ACC"])
        for sh in (1, 2, 3):
            S.op("dve", lambda e, mx=mx, sh=sh, w=w: e.scalar_tensor_tensor(
                out=ACC[:, sh:], in0=mx[:, 0:S_LEN - sh], scalar=w(3 - sh), in1=ACC[:, sh:], op0=ALU.mult, op1=ALU.add),
                reads=mkeys + ["ACC", "lconst"], writes=["ACC"])
        S.op("act", lambda e, c=c: e.activation(out=fm(k, XC, c), in_=ACC, func=AF.Silu), reads=["ACC"],
             writes=[("fm", XC, c, t) for t in range(NT)])
    for h in range(4):
        for t in range(NT):
            for g, src, dst in ((0, XC, QT), (1, XC, KT), (2, MX, VT)):
                b = next_bank(k)
                S.op("pe", lambda e, b=b, g=g, src=src, h=h, t=t: e.matmul(
                    k.bank(b), WQKV[:, g, h, :], fm(k, src, h, t * 512, 512), start=True, stop=True),
                    reads=["WQKV", ("fm", src, h, t)], writes=[("ps", b)])
                evac(k, fm(k, dst, h, t * 512, 512), k.bank(b), ("ps", b), ("fm", dst, h, t))
        for tt in range(NT128):
            b = next_bank(k)
            S.op("pe", lambda e, b=b, h=h, tt=tt: e.matmul(
                k.bank(b)[:, 0:128], fm(k, MX, h, tt * 128, 128), WQKV[:, 2, h, :], start=True, stop=True),
                reads=["WQKV", ("fm", MX, h, tt // 4)], writes=[("ps", b)])
            evac(k, Vall[:, tt, h, 0:128], k.bank(b)[:, 0:128], ("ps", b), ("V", tt, h))
    for h in range(4):
        S.op("dve", lambda e, h=h: e.tensor_scalar(fm(k, XC, h), fm(k, XC, h), k.skipc[:, h:h + 1], None, ALU.mult),
             reads=[("fm", XC, h, t) for t in range(NT)] + ["lconst"], writes=[("fm", XC, h, t) for t in range(NT)])
    S.barrier()
    for t in range(NT):
        for g, bb in ((0, t), (1, 4 + t)):
            for jj in range(12):
                src = (QT, KT, VT)[jj // 4]
                S.op("pe", lambda e, g=g, bb=bb, jj=jj, src=src, t=t: e.matmul(
                    k.bank(bb)[0:4, :], WIF[:, jj, g * 4:(g + 1) * 4], fm(k, src, jj % 4, t * 512, 512),
                    start=(jj == 0), stop=(jj == 11)), reads=["WIF"], writes=[("ps", bb)])
    T1 = k.f32(W_W, 2048)
    T2 = k.f32(W_W + 2048, 2048)
    T3 = k.f32(W_W + 5120, 2048)
    ONES = k.f32(W_W + 7168, 2048)
    S.op("dve", lambda e: e.memset(ONES[0:4, :], 1.0), writes=["ONES"])
    softplus_neg(k, T1, 4, [4, 5, 6, 7], k.gcols[0:4, 5:6], "T1")
    S.op("dve", lambda e: e.tensor_tensor_scan(T2[0:4, :], ONES[0:4, :], T1[0:4, :], 0.0, ALU.mult, ALU.add), reads=["T1", "ONES"], writes=["T2"])
    for t in range(NT):
        S.op("dve", lambda e, t=t: e.tensor_scalar(T1[0:4, t * 512:(t + 1) * 512], k.bank(t)[0:4, :], k.gcols[0:4, 1:2], None, ALU.add),
             reads=[("ps", t), "gcols", "T2"], writes=["T1"])
    S.op("dve", lambda e: e.tensor_tensor(out=T1[0:4, :], in0=T1[0:4, :], in1=T2[0:4, :], op=ALU.add), reads=["T1", "T2"], writes=["T1"])
    S.op("dve", lambda e: e.tensor_tensor_scan(T3[0:4, :], ONES[0:4, :], T1[0:4, :], 0.0, ALU.mult, ALU.max), reads=["T1", "ONES"], writes=["T3"])
    S.op("dve", lambda e: e.tensor_scalar(T3[0:4, :], T3[0:4, :], -1.0, None, ALU.mult), reads=["T3"], writes=["T3"])
    UR = ONES
    EM = k.f32(W_CONST + 2700, 64)
    UC = k.f32(W_CONST + 2780, 160)
    for j in range(NT):
        nb = T3[0:4, 512 * j - 1:512 * j] if j > 0 else 0.0
        S.op("act", lambda e, j=j, nb=nb: e.activation(out=T2[0:4, j * 512:(j + 1) * 512], in_=T2[0:4, j * 512:(j + 1) * 512],
                                                     func=AF.Exp, bias=nb, scale=1.0), reads=["T2", "T3"], writes=["T2"])
    for tt in range(NT128):
        S.op("pe", lambda e, tt=tt: e.transpose(k.bank(3)[:, tt * 4:(tt + 1) * 4], T2[0:4, tt * 128:(tt + 1) * 128], k.ident[0:4, 0:4]),
             reads=["T2", "ident"], writes=[("ps", 3)])
    S.op("dve", lambda e: e.tensor_copy(EM, k.bank(3)[:, 0:64]), reads=[("ps", 3)], writes=["EM"])
    for j in range(NT):
        nb = T3[0:4, 512 * j - 1:512 * j] if j > 0 else 0.0
        ncol = (j + 1) * 512
        S.op("act", lambda e, nb=nb, ncol=ncol: e.activation(out=UR[0:4, 0:ncol], in_=T1[0:4, 0:ncol], func=AF.Exp, bias=nb, scale=1.0),
             reads=["T1", "T3", "ONES"], writes=["ONES"])
        for i in range(4 * j + 4):
            idx = 2 * j * (j + 1) + i
            S.op("pe", lambda e, i=i, idx=idx: e.transpose(k.bank(2)[:, idx * 4:(idx + 1) * 4], UR[0:4, i * 128:(i + 1) * 128], k.ident[0:4, 0:4]),
                 reads=["ONES", "ident"], writes=[("ps", 2)])
    S.op("dve", lambda e: e.tensor_scalar(UC, k.bank(2)[:, 0:160], 128.0 ** -0.5, None, ALU.mult), reads=[("ps", 2)], writes=["UC"])
    S.barrier()
    k.tbanks = [3, 7]
    TB = SB + 1024
    blocks = []
    for h in range(4):

        def qf(t0, n, h=h):
            return fm(k, QT, h, t0, n)

        def kf(i, h=h):
            return fm(k, KT, h, i * 128, 128)

        def vf(i, h=h):
            return Vall[:, i, h, :]

        def fin(j, r, o, okey, h=h):
            tt = 4 * j + r
            c1, k1 = col(k)
            c2, k2 = col(k)
            c3, k3 = col(k)
            k._fp = getattr(k, "_fp", 0) + 1
            n = k._fp
            NUM = k.f32(TB + (n % 4) * 128, 128)
            TF = k.f32(TB + 640 + (n % 2) * 128, 128)
            HN = k.f32(TB + 896 + (n % 4) * 128, 128)
            ST6 = k.f32(TB + 512 + (n % 8) * 8, 6)
            MV = k.f32(TB + 576 + (n % 8) * 4, 2)
            hk, nk, fk, stk, mvk = ("HH", n % 4), ("HN", n % 4), ("TF", n % 2), ("ST6", n % 8), ("MV", n % 8)
            den = o[:, 128:129]
            S.op("dve", lambda e: e.tensor_tensor(out=c1, in0=den, in1=EM[:, tt * 4 + h:tt * 4 + h + 1], op=ALU.max), reads=[okey, "EM"], writes=[k1])
            S.op("dve", lambda e: e.scalar_tensor_tensor(out=c1, in0=den, scalar=-1.0, in1=c1, op0=ALU.mult, op1=ALU.max), reads=[okey, k1], writes=[k1])
            S.op("dve", lambda e: e.reciprocal(c1, c1), reads=[k1], writes=[k1])
            S.op("dve", lambda e: e.tensor_scalar(NUM, o[:, 0:128], c1, None, ALU.mult), reads=[okey, k1], writes=[hk])
            S.op("dve", lambda e: e.bn_stats(ST6, NUM), reads=[hk], writes=[stk])
            S.op("dve", lambda e: e.bn_aggr(MV, ST6), reads=[stk], writes=[mvk])

            def stage_b():
                S.op("act", lambda e: e.activation(out=c2, in_=MV[:, 1:2], func=AF.Ln, bias=LN_EPS, scale=1.0), reads=[mvk], writes=[k2])
                S.op("act", lambda e: e.activation(out=c2, in_=c2, func=AF.Exp, scale=-0.5), reads=[k2], writes=[k2])

            def post(pq, pkey):
                xs = fm(k, XC, h, tt * 128, 128)
                sz = fm(k, SZ, h, tt * 128, 128)
                S.op("dve", lambda e: e.scalar_tensor_tensor(out=TF, in0=pq, scalar=k.normc[:, h:h + 1], in1=xs, op0=ALU.mult, op1=ALU.add),
                     reads=[pkey, "lconst"], writes=[fk])
                S.op("dve", lambda e: e.tensor_tensor(out=sz, in0=TF, in1=sz, op=ALU.mult), reads=[fk], writes=[("fm", SZ, h, tt)])

            def stage_c():
                S.op("dve", lambda e: e.tensor_scalar(HN, NUM, MV[:, 0:1], c2, ALU.subtract, ALU.mult), reads=[hk, mvk, k2], writes=[nk])
                defer(k, 2, lambda: transpose_to_fm(k, HN, nk, None, None, post=post))
            defer(k, 2, stage_b)
            defer(k, 3, stage_c)

        def scale_fn(i, j, h=h):
            idx = 2 * j * (j + 1) + i
            return UC[:, idx * 4 + h:idx * 4 + h + 1]
        ps_ = AttnPass(k, qf=qf, kf=kf, aqf=None, akf=None, bias_fn=None, vf=vf, vkey=("V", "x"), dv=128, mode="exp", fin=fin,
                       sbanks=[0, 1, 2, 6], obanks=[(4, 5)], act_func=AF.Copy, scale_fn=scale_fn, mask_mul=True)
        blocks += ps_.blocks()
    run_blocks(k, blocks, L=3)
    S.barrier()


def merge_and_out(k, l):
    S, d = k.S, k.d
    OB = [NU(0), NU(2), NU(1)]
    MG = DU(0)
    GT = [k.f32(SB + i * 512, 512) for i in range(2)]
    ACC = k.f32(SB + 1024, 512)
    PB = k.f32(SB + 1536, 512)

    def load_mp(dd):
        s = dd % 2
        tile = k.bf(W_W + s * 2304, 4608)
        S.dma("pool", f"mp{s}", lambda e: e.dma_start(out=tile, in_=d["mpack"][l, dd, :, :]), writes=[("MP", s)])
        return tile

    tiles = {0: load_mp(0), 1: load_mp(1)}
    cnt = 0
    for dd in range(NKC):
        MP = tiles[dd]
        mkey = ("MP", dd % 2)
        for t in range(NT):
            for b in range(3):
                bg, bo = next_bank(k), next_bank(k)
                for kc in range(NKC):
                    S.op("pe", lambda e, bg=bg, b=b, kc=kc, t=t, MP=MP: e.matmul(
                        k.bank(bg), MP[:, b * 1536 + kc * 128:b * 1536 + (kc + 1) * 128], k.H(kc, t * 512, 512),
                        start=(kc == 0), stop=(kc == NKC - 1)), reads=[mkey, ("H", kc, t)], writes=[("ps", bg)])
                for kc in range(4):
                    S.op("pe", lambda e, bo=bo, b=b, kc=kc, t=t, MP=MP: e.matmul(
                        k.bank(bo), MP[:, b * 1536 + 1024 + kc * 128:b * 1536 + 1024 + (kc + 1) * 128], fm(k, OB[b], kc, t * 512, 512),
                        start=(kc == 0), stop=(kc == 3)), reads=[mkey], writes=[("ps", bo)])
                gt = GT[cnt % 2]
                gk = ("GT", cnt % 2)
                cnt += 1
                gbc = k.gb[:, b * 8 + dd:b * 8 + dd + 1]
                S.op("act", lambda e, gt=gt, bg=bg, gbc=gbc: e.activation(out=gt, in_=k.bank(bg), func=AF.Sigmoid, bias=gbc, scale=1.0),
                     reads=[("ps", bg), "lconst"], writes=[gk])
                if b == 0:
                    S.op("dve", lambda e, gt=gt, bo=bo: e.tensor_tensor(out=ACC, in0=gt, in1=k.bank(bo), op=ALU.mult),
                         reads=[gk, ("ps", bo)], writes=["MACC"])
                else:
                    S.op("dve", lambda e, gt=gt, bo=bo: e.tensor_tensor(out=PB, in0=gt, in1=k.bank(bo), op=ALU.mult),
                         reads=[gk, ("ps", bo)], writes=["MPB"])
                    dst = ACC if b == 1 else fm(k, MG, dd, t * 512, 512)
                    dkey = "MACC" if b == 1 else ("fm", MG, dd, t)
                    S.op("dve", lambda e, dst=dst: e.tensor_tensor(out=dst, in0=ACC, in1=PB, op=ALU.add),
                         reads=["MACC", "MPB"], writes=[dkey] + (["MACC"] if b == 2 else []))
        if dd + 2 < NKC:
            tiles[dd + 2] = load_mp(dd + 2)
    S.barrier()
    MN = NU(0)
    for i in range(4):
        S.op("dve" if i % 2 == 0 else "pool", lambda e, i=i: e.tensor_copy(k.bf(MN + i * 2048, 4096), k.bf(MG + i * 2048, 4096)),
             writes=[("MN", i)])
    S.barrier()
    XS = [k.f32(SB + i * 512, 512) for i in range(2)]

    WOT = [k.bf(W_W + dd * 512, 1024) for dd in range(NKC)]
    for dd in range(NKC):
        S.dma("pool", "wo", lambda e, dd=dd: e.dma_start(out=WOT[dd], in_=d["wout"][l, dd, :, :]), writes=[("WO", dd)])
    cnt = 0
    for t in range(NT):
        for dd in range(NKC):
            WO = WOT[dd]
            b = next_bank(k)
            xs = XS[cnt % 2]
            xk = ("XS", cnt % 2)
            cnt += 1
            S.dma("sp", f"xs{cnt % 2}", lambda e, xs=xs, dd=dd, t=t: e.dma_start(out=xs, in_=d["xspill"][:, dd, t * 512:(t + 1) * 512]),
                  writes=[xk])
            for kc in range(NKC):
                S.op("pe", lambda e, b=b, kc=kc, t=t, WO=WO: e.matmul(
                    k.bank(b), WO[:, kc * 128:(kc + 1) * 128], fm(k, MN, kc, t * 512, 512),
                    start=(kc == 0), stop=(kc == NKC - 1)), reads=[("WO", dd)], writes=[("ps", b)])
            S.op("dve", lambda e, b=b, xs=xs, dd=dd, t=t: e.tensor_tensor(out=k.X(dd, t), in0=k.bank(b), in1=xs, op=ALU.add),
                 reads=[("ps", b), xk], writes=[("X", dd, t)])


def spill_x(k):
    S, d = k.S, k.d
    for c in range(NKC):
        o = W_X + c * S_LEN
        S.dma("sp", "xsp", lambda e, c=c, o=o: e.dma_start(out=d["xspill"][:, c, :], in_=k.arena[:, o:o + S_LEN]),
              reads=[("X", c, t) for t in range(NT)], writes=["xspill"])


def mixer(k, l, branches=("ml", "diff", "fox")):
    S = k.S
    layer_consts(k, l)
    rmsnorm(k, l * 3 + 1, "H")
    spill_x(k)
    S.barrier()
    if "ml" in branches:
        mlstm_branch(k, l)
    if "diff" in branches:
        diff_branch(k, l)
    if "fox" in branches:
        fox_branch(k, l)
    merge_and_out(k, l)


def _chunk_rows(w):
    K_, N_ = w.shape
    return w.reshape(K_ // 128, 128, N_).transpose(1, 0, 2)


def _cols(v):
    return v.reshape(-1, 128).T


def prep_shared(inp):
    f = np.float32
    sh = {}
    for which in (1, 2):
        wg, wu, wd = inp[f"ffn{which}_w_gate"], inp[f"ffn{which}_w_up"], inp[f"ffn{which}_w_down"]
        gu = np.empty((DEPTH, NFC, 128, 2, NKC, 128), f)
        for l in range(DEPTH):
            for g, w in enumerate((wg[l], wu[l])):
                gu[l, :, :, g] = w.reshape(NKC, 128, NFC, 128).transpose(2, 1, 0, 3)
        sh[f"ffn{which}_gu"] = gu.reshape(DEPTH, NFC, 128, 2 * NKC * 128)
        sh[f"ffn{which}_wd"] = np.ascontiguousarray(wd.reshape(DEPTH, NFC, 128, D))
    vecs = []
    for l in range(DEPTH):
        vecs += [inp["ffn1_norm"][l], inp["mix_norm"][l], inp["ffn2_norm"][l]]
    vecs.append(inp["final_norm"])
    sh["normw"] = np.ascontiguousarray(np.stack([_cols(v) for v in vecs], axis=1).reshape(128, 56)).astype(f)
    sh["ident"] = np.eye(128, dtype=f)
    sh["ones"] = np.ones((128, 128), f)
    sh["tri"] = np.triu(np.ones((128, 128), f))
    sh["negtri"] = (np.tril(np.ones((128, 128), f), -1) * NEG_BIG).astype(f)
    w_in = inp["w_in"]
    sh["w_in"] = np.ascontiguousarray(np.stack([_chunk_rows(w_in[l]) for l in range(DEPTH)]))
    gb = np.empty((DEPTH, 128, 24), f)
    cw = np.empty((DEPTH, 128, 16), f)
    cb = np.empty((DEPTH, 128, 4), f)
    sk = np.empty((DEPTH, 128, 4), f)
    nmc = np.empty((DEPTH, 128, 4), f)
    gc = np.zeros((DEPTH, 8, 8), f)
    for l in range(DEPTH):
        for b in range(3):
            gb[l, :, b * 8:(b + 1) * 8] = _cols(inp["gate_bias"][l, b])
        for j in range(4):
            cw[l, :, j * 4:(j + 1) * 4] = _cols(inp["mlstm_conv_w"][l, j])
        cb[l] = _cols(inp["mlstm_conv_b"][l])
        sk[l] = _cols(inp["mlstm_skip"][l])
        nmc[l] = _cols(inp["mlstm_norm"][l])
        gc[l, :, 0] = inp["fox_b_f"][l]
        gc[l, 0:4, 1] = inp["mlstm_b_if"][l, 0:4]
        gc[l, 0:4, 2] = inp["mlstm_b_if"][l, 4:8]
    sh["gbcols"], sh["convw"], sh["convb"], sh["skipc"], sh["gcols"] = gb, cw, cb, sk, gc
    sh["normc"] = nmc
    sh["difflam"] = np.ascontiguousarray(np.concatenate(
        [inp["diff_lq1"], inp["diff_lk1"], inp["diff_lq2"], inp["diff_lk2"]], axis=1)).astype(f)
    sh["diff_subln"] = np.ascontiguousarray(inp["diff_subln"]).astype(f)
    sh["mlstm_norm"] = np.ascontiguousarray(inp["mlstm_norm"]).astype(f)
    slopes = [2.0 ** (-8.0 * (h + 1) / 4) for h in range(4)]
    ak = np.zeros((3, 4, 128), f)
    aq = np.zeros((3, 4, 512), f)
    relq = np.arange(512)
    for h in range(4):
        ak[0, h] = slopes[h] * np.arange(128)
        ak[1, h] = 1.0
        ak[2, h] = 1.0
        aq[0, h] = 1.0
        aq[1, h] = -slopes[h] * (128 * (relq // 128))
        aq[2, h] = -slopes[h] * (relq % 128)
    sh["alibi_k"] = ak.reshape(3, 512)
    sh["alibi_q"] = aq.reshape(3, 2048)
    tpos = np.arange(S_LEN)
    akf = np.zeros((4, 3, S_LEN), f)
    aqf = np.zeros((4, 3, S_LEN), f)
    for h in range(4):
        akf[h, 0] = slopes[h] * (tpos % 128)
        akf[h, 1] = 1.0
        akf[h, 2] = 1.0
        aqf[h, 0] = 1.0
        aqf[h, 1] = -slopes[h] * (128 * ((tpos % 512) // 128))
        aqf[h, 2] = -slopes[h] * (tpos % 128)
    sh["alibi_kf"], sh["alibi_qf"] = akf, aqf
    wqkv = np.empty((DEPTH, 128, 3, 4, 128), f)
    for g, nm in enumerate(("mlstm_wq", "mlstm_wk", "mlstm_wv")):
        wqkv[:, :, g] = inp[nm].transpose(0, 2, 1, 3)
    sh["wqkv"] = wqkv.reshape(DEPTH, 128, 1536)
    sh["wif"] = np.ascontiguousarray(inp["mlstm_w_if"].reshape(DEPTH, 12, 128, 8).transpose(0, 2, 1, 3)).reshape(DEPTH, 128, 96)
    mp = np.empty((DEPTH, NKC, 128, 3, 1536), f)
    wbs = (inp["w_branch_diff"], inp["w_branch_fox"], inp["w_branch_mlstm"])
    for l in range(DEPTH):
        for b in range(3):
            g = w_in[l][:, C_G + b * D:C_G + (b + 1) * D]
            mp[l, :, :, b, 0:1024] = g.reshape(NKC, 128, NKC, 128).transpose(2, 1, 0, 3).reshape(NKC, 128, 1024)
            mp[l, :, :, b, 1024:1536] = wbs[b][l].reshape(4, 128, NKC, 128).transpose(2, 1, 0, 3).reshape(NKC, 128, 512)
    sh["mpack"] = mp.reshape(DEPTH, NKC, 128, 4608)
    wo = np.empty((DEPTH, NKC, 128, 1024), f)
    for l in range(DEPTH):
        wo[l] = inp["w_out"][l].reshape(NKC, 128, NKC, 128).transpose(2, 1, 0, 3).reshape(NKC, 128, 1024)
    sh["wout"] = wo
    return sh


def build_program(shapes, plan):
    from contextlib import ExitStack
    nc = bass.Bass("TRN2", target_bir_lowering=False)
    dram = {}
    for name, shp in shapes.items():
        dram[name] = nc.dram_tensor(name, list(shp), F32, kind="ExternalInput").ap()
    dram["outT"] = nc.dram_tensor("outT", [128, NKC, S_LEN], F32, kind="ExternalOutput").ap()
    dram["xspill"] = nc.dram_tensor("xspill", [128, NKC, S_LEN], F32, kind="Internal").ap()
    for nm in ("augk_fox", "augq_fox", "augk_ml", "augq_ml"):
        dram[nm] = nc.dram_tensor(nm, [8, 6, S_LEN], BF16, kind="Internal").ap()
    with ExitStack() as es:
        sems = [es.enter_context(nc.semaphore(f"s{i}")) for i in range(70)]
        S = Sched(nc, sems)
        arena = nc.alloc_sbuf_tensor("arena", [128, ARENA_WORDS], F32)
        ps = es.enter_context(nc.psum_tensor("ps", [128, 4096], F32))
        k = K(nc, S, arena, ps, dram)
        plan(k)
        with nc.Block() as block:
            S.emit(block)
    return nc


def full_plan(k):
    build_consts(k)
    load_x(k)
    for l in range(DEPTH):
        rmsnorm(k, l * 3 + 0, "H")
        ffn(k, l, 1)
        mixer(k, l)
        rmsnorm(k, l * 3 + 2, "H")
        k.S.barrier()
        ffn(k, l, 2)
    rmsnorm(k, 6, "X")
    store_out(k)


def run(inputs, plan=full_plan, trace=False, cores=NB):
    sh = prep_shared(inputs)
    x = np.asarray(inputs["x"], np.float32)
    in_maps = []
    for b in range(cores):
        m = dict(sh)
        m["xT"] = np.ascontiguousarray(x[b].T.reshape(NKC, 128, S_LEN).transpose(1, 0, 2))
        in_maps.append(m)
    shapes = {n: a.shape for n, a in in_maps[0].items()}
    nc = build_program(shapes, plan)
    res = run_bass_kernel_spmd(nc, in_maps, core_ids=list(range(cores)), trace=trace)
    out = np.empty((cores, S_LEN, D), np.float32)
    for b in range(cores):
        o = res.results[b]["outT"]
        out[b] = o.transpose(1, 0, 2).reshape(D, S_LEN).T
    return out, res


def kernel(**inputs):
    out, _ = run(inputs)
    return out
```

```python
import bisect
import numpy as np
import concourse.bass as bass
import concourse.mybir as mybir
from concourse.bass_utils import run_bass_kernel_spmd

F32 = mybir.dt.float32
BF16 = mybir.dt.bfloat16
AF = mybir.ActivationFunctionType
ALU = mybir.AluOpType
AX = mybir.AxisListType


class _Ev:
    __slots__ = ("eng", "idx", "val", "clock")

    def __init__(self, eng, idx, val, clock):
        self.eng, self.idx, self.val, self.clock = eng, idx, val, clock


class _Eng:
    def __init__(self, name, sem, self_sync):
        self.name, self.sem, self.self_sync = name, sem, self_sync
        self.ops = []
        self.count = 0
        self.sig_idx = []
        self.sig_val = []
        self.n_inst = 0
        self.inst_rec = []
        self.clock = {}
        self.last_compute = None


class _Slot:
    def __init__(self, name, sem):
        self.name, self.sem, self.total = name, sem, 0


class _Res:
    __slots__ = ("w", "rs")

    def __init__(self):
        self.w, self.rs = None, []


class Sched:
    def __init__(self, nc, sems):
        self.nc = nc
        self._sems = list(sems)
        self.engs = {}
        for name, ss in (("pe", False), ("act", True), ("dve", True), ("pool", True), ("sp", False)):
            self.engs[name] = _Eng(name, self._sems.pop(), ss)
        self.slots = {}
        self.resd = {}

    def slot(self, name):
        s = self.slots.get(name)
        if s is None:
            s = _Slot(name, self._sems.pop())
            self.slots[name] = s
        return s

    def res(self, key):
        r = self.resd.get(key)
        if r is None:
            r = _Res()
            self.resd[key] = r
        return r

    def _value(self, ev):
        if ev.val is not None:
            return ev.val
        E = self.engs[ev.eng]
        j = bisect.bisect_left(E.sig_idx, ev.idx)
        if j < len(E.sig_idx):
            return E.sig_val[j]
        E.count += 1
        rec = E.inst_rec[ev.idx]
        rec[2], rec[3] = E.sem, 1
        E.sig_idx.append(ev.idx)
        E.sig_val.append(E.count)
        ev.val = E.count
        return ev.val

    def _wait_for(self, E, evs):
        need = {}
        for ev in evs:
            if ev is None:
                continue
            if ev.eng == E.name and not E.self_sync:
                continue
            if ev.eng in self.slots:
                v = self.slots[ev.eng].total
            else:
                v = self._value(ev)
            if E.clock.get(ev.eng, 0) >= v:
                continue
            if need.get(ev.eng, (0, None))[0] < v:
                need[ev.eng] = (v, ev)
        for name, (v, ev) in need.items():
            if E.clock.get(name, 0) >= v:
                continue
            sem = self.slots[name].sem if name in self.slots else self.engs[name].sem
            E.ops.append(["w", sem, v])
            E.clock[name] = v
            for k, cv in ev.clock.items():
                if E.clock.get(k, 0) < cv:
                    E.clock[k] = cv

    def _deps(self, reads, writes):
        evs = []
        for k in reads:
            r = self.res(k)
            if r.w is not None:
                evs.append(r.w)
        for k in writes:
            r = self.res(k)
            if r.w is not None:
                evs.append(r.w)
            evs.extend(r.rs)
        return evs

    def _commit(self, ev, reads, writes):
        for k in reads:
            self.res(k).rs.append(ev)
        for k in writes:
            r = self.res(k)
            r.w, r.rs = ev, []

    def op(self, eng, fn, reads=(), writes=()):
        E = self.engs[eng]
        self._wait_for(E, self._deps(reads, writes))
        rec = ["i", fn, None, 0]
        E.ops.append(rec)
        E.inst_rec.append(rec)
        idx = E.n_inst
        E.n_inst += 1
        E.last_compute = idx
        clk = dict(E.clock)
        ev = _Ev(eng, idx, None, clk)
        self._commit(ev, reads, writes)
        return ev

    def dma(self, queue, slot, fn, reads=(), writes=()):
        E = self.engs[queue]
        S = self.slot(slot) if isinstance(slot, str) else slot
        self._wait_for(E, self._deps(reads, writes))
        S.total += 16
        rec = ["i", fn, S.sem, 16]
        E.ops.append(rec)
        E.inst_rec.append(rec)
        E.n_inst += 1
        ev = _Ev(S.name, -1, S.total, dict(E.clock))
        self._commit(ev, reads, writes)
        return ev

    def barrier(self):
        evs = []
        for E in self.engs.values():
            if E.last_compute is not None:
                ev = _Ev(E.name, E.last_compute, None, dict(E.clock))
                self._value(ev)
                evs.append(ev)
        for S in self.slots.values():
            if S.total:
                evs.append(_Ev(S.name, -1, S.total, {}))
        for E in self.engs.values():
            self._wait_for(E, evs)
        self.resd = {}

    def wait_all_dma(self, eng, slots):
        E = self.engs[eng]
        evs = [_Ev(self.slots[s].name, -1, self.slots[s].total, {}) for s in slots if self.slots[s].total]
        self._wait_for(E, evs)

    def emit(self, block):
        def replay(E):
            def run(e):
                for rec in E.ops:
                    if rec[0] == "w":
                        e.wait_ge(rec[1], rec[2])
                    else:
                        ins = rec[1](e)
                        if rec[2] is not None:
                            ins.then_inc(rec[2], rec[3])
            return run
        block.tensor(replay(self.engs["pe"]))
        block.scalar(replay(self.engs["act"]))
        block.vector(replay(self.engs["dve"]))
        block.gpsimd(replay(self.engs["pool"]))
        block.sync(replay(self.engs["sp"]))


D = 1024
S_LEN = 2048
NB = 8
DEPTH = 2
DFF = 2816
NFC = DFF // 128
NKC = D // 128
NT = S_LEN // 512
NT128 = S_LEN // 128
RMS_EPS = 1e-6
LN_EPS = 1e-5
N_IN = 7176
FFN_GROUPS = [(0, 6), (6, 12), (12, 17), (17, 22)]

W_X = 0
W_H = W_X + 16384
W_CONST = W_H + 8192
W_PT = W_CONST + 3072
W_W = W_PT + 1024
W_N = W_W + 9216
W_END = W_N + 12288
ARENA_WORDS = W_END


class K:
    def __init__(self, nc, S, arena, ps, dram):
        self.nc, self.S, self.arena, self.ps, self.d = nc, S, arena, ps, dram
        self.ps_rr = 0

    def f32(self, off, n):
        return self.arena[:, off:off + n]

    def bf(self, off, n):
        return self.arena[:, off:off + (n + 1) // 2].bitcast(BF16)

    def bank(self, b):
        return self.ps[:, b * 512:(b + 1) * 512]

    def X(self, c, t):
        o = W_X + c * S_LEN + t * 512
        return self.arena[:, o:o + 512]

    def H(self, c, t0=0, n=S_LEN):
        return self.bf(W_H + c * (S_LEN // 2), S_LEN)[:, t0:t0 + n]

    def PT(self, i):
        return self.bf(W_PT + i * 256, 512)


def build_consts(k):
    S, d = k.S, k.d
    o = W_CONST
    k.ident = k.f32(o, 128); o += 128
    k.ones_bf = k.bf(o, 128); o += 64
    k.tri_bf = k.bf(o, 128); o += 64
    k.normw = k.f32(o, 7 * 8); o += 56
    k.const_end = o
    S.dma("sp", "c0", lambda e: e.dma_start(out=k.ident, in_=d["ident"][:, :]), writes=["ident"])
    S.dma("pool", "c1", lambda e: e.dma_start(out=k.ones_bf, in_=d["ones"][:, :]), writes=["ones"])
    S.dma("pool", "c1", lambda e: e.dma_start(out=k.tri_bf, in_=d["tri"][:, :]), writes=["tri"])
    S.dma("sp", "c0", lambda e: e.dma_start(out=k.normw, in_=d["normw"][:, :]), writes=["normw"])


def load_x(k):
    S, d = k.S, k.d
    for c in range(NKC):
        o = W_X + c * S_LEN
        S.dma("sp", "xld", lambda e, c=c, o=o: e.dma_start(out=k.arena[:, o:o + S_LEN], in_=d["xT"][:, c, :]),
              writes=[("X", c, t) for t in range(NT)])


def rmsnorm(k, widx, out_mode):
    S = k.S
    R0 = W_N + 8192
    for t in range(NT):
        for c in range(NKC):
            pt = k.PT((t * NKC + c) % 4)
            key = ("PT", (t * NKC + c) % 4)
            if c % 2 == 0:
                S.op("act", lambda e, pt=pt, c=c, t=t: e.activation(out=pt, in_=k.X(c, t), func=AF.Square),
                     reads=[("X", c, t)], writes=[key])
            else:
                S.op("dve", lambda e, pt=pt, c=c, t=t: e.tensor_tensor(out=pt, in0=k.X(c, t), in1=k.X(c, t), op=ALU.mult),
                     reads=[("X", c, t)], writes=[key])
            S.op("pe", lambda e, pt=pt, c=c, t=t: e.matmul(k.bank(t), k.ones_bf, pt, start=(c == 0), stop=(c == NKC - 1)),
                 reads=[key, "ones"], writes=[("ps", t)])
    for t in range(NT):
        R = k.f32(R0 + t * 512, 512)
        S.op("act", lambda e, R=R, t=t: e.activation(out=R, in_=k.bank(t), func=AF.Ln, bias=RMS_EPS, scale=1.0 / D),
             reads=[("ps", t)], writes=[("R", t)])
        S.op("act", lambda e, R=R: e.activation(out=R, in_=R, func=AF.Exp, scale=-0.5), reads=[("R", t)], writes=[("R", t)])
        for c in range(NKC):
            wcol = k.normw[:, widx * 8 + c:widx * 8 + c + 1]
            if out_mode == "H":
                S.op("dve", lambda e, R=R, c=c, t=t, wcol=wcol: e.scalar_tensor_tensor(
                    out=k.H(c, t * 512, 512), in0=k.X(c, t), scalar=wcol, in1=R, op0=ALU.mult, op1=ALU.mult),
                    reads=[("X", c, t), ("R", t), "normw"], writes=[("H", c, t)])
            else:
                S.op("dve", lambda e, R=R, c=c, t=t, wcol=wcol: e.scalar_tensor_tensor(
                    out=k.X(c, t), in0=k.X(c, t), scalar=wcol, in1=R, op0=ALU.mult, op1=ALU.mult),
                    reads=[("X", c, t), ("R", t), "normw"], writes=[("X", c, t)])


def ffn(k, l, which):
    S, d = k.S, k.d
    gu_d = d[f"ffn{which}_gu"]
    wd_d = d[f"ffn{which}_wd"]
    GU = [k.bf(W_W + i * 1024, 2048) for i in range(3)]
    WD = [k.bf(W_W + 3072 + i * 3072, 6 * 1024) for i in range(2)]
    A = [k.bf(W_N + i * 1024, 2048) for i in range(7)]
    SG = [k.f32(W_N + 7168 + i * 512, 512) for i in range(2)]

    def load_gu(c):
        s = c % 3
        S.dma("pool", f"gu{s}", lambda e: e.dma_start(out=GU[s], in_=gu_d[l, c, :, :]), writes=[("GU", s)])

    def load_wd(g):
        s = g % 2
        c0, c1 = FFN_GROUPS[g]
        for c in range(c0, c1):
            S.dma("pool", f"wd{s}", lambda e, c=c: e.dma_start(out=WD[s][:, (c - c0) * 1024:(c - c0 + 1) * 1024], in_=wd_d[l, c, :, :]),
                  writes=[("WD", s)])

    def gu_chunk(c):
        s = c % 3
        for t in range(NT):
            pr = (c * NT + t) % 3
            bg, bu = 2 * pr, 2 * pr + 1
            for g, b in ((0, bg), (1, bu)):
                for kc in range(NKC):
                    S.op("pe", lambda e, g=g, b=b, kc=kc, t=t: e.matmul(
                        k.bank(b), GU[s][:, (g * 8 + kc) * 128:(g * 8 + kc + 1) * 128], k.H(kc, t * 512, 512),
                        start=(kc == 0), stop=(kc == NKC - 1)),
                        reads=[("GU", s), ("H", kc, t)], writes=[("ps", b)])
            sg = SG[(c * NT + t) % 2]
            sgk = ("SG", (c * NT + t) % 2)
            S.op("act", lambda e, sg=sg, bg=bg: e.activation(out=sg, in_=k.bank(bg), func=AF.Silu),
                 reads=[("ps", bg)], writes=[sgk])
            S.op("dve", lambda e, sg=sg, bu=bu, t=t: e.tensor_tensor(
                out=A[c % 7][:, t * 512:(t + 1) * 512], in0=sg, in1=k.bank(bu), op=ALU.mult),
                reads=[sgk, ("ps", bu)], writes=[("A", c % 7, t)])

    dn_cnt = [0]

    def down(g):
        s = g % 2
        c0, c1 = FFN_GROUPS[g]
        for t in range(NT):
            for dd in range(NKC):
                b = 6 + dn_cnt[0] % 2
                dn_cnt[0] += 1
                for c in range(c0, c1):
                    S.op("pe", lambda e, b=b, c=c, dd=dd, t=t: e.matmul(
                        k.bank(b), WD[s][:, (c - c0) * 1024 + dd * 128:(c - c0) * 1024 + (dd + 1) * 128],
                        A[c % 7][:, t * 512:(t + 1) * 512], start=(c == c0), stop=(c == c1 - 1)),
                        reads=[("WD", s), ("A", c % 7, t)], writes=[("ps", b)])
                S.op("dve", lambda e, b=b, dd=dd, t=t: e.scalar_tensor_tensor(
                    out=k.X(dd, t), in0=k.bank(b), scalar=0.5, in1=k.X(dd, t), op0=ALU.mult, op1=ALU.add),
                    reads=[("ps", b), ("X", dd, t)], writes=[("X", dd, t)])

    for c in range(3):
        load_gu(c)
    load_wd(0)
    load_wd(1)
    grp_of = {}
    for g, (c0, c1) in enumerate(FFN_GROUPS):
        for c in range(c0, c1):
            grp_of[c] = g
    for c in range(NFC):
        gu_chunk(c)
        if c + 3 < NFC:
            load_gu(c + 3)
        g = grp_of[c]
        if c == FFN_GROUPS[g][0] and g > 0:
            down(g - 1)
            if g + 1 < len(FFN_GROUPS):
                load_wd(g + 1)
    down(len(FFN_GROUPS) - 1)


def store_out(k):
    S, d = k.S, k.d
    for c in range(NKC):
        o = W_X + c * S_LEN
        S.dma("sp", "ost", lambda e, c=c, o=o: e.dma_start(out=d["outT"][:, c, :], in_=k.arena[:, o:o + S_LEN]),
              reads=[("X", c, t) for t in range(NT)])
    S.wait_all_dma("sp", ["ost"])


U = 4096
W_SCR = W_END + 64
ARENA_WORDS = W_END + 2560
SB = W_SCR + 64
NEG_BIG = -30000.0
WARM_FILL = 0
C_DQ, C_DK, C_DV = 0, 512, 1024
C_FQ, C_FK, C_FV, C_FF = 1536, 2048, 2560, 3072
C_MX, C_MZ, C_G = 3080, 3592, 4104


def DU(i):
    return W_X + i * U


def NU(i):
    return W_N + i * U


def fm(k, off, c, t0=0, n=S_LEN):
    return k.bf(off + c * (S_LEN // 2), S_LEN)[:, t0:t0 + n]


def next_bank(k):
    b = k.ps_rr % 8
    k.ps_rr += 1
    return b


def col(k):
    i = getattr(k, "_col_rr", 0)
    k._col_rr = i + 1
    i %= 48
    return k.arena[:, W_SCR + i:W_SCR + i + 1], ("col", i)


def evac(k, dst, src, src_key, dst_key, scale=None, func=None, eng=None):
    S = k.S
    if eng is None:
        k._ev_rr = getattr(k, "_ev_rr", 0) + 1
        eng = "act" if (func is not None or k._ev_rr % 2 == 0) else "dve"
    if eng == "act":
        f = func if func is not None else AF.Copy
        sc = 1.0 if scale is None else scale
        S.op("act", lambda e: e.activation(out=dst, in_=src, func=f, scale=sc), reads=[src_key], writes=[dst_key])
    else:
        if scale is None:
            S.op("dve", lambda e: e.tensor_copy(dst, src), reads=[src_key], writes=[dst_key])
        else:
            S.op("dve", lambda e: e.tensor_scalar(dst, src, scale, None, ALU.mult), reads=[src_key], writes=[dst_key])


def wcol_jobs(k, l, jobs):
    S, d = k.S, k.d

    def load(i):
        c0, nc_, _ = jobs[i]
        s = i % 2
        tile = k.bf(W_W + s * 2048, 4096).rearrange("p (k c) -> p k c", k=8)
        S.dma("pool", f"wc{s}", lambda e: e.dma_start(out=tile[:, :, 0:nc_], in_=d["w_in"][l, :, :, c0:c0 + nc_]),
              writes=[("WC", s)])
        return tile

    tiles = {}
    for i in range(min(2, len(jobs))):
        tiles[i] = load(i)
    for i in range(len(jobs)):
        jobs[i][2](tiles[i], ("WC", i % 2))
        if i + 2 < len(jobs):
            tiles[i + 2] = load(i + 2)


def h_fm(dst_off, scale=None, func=None):
    def mk(k, c_base, nchunks=4):
        def handler(W, wkey):
            S = k.S
            for m in range(nchunks):
                for t in range(NT):
                    b = next_bank(k)
                    for kc in range(NKC):
                        S.op("pe", lambda e, b=b, m=m, kc=kc, t=t: e.matmul(
                            k.bank(b), W[:, kc, m * 128:(m + 1) * 128], k.H(kc, t * 512, 512),
                            start=(kc == 0), stop=(kc == NKC - 1)),
                            reads=[wkey, ("H", kc, t)], writes=[("ps", b)])
                    evac(k, fm(k, dst_off, c_base + m, t * 512, 512), k.bank(b), ("ps", b),
                         ("fm", dst_off, c_base + m, t), scale=scale, func=func)
        return handler
    return mk


def h_tm(k, vbuf_fn, nh, dv):
    def handler(W, wkey):
        S = k.S
        for tt in range(NT128):
            b = next_bank(k)
            for kc in range(NKC):
                S.op("pe", lambda e, b=b, kc=kc, tt=tt: e.matmul(
                    k.bank(b), k.H(kc, tt * 128, 128), W[:, kc, 0:512],
                    start=(kc == 0), stop=(kc == NKC - 1)),
                    reads=[wkey, ("H", kc, tt // 4)], writes=[("ps", b)])
            dst = vbuf_fn(tt)[:, :, 0:dv]
            src = k.bank(b).rearrange("p (h e) -> p h e", h=nh)
            evac(k, dst, src, ("ps", b), ("V", tt))
    return handler


class AttnPass:
    def __init__(self, k, *, qf, kf, aqf, akf, bias_fn, vf, vkey, dv, mode, fin, sbanks, obanks, scale=1.0, augkey=None, pre=None, act_func=None, scale_fn=None, mask_mul=False):
        self.__dict__.update(locals())
        self.state = {}
        self.per_bank = 2 if dv == 128 else 4

    def blocks(self):
        return [(self, j, i) for j in range(4) for i in range(4 * j + 4)]

    def O(self, j, r):
        pb = self.per_bank
        ob = self.obanks[j % len(self.obanks)][r // pb]
        c0 = (r % pb) * (self.dv + 1)
        return self.k.bank(ob)[:, c0:c0 + self.dv + 1], ("ps", ob)

    def emit_s(self, j, i):
        k, S = self.k, self.k.S
        if self.pre is not None:
            pl = self.pre if isinstance(self.pre, list) else [self.pre]
            self.pre = None
            for f_ in pl:
                f_()
        r0 = max(0, i - 4 * j)
        n0 = r0 * 128
        ncol = 512 - n0
        diag = i >= 4 * j
        k._blk = getattr(k, "_blk", 0) + 1
        pti = k._blk % 4
        pt = k.PT(pti)
        ptk = ("PT", pti)
        if self.mode == "exp":
            sb = self.sbanks[k._blk % len(self.sbanks)]
            has_aug = self.akf is not None
            for rep in range(1 + WARM_FILL):
                real = (rep == WARM_FILL)
                S.op("pe", lambda e, real=real: e.matmul(k.bank(sb)[:, n0:512], self.kf(i), self.qf(j * 512 + n0, ncol),
                                                         start=True, stop=((not has_aug and (not diag or self.mask_mul)) or not real)),
                     reads=([self.augkey] if (self.augkey is not None and not has_aug) else []), writes=[("ps", sb)])
            if has_aug:
                S.op("pe", lambda e: e.matmul(k.bank(sb)[:, n0:512], self.akf(i), self.aqf(j, n0, ncol),
                                              start=False, stop=(not diag)),
                     reads=[self.augkey], writes=[("ps", sb)])
            if diag and not self.mask_mul:
                S.op("pe", lambda e: e.matmul(k.bank(sb)[:, n0:n0 + 128], k.ident_bf, k.negtri_bf, start=False, stop=True),
                     reads=["identbf", "negtri"], writes=[("ps", sb)])
            bias = float(self.bias_fn(i, j)) if self.bias_fn is not None else 0.0
            if self.act_func is None:
                S.op("act", lambda e: e.activation(out=pt[:, n0:512], in_=k.bank(sb)[:, n0:512], func=AF.Exp, bias=bias, scale=1.0),
                     reads=[("ps", sb)], writes=[ptk])
            else:
                sc_ap = self.scale_fn(i, j)
                S.op("act", lambda e: e.activation(out=pt[:, n0:512], in_=k.bank(sb)[:, n0:512], func=self.act_func, scale=sc_ap),
                     reads=[("ps", sb), "UC"], writes=[ptk])
            if diag and self.mask_mul:
                S.op("pool", lambda e: e.tensor_tensor(out=pt[:, n0:n0 + 128], in0=pt[:, n0:n0 + 128], in1=k.tri_bf, op=ALU.mult),
                     reads=[ptk, "tri"], writes=[ptk])
        else:
            pr = self.sbanks[k._blk % len(self.sbanks)]
            sb, eb = pr
            S.op("pe", lambda e: e.matmul(k.bank(sb)[:, n0:512], self.kf(i), self.qf(j * 512 + n0, ncol), start=True, stop=True),
                 reads=[], writes=[("ps", sb)])
            S.op("pe", lambda e: e.matmul(k.bank(eb)[:, n0:512], self.akf(i), self.aqf(j, n0, ncol), start=True, stop=(not diag)),
                 reads=[self.augkey], writes=[("ps", eb)])
            if diag:
                S.op("pe", lambda e: e.matmul(k.bank(eb)[:, n0:n0 + 128], k.ident_bf, k.negtri_bf, start=False, stop=True),
                     reads=["identbf", "negtri"], writes=[("ps", eb)])
            eti = k._blk % 2
            et = k.f32(SB + eti * 512, 512)
            S.op("act", lambda e: e.activation(out=et[:, n0:512], in_=k.bank(eb)[:, n0:512], func=AF.Exp),
                 reads=[("ps", eb)], writes=[("ET", eti)])
            sc = self.scale
            S.op("dve", lambda e: e.scalar_tensor_tensor(out=pt[:, n0:512], in0=k.bank(sb)[:, n0:512], scalar=sc,
                                                         in1=et[:, n0:512], op0=ALU.mult, op1=ALU.mult),
                 reads=[("ps", sb), ("ET", eti)], writes=[ptk])
        self.state[(j, i)] = (pt, ptk, r0)

    def emit_pv(self, j, i):
        k, S = self.k, self.k.S
        pt, ptk, r0 = self.state.pop((j, i))
        for r in range(r0, 4):
            o, okey = self.O(j, r)
            last = (i == 4 * j + r)
            S.op("pe", lambda e, r=r, o=o, last=last: e.matmul(o, pt[:, r * 128:(r + 1) * 128], self.vf(i), start=(i == 0 and r % self.per_bank == 0), stop=last, skip_group_check=True),
                 reads=[ptk, self.vkey], writes=[okey])
            if last:
                self.fin(j, r, o, okey)


def run_blocks(k, blocks, L=2):
    n = len(blocks)
    for idx in range(n + L):
        if idx < n:
            p, j, i = blocks[idx]
            p.emit_s(j, i)
        if idx >= L:
            p, j, i = blocks[idx - L]
            p.emit_pv(j, i)
        tick(k)
    flush_deferred(k)


def defer(k, n, fn):
    k._dq = getattr(k, "_dq", [])
    k._dq.append([n, fn])


def tick(k):
    dq = getattr(k, "_dq", [])
    k._dq = []
    keep = []
    for it in dq:
        it[0] -= 1
        if it[0] <= 0:
            it[1]()
        else:
            keep.append(it)
    k._dq = keep + k._dq


def flush_deferred(k):
    while getattr(k, "_dq", []):
        dq = k._dq
        k._dq = []
        for it in dq:
            it[1]()


def transpose_to_fm(k, src, src_key, dst, dst_key, post=None):
    S = k.S
    q = getattr(k, "_tq", 0)
    k._tq = q + 1
    tb = k.tbanks[q % len(k.tbanks)]
    pq = k.bank(tb)[:, 0:128]
    pkey = ("ps", tb)
    S.op("pe", lambda e: e.transpose(pq, src, k.ident), reads=[src_key, "ident"], writes=[pkey])
    if post is None:
        evac(k, dst, pq, pkey, dst_key, eng="act")
    else:
        post(pq, pkey)


def softplus_neg(k, T, nh, banks, negb_col, key):
    S = k.S
    for t in range(NT):
        b = banks[t]
        S.op("act", lambda e, b=b, t=t: e.activation(out=T[0:nh, t * 512:(t + 1) * 512], in_=k.bank(b)[0:nh, :],
                                                    func=AF.Exp, bias=negb_col, scale=-1.0),
             reads=[("ps", b), "gcols"], writes=[key])
    S.op("act", lambda e: e.activation(out=T[0:nh, :], in_=T[0:nh, :], func=AF.Ln, bias=1.0, scale=1.0),
         reads=[key], writes=[key])


def build_aug(k, kvec, kkey, qvec, qkey, nh, spl_off, tag):
    S, d = k.S, k.d
    dk, dq = d["augk_" + tag], d["augq_" + tag]
    SPL = [k.bf(spl_off + i * 1024, 2048) for i in range(4)]
    ones = SPL[3]
    S.op("dve", lambda e: e.memset(ones[0:nh, :], 1.0), writes=[("SPL", 3)])
    for r in range(3):
        S.dma("sp", "augw", lambda e, r=r: e.dma_start(out=dk[0:nh, 3 + r, :], in_=ones[0:nh, :]), reads=[("SPL", 3)], writes=[("augd", tag)])
        S.dma("sp", "augw", lambda e, r=r: e.dma_start(out=dq[0:nh, r, :], in_=ones[0:nh, :]), reads=[("SPL", 3)], writes=[("augd", tag)])
    cnt = 0
    for vec, vkey, dram, r0 in ((kvec, kkey, dk, 0), (qvec, qkey, dq, 3)):
        for r in range(3):
            si = cnt % 3
            cnt += 1
            spl = SPL[si]
            S.op("dve", lambda e, spl=spl, vec=vec: e.tensor_copy(spl[0:nh, :], vec[0:nh, :]), reads=[vkey], writes=[("SPL", si)])
            if r < 2:
                S.op("dve", lambda e, spl=spl, vec=vec: e.tensor_tensor(out=vec[0:nh, :], in0=vec[0:nh, :], in1=spl[0:nh, :], op=ALU.subtract),
                     reads=[vkey, ("SPL", si)], writes=[vkey])
            S.dma("sp", "augw", lambda e, spl=spl, dram=dram, rr=r0 + r: e.dma_start(out=dram[0:nh, rr, :], in_=spl[0:nh, :]),
                  reads=[("SPL", si)], writes=[("augd", tag)])


def aug_views(k, slot):
    ak = k.bf(W_W + 5120 + slot * 2048, 2048)
    aq = k.bf(W_W + 5120 + slot * 2048 + 1024, 2048)
    akf = lambda i: ak[0:6, i * 128:(i + 1) * 128]
    aqf = lambda j, n0, n: aq[0:6, j * 512 + n0:j * 512 + n0 + n]
    return akf, aqf


def load_aug(k, tag, h, slot):
    S, d = k.S, k.d
    ak = k.bf(W_W + 5120 + slot * 2048, 2048)
    aq = k.bf(W_W + 5120 + slot * 2048 + 1024, 2048)
    S.dma("sp", f"augl{slot}", lambda e: e.dma_start(out=ak[0:6, :], in_=d["augk_" + tag][h, :, :]), reads=[("augd", tag)], writes=[("AUGS", slot)])
    S.dma("sp", f"augl{slot}", lambda e: e.dma_start(out=aq[0:6, :], in_=d["augq_" + tag][h, :, :]), reads=[("augd", tag)], writes=[("AUGS", slot)])


HB_OFF = [W_W + 0, W_W + 2048, W_W + 5120, W_W + 7168]


def hb_views(k, slot):
    return k.bf(HB_OFF[slot], 2048), k.bf(HB_OFF[slot] + 1024, 2048)


def hb_init(k):
    S = k.S
    for s_ in range(4):
        qb, kb = hb_views(k, s_)
        S.op("act", lambda e, qb=qb: e.memzero(qb), writes=[("HB", s_)])
        S.op("act", lambda e, kb=kb: e.memzero(kb), writes=[("HB", s_)])


def hb_build(k, slot, par, q_src, k_src, augq_ap, augk_ap, R, queue):
    S = k.S
    qb, kb = hb_views(k, slot)
    r0 = par * 64
    a0 = 64 if par == 0 else 0
    S.dma("sp", f"hbsp{slot}", lambda e: e.dma_start(out=qb[r0:r0 + 64, :], in_=q_src[r0:r0 + 64, :]), writes=[("HB", slot)])
    S.dma("sp", f"hbsp{slot}", lambda e: e.dma_start(out=kb[r0:r0 + 64, :], in_=k_src[r0:r0 + 64, :]), writes=[("HB", slot)])
    S.dma(queue, f"hb{queue}{slot}", lambda e: e.dma_start(out=qb[a0:a0 + R, :], in_=augq_ap), writes=[("HB", slot)])
    S.dma(queue, f"hb{queue}{slot}", lambda e: e.dma_start(out=kb[a0:a0 + R, :], in_=augk_ap), writes=[("HB", slot)])


def layer_consts(k, l):
    S, d = k.S, k.d
    o = k.const_end
    k.gb = k.f32(o, 24); o += 24
    k.convw = k.f32(o, 16); o += 16
    k.convb = k.f32(o, 4); o += 4
    k.skipc = k.f32(o, 4); o += 4
    k.normc = k.f32(o, 4); o += 4
    k.gcols = k.f32(o, 8); o += 8
    k.lamt = k.f32(o, 256); o += 256
    k.lamc = k.f32(o, 8); o += 8
    k.subln = k.f32(o, 128); o += 128
    k.normB = k.f32(o, 512); o += 512
    k.AKd = k.bf(o, 4 * 128); o += 256
    k.AQd = k.bf(o, 4 * 512); o += 1024
    k.ident_bf = k.bf(o, 128); o += 64
    k.negtri_bf = k.bf(o, 128); o += 64
    assert o <= W_CONST + 3072, o
    S.dma("sp", "lc", lambda e: e.dma_start(out=k.gb, in_=d["gbcols"][l, :, :]), writes=["lconst"])
    S.dma("sp", "lc", lambda e: e.dma_start(out=k.convw, in_=d["convw"][l, :, :]), writes=["lconst"])
    S.dma("sp", "lc", lambda e: e.dma_start(out=k.convb, in_=d["convb"][l, :, :]), writes=["lconst"])
    S.dma("sp", "lc", lambda e: e.dma_start(out=k.skipc, in_=d["skipc"][l, :, :]), writes=["lconst"])
    S.dma("sp", "lc", lambda e: e.dma_start(out=k.normc, in_=d["normc"][l, :, :]), writes=["lconst"])
    S.dma("sp", "lc", lambda e: e.dma_start(out=k.gcols[0:8, :], in_=d["gcols"][l, :, :]), writes=["gcols"])
    S.dma("sp", "lc", lambda e: e.dma_start(out=k.lamt, in_=d["difflam"][l:l + 1, :].partition_broadcast(128)), writes=["lamt"])
    S.dma("sp", "lc", lambda e: e.dma_start(out=k.subln, in_=d["diff_subln"][l:l + 1, :].partition_broadcast(128)), writes=["subln"])
    S.dma("sp", "lc", lambda e: e.dma_start(out=k.normB, in_=d["mlstm_norm"][l:l + 1, :].partition_broadcast(128)), writes=["normB"])
    S.op("dve", lambda e: e.tensor_scalar(k.gcols[0:8, 4:5], k.gcols[0:8, 0:1], -1.0, None, ALU.mult), reads=["gcols"], writes=["gcols"])
    S.op("dve", lambda e: e.tensor_scalar(k.gcols[0:8, 5:6], k.gcols[0:8, 2:3], -1.0, None, ALU.mult), reads=["gcols"], writes=["gcols"])
    if l == 0:
        S.dma("pool", "lc2", lambda e: e.dma_start(out=k.AKd[0:3, :], in_=d["alibi_k"][:, :]), writes=["alibi"])
        S.dma("pool", "lc2", lambda e: e.dma_start(out=k.AQd[0:3, :], in_=d["alibi_q"][:, :]), writes=["alibi"])
        S.dma("pool", "lc2", lambda e: e.dma_start(out=k.ident_bf, in_=d["ident"][:, :]), writes=["identbf"])
        S.dma("pool", "lc2", lambda e: e.dma_start(out=k.negtri_bf, in_=d["negtri"][:, :]), writes=["negtri"])
    import math
    lam_init = 0.8 - 0.6 * math.exp(-0.3 * l)
    lt, lc = k.lamt, k.lamc
    S.op("dve", lambda e: e.tensor_tensor(out=lt[:, 0:64], in0=lt[:, 0:64], in1=lt[:, 64:128], op=ALU.mult), reads=["lamt"], writes=["lamt"])
    S.op("dve", lambda e: e.tensor_tensor(out=lt[:, 128:192], in0=lt[:, 128:192], in1=lt[:, 192:256], op=ALU.mult), reads=["lamt"], writes=["lamt"])
    S.op("dve", lambda e: e.reduce_sum(out=lc[:, 0:1], in_=lt[:, 0:64], axis=AX.X), reads=["lamt"], writes=["lamc"])
    S.op("dve", lambda e: e.reduce_sum(out=lc[:, 1:2], in_=lt[:, 128:192], axis=AX.X), reads=["lamt"], writes=["lamc"])
    S.op("act", lambda e: e.activation(out=lc[:, 2:4], in_=lc[:, 0:2], func=AF.Exp), reads=["lamc"], writes=["lamc"])
    S.op("dve", lambda e: e.tensor_tensor(out=lc[:, 4:5], in0=lc[:, 3:4], in1=lc[:, 2:3], op=ALU.subtract), reads=["lamc"], writes=["lamc"])
    S.op("dve", lambda e: e.tensor_scalar(lc[:, 5:6], lc[:, 4:5], -lam_init, None, ALU.add), reads=["lamc"], writes=["neglam"])
    S.op("dve", lambda e: e.tensor_scalar(k.subln, k.subln, 1.0 - lam_init, None, ALU.mult), reads=["subln"], writes=["subln"])
    k.neglam = lc[:, 5:6]


def diff_branch(k, l):
    S = k.S
    QT, KT, VO, OD = DU(0), DU(1), DU(2), NU(0)
    V = k.bf(VO, 16 * 4 * 129).rearrange("p (t h e) -> p t h e", t=16, h=4)
    S.op("dve", lambda e: e.memset(k.bf(VO, 16 * 4 * 129), 1.0), writes=[("V", "all")])
    S.barrier()
    wcol_jobs(k, l, [
        (C_DQ, 512, h_fm(QT)(k, 0)),
        (C_DK, 512, h_fm(KT, scale=0.125)(k, 0)),
        (C_DV, 512, h_tm(k, lambda tt: V[:, tt, :, :], 4, 128)),
    ])
    S.barrier()
    k.tbanks = [3]
    slopes = [2.0 ** (-8.0 * (h + 1) / 4) for h in range(4)]
    A1 = [[k.f32(SB + (p * 4 + r) * 128, 128) for r in range(4)] for p in range(2)]
    TB = SB + 1024
    blocks = []
    pres = []
    first_pass = []
    hb_init(k)
    for h in range(4):
        passes = []
        for c in range(2):
            slot = c + 2 * (h % 2)
            qb, kb = hb_views(k, slot)

            def qf(t0, n, qb=qb):
                return qb[:, t0:t0 + n]

            def kf(i, kb=kb):
                return kb[:, i * 128:(i + 1) * 128]

            pres.append(lambda h=h, c=c, slot=slot: hb_build(k, slot, c, fm(k, QT, h), fm(k, KT, h),
                                                              k.d["alibi_qf"][h, :, :], k.d["alibi_kf"][h, :, :], 3, "pool"))

            def bias_fn(i, j, h=h):
                return slopes[h] * (128.0 * i - 512.0 * j)

            def vf(i, h=h):
                return V[:, i, h, :]

            if c == 0:
                def fin(j, r, o, okey, h=h):
                    rc, rk = col(k)
                    a1 = A1[j % 2][r]
                    S.op("dve", lambda e: e.reciprocal(rc, o[:, 128:129]), reads=[okey], writes=[rk])
                    S.op("dve", lambda e: e.tensor_scalar(a1, o[:, 0:128], rc, None, ALU.mult), reads=[okey, rk], writes=[("A1", j % 2, r)])
            else:
                def fin(j, r, o, okey, h=h):
                    rc, rk = col(k)
                    sc, sk = col(k)
                    k._fp = getattr(k, "_fp", 0) + 1
                    n = k._fp
                    tmp = k.f32(TB + (n % 5) * 128, 128)
                    sq = k.f32(TB + 640 + (n % 2) * 128, 128)
                    ot = k.f32(TB + 896 + (n % 4) * 128, 128)
                    tk_, sqk, otk = ("TMP", n % 5), ("SQ", n % 2), ("OT", n % 4)
                    a1 = A1[j % 2][r]
                    tt = 4 * j + r
                    S.op("dve", lambda e: e.reciprocal(rc, o[:, 128:129]), reads=[okey], writes=[rk])
                    S.op("dve", lambda e: e.tensor_scalar(tmp, o[:, 0:128], rc, None, ALU.mult), reads=[okey, rk], writes=[tk_])
                    S.op("dve", lambda e: e.scalar_tensor_tensor(out=tmp, in0=tmp, scalar=k.neglam, in1=a1, op0=ALU.mult, op1=ALU.add),
                         reads=[tk_, ("A1", j % 2, r), "neglam"], writes=[tk_])
                    S.op("dve", lambda e: e.tensor_tensor(out=sq, in0=tmp, in1=tmp, op=ALU.mult), reads=[tk_], writes=[sqk])
                    S.op("dve", lambda e: e.reduce_sum(out=sc, in_=sq, axis=AX.X), reads=[sqk], writes=[sk])

                    def stage_b():
                        S.op("act", lambda e: e.activation(out=sc, in_=sc, func=AF.Ln, bias=RMS_EPS, scale=1.0 / 128), reads=[sk], writes=[sk])
                        S.op("act", lambda e: e.activation(out=sc, in_=sc, func=AF.Exp, scale=-0.5), reads=[sk], writes=[sk])

                    def stage_c():
                        S.op("dve", lambda e: e.scalar_tensor_tensor(out=ot, in0=tmp, scalar=sc, in1=k.subln, op0=ALU.mult, op1=ALU.mult),
                             reads=[tk_, sk, "subln"], writes=[otk])
                        defer(k, 2, lambda: transpose_to_fm(k, ot, otk, fm(k, OD, h, tt * 128, 128), ("fm", OD, h, tt)))
                    defer(k, 2, stage_b)
                    defer(k, 3, stage_c)
            passes.append(AttnPass(k, qf=qf, kf=kf, aqf=None, akf=None, bias_fn=bias_fn, vf=vf, vkey=("V", "x"), dv=128,
                                   mode="exp", fin=fin, sbanks=[0, 1, 2], obanks=[(4, 5)] if c == 0 else [(6, 7)], augkey=("HB", slot)))
        first_pass.append(passes[0])
        b0, b1 = passes[0].blocks(), passes[1].blocks()
        for j in range(4):
            blocks += [b for b in b0 if b[1] == j]
            blocks += [b for b in b1 if b[1] == j]
    for u in range(4):
        first_pass[u].pre = (pres[0:4] if u == 0 else (pres[2 * u + 2:2 * u + 4] if u < 3 else None))
    run_blocks(k, blocks)
    S.barrier()


def fox_branch(k, l):
    S, d = k.S, k.d
    QT, KT, VO, OF = DU(0), DU(1), DU(2), NU(2)
    V = k.bf(VO, 16 * 8 * 65).rearrange("p (t h e) -> p t h e", t=16, h=8)
    WFF = k.bf(W_W + 4096, 64).rearrange("p (k c) -> p k c", k=8)
    S.op("dve", lambda e: e.memset(k.bf(VO, 16 * 8 * 65), 1.0), writes=[("V", "all")])
    S.dma("pool", "wff", lambda e: e.dma_start(out=WFF, in_=d["w_in"][l, :, :, C_FF:C_FF + 8]), writes=["WFF"])
    S.barrier()
    wcol_jobs(k, l, [
        (C_FQ, 512, h_fm(QT)(k, 0)),
        (C_FK, 512, h_fm(KT, scale=0.125)(k, 0)),
        (C_FV, 512, h_tm(k, lambda tt: V[:, tt, :, :], 8, 64)),
    ])
    for t in range(NT):
        for kc in range(NKC):
            S.op("pe", lambda e, kc=kc, t=t: e.matmul(k.bank(t)[0:8, :], WFF[:, kc, :], k.H(kc, t * 512, 512),
                                                      start=(kc == 0), stop=(kc == NKC - 1)),
                 reads=["WFF", ("H", kc, t)], writes=[("ps", t)])
    S.barrier()
    T1 = k.f32(W_W, 2048)
    T2 = k.f32(W_W + 2048, 2048)
    ONES = k.f32(W_W + 7168, 2048)
    S.op("dve", lambda e: e.memset(ONES[0:8, :], 1.0), writes=["ONES"])
    softplus_neg(k, T1, 8, [0, 1, 2, 3], k.gcols[0:8, 4:5], "T1")
    S.op("dve", lambda e: e.tensor_tensor_scan(T2[0:8, :], ONES[0:8, :], T1[0:8, :], 0.0, ALU.mult, ALU.add), reads=["T1", "ONES"], writes=["T2"])
    S.op("dve", lambda e: e.tensor_scalar(T1[0:8, :], T2[0:8, :], -1.0, None, ALU.mult), reads=["T2"], writes=["T1"])
    build_aug(k, T2, "T2", T1, "T1", 8, NU(2), "fox")
    S.barrier()
    k.tbanks = [3, 7]
    OTB = [[k.f32(SB + (p * 4 + r) * 128, 128) for r in range(4)] for p in range(2)]
    blocks = []
    pres = []
    first_pass = []
    hb_init(k)
    for pr in range(4):
        passes = []
        for c in range(2):
            hd = 2 * pr + c
            slot = c + 2 * (pr % 2)
            qb, kb = hb_views(k, slot)

            def qf(t0, n, qb=qb):
                return qb[:, t0:t0 + n]

            def kf(i, kb=kb):
                return kb[:, i * 128:(i + 1) * 128]

            pres.append(lambda pr=pr, c=c, slot=slot, hd=hd: hb_build(k, slot, c, fm(k, QT, pr), fm(k, KT, pr),
                                                                      k.d["augq_fox"][hd, :, :], k.d["augk_fox"][hd, :, :], 6, "sp"))

            def vf(i, hd=hd):
                return V[:, i, hd, :]

            def fin(j, r, o, okey, pr=pr, c=c):
                rc, rk = col(k)
                ot = OTB[j % 2][r]
                otk = ("OTB", j % 2, r)
                S.op("dve", lambda e: e.reciprocal(rc, o[:, 64:65]), reads=[okey], writes=[rk])
                S.op("dve", lambda e: e.tensor_scalar(ot[:, c * 64:(c + 1) * 64], o[:, 0:64], rc, None, ALU.mult), reads=[okey, rk], writes=[otk])
                if c == 1:
                    tt = 4 * j + r
                    defer(k, 3, lambda: transpose_to_fm(k, ot, otk, fm(k, OF, pr, tt * 128, 128), ("fm", OF, pr, tt)))
            passes.append(AttnPass(k, qf=qf, kf=kf, aqf=None, akf=None, bias_fn=None, vf=vf, vkey=("V", "x"), dv=64,
                                   mode="exp", fin=fin, sbanks=[0, 1, 2, 6], obanks=[(4,)] if c == 0 else [(5,)], augkey=("HB", slot)))
        first_pass.append(passes[0])
        b0, b1 = passes[0].blocks(), passes[1].blocks()
        for j in range(4):
            blocks += [b for b in b0 if b[1] == j]
            blocks += [b for b in b1 if b[1] == j]
    for u in range(4):
        first_pass[u].pre = (pres[0:4] if u == 0 else (pres[2 * u + 2:2 * u + 4] if u < 3 else None))
    run_blocks(k, blocks, L=3)
    S.barrier()


def mlstm_branch(k, l):
    S, d = k.S, k.d
    QT, KT, VT, MX, XC, SZ, VA = DU(0), DU(1), DU(2), DU(3), NU(0), NU(1), NU(2)
    Vall = k.bf(VA, 16 * 4 * 129).rearrange("p (t h e) -> p t h e", t=16, h=4)
    WQKV = k.bf(W_W + 4096, 1536).rearrange("p (g h e) -> p g h e", g=3, h=4)
    WIF = k.bf(W_W + 4096 + 768, 96).rearrange("p (j o) -> p j o", j=12)
    S.op("dve", lambda e: e.memset(k.bf(VA, 16 * 4 * 129), 1.0), writes=[("V", "all")])
    S.dma("pool", "wqkv", lambda e: e.dma_start(out=k.bf(W_W + 4096, 1536), in_=d["wqkv"][l, :, :]), writes=["WQKV"])
    S.dma("pool", "wqkv", lambda e: e.dma_start(out=k.bf(W_W + 4096 + 768, 96), in_=d["wif"][l, :, :]), writes=["WIF"])
    S.barrier()
    wcol_jobs(k, l, [
        (C_MX, 512, h_fm(MX)(k, 0)),
        (C_MZ, 512, h_fm(SZ, func=AF.Silu)(k, 0)),
    ])
    ACC = k.f32(SB, 2048)
    for c in range(4):
        mx = fm(k, MX, c)
        mkeys = [("fm", MX, c, t) for t in range(NT)]
        w = lambda j, c=c: k.convw[:, j * 4 + c:j * 4 + c + 1]
        S.op("dve", lambda e, mx=mx, c=c, w=w: e.tensor_scalar(ACC, mx, w(3), k.convb[:, c:c + 1], ALU.mult, ALU.add),
             reads=mkeys + ["lconst"], writes=["ACC"])
        for sh in (1, 2, 3):
            S.op("dve", lambda e, mx=mx, sh=sh, w=w: e.scalar_tensor_tensor(
                out=ACC[:, sh:], in0=mx[:, 0:S_LEN - sh], scalar=w(3 - sh), in1=ACC[:, sh:], op0=ALU.mult, op1=ALU.add),
                reads=mkeys + ["ACC", "lconst"], writes=["ACC"])
        S.op("act", lambda e, c=c: e.activation(out=fm(k, XC, c), in_=ACC, func=AF.Silu), reads=["ACC"],
             writes=[("fm", XC, c, t) for t in range(NT)])
    for h in range(4):
        for t in range(NT):
            for g, src, dst in ((0, XC, QT), (1, XC, KT), (2, MX, VT)):
                b = next_bank(k)
                S.op("pe", lambda e, b=b, g=g, src=src, h=h, t=t: e.matmul(
                    k.bank(b), WQKV[:, g, h, :], fm(k, src, h, t * 512, 512), start=True, stop=True),
                    reads=["WQKV", ("fm", src, h, t)], writes=[("ps", b)])
                evac(k, fm(k, dst, h, t * 512, 512), k.bank(b), ("ps", b), ("fm", dst, h, t))
        for tt in range(NT128):
            b = next_bank(k)
            S.op("pe", lambda e, b=b, h=h, tt=tt: e.matmul(
                k.bank(b)[:, 0:128], fm(k, MX, h, tt * 128, 128), WQKV[:, 2, h, :], start=True, stop=True),
                reads=["WQKV", ("fm", MX, h, tt // 4)], writes=[("ps", b)])
            evac(k, Vall[:, tt, h, 0:128], k.bank(b)[:, 0:128], ("ps", b), ("V", tt, h))
    for h in range(4):
        S.op("dve", lambda e, h=h: e.tensor_scalar(fm(k, XC, h), fm(k, XC, h), k.skipc[:, h:h + 1], None, ALU.mult),
             reads=[("fm", XC, h, t) for t in range(NT)] + ["lconst"], writes=[("fm", XC, h, t) for t in range(NT)])
    S.barrier()
    for t in range(NT):
        for g, bb in ((0, t), (1, 4 + t)):
            for jj in range(12):
                src = (QT, KT, VT)[jj // 4]
                S.op("pe", lambda e, g=g, bb=bb, jj=jj, src=src, t=t: e.matmul(
                    k.bank(bb)[0:4, :], WIF[:, jj, g * 4:(g + 1) * 4], fm(k, src, jj % 4, t * 512, 512),
                    start=(jj == 0), stop=(jj == 11)), reads=["WIF"], writes=[("ps", bb)])
    T1 = k.f32(W_W, 2048)
    T2 = k.f32(W_W + 2048, 2048)
    T3 = k.f32(W_W + 5120, 2048)
    ONES = k.f32(W_W + 7168, 2048)
    S.op("dve", lambda e: e.memset(ONES[0:4, :], 1.0), writes=["ONES"])
    softplus_neg(k, T1, 4, [4, 5, 6, 7], k.gcols[0:4, 5:6], "T1")
    S.op("dve", lambda e: e.tensor_tensor_scan(T2[0:4, :], ONES[0:4, :], T1[0:4, :], 0.0, ALU.mult, ALU.add), reads=["T1", "ONES"], writes=["T2"])
    for t in range(NT):
        S.op("dve", lambda e, t=t: e.tensor_scalar(T1[0:4, t * 512:(t + 1) * 512], k.bank(t)[0:4, :], k.gcols[0:4, 1:2], None, ALU.add),
             reads=[("ps", t), "gcols", "T2"], writes=["T1"])
    S.op("dve", lambda e: e.tensor_tensor(out=T1[0:4, :], in0=T1[0:4, :], in1=T2[0:4, :], op=ALU.add), reads=["T1", "T2"], writes=["T1"])
    S.op("dve", lambda e: e.tensor_tensor_scan(T3[0:4, :], ONES[0:4, :], T1[0:4, :], 0.0, ALU.mult, ALU.max), reads=["T1", "ONES"], writes=["T3"])
    S.op("dve", lambda e: e.tensor_scalar(T3[0:4, :], T3[0:4, :], -1.0, None, ALU.mult), reads=["T3"], writes=["T3"])
    UR = ONES
    EM = k.f32(W_CONST + 2700, 64)
    UC = k.f32(W_CONST + 2780, 160)
    for j in range(NT):
        nb = T3[0:4, 512 * j - 1:512 * j] if j > 0 else 0.0
        S.op("act", lambda e, j=j, nb=nb: e.activation(out=T2[0:4, j * 512:(j + 1) * 512], in_=T2[0:4, j * 512:(j + 1) * 512],
                                                     func=AF.Exp, bias=nb, scale=1.0), reads=["T2", "T3"], writes=["T2"])
    for tt in range(NT128):
        S.op("pe", lambda e, tt=tt: e.transpose(k.bank(3)[:, tt * 4:(tt + 1) * 4], T2[0:4, tt * 128:(tt + 1) * 128], k.ident[0:4, 0:4]),
             reads=["T2", "ident"], writes=[("ps", 3)])
    S.op("dve", lambda e: e.tensor_copy(EM, k.bank(3)[:, 0:64]), reads=[("ps", 3)], writes=["EM"])
    for j in range(NT):
        nb = T3[0:4, 512 * j - 1:512 * j] if j > 0 else 0.0
        ncol = (j + 1) * 512
        S.op("act", lambda e, nb=nb, ncol=ncol: e.activation(out=UR[0:4, 0:ncol], in_=T1[0:4, 0:ncol], func=AF.Exp, bias=nb, scale=1.0),
             reads=["T1", "T3", "ONES"], writes=["ONES"])
        for i in range(4 * j + 4):
            idx = 2 * j * (j + 1) + i
            S.op("pe", lambda e, i=i, idx=idx: e.transpose(k.bank(2)[:, idx * 4:(idx + 1) * 4], UR[0:4, i * 128:(i + 1) * 128], k.ident[0:4, 0:4]),
                 reads=["ONES", "ident"], writes=[("ps", 2)])
    S.op("dve", lambda e: e.tensor_scalar(UC, k.bank(2)[:, 0:160], 128.0 ** -0.5, None, ALU.mult), reads=[("ps", 2)], writes=["UC"])
    S.barrier()
    k.tbanks = [3, 7]
    TB = SB + 1024
    blocks = []
    for h in range(4):

        def qf(t0, n, h=h):
            return fm(k, QT, h, t0, n)

        def kf(i, h=h):
            return fm(k, KT, h, i * 128, 128)

        def vf(i, h=h):
            return Vall[:, i, h, :]

        def fin(j, r, o, okey, h=h):
            tt = 4 * j + r
            c1, k1 = col(k)
            c2, k2 = col(k)
            c3, k3 = col(k)
            k._fp = getattr(k, "_fp", 0) + 1
            n = k._fp
            NUM = k.f32(TB + (n % 4) * 128, 128)
            TF = k.f32(TB + 640 + (n % 2) * 128, 128)
            HN = k.f32(TB + 896 + (n % 4) * 128, 128)
            ST6 = k.f32(TB + 512 + (n % 8) * 8, 6)
            MV = k.f32(TB + 576 + (n % 8) * 4, 2)
            hk, nk, fk, stk, mvk = ("HH", n % 4), ("HN", n % 4), ("TF", n % 2), ("ST6", n % 8), ("MV", n % 8)
            den = o[:, 128:129]
            S.op("dve", lambda e: e.tensor_tensor(out=c1, in0=den, in1=EM[:, tt * 4 + h:tt * 4 + h + 1], op=ALU.max), reads=[okey, "EM"], writes=[k1])
            S.op("dve", lambda e: e.scalar_tensor_tensor(out=c1, in0=den, scalar=-1.0, in1=c1, op0=ALU.mult, op1=ALU.max), reads=[okey, k1], writes=[k1])
            S.op("dve", lambda e: e.reciprocal(c1, c1), reads=[k1], writes=[k1])
            S.op("dve", lambda e: e.tensor_scalar(NUM, o[:, 0:128], c1, None, ALU.mult), reads=[okey, k1], writes=[hk])
            S.op("dve", lambda e: e.bn_stats(ST6, NUM), reads=[hk], writes=[stk])
            S.op("dve", lambda e: e.bn_aggr(MV, ST6), reads=[stk], writes=[mvk])

            def stage_b():
                S.op("act", lambda e: e.activation(out=c2, in_=MV[:, 1:2], func=AF.Ln, bias=LN_EPS, scale=1.0), reads=[mvk], writes=[k2])
                S.op("act", lambda e: e.activation(out=c2, in_=c2, func=AF.Exp, scale=-0.5), reads=[k2], writes=[k2])

            def post(pq, pkey):
                xs = fm(k, XC, h, tt * 128, 128)
                sz = fm(k, SZ, h, tt * 128, 128)
                S.op("dve", lambda e: e.scalar_tensor_tensor(out=TF, in0=pq, scalar=k.normc[:, h:h + 1], in1=xs, op0=ALU.mult, op1=ALU.add),
                     reads=[pkey, "lconst"], writes=[fk])
                S.op("dve", lambda e: e.tensor_tensor(out=sz, in0=TF, in1=sz, op=ALU.mult), reads=[fk], writes=[("fm", SZ, h, tt)])

            def stage_c():
                S.op("dve", lambda e: e.tensor_scalar(HN, NUM, MV[:, 0:1], c2, ALU.subtract, ALU.mult), reads=[hk, mvk, k2], writes=[nk])
                defer(k, 2, lambda: transpose_to_fm(k, HN, nk, None, None, post=post))
            defer(k, 2, stage_b)
            defer(k, 3, stage_c)

        def scale_fn(i, j, h=h):
            idx = 2 * j * (j + 1) + i
            return UC[:, idx * 4 + h:idx * 4 + h + 1]
        ps_ = AttnPass(k, qf=qf, kf=kf, aqf=None, akf=None, bias_fn=None, vf=vf, vkey=("V", "x"), dv=128, mode="exp", fin=fin,
                       sbanks=[0, 1, 2, 6], obanks=[(4, 5)], act_func=AF.Copy, scale_fn=scale_fn, mask_mul=True)
        blocks += ps_.blocks()
    run_blocks(k, blocks, L=3)
    S.barrier()


def merge_and_out(k, l):
    S, d = k.S, k.d
    OB = [NU(0), NU(2), NU(1)]
    MG = DU(0)
    GT = [k.f32(SB + i * 512, 512) for i in range(2)]
    ACC = k.f32(SB + 1024, 512)
    PB = k.f32(SB + 1536, 512)

    def load_mp(dd):
        s = dd % 2
        tile = k.bf(W_W + s * 2304, 4608)
        S.dma("pool", f"mp{s}", lambda e: e.dma_start(out=tile, in_=d["mpack"][l, dd, :, :]), writes=[("MP", s)])
        return tile

    tiles = {0: load_mp(0), 1: load_mp(1)}
    cnt = 0
    for dd in range(NKC):
        MP = tiles[dd]
        mkey = ("MP", dd % 2)
        for t in range(NT):
            for b in range(3):
                bg, bo = next_bank(k), next_bank(k)
                for kc in range(NKC):
                    S.op("pe", lambda e, bg=bg, b=b, kc=kc, t=t, MP=MP: e.matmul(
                        k.bank(bg), MP[:, b * 1536 + kc * 128:b * 1536 + (kc + 1) * 128], k.H(kc, t * 512, 512),
                        start=(kc == 0), stop=(kc == NKC - 1)), reads=[mkey, ("H", kc, t)], writes=[("ps", bg)])
                for kc in range(4):
                    S.op("pe", lambda e, bo=bo, b=b, kc=kc, t=t, MP=MP: e.matmul(
                        k.bank(bo), MP[:, b * 1536 + 1024 + kc * 128:b * 1536 + 1024 + (kc + 1) * 128], fm(k, OB[b], kc, t * 512, 512),
                        start=(kc == 0), stop=(kc == 3)), reads=[mkey], writes=[("ps", bo)])
                gt = GT[cnt % 2]
                gk = ("GT", cnt % 2)
                cnt += 1
                gbc = k.gb[:, b * 8 + dd:b * 8 + dd + 1]
                S.op("act", lambda e, gt=gt, bg=bg, gbc=gbc: e.activation(out=gt, in_=k.bank(bg), func=AF.Sigmoid, bias=gbc, scale=1.0),
                     reads=[("ps", bg), "lconst"], writes=[gk])
                if b == 0:
                    S.op("dve", lambda e, gt=gt, bo=bo: e.tensor_tensor(out=ACC, in0=gt, in1=k.bank(bo), op=ALU.mult),
                         reads=[gk, ("ps", bo)], writes=["MACC"])
                else:
                    S.op("dve", lambda e, gt=gt, bo=bo: e.tensor_tensor(out=PB, in0=gt, in1=k.bank(bo), op=ALU.mult),
                         reads=[gk, ("ps", bo)], writes=["MPB"])
                    dst = ACC if b == 1 else fm(k, MG, dd, t * 512, 512)
                    dkey = "MACC" if b == 1 else ("fm", MG, dd, t)
                    S.op("dve", lambda e, dst=dst: e.tensor_tensor(out=dst, in0=ACC, in1=PB, op=ALU.add),
                         reads=["MACC", "MPB"], writes=[dkey] + (["MACC"] if b == 2 else []))
        if dd + 2 < NKC:
            tiles[dd + 2] = load_mp(dd + 2)
    S.barrier()
    MN = NU(0)
    for i in range(4):
        S.dma("sp", "mncp", lambda e, i=i: e.dma_start(out=k.bf(MN + i * 2048, 4096), in_=k.bf(MG + i * 2048, 4096)),
              writes=[("MN", i)])
    S.barrier()
    XS = [k.f32(SB + i * 512, 512) for i in range(2)]

    WOT = [k.bf(W_W + dd * 512, 1024) for dd in range(NKC)]
    for dd in range(NKC):
        S.dma("pool", "wo", lambda e, dd=dd: e.dma_start(out=WOT[dd], in_=d["wout"][l, dd, :, :]), writes=[("WO", dd)])
    cnt = 0
    for t in range(NT):
        for dd in range(NKC):
            WO = WOT[dd]
            b = next_bank(k)
            xs = XS[cnt % 2]
            xk = ("XS", cnt % 2)
            cnt += 1
            S.dma("sp", f"xs{cnt % 2}", lambda e, xs=xs, dd=dd, t=t: e.dma_start(out=xs, in_=d["xspill"][:, dd, t * 512:(t + 1) * 512]),
                  writes=[xk])
            for kc in range(NKC):
                S.op("pe", lambda e, b=b, kc=kc, t=t, WO=WO: e.matmul(
                    k.bank(b), WO[:, kc * 128:(kc + 1) * 128], fm(k, MN, kc, t * 512, 512),
                    start=(kc == 0), stop=(kc == NKC - 1)), reads=[("WO", dd)], writes=[("ps", b)])
            S.op("dve", lambda e, b=b, xs=xs, dd=dd, t=t: e.tensor_tensor(out=k.X(dd, t), in0=k.bank(b), in1=xs, op=ALU.add),
                 reads=[("ps", b), xk], writes=[("X", dd, t)])


def spill_x(k):
    S, d = k.S, k.d
    for c in range(NKC):
        o = W_X + c * S_LEN
        S.dma("sp", "xsp", lambda e, c=c, o=o: e.dma_start(out=d["xspill"][:, c, :], in_=k.arena[:, o:o + S_LEN]),
              reads=[("X", c, t) for t in range(NT)], writes=["xspill"])


def mixer(k, l, branches=("ml", "diff", "fox")):
    S = k.S
    layer_consts(k, l)
    rmsnorm(k, l * 3 + 1, "H")
    spill_x(k)
    S.barrier()
    if "ml" in branches:
        mlstm_branch(k, l)
    if "diff" in branches:
        diff_branch(k, l)
    if "fox" in branches:
        fox_branch(k, l)
    merge_and_out(k, l)


def _chunk_rows(w):
    K_, N_ = w.shape
    return w.reshape(K_ // 128, 128, N_).transpose(1, 0, 2)


def _cols(v):
    return v.reshape(-1, 128).T


def prep_shared(inp):
    f = np.float32
    sh = {}
    for which in (1, 2):
        wg, wu, wd = inp[f"ffn{which}_w_gate"], inp[f"ffn{which}_w_up"], inp[f"ffn{which}_w_down"]
        gu = np.empty((DEPTH, NFC, 128, 2, NKC, 128), f)
        for l in range(DEPTH):
            for g, w in enumerate((wg[l], wu[l])):
                gu[l, :, :, g] = w.reshape(NKC, 128, NFC, 128).transpose(2, 1, 0, 3)
        sh[f"ffn{which}_gu"] = gu.reshape(DEPTH, NFC, 128, 2 * NKC * 128)
        sh[f"ffn{which}_wd"] = np.ascontiguousarray(wd.reshape(DEPTH, NFC, 128, D))
    vecs = []
    for l in range(DEPTH):
        vecs += [inp["ffn1_norm"][l], inp["mix_norm"][l], inp["ffn2_norm"][l]]
    vecs.append(inp["final_norm"])
    sh["normw"] = np.ascontiguousarray(np.stack([_cols(v) for v in vecs], axis=1).reshape(128, 56)).astype(f)
    sh["ident"] = np.eye(128, dtype=f)
    sh["ones"] = np.ones((128, 128), f)
    sh["tri"] = np.triu(np.ones((128, 128), f))
    sh["negtri"] = (np.tril(np.ones((128, 128), f), -1) * NEG_BIG).astype(f)
    w_in = inp["w_in"]
    sh["w_in"] = np.ascontiguousarray(np.stack([_chunk_rows(w_in[l]) for l in range(DEPTH)]))
    gb = np.empty((DEPTH, 128, 24), f)
    cw = np.empty((DEPTH, 128, 16), f)
    cb = np.empty((DEPTH, 128, 4), f)
    sk = np.empty((DEPTH, 128, 4), f)
    nmc = np.empty((DEPTH, 128, 4), f)
    gc = np.zeros((DEPTH, 8, 8), f)
    for l in range(DEPTH):
        for b in range(3):
            gb[l, :, b * 8:(b + 1) * 8] = _cols(inp["gate_bias"][l, b])
        for j in range(4):
            cw[l, :, j * 4:(j + 1) * 4] = _cols(inp["mlstm_conv_w"][l, j])
        cb[l] = _cols(inp["mlstm_conv_b"][l])
        sk[l] = _cols(inp["mlstm_skip"][l])
        nmc[l] = _cols(inp["mlstm_norm"][l])
        gc[l, :, 0] = inp["fox_b_f"][l]
        gc[l, 0:4, 1] = inp["mlstm_b_if"][l, 0:4]
        gc[l, 0:4, 2] = inp["mlstm_b_if"][l, 4:8]
    sh["gbcols"], sh["convw"], sh["convb"], sh["skipc"], sh["gcols"] = gb, cw, cb, sk, gc
    sh["normc"] = nmc
    sh["difflam"] = np.ascontiguousarray(np.concatenate(
        [inp["diff_lq1"], inp["diff_lk1"], inp["diff_lq2"], inp["diff_lk2"]], axis=1)).astype(f)
    sh["diff_subln"] = np.ascontiguousarray(inp["diff_subln"]).astype(f)
    sh["mlstm_norm"] = np.ascontiguousarray(inp["mlstm_norm"]).astype(f)
    slopes = [2.0 ** (-8.0 * (h + 1) / 4) for h in range(4)]
    ak = np.zeros((3, 4, 128), f)
    aq = np.zeros((3, 4, 512), f)
    relq = np.arange(512)
    for h in range(4):
        ak[0, h] = slopes[h] * np.arange(128)
        ak[1, h] = 1.0
        ak[2, h] = 1.0
        aq[0, h] = 1.0
        aq[1, h] = -slopes[h] * (128 * (relq // 128))
        aq[2, h] = -slopes[h] * (relq % 128)
    sh["alibi_k"] = ak.reshape(3, 512)
    sh["alibi_q"] = aq.reshape(3, 2048)
    tpos = np.arange(S_LEN)
    akf = np.zeros((4, 3, S_LEN), f)
    aqf = np.zeros((4, 3, S_LEN), f)
    for h in range(4):
        akf[h, 0] = slopes[h] * (tpos % 128)
        akf[h, 1] = 1.0
        akf[h, 2] = 1.0
        aqf[h, 0] = 1.0
        aqf[h, 1] = -slopes[h] * (128 * ((tpos % 512) // 128))
        aqf[h, 2] = -slopes[h] * (tpos % 128)
    sh["alibi_kf"], sh["alibi_qf"] = akf, aqf
    wqkv = np.empty((DEPTH, 128, 3, 4, 128), f)
    for g, nm in enumerate(("mlstm_wq", "mlstm_wk", "mlstm_wv")):
        wqkv[:, :, g] = inp[nm].transpose(0, 2, 1, 3)
    sh["wqkv"] = wqkv.reshape(DEPTH, 128, 1536)
    sh["wif"] = np.ascontiguousarray(inp["mlstm_w_if"].reshape(DEPTH, 12, 128, 8).transpose(0, 2, 1, 3)).reshape(DEPTH, 128, 96)
    mp = np.empty((DEPTH, NKC, 128, 3, 1536), f)
    wbs = (inp["w_branch_diff"], inp["w_branch_fox"], inp["w_branch_mlstm"])
    for l in range(DEPTH):
        for b in range(3):
            g = w_in[l][:, C_G + b * D:C_G + (b + 1) * D]
            mp[l, :, :, b, 0:1024] = g.reshape(NKC, 128, NKC, 128).transpose(2, 1, 0, 3).reshape(NKC, 128, 1024)
            mp[l, :, :, b, 1024:1536] = wbs[b][l].reshape(4, 128, NKC, 128).transpose(2, 1, 0, 3).reshape(NKC, 128, 512)
    sh["mpack"] = mp.reshape(DEPTH, NKC, 128, 4608)
    wo = np.empty((DEPTH, NKC, 128, 1024), f)
    for l in range(DEPTH):
        wo[l] = inp["w_out"][l].reshape(NKC, 128, NKC, 128).transpose(2, 1, 0, 3).reshape(NKC, 128, 1024)
    sh["wout"] = wo
    return sh


def build_program(shapes, plan):
    from contextlib import ExitStack
    nc = bass.Bass("TRN2", target_bir_lowering=False)
    dram = {}
    for name, shp in shapes.items():
        dram[name] = nc.dram_tensor(name, list(shp), F32, kind="ExternalInput").ap()
    dram["outT"] = nc.dram_tensor("outT", [128, NKC, S_LEN], F32, kind="ExternalOutput").ap()
    dram["xspill"] = nc.dram_tensor("xspill", [128, NKC, S_LEN], F32, kind="Internal").ap()
    for nm in ("augk_fox", "augq_fox", "augk_ml", "augq_ml"):
        dram[nm] = nc.dram_tensor(nm, [8, 6, S_LEN], BF16, kind="Internal").ap()
    with ExitStack() as es:
        sems = [es.enter_context(nc.semaphore(f"s{i}")) for i in range(70)]
        S = Sched(nc, sems)
        arena = nc.alloc_sbuf_tensor("arena", [128, ARENA_WORDS], F32)
        ps = es.enter_context(nc.psum_tensor("ps", [128, 4096], F32))
        k = K(nc, S, arena, ps, dram)
        plan(k)
        with nc.Block() as block:
            S.emit(block)
    return nc


def full_plan(k):
    build_consts(k)
    load_x(k)
    for l in range(DEPTH):
        rmsnorm(k, l * 3 + 0, "H")
        ffn(k, l, 1)
        mixer(k, l)
        rmsnorm(k, l * 3 + 2, "H")
        k.S.barrier()
        ffn(k, l, 2)
    rmsnorm(k, 6, "X")
    store_out(k)


def run(inputs, plan=full_plan, trace=False, cores=NB):
    sh = prep_shared(inputs)
    x = np.asarray(inputs["x"], np.float32)
    in_maps = []
    for b in range(cores):
        m = dict(sh)
        m["xT"] = np.ascontiguousarray(x[b].T.reshape(NKC, 128, S_LEN).transpose(1, 0, 2))
        in_maps.append(m)
    shapes = {n: a.shape for n, a in in_maps[0].items()}
    nc = build_program(shapes, plan)
    res = run_bass_kernel_spmd(nc, in_maps, core_ids=list(range(cores)), trace=trace)
    out = np.empty((cores, S_LEN, D), np.float32)
    for b in range(cores):
        o = res.results[b]["outT"]
        out[b] = o.transpose(1, 0, 2).reshape(D, S_LEN).T
    return out, res


def kernel(**inputs):
    out, _ = run(inputs)
    return out
```

```python
import bisect
import numpy as np
import concourse.bass as bass
import concourse.mybir as mybir
from concourse.bass_utils import run_bass_kernel_spmd

F32 = mybir.dt.float32
BF16 = mybir.dt.bfloat16
AF = mybir.ActivationFunctionType
ALU = mybir.AluOpType
AX = mybir.AxisListType


class _Ev:
    __slots__ = ("eng", "idx", "val", "clock")

    def __init__(self, eng, idx, val, clock):
        self.eng, self.idx, self.val, self.clock = eng, idx, val, clock


class _Eng:
    def __init__(self, name, sem, self_sync):
        self.name, self.sem, self.self_sync = name, sem, self_sync
        self.ops = []
        self.count = 0
        self.sig_idx = []
        self.sig_val = []
        self.n_inst = 0
        self.inst_rec = []
        self.clock = {}
        self.last_compute = None


class _Slot:
    def __init__(self, name, sem):
        self.name, self.sem, self.total = name, sem, 0


class _Res:
    __slots__ = ("w", "rs")

    def __init__(self):
        self.w, self.rs = None, []


class Sched:
    def __init__(self, nc, sems):
        self.nc = nc
        self._sems = list(sems)
        self.engs = {}
        for name, ss in (("pe", False), ("act", True), ("dve", True), ("pool", True), ("sp", False)):
            self.engs[name] = _Eng(name, self._sems.pop(), ss)
        self.slots = {}
        self.resd = {}

    def slot(self, name):
        s = self.slots.get(name)
        if s is None:
            s = _Slot(name, self._sems.pop())
            self.slots[name] = s
        return s

    def res(self, key):
        r = self.resd.get(key)
        if r is None:
            r = _Res()
            self.resd[key] = r
        return r

    def _value(self, ev):
        if ev.val is not None:
            return ev.val
        E = self.engs[ev.eng]
        j = bisect.bisect_left(E.sig_idx, ev.idx)
        if j < len(E.sig_idx):
            return E.sig_val[j]
        E.count += 1
        rec = E.inst_rec[ev.idx]
        rec[2], rec[3] = E.sem, 1
        E.sig_idx.append(ev.idx)
        E.sig_val.append(E.count)
        ev.val = E.count
        return ev.val

    def _wait_for(self, E, evs):
        need = {}
        for ev in evs:
            if ev is None:
                continue
            if ev.eng == E.name and not E.self_sync:
                continue
            if ev.eng in self.slots:
                v = self.slots[ev.eng].total
            else:
                v = self._value(ev)
            if E.clock.get(ev.eng, 0) >= v:
                continue
            if need.get(ev.eng, (0, None))[0] < v:
                need[ev.eng] = (v, ev)
        for name, (v, ev) in need.items():
            if E.clock.get(name, 0) >= v:
                continue
            sem = self.slots[name].sem if name in self.slots else self.engs[name].sem
            E.ops.append(["w", sem, v])
            E.clock[name] = v
            for k, cv in ev.clock.items():
                if E.clock.get(k, 0) < cv:
                    E.clock[k] = cv

    def _deps(self, reads, writes):
        evs = []
        for k in reads:
            r = self.res(k)
            if r.w is not None:
                evs.append(r.w)
        for k in writes:
            r = self.res(k)
            if r.w is not None:
                evs.append(r.w)
            evs.extend(r.rs)
        return evs

    def _commit(self, ev, reads, writes):
        for k in reads:
            self.res(k).rs.append(ev)
        for k in writes:
            r = self.res(k)
            r.w, r.rs = ev, []

    def op(self, eng, fn, reads=(), writes=()):
        E = self.engs[eng]
        self._wait_for(E, self._deps(reads, writes))
        rec = ["i", fn, None, 0]
        E.ops.append(rec)
        E.inst_rec.append(rec)
        idx = E.n_inst
        E.n_inst += 1
        E.last_compute = idx
        clk = dict(E.clock)
        ev = _Ev(eng, idx, None, clk)
        self._commit(ev, reads, writes)
        return ev

    def dma(self, queue, slot, fn, reads=(), writes=()):
        E = self.engs[queue]
        S = self.slot(slot) if isinstance(slot, str) else slot
        self._wait_for(E, self._deps(reads, writes))
        S.total += 16
        rec = ["i", fn, S.sem, 16]
        E.ops.append(rec)
        E.inst_rec.append(rec)
        E.n_inst += 1
        ev = _Ev(S.name, -1, S.total, dict(E.clock))
        self._commit(ev, reads, writes)
        return ev

    def barrier(self):
        evs = []
        for E in self.engs.values():
            if E.last_compute is not None:
                ev = _Ev(E.name, E.last_compute, None, dict(E.clock))
                self._value(ev)
                evs.append(ev)
        for S in self.slots.values():
            if S.total:
                evs.append(_Ev(S.name, -1, S.total, {}))
        for E in self.engs.values():
            self._wait_for(E, evs)
        self.resd = {}

    def wait_all_dma(self, eng, slots):
        E = self.engs[eng]
        evs = [_Ev(self.slots[s].name, -1, self.slots[s].total, {}) for s in slots if self.slots[s].total]
        self._wait_for(E, evs)

    def emit(self, block):
        def replay(E):
            def run(e):
                for rec in E.ops:
                    if rec[0] == "w":
                        e.wait_ge(rec[1], rec[2])
                    else:
                        ins = rec[1](e)
                        if rec[2] is not None:
                            ins.then_inc(rec[2], rec[3])
            return run
        block.tensor(replay(self.engs["pe"]))
        block.scalar(replay(self.engs["act"]))
        block.vector(replay(self.engs["dve"]))
        block.gpsimd(replay(self.engs["pool"]))
        block.sync(replay(self.engs["sp"]))


D = 1024
S_LEN = 2048
NB = 8
DEPTH = 2
DFF = 2816
NFC = DFF // 128
NKC = D // 128
NT = S_LEN // 512
NT128 = S_LEN // 128
RMS_EPS = 1e-6
LN_EPS = 1e-5
N_IN = 7176
FFN_GROUPS = [(0, 6), (6, 12), (12, 17), (17, 22)]

W_X = 0
W_H = W_X + 16384
W_CONST = W_H + 8192
W_PT = W_CONST + 3072
W_W = W_PT + 1024
W_N = W_W + 9216
W_END = W_N + 12288
ARENA_WORDS = W_END


class K:
    def __init__(self, nc, S, arena, ps, dram):
        self.nc, self.S, self.arena, self.ps, self.d = nc, S, arena, ps, dram
        self.ps_rr = 0

    def f32(self, off, n):
        return self.arena[:, off:off + n]

    def bf(self, off, n):
        return self.arena[:, off:off + (n + 1) // 2].bitcast(BF16)

    def bank(self, b):
        return self.ps[:, b * 512:(b + 1) * 512]

    def X(self, c, t):
        o = W_X + c * S_LEN + t * 512
        return self.arena[:, o:o + 512]

    def H(self, c, t0=0, n=S_LEN):
        return self.bf(W_H + c * (S_LEN // 2), S_LEN)[:, t0:t0 + n]

    def PT(self, i):
        return self.bf(W_PT + i * 256, 512)


def build_consts(k):
    S, d = k.S, k.d
    o = W_CONST
    k.ident = k.f32(o, 128); o += 128
    k.ones_bf = k.bf(o, 128); o += 64
    k.tri_bf = k.bf(o, 128); o += 64
    k.normw = k.f32(o, 7 * 8); o += 56
    k.const_end = o
    S.dma("sp", "c0", lambda e: e.dma_start(out=k.ident, in_=d["ident"][:, :]), writes=["ident"])
    S.dma("pool", "c1", lambda e: e.dma_start(out=k.ones_bf, in_=d["ones"][:, :]), writes=["ones"])
    S.dma("pool", "c1", lambda e: e.dma_start(out=k.tri_bf, in_=d["tri"][:, :]), writes=["tri"])
    S.dma("sp", "c0", lambda e: e.dma_start(out=k.normw, in_=d["normw"][:, :]), writes=["normw"])


def load_x(k):
    S, d = k.S, k.d
    for c in range(NKC):
        o = W_X + c * S_LEN
        S.dma("sp", "xld", lambda e, c=c, o=o: e.dma_start(out=k.arena[:, o:o + S_LEN], in_=d["xT"][:, c, :]),
              writes=[("X", c, t) for t in range(NT)])


def rmsnorm(k, widx, out_mode):
    S = k.S
    R0 = W_N + 8192
    for t in range(NT):
        for c in range(NKC):
            pt = k.PT((t * NKC + c) % 4)
            key = ("PT", (t * NKC + c) % 4)
            if c % 2 == 0:
                S.op("act", lambda e, pt=pt, c=c, t=t: e.activation(out=pt, in_=k.X(c, t), func=AF.Square),
                     reads=[("X", c, t)], writes=[key])
            else:
                S.op("dve", lambda e, pt=pt, c=c, t=t: e.tensor_tensor(out=pt, in0=k.X(c, t), in1=k.X(c, t), op=ALU.mult),
                     reads=[("X", c, t)], writes=[key])
            S.op("pe", lambda e, pt=pt, c=c, t=t: e.matmul(k.bank(t), k.ones_bf, pt, start=(c == 0), stop=(c == NKC - 1)),
                 reads=[key, "ones"], writes=[("ps", t)])
    for t in range(NT):
        R = k.f32(R0 + t * 512, 512)
        S.op("act", lambda e, R=R, t=t: e.activation(out=R, in_=k.bank(t), func=AF.Ln, bias=RMS_EPS, scale=1.0 / D),
             reads=[("ps", t)], writes=[("R", t)])
        S.op("act", lambda e, R=R: e.activation(out=R, in_=R, func=AF.Exp, scale=-0.5), reads=[("R", t)], writes=[("R", t)])
        for c in range(NKC):
            wcol = k.normw[:, widx * 8 + c:widx * 8 + c + 1]
            if out_mode == "H":
                S.op("dve", lambda e, R=R, c=c, t=t, wcol=wcol: e.scalar_tensor_tensor(
                    out=k.H(c, t * 512, 512), in0=k.X(c, t), scalar=wcol, in1=R, op0=ALU.mult, op1=ALU.mult),
                    reads=[("X", c, t), ("R", t), "normw"], writes=[("H", c, t)])
            else:
                S.op("dve", lambda e, R=R, c=c, t=t, wcol=wcol: e.scalar_tensor_tensor(
                    out=k.X(c, t), in0=k.X(c, t), scalar=wcol, in1=R, op0=ALU.mult, op1=ALU.mult),
                    reads=[("X", c, t), ("R", t), "normw"], writes=[("X", c, t)])


def ffn(k, l, which):
    S, d = k.S, k.d
    gu_d = d[f"ffn{which}_gu"]
    wd_d = d[f"ffn{which}_wd"]
    GU = [k.bf(W_W + i * 1024, 2048) for i in range(3)]
    WD = [k.bf(W_W + 3072 + i * 3072, 6 * 1024) for i in range(2)]
    A = [k.bf(W_N + i * 1024, 2048) for i in range(7)]
    SG = [k.f32(W_N + 7168 + i * 512, 512) for i in range(2)]

    def load_gu(c):
        s = c % 3
        S.dma("pool", f"gu{s}", lambda e: e.dma_start(out=GU[s], in_=gu_d[l, c, :, :]), writes=[("GU", s)])

    def load_wd(g):
        s = g % 2
        c0, c1 = FFN_GROUPS[g]
        for c in range(c0, c1):
            S.dma("pool", f"wd{s}", lambda e, c=c: e.dma_start(out=WD[s][:, (c - c0) * 1024:(c - c0 + 1) * 1024], in_=wd_d[l, c, :, :]),
                  writes=[("WD", s)])

    def gu_chunk(c):
        s = c % 3
        for t in range(NT):
            pr = (c * NT + t) % 3
            bg, bu = 2 * pr, 2 * pr + 1
            for g, b in ((0, bg), (1, bu)):
                for kc in range(NKC):
                    S.op("pe", lambda e, g=g, b=b, kc=kc, t=t: e.matmul(
                        k.bank(b), GU[s][:, (g * 8 + kc) * 128:(g * 8 + kc + 1) * 128], k.H(kc, t * 512, 512),
                        start=(kc == 0), stop=(kc == NKC - 1)),
                        reads=[("GU", s), ("H", kc, t)], writes=[("ps", b)])
            sg = SG[(c * NT + t) % 2]
            sgk = ("SG", (c * NT + t) % 2)
            S.op("act", lambda e, sg=sg, bg=bg: e.activation(out=sg, in_=k.bank(bg), func=AF.Silu),
                 reads=[("ps", bg)], writes=[sgk])
            S.op("dve", lambda e, sg=sg, bu=bu, t=t: e.tensor_tensor(
                out=A[c % 7][:, t * 512:(t + 1) * 512], in0=sg, in1=k.bank(bu), op=ALU.mult),
                reads=[sgk, ("ps", bu)], writes=[("A", c % 7, t)])

    dn_cnt = [0]

    def down(g):
        s = g % 2
        c0, c1 = FFN_GROUPS[g]
        for t in range(NT):
            for dd in range(NKC):
                b = 6 + dn_cnt[0] % 2
                dn_cnt[0] += 1
                for c in range(c0, c1):
                    S.op("pe", lambda e, b=b, c=c, dd=dd, t=t: e.matmul(
                        k.bank(b), WD[s][:, (c - c0) * 1024 + dd * 128:(c - c0) * 1024 + (dd + 1) * 128],
                        A[c % 7][:, t * 512:(t + 1) * 512], start=(c == c0), stop=(c == c1 - 1)),
                        reads=[("WD", s), ("A", c % 7, t)], writes=[("ps", b)])
                S.op("dve", lambda e, b=b, dd=dd, t=t: e.scalar_tensor_tensor(
                    out=k.X(dd, t), in0=k.bank(b), scalar=0.5, in1=k.X(dd, t), op0=ALU.mult, op1=ALU.add),
                    reads=[("ps", b), ("X", dd, t)], writes=[("X", dd, t)])

    for c in range(3):
        load_gu(c)
    load_wd(0)
    load_wd(1)
    grp_of = {}
    for g, (c0, c1) in enumerate(FFN_GROUPS):
        for c in range(c0, c1):
            grp_of[c] = g
    for c in range(NFC):
        gu_chunk(c)
        if c + 3 < NFC:
            load_gu(c + 3)
        g = grp_of[c]
        if c == FFN_GROUPS[g][0] and g > 0:
            down(g - 1)
            if g + 1 < len(FFN_GROUPS):
                load_wd(g + 1)
    down(len(FFN_GROUPS) - 1)


def store_out(k):
    S, d = k.S, k.d
    for c in range(NKC):
        o = W_X + c * S_LEN
        S.dma("sp", "ost", lambda e, c=c, o=o: e.dma_start(out=d["outT"][:, c, :], in_=k.arena[:, o:o + S_LEN]),
              reads=[("X", c, t) for t in range(NT)])
    S.wait_all_dma("sp", ["ost"])


U = 4096
W_SCR = W_END + 64
ARENA_WORDS = W_END + 2560
SB = W_SCR + 64
NEG_BIG = -30000.0
WARM_FILL = 0
C_DQ, C_DK, C_DV = 0, 512, 1024
C_FQ, C_FK, C_FV, C_FF = 1536, 2048, 2560, 3072
C_MX, C_MZ, C_G = 3080, 3592, 4104


def DU(i):
    return W_X + i * U


def NU(i):
    return W_N + i * U


def fm(k, off, c, t0=0, n=S_LEN):
    return k.bf(off + c * (S_LEN // 2), S_LEN)[:, t0:t0 + n]


def next_bank(k):
    b = k.ps_rr % 8
    k.ps_rr += 1
    return b


def col(k):
    i = getattr(k, "_col_rr", 0)
    k._col_rr = i + 1
    i %= 48
    return k.arena[:, W_SCR + i:W_SCR + i + 1], ("col", i)


def evac(k, dst, src, src_key, dst_key, scale=None, func=None, eng=None):
    S = k.S
    if eng is None:
        k._ev_rr = getattr(k, "_ev_rr", 0) + 1
        eng = "act" if (func is not None or k._ev_rr % 2 == 0) else "dve"
    if eng == "act":
        f = func if func is not None else AF.Copy
        sc = 1.0 if scale is None else scale
        S.op("act", lambda e: e.activation(out=dst, in_=src, func=f, scale=sc), reads=[src_key], writes=[dst_key])
    else:
        if scale is None:
            S.op("dve", lambda e: e.tensor_copy(dst, src), reads=[src_key], writes=[dst_key])
        else:
            S.op("dve", lambda e: e.tensor_scalar(dst, src, scale, None, ALU.mult), reads=[src_key], writes=[dst_key])


def wcol_jobs(k, l, jobs):
    S, d = k.S, k.d

    def load(i):
        c0, nc_, _ = jobs[i]
        s = i % 2
        tile = k.bf(W_W + s * 2048, 4096).rearrange("p (k c) -> p k c", k=8)
        S.dma("pool", f"wc{s}", lambda e: e.dma_start(out=tile[:, :, 0:nc_], in_=d["w_in"][l, :, :, c0:c0 + nc_]),
              writes=[("WC", s)])
        return tile

    tiles = {}
    for i in range(min(2, len(jobs))):
        tiles[i] = load(i)
    for i in range(len(jobs)):
        jobs[i][2](tiles[i], ("WC", i % 2))
        if i + 2 < len(jobs):
            tiles[i + 2] = load(i + 2)


def h_fm(dst_off, scale=None, func=None):
    def mk(k, c_base, nchunks=4):
        def handler(W, wkey):
            S = k.S
            for m in range(nchunks):
                for t in range(NT):
                    b = next_bank(k)
                    for kc in range(NKC):
                        S.op("pe", lambda e, b=b, m=m, kc=kc, t=t: e.matmul(
                            k.bank(b), W[:, kc, m * 128:(m + 1) * 128], k.H(kc, t * 512, 512),
                            start=(kc == 0), stop=(kc == NKC - 1)),
                            reads=[wkey, ("H", kc, t)], writes=[("ps", b)])
                    evac(k, fm(k, dst_off, c_base + m, t * 512, 512), k.bank(b), ("ps", b),
                         ("fm", dst_off, c_base + m, t), scale=scale, func=func)
        return handler
    return mk


def h_tm(k, vbuf_fn, nh, dv):
    def handler(W, wkey):
        S = k.S
        for tt in range(NT128):
            b = next_bank(k)
            for kc in range(NKC):
                S.op("pe", lambda e, b=b, kc=kc, tt=tt: e.matmul(
                    k.bank(b), k.H(kc, tt * 128, 128), W[:, kc, 0:512],
                    start=(kc == 0), stop=(kc == NKC - 1)),
                    reads=[wkey, ("H", kc, tt // 4)], writes=[("ps", b)])
            dst = vbuf_fn(tt)[:, :, 0:dv]
            src = k.bank(b).rearrange("p (h e) -> p h e", h=nh)
            evac(k, dst, src, ("ps", b), ("V", tt))
    return handler


class AttnPass:
    def __init__(self, k, *, qf, kf, aqf, akf, bias_fn, vf, vkey, dv, mode, fin, sbanks, obanks, scale=1.0, augkey=None, pre=None, act_func=None, scale_fn=None, mask_mul=False):
        self.__dict__.update(locals())
        self.state = {}
        self.per_bank = 2 if dv == 128 else 4

    def blocks(self):
        return [(self, j, i) for j in range(4) for i in range(4 * j + 4)]

    def O(self, j, r):
        pb = self.per_bank
        ob = self.obanks[j % len(self.obanks)][r // pb]
        c0 = (r % pb) * (self.dv + 1)
        return self.k.bank(ob)[:, c0:c0 + self.dv + 1], ("ps", ob)

    def emit_s(self, j, i):
        k, S = self.k, self.k.S
        if self.pre is not None:
            pl = self.pre if isinstance(self.pre, list) else [self.pre]
            self.pre = None
            for f_ in pl:
                f_()
        r0 = max(0, i - 4 * j)
        n0 = r0 * 128
        ncol = 512 - n0
        diag = i >= 4 * j
        k._blk = getattr(k, "_blk", 0) + 1
        pti = k._blk % 4
        pt = k.PT(pti)
        ptk = ("PT", pti)
        if self.mode == "exp":
            sb = self.sbanks[k._blk % len(self.sbanks)]
            has_aug = self.akf is not None
            for rep in range(1 + WARM_FILL):
                real = (rep == WARM_FILL)
                S.op("pe", lambda e, real=real: e.matmul(k.bank(sb)[:, n0:512], self.kf(i), self.qf(j * 512 + n0, ncol),
                                                         start=True, stop=((not has_aug and (not diag or self.mask_mul)) or not real)),
                     reads=([self.augkey] if (self.augkey is not None and not has_aug) else []), writes=[("ps", sb)])
            if has_aug:
                S.op("pe", lambda e: e.matmul(k.bank(sb)[:, n0:512], self.akf(i), self.aqf(j, n0, ncol),
                                              start=False, stop=(not diag)),
                     reads=[self.augkey], writes=[("ps", sb)])
            if diag and not self.mask_mul:
                S.op("pe", lambda e: e.matmul(k.bank(sb)[:, n0:n0 + 128], k.ident_bf, k.negtri_bf, start=False, stop=True),
                     reads=["identbf", "negtri"], writes=[("ps", sb)])
            bias = float(self.bias_fn(i, j)) if self.bias_fn is not None else 0.0
            if self.act_func is None:
                S.op("act", lambda e: e.activation(out=pt[:, n0:512], in_=k.bank(sb)[:, n0:512], func=AF.Exp, bias=bias, scale=1.0),
                     reads=[("ps", sb)], writes=[ptk])
            else:
                sc_ap = self.scale_fn(i, j)
                S.op("act", lambda e: e.activation(out=pt[:, n0:512], in_=k.bank(sb)[:, n0:512], func=self.act_func, scale=sc_ap),
                     reads=[("ps", sb), "UC"], writes=[ptk])
            if diag and self.mask_mul:
                S.op("pool", lambda e: e.tensor_tensor(out=pt[:, n0:n0 + 128], in0=pt[:, n0:n0 + 128], in1=k.tri_bf, op=ALU.mult),
                     reads=[ptk, "tri"], writes=[ptk])
        else:
            pr = self.sbanks[k._blk % len(self.sbanks)]
            sb, eb = pr
            S.op("pe", lambda e: e.matmul(k.bank(sb)[:, n0:512], self.kf(i), self.qf(j * 512 + n0, ncol), start=True, stop=True),
                 reads=[], writes=[("ps", sb)])
            S.op("pe", lambda e: e.matmul(k.bank(eb)[:, n0:512], self.akf(i), self.aqf(j, n0, ncol), start=True, stop=(not diag)),
                 reads=[self.augkey], writes=[("ps", eb)])
            if diag:
                S.op("pe", lambda e: e.matmul(k.bank(eb)[:, n0:n0 + 128], k.ident_bf, k.negtri_bf, start=False, stop=True),
                     reads=["identbf", "negtri"], writes=[("ps", eb)])
            eti = k._blk % 2
            et = k.f32(SB + eti * 512, 512)
            S.op("act", lambda e: e.activation(out=et[:, n0:512], in_=k.bank(eb)[:, n0:512], func=AF.Exp),
                 reads=[("ps", eb)], writes=[("ET", eti)])
            sc = self.scale
            S.op("dve", lambda e: e.scalar_tensor_tensor(out=pt[:, n0:512], in0=k.bank(sb)[:, n0:512], scalar=sc,
                                                         in1=et[:, n0:512], op0=ALU.mult, op1=ALU.mult),
                 reads=[("ps", sb), ("ET", eti)], writes=[ptk])
        self.state[(j, i)] = (pt, ptk, r0)

    def emit_pv(self, j, i):
        k, S = self.k, self.k.S
        pt, ptk, r0 = self.state.pop((j, i))
        for r in range(r0, 4):
            o, okey = self.O(j, r)
            last = (i == 4 * j + r)
            S.op("pe", lambda e, r=r, o=o, last=last: e.matmul(o, pt[:, r * 128:(r + 1) * 128], self.vf(i), start=(i == 0 and r % self.per_bank == 0), stop=last, skip_group_check=True),
                 reads=[ptk, self.vkey], writes=[okey])
            if last:
                self.fin(j, r, o, okey)


def run_blocks(k, blocks, L=2):
    n = len(blocks)
    for idx in range(n + L):
        if idx < n:
            p, j, i = blocks[idx]
            p.emit_s(j, i)
        if idx >= L:
            p, j, i = blocks[idx - L]
            p.emit_pv(j, i)
        tick(k)
    flush_deferred(k)


def defer(k, n, fn):
    k._dq = getattr(k, "_dq", [])
    k._dq.append([n, fn])


def tick(k):
    dq = getattr(k, "_dq", [])
    k._dq = []
    keep = []
    for it in dq:
        it[0] -= 1
        if it[0] <= 0:
            it[1]()
        else:
            keep.append(it)
    k._dq = keep + k._dq


def flush_deferred(k):
    while getattr(k, "_dq", []):
        dq = k._dq
        k._dq = []
        for it in dq:
            it[1]()


def transpose_to_fm(k, src, src_key, dst, dst_key, post=None):
    S = k.S
    q = getattr(k, "_tq", 0)
    k._tq = q + 1
    tb = k.tbanks[q % len(k.tbanks)]
    pq = k.bank(tb)[:, 0:128]
    pkey = ("ps", tb)
    S.op("pe", lambda e: e.transpose(pq, src, k.ident), reads=[src_key, "ident"], writes=[pkey])
    if post is None:
        evac(k, dst, pq, pkey, dst_key, eng="act")
    else:
        post(pq, pkey)


def softplus_neg(k, T, nh, banks, negb_col, key):
    S = k.S
    for t in range(NT):
        b = banks[t]
        S.op("act", lambda e, b=b, t=t: e.activation(out=T[0:nh, t * 512:(t + 1) * 512], in_=k.bank(b)[0:nh, :],
                                                    func=AF.Exp, bias=negb_col, scale=-1.0),
             reads=[("ps", b), "gcols"], writes=[key])
    S.op("act", lambda e: e.activation(out=T[0:nh, :], in_=T[0:nh, :], func=AF.Ln, bias=1.0, scale=1.0),
         reads=[key], writes=[key])


def build_aug(k, kvec, kkey, qvec, qkey, nh, spl_off, tag):
    S, d = k.S, k.d
    dk, dq = d["augk_" + tag], d["augq_" + tag]
    SPL = [k.bf(spl_off + i * 1024, 2048) for i in range(4)]
    ones = SPL[3]
    S.op("dve", lambda e: e.memset(ones[0:nh, :], 1.0), writes=[("SPL", 3)])
    for r in range(3):
        S.dma("sp", "augw", lambda e, r=r: e.dma_start(out=dk[0:nh, 3 + r, :], in_=ones[0:nh, :]), reads=[("SPL", 3)], writes=[("augd", tag)])
        S.dma("sp", "augw", lambda e, r=r: e.dma_start(out=dq[0:nh, r, :], in_=ones[0:nh, :]), reads=[("SPL", 3)], writes=[("augd", tag)])
    cnt = 0
    for vec, vkey, dram, r0 in ((kvec, kkey, dk, 0), (qvec, qkey, dq, 3)):
        for r in range(3):
            si = cnt % 3
            cnt += 1
            spl = SPL[si]
            S.op("dve", lambda e, spl=spl, vec=vec: e.tensor_copy(spl[0:nh, :], vec[0:nh, :]), reads=[vkey], writes=[("SPL", si)])
            if r < 2:
                S.op("dve", lambda e, spl=spl, vec=vec: e.tensor_tensor(out=vec[0:nh, :], in0=vec[0:nh, :], in1=spl[0:nh, :], op=ALU.subtract),
                     reads=[vkey, ("SPL", si)], writes=[vkey])
            S.dma("sp", "augw", lambda e, spl=spl, dram=dram, rr=r0 + r: e.dma_start(out=dram[0:nh, rr, :], in_=spl[0:nh, :]),
                  reads=[("SPL", si)], writes=[("augd", tag)])


def aug_views(k, slot):
    ak = k.bf(W_W + 5120 + slot * 2048, 2048)
    aq = k.bf(W_W + 5120 + slot * 2048 + 1024, 2048)
    akf = lambda i: ak[0:6, i * 128:(i + 1) * 128]
    aqf = lambda j, n0, n: aq[0:6, j * 512 + n0:j * 512 + n0 + n]
    return akf, aqf


def load_aug(k, tag, h, slot):
    S, d = k.S, k.d
    ak = k.bf(W_W + 5120 + slot * 2048, 2048)
    aq = k.bf(W_W + 5120 + slot * 2048 + 1024, 2048)
    S.dma("sp", f"augl{slot}", lambda e: e.dma_start(out=ak[0:6, :], in_=d["augk_" + tag][h, :, :]), reads=[("augd", tag)], writes=[("AUGS", slot)])
    S.dma("sp", f"augl{slot}", lambda e: e.dma_start(out=aq[0:6, :], in_=d["augq_" + tag][h, :, :]), reads=[("augd", tag)], writes=[("AUGS", slot)])


HB_OFF = [W_W + 0, W_W + 2048, W_W + 5120, W_W + 7168]


def hb_views(k, slot):
    return k.bf(HB_OFF[slot], 2048), k.bf(HB_OFF[slot] + 1024, 2048)


def hb_init(k):
    S = k.S
    for s_ in range(4):
        qb, kb = hb_views(k, s_)
        S.op("act", lambda e, qb=qb: e.memzero(qb), writes=[("HB", s_)])
        S.op("act", lambda e, kb=kb: e.memzero(kb), writes=[("HB", s_)])


def hb_build(k, slot, par, q_src, k_src, augq_ap, augk_ap, R, queue):
    S = k.S
    qb, kb = hb_views(k, slot)
    r0 = par * 64
    a0 = 64 if par == 0 else 0
    S.dma("sp", f"hbsp{slot}", lambda e: e.dma_start(out=qb[r0:r0 + 64, :], in_=q_src[r0:r0 + 64, :]), writes=[("HB", slot)])
    S.dma("sp", f"hbsp{slot}", lambda e: e.dma_start(out=kb[r0:r0 + 64, :], in_=k_src[r0:r0 + 64, :]), writes=[("HB", slot)])
    S.dma(queue, f"hb{queue}{slot}", lambda e: e.dma_start(out=qb[a0:a0 + R, :], in_=augq_ap), writes=[("HB", slot)])
    S.dma(queue, f"hb{queue}{slot}", lambda e: e.dma_start(out=kb[a0:a0 + R, :], in_=augk_ap), writes=[("HB", slot)])


def layer_consts(k, l):
    S, d = k.S, k.d
    o = k.const_end
    k.gb = k.f32(o, 24); o += 24
    k.convw = k.f32(o, 16); o += 16
    k.convb = k.f32(o, 4); o += 4
    k.skipc = k.f32(o, 4); o += 4
    k.normc = k.f32(o, 4); o += 4
    k.gcols = k.f32(o, 8); o += 8
    k.lamt = k.f32(o, 256); o += 256
    k.lamc = k.f32(o, 8); o += 8
    k.subln = k.f32(o, 128); o += 128
    k.normB = k.f32(o, 512); o += 512
    k.AKd = k.bf(o, 4 * 128); o += 256
    k.AQd = k.bf(o, 4 * 512); o += 1024
    k.ident_bf = k.bf(o, 128); o += 64
    k.negtri_bf = k.bf(o, 128); o += 64
    assert o <= W_CONST + 3072, o
    S.dma("sp", "lc", lambda e: e.dma_start(out=k.gb, in_=d["gbcols"][l, :, :]), writes=["lconst"])
    S.dma("sp", "lc", lambda e: e.dma_start(out=k.convw, in_=d["convw"][l, :, :]), writes=["lconst"])
    S.dma("sp", "lc", lambda e: e.dma_start(out=k.convb, in_=d["convb"][l, :, :]), writes=["lconst"])
    S.dma("sp", "lc", lambda e: e.dma_start(out=k.skipc, in_=d["skipc"][l, :, :]), writes=["lconst"])
    S.dma("sp", "lc", lambda e: e.dma_start(out=k.normc, in_=d["normc"][l, :, :]), writes=["lconst"])
    S.dma("sp", "lc", lambda e: e.dma_start(out=k.gcols[0:8, :], in_=d["gcols"][l, :, :]), writes=["gcols"])
    S.dma("sp", "lc", lambda e: e.dma_start(out=k.lamt, in_=d["difflam"][l:l + 1, :].partition_broadcast(128)), writes=["lamt"])
    S.dma("sp", "lc", lambda e: e.dma_start(out=k.subln, in_=d["diff_subln"][l:l + 1, :].partition_broadcast(128)), writes=["subln"])
    S.dma("sp", "lc", lambda e: e.dma_start(out=k.normB, in_=d["mlstm_norm"][l:l + 1, :].partition_broadcast(128)), writes=["normB"])
    S.op("dve", lambda e: e.tensor_scalar(k.gcols[0:8, 4:5], k.gcols[0:8, 0:1], -1.0, None, ALU.mult), reads=["gcols"], writes=["gcols"])
    S.op("dve", lambda e: e.tensor_scalar(k.gcols[0:8, 5:6], k.gcols[0:8, 2:3], -1.0, None, ALU.mult), reads=["gcols"], writes=["gcols"])
    if l == 0:
        S.dma("pool", "lc2", lambda e: e.dma_start(out=k.AKd[0:3, :], in_=d["alibi_k"][:, :]), writes=["alibi"])
        S.dma("pool", "lc2", lambda e: e.dma_start(out=k.AQd[0:3, :], in_=d["alibi_q"][:, :]), writes=["alibi"])
        S.dma("pool", "lc2", lambda e: e.dma_start(out=k.ident_bf, in_=d["ident"][:, :]), writes=["identbf"])
        S.dma("pool", "lc2", lambda e: e.dma_start(out=k.negtri_bf, in_=d["negtri"][:, :]), writes=["negtri"])
    import math
    lam_init = 0.8 - 0.6 * math.exp(-0.3 * l)
    lt, lc = k.lamt, k.lamc
    S.op("dve", lambda e: e.tensor_tensor(out=lt[:, 0:64], in0=lt[:, 0:64], in1=lt[:, 64:128], op=ALU.mult), reads=["lamt"], writes=["lamt"])
    S.op("dve", lambda e: e.tensor_tensor(out=lt[:, 128:192], in0=lt[:, 128:192], in1=lt[:, 192:256], op=ALU.mult), reads=["lamt"], writes=["lamt"])
    S.op("dve", lambda e: e.reduce_sum(out=lc[:, 0:1], in_=lt[:, 0:64], axis=AX.X), reads=["lamt"], writes=["lamc"])
    S.op("dve", lambda e: e.reduce_sum(out=lc[:, 1:2], in_=lt[:, 128:192], axis=AX.X), reads=["lamt"], writes=["lamc"])
    S.op("act", lambda e: e.activation(out=lc[:, 2:4], in_=lc[:, 0:2], func=AF.Exp), reads=["lamc"], writes=["lamc"])
    S.op("dve", lambda e: e.tensor_tensor(out=lc[:, 4:5], in0=lc[:, 3:4], in1=lc[:, 2:3], op=ALU.subtract), reads=["lamc"], writes=["lamc"])
    S.op("dve", lambda e: e.tensor_scalar(lc[:, 5:6], lc[:, 4:5], -lam_init, None, ALU.add), reads=["lamc"], writes=["neglam"])
    S.op("dve", lambda e: e.tensor_scalar(k.subln, k.subln, 1.0 - lam_init, None, ALU.mult), reads=["subln"], writes=["subln"])
    k.neglam = lc[:, 5:6]


def diff_branch(k, l):
    S = k.S
    QT, KT, VO, OD = DU(0), DU(1), DU(2), NU(0)
    V = k.bf(VO, 16 * 4 * 129).rearrange("p (t h e) -> p t h e", t=16, h=4)
    S.op("dve", lambda e: e.memset(V[:, :, :, 128:129], 1.0), writes=[("V", "ones")])
    wcol_jobs(k, l, [
        (C_DQ, 512, h_fm(QT)(k, 0)),
        (C_DK, 512, h_fm(KT, scale=0.125)(k, 0)),
        (C_DV, 512, h_tm(k, lambda tt: V[:, tt, :, :], 4, 128)),
    ])
    S.barrier()
    k.tbanks = [3]
    slopes = [2.0 ** (-8.0 * (h + 1) / 4) for h in range(4)]
    A1 = [[k.f32(SB + (p * 4 + r) * 128, 128) for r in range(4)] for p in range(2)]
    TB = SB + 1024
    blocks = []
    pres = []
    first_pass = []
    hb_init(k)
    for h in range(4):
        passes = []
        for c in range(2):
            slot = c + 2 * (h % 2)
            qb, kb = hb_views(k, slot)

            def qf(t0, n, qb=qb):
                return qb[:, t0:t0 + n]

            def kf(i, kb=kb):
                return kb[:, i * 128:(i + 1) * 128]

            pres.append(lambda h=h, c=c, slot=slot: hb_build(k, slot, c, fm(k, QT, h), fm(k, KT, h),
                                                              k.d["alibi_qf"][h, :, :], k.d["alibi_kf"][h, :, :], 3, "pool"))

            def bias_fn(i, j, h=h):
                return slopes[h] * (128.0 * i - 512.0 * j)

            def vf(i, h=h):
                return V[:, i, h, :]

            if c == 0:
                def fin(j, r, o, okey, h=h):
                    rc, rk = col(k)
                    a1 = A1[j % 2][r]
                    S.op("dve", lambda e: e.reciprocal(rc, o[:, 128:129]), reads=[okey], writes=[rk])
                    S.op("dve", lambda e: e.tensor_scalar(a1, o[:, 0:128], rc, None, ALU.mult), reads=[okey, rk], writes=[("A1", j % 2, r)])
            else:
                def fin(j, r, o, okey, h=h):
                    rc, rk = col(k)
                    sc, sk = col(k)
                    k._fp = getattr(k, "_fp", 0) + 1
                    n = k._fp
                    tmp = k.f32(TB + (n % 5) * 128, 128)
                    sq = k.f32(TB + 640 + (n % 2) * 128, 128)
                    ot = k.f32(TB + 896 + (n % 4) * 128, 128)
                    tk_, sqk, otk = ("TMP", n % 5), ("SQ", n % 2), ("OT", n % 4)
                    a1 = A1[j % 2][r]
                    tt = 4 * j + r
                    S.op("dve", lambda e: e.reciprocal(rc, o[:, 128:129]), reads=[okey], writes=[rk])
                    S.op("dve", lambda e: e.tensor_scalar(tmp, o[:, 0:128], rc, None, ALU.mult), reads=[okey, rk], writes=[tk_])
                    S.op("dve", lambda e: e.scalar_tensor_tensor(out=tmp, in0=tmp, scalar=k.neglam, in1=a1, op0=ALU.mult, op1=ALU.add),
                         reads=[tk_, ("A1", j % 2, r), "neglam"], writes=[tk_])
                    S.op("dve", lambda e: e.tensor_tensor(out=sq, in0=tmp, in1=tmp, op=ALU.mult), reads=[tk_], writes=[sqk])
                    S.op("dve", lambda e: e.reduce_sum(out=sc, in_=sq, axis=AX.X), reads=[sqk], writes=[sk])

                    def stage_b():
                        S.op("act", lambda e: e.activation(out=sc, in_=sc, func=AF.Ln, bias=RMS_EPS, scale=1.0 / 128), reads=[sk], writes=[sk])
                        S.op("act", lambda e: e.activation(out=sc, in_=sc, func=AF.Exp, scale=-0.5), reads=[sk], writes=[sk])

                    def stage_c():
                        S.op("dve", lambda e: e.scalar_tensor_tensor(out=ot, in0=tmp, scalar=sc, in1=k.subln, op0=ALU.mult, op1=ALU.mult),
                             reads=[tk_, sk, "subln"], writes=[otk])
                        defer(k, 2, lambda: transpose_to_fm(k, ot, otk, fm(k, OD, h, tt * 128, 128), ("fm", OD, h, tt)))
                    defer(k, 2, stage_b)
                    defer(k, 3, stage_c)
            passes.append(AttnPass(k, qf=qf, kf=kf, aqf=None, akf=None, bias_fn=bias_fn, vf=vf, vkey=("V", "x"), dv=128,
                                   mode="exp", fin=fin, sbanks=[0, 1, 2], obanks=[(4, 5)] if c == 0 else [(6, 7)], augkey=("HB", slot)))
        first_pass.append(passes[0])
        b0, b1 = passes[0].blocks(), passes[1].blocks()
        for j in range(4):
            blocks += [b for b in b0 if b[1] == j]
            blocks += [b for b in b1 if b[1] == j]
    for u in range(4):
        first_pass[u].pre = (pres[0:4] if u == 0 else (pres[2 * u + 2:2 * u + 4] if u < 3 else None))
    run_blocks(k, blocks)
    S.barrier()


def fox_branch(k, l):
    S, d = k.S, k.d
    QT, KT, VO, OF = DU(0), DU(1), DU(2), NU(2)
    V = k.bf(VO, 16 * 8 * 65).rearrange("p (t h e) -> p t h e", t=16, h=8)
    WFF = k.bf(W_W + 4096, 64).rearrange("p (k c) -> p k c", k=8)
    S.op("dve", lambda e: e.memset(V[:, :, :, 64:65], 1.0), writes=[("V", "ones")])
    S.dma("pool", "wff", lambda e: e.dma_start(out=WFF, in_=d["w_in"][l, :, :, C_FF:C_FF + 8]), writes=["WFF"])
    S.barrier()
    wcol_jobs(k, l, [
        (C_FQ, 512, h_fm(QT)(k, 0)),
        (C_FK, 512, h_fm(KT, scale=0.125)(k, 0)),
        (C_FV, 512, h_tm(k, lambda tt: V[:, tt, :, :], 8, 64)),
    ])
    for t in range(NT):
        for kc in range(NKC):
            S.op("pe", lambda e, kc=kc, t=t: e.matmul(k.bank(t)[0:8, :], WFF[:, kc, :], k.H(kc, t * 512, 512),
                                                      start=(kc == 0), stop=(kc == NKC - 1)),
                 reads=["WFF", ("H", kc, t)], writes=[("ps", t)])
    S.barrier()
    T1 = k.f32(W_W, 2048)
    T2 = k.f32(W_W + 2048, 2048)
    ONES = k.f32(W_W + 7168, 2048)
    S.op("dve", lambda e: e.memset(ONES[0:8, :], 1.0), writes=["ONES"])
    softplus_neg(k, T1, 8, [0, 1, 2, 3], k.gcols[0:8, 4:5], "T1")
    S.op("dve", lambda e: e.tensor_tensor_scan(T2[0:8, :], ONES[0:8, :], T1[0:8, :], 0.0, ALU.mult, ALU.add), reads=["T1", "ONES"], writes=["T2"])
    S.op("dve", lambda e: e.tensor_scalar(T1[0:8, :], T2[0:8, :], -1.0, None, ALU.mult), reads=["T2"], writes=["T1"])
    build_aug(k, T2, "T2", T1, "T1", 8, NU(2), "fox")
    S.barrier()
    k.tbanks = [3, 7]
    OTB = [[k.f32(SB + (p * 4 + r) * 128, 128) for r in range(4)] for p in range(2)]
    blocks = []
    pres = []
    first_pass = []
    hb_init(k)
    for pr in range(4):
        passes = []
        for c in range(2):
            hd = 2 * pr + c
            slot = c + 2 * (pr % 2)
            qb, kb = hb_views(k, slot)

            def qf(t0, n, qb=qb):
                return qb[:, t0:t0 + n]

            def kf(i, kb=kb):
                return kb[:, i * 128:(i + 1) * 128]

            pres.append(lambda pr=pr, c=c, slot=slot, hd=hd: hb_build(k, slot, c, fm(k, QT, pr), fm(k, KT, pr),
                                                                      k.d["augq_fox"][hd, :, :], k.d["augk_fox"][hd, :, :], 6, "sp"))

            def vf(i, hd=hd):
                return V[:, i, hd, :]

            def fin(j, r, o, okey, pr=pr, c=c):
                rc, rk = col(k)
                ot = OTB[j % 2][r]
                otk = ("OTB", j % 2, r)
                S.op("dve", lambda e: e.reciprocal(rc, o[:, 64:65]), reads=[okey], writes=[rk])
                S.op("dve", lambda e: e.tensor_scalar(ot[:, c * 64:(c + 1) * 64], o[:, 0:64], rc, None, ALU.mult), reads=[okey, rk], writes=[otk])
                if c == 1:
                    tt = 4 * j + r
                    defer(k, 3, lambda: transpose_to_fm(k, ot, otk, fm(k, OF, pr, tt * 128, 128), ("fm", OF, pr, tt)))
            passes.append(AttnPass(k, qf=qf, kf=kf, aqf=None, akf=None, bias_fn=None, vf=vf, vkey=("V", "x"), dv=64,
                                   mode="exp", fin=fin, sbanks=[0, 1, 2, 6], obanks=[(4,)] if c == 0 else [(5,)], augkey=("HB", slot)))
        first_pass.append(passes[0])
        b0, b1 = passes[0].blocks(), passes[1].blocks()
        for j in range(4):
            blocks += [b for b in b0 if b[1] == j]
            blocks += [b for b in b1 if b[1] == j]
    for u in range(4):
        first_pass[u].pre = (pres[0:4] if u == 0 else (pres[2 * u + 2:2 * u + 4] if u < 3 else None))
    run_blocks(k, blocks, L=3)
    S.barrier()


def mlstm_branch(k, l):
    S, d = k.S, k.d
    QT, KT, VT, MX, XC, SZ, VA = DU(0), DU(1), DU(2), DU(3), NU(0), NU(1), NU(2)
    Vall = k.bf(VA, 16 * 4 * 129).rearrange("p (t h e) -> p t h e", t=16, h=4)
    WQKV = k.bf(W_W + 4096, 1536).rearrange("p (g h e) -> p g h e", g=3, h=4)
    WIF = k.bf(W_W + 4096 + 768, 96).rearrange("p (j o) -> p j o", j=12)
    S.op("dve", lambda e: e.memset(Vall[:, :, :, 128:129], 1.0), writes=[("V", "ones")])
    S.dma("pool", "wqkv", lambda e: e.dma_start(out=k.bf(W_W + 4096, 1536), in_=d["wqkv"][l, :, :]), writes=["WQKV"])
    S.dma("pool", "wqkv", lambda e: e.dma_start(out=k.bf(W_W + 4096 + 768, 96), in_=d["wif"][l, :, :]), writes=["WIF"])
    S.barrier()
    wcol_jobs(k, l, [
        (C_MX, 512, h_fm(MX)(k, 0)),
        (C_MZ, 512, h_fm(SZ, func=AF.Silu)(k, 0)),
    ])
    ACC = k.f32(SB, 2048)
    for c in range(4):
        mx = fm(k, MX, c)
        mkeys = [("fm", MX, c, t) for t in range(NT)]
        w = lambda j, c=c: k.convw[:, j * 4 + c:j * 4 + c + 1]
        S.op("dve", lambda e, mx=mx, c=c, w=w: e.tensor_scalar(ACC, mx, w(3), k.convb[:, c:c + 1], ALU.mult, ALU.add),
             reads=mkeys + ["lconst"], writes=["ACC"])
        for sh in (1, 2, 3):
            S.op("dve", lambda e, mx=mx, sh=sh, w=w: e.scalar_tensor_tensor(
                out=ACC[:, sh:], in0=mx[:, 0:S_LEN - sh], scalar=w(3 - sh), in1=ACC[:, sh:], op0=ALU.mult, op1=ALU.add),
                reads=mkeys + ["ACC", "lconst"], writes=["ACC"])
        S.op("act", lambda e, c=c: e.activation(out=fm(k, XC, c), in_=ACC, func=AF.Silu), reads=["ACC"],
             writes=[("fm", XC, c, t) for t in range(NT)])
    for h in range(4):
        for t in range(NT):
            for g, src, dst in ((0, XC, QT), (1, XC, KT), (2, MX, VT)):
                b = next_bank(k)
                S.op("pe", lambda e, b=b, g=g, src=src, h=h, t=t: e.matmul(
                    k.bank(b), WQKV[:, g, h, :], fm(k, src, h, t * 512, 512), start=True, stop=True),
                    reads=["WQKV", ("fm", src, h, t)], writes=[("ps", b)])
                evac(k, fm(k, dst, h, t * 512, 512), k.bank(b), ("ps", b), ("fm", dst, h, t))
        for tt in range(NT128):
            b = next_bank(k)
            S.op("pe", lambda e, b=b, h=h, tt=tt: e.matmul(
                k.bank(b)[:, 0:128], fm(k, MX, h, tt * 128, 128), WQKV[:, 2, h, :], start=True, stop=True),
                reads=["WQKV", ("fm", MX, h, tt // 4)], writes=[("ps", b)])
            evac(k, Vall[:, tt, h, 0:128], k.bank(b)[:, 0:128], ("ps", b), ("V", tt, h))
    for h in range(4):
        S.op("dve", lambda e, h=h: e.tensor_scalar(fm(k, XC, h), fm(k, XC, h), k.skipc[:, h:h + 1], None, ALU.mult),
             reads=[("fm", XC, h, t) for t in range(NT)] + ["lconst"], writes=[("fm", XC, h, t) for t in range(NT)])
    S.barrier()
    for t in range(NT):
        for g, bb in ((0, t), (1, 4 + t)):
            for jj in range(12):
                src = (QT, KT, VT)[jj // 4]
                S.op("pe", lambda e, g=g, bb=bb, jj=jj, src=src, t=t: e.matmul(
                    k.bank(bb)[0:4, :], WIF[:, jj, g * 4:(g + 1) * 4], fm(k, src, jj % 4, t * 512, 512),
                    start=(jj == 0), stop=(jj == 11)), reads=["WIF"], writes=[("ps", bb)])
    T1 = k.f32(W_W, 2048)
    T2 = k.f32(W_W + 2048, 2048)
    T3 = k.f32(W_W + 5120, 2048)
    ONES = k.f32(W_W + 7168, 2048)
    S.op("dve", lambda e: e.memset(ONES[0:4, :], 1.0), writes=["ONES"])
    softplus_neg(k, T1, 4, [4, 5, 6, 7], k.gcols[0:4, 5:6], "T1")
    S.op("dve", lambda e: e.tensor_tensor_scan(T2[0:4, :], ONES[0:4, :], T1[0:4, :], 0.0, ALU.mult, ALU.add), reads=["T1", "ONES"], writes=["T2"])
    for t in range(NT):
        S.op("dve", lambda e, t=t: e.tensor_scalar(T1[0:4, t * 512:(t + 1) * 512], k.bank(t)[0:4, :], k.gcols[0:4, 1:2], None, ALU.add),
             reads=[("ps", t), "gcols", "T2"], writes=["T1"])
    S.op("dve", lambda e: e.tensor_tensor(out=T1[0:4, :], in0=T1[0:4, :], in1=T2[0:4, :], op=ALU.add), reads=["T1", "T2"], writes=["T1"])
    S.op("dve", lambda e: e.tensor_tensor_scan(T3[0:4, :], ONES[0:4, :], T1[0:4, :], 0.0, ALU.mult, ALU.max), reads=["T1", "ONES"], writes=["T3"])
    S.op("dve", lambda e: e.tensor_scalar(T3[0:4, :], T3[0:4, :], -1.0, None, ALU.mult), reads=["T3"], writes=["T3"])
    UR = ONES
    EM = k.f32(W_CONST + 2700, 64)
    UC = k.f32(W_CONST + 2780, 160)
    for j in range(NT):
        nb = T3[0:4, 512 * j - 1:512 * j] if j > 0 else 0.0
        S.op("act", lambda e, j=j, nb=nb: e.activation(out=T2[0:4, j * 512:(j + 1) * 512], in_=T2[0:4, j * 512:(j + 1) * 512],
                                                     func=AF.Exp, bias=nb, scale=1.0), reads=["T2", "T3"], writes=["T2"])
    for tt in range(NT128):
        S.op("pe", lambda e, tt=tt: e.transpose(k.bank(3)[:, tt * 4:(tt + 1) * 4], T2[0:4, tt * 128:(tt + 1) * 128], k.ident[0:4, 0:4]),
             reads=["T2", "ident"], writes=[("ps", 3)])
    S.op("dve", lambda e: e.tensor_copy(EM, k.bank(3)[:, 0:64]), reads=[("ps", 3)], writes=["EM"])
    for j in range(NT):
        nb = T3[0:4, 512 * j - 1:512 * j] if j > 0 else 0.0
        ncol = (j + 1) * 512
        S.op("act", lambda e, nb=nb, ncol=ncol: e.activation(out=UR[0:4, 0:ncol], in_=T1[0:4, 0:ncol], func=AF.Exp, bias=nb, scale=1.0),
             reads=["T1", "T3", "ONES"], writes=["ONES"])
        for i in range(4 * j + 4):
            idx = 2 * j * (j + 1) + i
            S.op("pe", lambda e, i=i, idx=idx: e.transpose(k.bank(2)[:, idx * 4:(idx + 1) * 4], UR[0:4, i * 128:(i + 1) * 128], k.ident[0:4, 0:4]),
                 reads=["ONES", "ident"], writes=[("ps", 2)])
    S.op("dve", lambda e: e.tensor_scalar(UC, k.bank(2)[:, 0:160], 128.0 ** -0.5, None, ALU.mult), reads=[("ps", 2)], writes=["UC"])
    S.barrier()
    k.tbanks = [3, 7]
    TB = SB + 1024
    blocks = []
    for h in range(4):

        def qf(t0, n, h=h):
            return fm(k, QT, h, t0, n)

        def kf(i, h=h):
            return fm(k, KT, h, i * 128, 128)

        def vf(i, h=h):
            return Vall[:, i, h, :]

        def fin(j, r, o, okey, h=h):
            tt = 4 * j + r
            c1, k1 = col(k)
            c2, k2 = col(k)
            c3, k3 = col(k)
            k._fp = getattr(k, "_fp", 0) + 1
            n = k._fp
            NUM = k.f32(TB + (n % 4) * 128, 128)
            TF = k.f32(TB + 640 + (n % 2) * 128, 128)
            HN = k.f32(TB + 896 + (n % 4) * 128, 128)
            ST6 = k.f32(TB + 512 + (n % 8) * 8, 6)
            MV = k.f32(TB + 576 + (n % 8) * 4, 2)
            hk, nk, fk, stk, mvk = ("HH", n % 4), ("HN", n % 4), ("TF", n % 2), ("ST6", n % 8), ("MV", n % 8)
            den = o[:, 128:129]
            S.op("dve", lambda e: e.tensor_tensor(out=c1, in0=den, in1=EM[:, tt * 4 + h:tt * 4 + h + 1], op=ALU.max), reads=[okey, "EM"], writes=[k1])
            S.op("dve", lambda e: e.scalar_tensor_tensor(out=c1, in0=den, scalar=-1.0, in1=c1, op0=ALU.mult, op1=ALU.max), reads=[okey, k1], writes=[k1])
            S.op("dve", lambda e: e.reciprocal(c1, c1), reads=[k1], writes=[k1])
            S.op("dve", lambda e: e.tensor_scalar(NUM, o[:, 0:128], c1, None, ALU.mult), reads=[okey, k1], writes=[hk])
            S.op("dve", lambda e: e.bn_stats(ST6, NUM), reads=[hk], writes=[stk])
            S.op("dve", lambda e: e.bn_aggr(MV, ST6), reads=[stk], writes=[mvk])

            def stage_b():
                S.op("act", lambda e: e.activation(out=c2, in_=MV[:, 1:2], func=AF.Ln, bias=LN_EPS, scale=1.0), reads=[mvk], writes=[k2])
                S.op("act", lambda e: e.activation(out=c2, in_=c2, func=AF.Exp, scale=-0.5), reads=[k2], writes=[k2])

            def post(pq, pkey):
                xs = fm(k, XC, h, tt * 128, 128)
                sz = fm(k, SZ, h, tt * 128, 128)
                S.op("dve", lambda e: e.scalar_tensor_tensor(out=TF, in0=pq, scalar=k.normc[:, h:h + 1], in1=xs, op0=ALU.mult, op1=ALU.add),
                     reads=[pkey, "lconst"], writes=[fk])
                S.op("dve", lambda e: e.tensor_tensor(out=sz, in0=TF, in1=sz, op=ALU.mult), reads=[fk], writes=[("fm", SZ, h, tt)])

            def stage_c():
                S.op("dve", lambda e: e.tensor_scalar(HN, NUM, MV[:, 0:1], c2, ALU.subtract, ALU.mult), reads=[hk, mvk, k2], writes=[nk])
                defer(k, 2, lambda: transpose_to_fm(k, HN, nk, None, None, post=post))
            defer(k, 2, stage_b)
            defer(k, 3, stage_c)

        def scale_fn(i, j, h=h):
            idx = 2 * j * (j + 1) + i
            return UC[:, idx * 4 + h:idx * 4 + h + 1]
        ps_ = AttnPass(k, qf=qf, kf=kf, aqf=None, akf=None, bias_fn=None, vf=vf, vkey=("V", "x"), dv=128, mode="exp", fin=fin,
                       sbanks=[0, 1, 2, 6], obanks=[(4, 5)], act_func=AF.Copy, scale_fn=scale_fn, mask_mul=True)
        blocks += ps_.blocks()
    run_blocks(k, blocks, L=3)
    S.barrier()


def merge_and_out(k, l):
    S, d = k.S, k.d
    OB = [NU(0), NU(2), NU(1)]
    MG = DU(0)
    GT = [k.f32(SB + i * 512, 512) for i in range(2)]
    ACC = k.f32(SB + 1024, 512)
    PB = k.f32(SB + 1536, 512)

    def load_mp(dd):
        s = dd % 2
        tile = k.bf(W_W + s * 2304, 4608)
        S.dma("pool", f"mp{s}", lambda e: e.dma_start(out=tile, in_=d["mpack"][l, dd, :, :]), writes=[("MP", s)])
        return tile

    tiles = {0: load_mp(0), 1: load_mp(1)}
    cnt = 0
    for dd in range(NKC):
        MP = tiles[dd]
        mkey = ("MP", dd % 2)
        for t in range(NT):
            for b in range(3):
                bg, bo = next_bank(k), next_bank(k)
                for kc in range(NKC):
                    S.op("pe", lambda e, bg=bg, b=b, kc=kc, t=t, MP=MP: e.matmul(
                        k.bank(bg), MP[:, b * 1536 + kc * 128:b * 1536 + (kc + 1) * 128], k.H(kc, t * 512, 512),
                        start=(kc == 0), stop=(kc == NKC - 1)), reads=[mkey, ("H", kc, t)], writes=[("ps", bg)])
                for kc in range(4):
                    S.op("pe", lambda e, bo=bo, b=b, kc=kc, t=t, MP=MP: e.matmul(
                        k.bank(bo), MP[:, b * 1536 + 1024 + kc * 128:b * 1536 + 1024 + (kc + 1) * 128], fm(k, OB[b], kc, t * 512, 512),
                        start=(kc == 0), stop=(kc == 3)), reads=[mkey], writes=[("ps", bo)])
                gt = GT[cnt % 2]
                gk = ("GT", cnt % 2)
                cnt += 1
                gbc = k.gb[:, b * 8 + dd:b * 8 + dd + 1]
                S.op("act", lambda e, gt=gt, bg=bg, gbc=gbc: e.activation(out=gt, in_=k.bank(bg), func=AF.Sigmoid, bias=gbc, scale=1.0),
                     reads=[("ps", bg), "lconst"], writes=[gk])
                if b == 0:
                    S.op("dve", lambda e, gt=gt, bo=bo: e.tensor_tensor(out=ACC, in0=gt, in1=k.bank(bo), op=ALU.mult),
                         reads=[gk, ("ps", bo)], writes=["MACC"])
                else:
                    S.op("dve", lambda e, gt=gt, bo=bo: e.tensor_tensor(out=PB, in0=gt, in1=k.bank(bo), op=ALU.mult),
                         reads=[gk, ("ps", bo)], writes=["MPB"])
                    dst = ACC if b == 1 else fm(k, MG, dd, t * 512, 512)
                    dkey = "MACC" if b == 1 else ("fm", MG, dd, t)
                    S.op("dve", lambda e, dst=dst: e.tensor_tensor(out=dst, in0=ACC, in1=PB, op=ALU.add),
                         reads=["MACC", "MPB"], writes=[dkey] + (["MACC"] if b == 2 else []))
        if dd + 2 < NKC:
            tiles[dd + 2] = load_mp(dd + 2)
    S.barrier()
    MN = NU(0)
    for i in range(4):
        S.dma("sp", "mncp", lambda e, i=i: e.dma_start(out=k.bf(MN + i * 2048, 4096), in_=k.bf(MG + i * 2048, 4096)),
              writes=[("MN", i)])
    S.barrier()
    XS = [k.f32(SB + i * 512, 512) for i in range(2)]

    WOT = [k.bf(W_W + dd * 512, 1024) for dd in range(NKC)]
    for dd in range(NKC):
        S.dma("pool", "wo", lambda e, dd=dd: e.dma_start(out=WOT[dd], in_=d["wout"][l, dd, :, :]), writes=[("WO", dd)])
    cnt = 0
    for t in range(NT):
        for dd in range(NKC):
            WO = WOT[dd]
            b = next_bank(k)
            xs = XS[cnt % 2]
            xk = ("XS", cnt % 2)
            cnt += 1
            S.dma("sp", f"xs{cnt % 2}", lambda e, xs=xs, dd=dd, t=t: e.dma_start(out=xs, in_=d["xspill"][:, dd, t * 512:(t + 1) * 512]),
                  writes=[xk])
            for kc in range(NKC):
                S.op("pe", lambda e, b=b, kc=kc, t=t, WO=WO: e.matmul(
                    k.bank(b), WO[:, kc * 128:(kc + 1) * 128], fm(k, MN, kc, t * 512, 512),
                    start=(kc == 0), stop=(kc == NKC - 1)), reads=[("WO", dd)], writes=[("ps", b)])
            S.op("dve", lambda e, b=b, xs=xs, dd=dd, t=t: e.tensor_tensor(out=k.X(dd, t), in0=k.bank(b), in1=xs, op=ALU.add),
                 reads=[("ps", b), xk], writes=[("X", dd, t)])


def spill_x(k):
    S, d = k.S, k.d
    for c in range(NKC):
        o = W_X + c * S_LEN
        S.dma("sp", "xsp", lambda e, c=c, o=o: e.dma_start(out=d["xspill"][:, c, :], in_=k.arena[:, o:o + S_LEN]),
              reads=[("X", c, t) for t in range(NT)], writes=["xspill"])


def mixer(k, l, branches=("ml", "diff", "fox")):
    S = k.S
    layer_consts(k, l)
    rmsnorm(k, l * 3 + 1, "H")
    spill_x(k)
    S.barrier()
    if "ml" in branches:
        mlstm_branch(k, l)
    if "diff" in branches:
        diff_branch(k, l)
    if "fox" in branches:
        fox_branch(k, l)
    merge_and_out(k, l)


def _chunk_rows(w):
    K_, N_ = w.shape
    return w.reshape(K_ // 128, 128, N_).transpose(1, 0, 2)


def _cols(v):
    return v.reshape(-1, 128).T


def prep_shared(inp):
    f = np.float32
    sh = {}
    for which in (1, 2):
        wg, wu, wd = inp[f"ffn{which}_w_gate"], inp[f"ffn{which}_w_up"], inp[f"ffn{which}_w_down"]
        gu = np.empty((DEPTH, NFC, 128, 2, NKC, 128), f)
        for l in range(DEPTH):
            for g, w in enumerate((wg[l], wu[l])):
                gu[l, :, :, g] = w.reshape(NKC, 128, NFC, 128).transpose(2, 1, 0, 3)
        sh[f"ffn{which}_gu"] = gu.reshape(DEPTH, NFC, 128, 2 * NKC * 128)
        sh[f"ffn{which}_wd"] = np.ascontiguousarray(wd.reshape(DEPTH, NFC, 128, D))
    vecs = []
    for l in range(DEPTH):
        vecs += [inp["ffn1_norm"][l], inp["mix_norm"][l], inp["ffn2_norm"][l]]
    vecs.append(inp["final_norm"])
    sh["normw"] = np.ascontiguousarray(np.stack([_cols(v) for v in vecs], axis=1).reshape(128, 56)).astype(f)
    sh["ident"] = np.eye(128, dtype=f)
    sh["ones"] = np.ones((128, 128), f)
    sh["tri"] = np.triu(np.ones((128, 128), f))
    sh["negtri"] = (np.tril(np.ones((128, 128), f), -1) * NEG_BIG).astype(f)
    w_in = inp["w_in"]
    sh["w_in"] = np.ascontiguousarray(np.stack([_chunk_rows(w_in[l]) for l in range(DEPTH)]))
    gb = np.empty((DEPTH, 128, 24), f)
    cw = np.empty((DEPTH, 128, 16), f)
    cb = np.empty((DEPTH, 128, 4), f)
    sk = np.empty((DEPTH, 128, 4), f)
    nmc = np.empty((DEPTH, 128, 4), f)
    gc = np.zeros((DEPTH, 8, 8), f)
    for l in range(DEPTH):
        for b in range(3):
            gb[l, :, b * 8:(b + 1) * 8] = _cols(inp["gate_bias"][l, b])
        for j in range(4):
            cw[l, :, j * 4:(j + 1) * 4] = _cols(inp["mlstm_conv_w"][l, j])
        cb[l] = _cols(inp["mlstm_conv_b"][l])
        sk[l] = _cols(inp["mlstm_skip"][l])
        nmc[l] = _cols(inp["mlstm_norm"][l])
        gc[l, :, 0] = inp["fox_b_f"][l]
        gc[l, 0:4, 1] = inp["mlstm_b_if"][l, 0:4]
        gc[l, 0:4, 2] = inp["mlstm_b_if"][l, 4:8]
    sh["gbcols"], sh["convw"], sh["convb"], sh["skipc"], sh["gcols"] = gb, cw, cb, sk, gc
    sh["normc"] = nmc
    sh["difflam"] = np.ascontiguousarray(np.concatenate(
        [inp["diff_lq1"], inp["diff_lk1"], inp["diff_lq2"], inp["diff_lk2"]], axis=1)).astype(f)
    sh["diff_subln"] = np.ascontiguousarray(inp["diff_subln"]).astype(f)
    sh["mlstm_norm"] = np.ascontiguousarray(inp["mlstm_norm"]).astype(f)
    slopes = [2.0 ** (-8.0 * (h + 1) / 4) for h in range(4)]
    ak = np.zeros((3, 4, 128), f)
    aq = np.zeros((3, 4, 512), f)
    relq = np.arange(512)
    for h in range(4):
        ak[0, h] = slopes[h] * np.arange(128)
        ak[1, h] = 1.0
        ak[2, h] = 1.0
        aq[0, h] = 1.0
        aq[1, h] = -slopes[h] * (128 * (relq // 128))
        aq[2, h] = -slopes[h] * (relq % 128)
    sh["alibi_k"] = ak.reshape(3, 512)
    sh["alibi_q"] = aq.reshape(3, 2048)
    tpos = np.arange(S_LEN)
    akf = np.zeros((4, 3, S_LEN), f)
    aqf = np.zeros((4, 3, S_LEN), f)
    for h in range(4):
        akf[h, 0] = slopes[h] * (tpos % 128)
        akf[h, 1] = 1.0
        akf[h, 2] = 1.0
        aqf[h, 0] = 1.0
        aqf[h, 1] = -slopes[h] * (128 * ((tpos % 512) // 128))
        aqf[h, 2] = -slopes[h] * (tpos % 128)
    sh["alibi_kf"], sh["alibi_qf"] = akf, aqf
    wqkv = np.empty((DEPTH, 128, 3, 4, 128), f)
    for g, nm in enumerate(("mlstm_wq", "mlstm_wk", "mlstm_wv")):
        wqkv[:, :, g] = inp[nm].transpose(0, 2, 1, 3)
    sh["wqkv"] = wqkv.reshape(DEPTH, 128, 1536)
    sh["wif"] = np.ascontiguousarray(inp["mlstm_w_if"].reshape(DEPTH, 12, 128, 8).transpose(0, 2, 1, 3)).reshape(DEPTH, 128, 96)
    mp = np.empty((DEPTH, NKC, 128, 3, 1536), f)
    wbs = (inp["w_branch_diff"], inp["w_branch_fox"], inp["w_branch_mlstm"])
    for l in range(DEPTH):
        for b in range(3):
            g = w_in[l][:, C_G + b * D:C_G + (b + 1) * D]
            mp[l, :, :, b, 0:1024] = g.reshape(NKC, 128, NKC, 128).transpose(2, 1, 0, 3).reshape(NKC, 128, 1024)
            mp[l, :, :, b, 1024:1536] = wbs[b][l].reshape(4, 128, NKC, 128).transpose(2, 1, 0, 3).reshape(NKC, 128, 512)
    sh["mpack"] = mp.reshape(DEPTH, NKC, 128, 4608)
    wo = np.empty((DEPTH, NKC, 128, 1024), f)
    for l in range(DEPTH):
        wo[l] = inp["w_out"][l].reshape(NKC, 128, NKC, 128).transpose(2, 1, 0, 3).reshape(NKC, 128, 1024)
    sh["wout"] = wo
    return sh


def build_program(shapes, plan):
    from contextlib import ExitStack
    nc = bass.Bass("TRN2", target_bir_lowering=False)
    dram = {}
    for name, shp in shapes.items():
        dram[name] = nc.dram_tensor(name, list(shp), F32, kind="ExternalInput").ap()
    dram["outT"] = nc.dram_tensor("outT", [128, NKC, S_LEN], F32, kind="ExternalOutput").ap()
    dram["xspill"] = nc.dram_tensor("xspill", [128, NKC, S_LEN], F32, kind="Internal").ap()
    for nm in ("augk_fox", "augq_fox", "augk_ml", "augq_ml"):
        dram[nm] = nc.dram_tensor(nm, [8, 6, S_LEN], BF16, kind="Internal").ap()
    with ExitStack() as es:
        sems = [es.enter_context(nc.semaphore(f"s{i}")) for i in range(70)]
        S = Sched(nc, sems)
        arena = nc.alloc_sbuf_tensor("arena", [128, ARENA_WORDS], F32)
        ps = es.enter_context(nc.psum_tensor("ps", [128, 4096], F32))
        k = K(nc, S, arena, ps, dram)
        plan(k)
        with nc.Block() as block:
            S.emit(block)
    return nc


def full_plan(k):
    build_consts(k)
    load_x(k)
    for l in range(DEPTH):
        rmsnorm(k, l * 3 + 0, "H")
        ffn(k, l, 1)
        mixer(k, l)
        rmsnorm(k, l * 3 + 2, "H")
        k.S.barrier()
        ffn(k, l, 2)
    rmsnorm(k, 6, "X")
    store_out(k)


def run(inputs, plan=full_plan, trace=False, cores=NB):
    sh = prep_shared(inputs)
    x = np.asarray(inputs["x"], np.float32)
    in_maps = []
    for b in range(cores):
        m = dict(sh)
        m["xT"] = np.ascontiguousarray(x[b].T.reshape(NKC, 128, S_LEN).transpose(1, 0, 2))
        in_maps.append(m)
    shapes = {n: a.shape for n, a in in_maps[0].items()}
    nc = build_program(shapes, plan)
    res = run_bass_kernel_spmd(nc, in_maps, core_ids=list(range(cores)), trace=trace)
    out = np.empty((cores, S_LEN, D), np.float32)
    for b in range(cores):
        o = res.results[b]["outT"]
        out[b] = o.transpose(1, 0, 2).reshape(D, S_LEN).T
    return out, res


def kernel(**inputs):
    out, _ = run(inputs)
    return out
```

```python
import bisect
import numpy as np
import concourse.bass as bass
import concourse.mybir as mybir
from concourse.bass_utils import run_bass_kernel_spmd

F32 = mybir.dt.float32
BF16 = mybir.dt.bfloat16
AF = mybir.ActivationFunctionType
ALU = mybir.AluOpType
AX = mybir.AxisListType


class _Ev:
    __slots__ = ("eng", "idx", "val", "clock")

    def __init__(self, eng, idx, val, clock):
        self.eng, self.idx, self.val, self.clock = eng, idx, val, clock


class _Eng:
    def __init__(self, name, sem, self_sync):
        self.name, self.sem, self.self_sync = name, sem, self_sync
        self.ops = []
        self.count = 0
        self.sig_idx = []
        self.sig_val = []
        self.n_inst = 0
        self.inst_rec = []
        self.clock = {}
        self.last_compute = None


class _Slot:
    def __init__(self, name, sem):
        self.name, self.sem, self.total = name, sem, 0


class _Res:
    __slots__ = ("w", "rs")

    def __init__(self):
        self.w, self.rs = None, []


class Sched:
    def __init__(self, nc, sems):
        self.nc = nc
        self._sems = list(sems)
        self.engs = {}
        for name, ss in (("pe", False), ("act", True), ("dve", True), ("pool", True), ("sp", False)):
            self.engs[name] = _Eng(name, self._sems.pop(), ss)
        self.slots = {}
        self.resd = {}

    def slot(self, name):
        s = self.slots.get(name)
        if s is None:
            s = _Slot(name, self._sems.pop())
            self.slots[name] = s
        return s

    def res(self, key):
        r = self.resd.get(key)
        if r is None:
            r = _Res()
            self.resd[key] = r
        return r

    def _value(self, ev):
        if ev.val is not None:
            return ev.val
        E = self.engs[ev.eng]
        j = bisect.bisect_left(E.sig_idx, ev.idx)
        if j < len(E.sig_idx):
            return E.sig_val[j]
        E.count += 1
        rec = E.inst_rec[ev.idx]
        rec[2], rec[3] = E.sem, 1
        E.sig_idx.append(ev.idx)
        E.sig_val.append(E.count)
        ev.val = E.count
        return ev.val

    def _wait_for(self, E, evs):
        need = {}
        for ev in evs:
            if ev is None:
                continue
            if ev.eng == E.name and not E.self_sync:
                continue
            if ev.eng in self.slots:
                v = self.slots[ev.eng].total
            else:
                v = self._value(ev)
            if E.clock.get(ev.eng, 0) >= v:
                continue
            if need.get(ev.eng, (0, None))[0] < v:
                need[ev.eng] = (v, ev)
        for name, (v, ev) in need.items():
            if E.clock.get(name, 0) >= v:
                continue
            sem = self.slots[name].sem if name in self.slots else self.engs[name].sem
            E.ops.append(["w", sem, v])
            E.clock[name] = v
            for k, cv in ev.clock.items():
                if E.clock.get(k, 0) < cv:
                    E.clock[k] = cv

    def _deps(self, reads, writes):
        evs = []
        for k in reads:
            r = self.res(k)
            if r.w is not None:
                evs.append(r.w)
        for k in writes:
            r = self.res(k)
            if r.w is not None:
                evs.append(r.w)
            evs.extend(r.rs)
        return evs

    def _commit(self, ev, reads, writes):
        for k in reads:
            self.res(k).rs.append(ev)
        for k in writes:
            r = self.res(k)
            r.w, r.rs = ev, []

    def op(self, eng, fn, reads=(), writes=()):
        E = self.engs[eng]
        self._wait_for(E, self._deps(reads, writes))
        rec = ["i", fn, None, 0]
        E.ops.append(rec)
        E.inst_rec.append(rec)
        idx = E.n_inst
        E.n_inst += 1
        E.last_compute = idx
        clk = dict(E.clock)
        ev = _Ev(eng, idx, None, clk)
        self._commit(ev, reads, writes)
        return ev

    def dma(self, queue, slot, fn, reads=(), writes=()):
        E = self.engs[queue]
        S = self.slot(slot) if isinstance(slot, str) else slot
        self._wait_for(E, self._deps(reads, writes))
        S.total += 16
        rec = ["i", fn, S.sem, 16]
        E.ops.append(rec)
        E.inst_rec.append(rec)
        E.n_inst += 1
        ev = _Ev(S.name, -1, S.total, dict(E.clock))
        self._commit(ev, reads, writes)
        return ev

    def barrier(self):
        evs = []
        for E in self.engs.values():
            if E.last_compute is not None:
                ev = _Ev(E.name, E.last_compute, None, dict(E.clock))
                self._value(ev)
                evs.append(ev)
        for S in self.slots.values():
            if S.total:
                evs.append(_Ev(S.name, -1, S.total, {}))
        for E in self.engs.values():
            self._wait_for(E, evs)
        self.resd = {}

    def wait_all_dma(self, eng, slots):
        E = self.engs[eng]
        evs = [_Ev(self.slots[s].name, -1, self.slots[s].total, {}) for s in slots if self.slots[s].total]
        self._wait_for(E, evs)

    def emit(self, block):
        def replay(E):
            def run(e):
                for rec in E.ops:
                    if rec[0] == "w":
                        e.wait_ge(rec[1], rec[2])
                    else:
                        ins = rec[1](e)
                        if rec[2] is not None:
                            ins.then_inc(rec[2], rec[3])
            return run
        block.tensor(replay(self.engs["pe"]))
        block.scalar(replay(self.engs["act"]))
        block.vector(replay(self.engs["dve"]))
        block.gpsimd(replay(self.engs["pool"]))
        block.sync(replay(self.engs["sp"]))


D = 1024
S_LEN = 2048
NB = 8
DEPTH = 2
DFF = 2816
NFC = DFF // 128
NKC = D // 128
NT = S_LEN // 512
NT128 = S_LEN // 128
RMS_EPS = 1e-6
LN_EPS = 1e-5
N_IN = 7176
FFN_GROUPS = [(0, 6), (6, 12), (12, 17), (17, 22)]

W_X = 0
W_H = W_X + 16384
W_CONST = W_H + 8192
W_PT = W_CONST + 3072
W_W = W_PT + 1024
W_N = W_W + 9216
W_END = W_N + 12288
ARENA_WORDS = W_END


class K:
    def __init__(self, nc, S, arena, ps, dram):
        self.nc, self.S, self.arena, self.ps, self.d = nc, S, arena, ps, dram
        self.ps_rr = 0

    def f32(self, off, n):
        return self.arena[:, off:off + n]

    def bf(self, off, n):
        return self.arena[:, off:off + (n + 1) // 2].bitcast(BF16)

    def bank(self, b):
        return self.ps[:, b * 512:(b + 1) * 512]

    def X(self, c, t):
        o = W_X + c * S_LEN + t * 512
        return self.arena[:, o:o + 512]

    def H(self, c, t0=0, n=S_LEN):
        return self.bf(W_H + c * (S_LEN // 2), S_LEN)[:, t0:t0 + n]

    def PT(self, i):
        return self.bf(W_PT + i * 256, 512)


def build_consts(k):
    S, d = k.S, k.d
    o = W_CONST
    k.ident = k.f32(o, 128); o += 128
    k.ones_bf = k.bf(o, 128); o += 64
    k.tri_bf = k.bf(o, 128); o += 64
    k.normw = k.f32(o, 7 * 8); o += 56
    k.const_end = o
    S.dma("sp", "c0", lambda e: e.dma_start(out=k.ident, in_=d["ident"][:, :]), writes=["ident"])
    S.dma("pool", "c1", lambda e: e.dma_start(out=k.ones_bf, in_=d["ones"][:, :]), writes=["ones"])
    S.dma("pool", "c1", lambda e: e.dma_start(out=k.tri_bf, in_=d["tri"][:, :]), writes=["tri"])
    S.dma("sp", "c0", lambda e: e.dma_start(out=k.normw, in_=d["normw"][:, :]), writes=["normw"])


def load_x(k):
    S, d = k.S, k.d
    for c in range(NKC):
        o = W_X + c * S_LEN
        S.dma("sp", "xld", lambda e, c=c, o=o: e.dma_start(out=k.arena[:, o:o + S_LEN], in_=d["xT"][:, c, :]),
              writes=[("X", c, t) for t in range(NT)])


def rmsnorm(k, widx, out_mode):
    S = k.S
    R0 = W_N + 8192
    for t in range(NT):
        for c in range(NKC):
            pt = k.PT((t * NKC + c) % 4)
            key = ("PT", (t * NKC + c) % 4)
            if c % 2 == 0:
                S.op("act", lambda e, pt=pt, c=c, t=t: e.activation(out=pt, in_=k.X(c, t), func=AF.Square),
                     reads=[("X", c, t)], writes=[key])
            else:
                S.op("dve", lambda e, pt=pt, c=c, t=t: e.tensor_tensor(out=pt, in0=k.X(c, t), in1=k.X(c, t), op=ALU.mult),
                     reads=[("X", c, t)], writes=[key])
            S.op("pe", lambda e, pt=pt, c=c, t=t: e.matmul(k.bank(t), k.ones_bf, pt, start=(c == 0), stop=(c == NKC - 1)),
                 reads=[key, "ones"], writes=[("ps", t)])
    for t in range(NT):
        R = k.f32(R0 + t * 512, 512)
        S.op("act", lambda e, R=R, t=t: e.activation(out=R, in_=k.bank(t), func=AF.Ln, bias=RMS_EPS, scale=1.0 / D),
             reads=[("ps", t)], writes=[("R", t)])
        S.op("act", lambda e, R=R: e.activation(out=R, in_=R, func=AF.Exp, scale=-0.5), reads=[("R", t)], writes=[("R", t)])
        for c in range(NKC):
            wcol = k.normw[:, widx * 8 + c:widx * 8 + c + 1]
            if out_mode == "H":
                S.op("dve", lambda e, R=R, c=c, t=t, wcol=wcol: e.scalar_tensor_tensor(
                    out=k.H(c, t * 512, 512), in0=k.X(c, t), scalar=wcol, in1=R, op0=ALU.mult, op1=ALU.mult),
                    reads=[("X", c, t), ("R", t), "normw"], writes=[("H", c, t)])
            else:
                S.op("dve", lambda e, R=R, c=c, t=t, wcol=wcol: e.scalar_tensor_tensor(
                    out=k.X(c, t), in0=k.X(c, t), scalar=wcol, in1=R, op0=ALU.mult, op1=ALU.mult),
                    reads=[("X", c, t), ("R", t), "normw"], writes=[("X", c, t)])


def ffn(k, l, which):
    S, d = k.S, k.d
    gu_d = d[f"ffn{which}_gu"]
    wd_d = d[f"ffn{which}_wd"]
    GU = [k.bf(W_W + i * 1024, 2048) for i in range(3)]
    WD = [k.bf(W_W + 3072 + i * 3072, 6 * 1024) for i in range(2)]
    A = [k.bf(W_N + i * 1024, 2048) for i in range(7)]
    SG = [k.f32(W_N + 7168 + i * 512, 512) for i in range(2)]

    def load_gu(c):
        s = c % 3
        S.dma("pool", f"gu{s}", lambda e: e.dma_start(out=GU[s], in_=gu_d[l, c, :, :]), writes=[("GU", s)])

    def load_wd(g):
        s = g % 2
        c0, c1 = FFN_GROUPS[g]
        for c in range(c0, c1):
            S.dma("pool", f"wd{s}", lambda e, c=c: e.dma_start(out=WD[s][:, (c - c0) * 1024:(c - c0 + 1) * 1024], in_=wd_d[l, c, :, :]),
                  writes=[("WD", s)])

    def gu_chunk(c):
        s = c % 3
        for t in range(NT):
            pr = (c * NT + t) % 3
            bg, bu = 2 * pr, 2 * pr + 1
            for g, b in ((0, bg), (1, bu)):
                for kc in range(NKC):
                    S.op("pe", lambda e, g=g, b=b, kc=kc, t=t: e.matmul(
                        k.bank(b), GU[s][:, (g * 8 + kc) * 128:(g * 8 + kc + 1) * 128], k.H(kc, t * 512, 512),
                        start=(kc == 0), stop=(kc == NKC - 1)),
                        reads=[("GU", s), ("H", kc, t)], writes=[("ps", b)])
            sg = SG[(c * NT + t) % 2]
            sgk = ("SG", (c * NT + t) % 2)
            S.op("act", lambda e, sg=sg, bg=bg: e.activation(out=sg, in_=k.bank(bg), func=AF.Silu),
                 reads=[("ps", bg)], writes=[sgk])
            S.op("dve", lambda e, sg=sg, bu=bu, t=t: e.tensor_tensor(
                out=A[c % 7][:, t * 512:(t + 1) * 512], in0=sg, in1=k.bank(bu), op=ALU.mult),
                reads=[sgk, ("ps", bu)], writes=[("A", c % 7, t)])

    dn_cnt = [0]

    def down(g):
        s = g % 2
        c0, c1 = FFN_GROUPS[g]
        for t in range(NT):
            for dd in range(NKC):
                b = 6 + dn_cnt[0] % 2
                dn_cnt[0] += 1
                for c in range(c0, c1):
                    S.op("pe", lambda e, b=b, c=c, dd=dd, t=t: e.matmul(
                        k.bank(b), WD[s][:, (c - c0) * 1024 + dd * 128:(c - c0) * 1024 + (dd + 1) * 128],
                        A[c % 7][:, t * 512:(t + 1) * 512], start=(c == c0), stop=(c == c1 - 1)),
                        reads=[("WD", s), ("A", c % 7, t)], writes=[("ps", b)])
                S.op("dve", lambda e, b=b, dd=dd, t=t: e.scalar_tensor_tensor(
                    out=k.X(dd, t), in0=k.bank(b), scalar=0.5, in1=k.X(dd, t), op0=ALU.mult, op1=ALU.add),
                    reads=[("ps", b), ("X", dd, t)], writes=[("X", dd, t)])

    for c in range(3):
        load_gu(c)
    load_wd(0)
    load_wd(1)
    grp_of = {}
    for g, (c0, c1) in enumerate(FFN_GROUPS):
        for c in range(c0, c1):
            grp_of[c] = g
    for c in range(NFC):
        gu_chunk(c)
        if c + 3 < NFC:
            load_gu(c + 3)
        g = grp_of[c]
        if c == FFN_GROUPS[g][0] and g > 0:
            down(g - 1)
            if g + 1 < len(FFN_GROUPS):
                load_wd(g + 1)
    down(len(FFN_GROUPS) - 1)


def store_out(k):
    S, d = k.S, k.d
    for c in range(NKC):
        o = W_X + c * S_LEN
        S.dma("sp", "ost", lambda e, c=c, o=o: e.dma_start(out=d["outT"][:, c, :], in_=k.arena[:, o:o + S_LEN]),
              reads=[("X", c, t) for t in range(NT)])
    S.wait_all_dma("sp", ["ost"])


U = 4096
W_SCR = W_END + 64
ARENA_WORDS = W_END + 2560
SB = W_SCR + 64
NEG_BIG = -30000.0
WARM_FILL = 0
C_DQ, C_DK, C_DV = 0, 512, 1024
C_FQ, C_FK, C_FV, C_FF = 1536, 2048, 2560, 3072
C_MX, C_MZ, C_G = 3080, 3592, 4104


def DU(i):
    return W_X + i * U


def NU(i):
    return W_N + i * U


def fm(k, off, c, t0=0, n=S_LEN):
    return k.bf(off + c * (S_LEN // 2), S_LEN)[:, t0:t0 + n]


def next_bank(k):
    b = k.ps_rr % 8
    k.ps_rr += 1
    return b


def col(k):
    i = getattr(k, "_col_rr", 0)
    k._col_rr = i + 1
    i %= 48
    return k.arena[:, W_SCR + i:W_SCR + i + 1], ("col", i)


def evac(k, dst, src, src_key, dst_key, scale=None, func=None, eng=None):
    S = k.S
    if eng is None:
        k._ev_rr = getattr(k, "_ev_rr", 0) + 1
        eng = "act" if (func is not None or k._ev_rr % 2 == 0) else "dve"
    if eng == "act":
        f = func if func is not None else AF.Copy
        sc = 1.0 if scale is None else scale
        S.op("act", lambda e: e.activation(out=dst, in_=src, func=f, scale=sc), reads=[src_key], writes=[dst_key])
    else:
        if scale is None:
            S.op("dve", lambda e: e.tensor_copy(dst, src), reads=[src_key], writes=[dst_key])
        else:
            S.op("dve", lambda e: e.tensor_scalar(dst, src, scale, None, ALU.mult), reads=[src_key], writes=[dst_key])


def wcol_jobs(k, l, jobs):
    S, d = k.S, k.d

    def load(i):
        c0, nc_, _ = jobs[i]
        s = i % 2
        tile = k.bf(W_W + s * 2048, 4096).rearrange("p (k c) -> p k c", k=8)
        S.dma("pool", f"wc{s}", lambda e: e.dma_start(out=tile[:, :, 0:nc_], in_=d["w_in"][l, :, :, c0:c0 + nc_]),
              writes=[("WC", s)])
        return tile

    tiles = {}
    for i in range(min(2, len(jobs))):
        tiles[i] = load(i)
    for i in range(len(jobs)):
        jobs[i][2](tiles[i], ("WC", i % 2))
        if i + 2 < len(jobs):
            tiles[i + 2] = load(i + 2)


def h_fm(dst_off, scale=None, func=None):
    def mk(k, c_base, nchunks=4):
        def handler(W, wkey):
            S = k.S
            for m in range(nchunks):
                for t in range(NT):
                    b = next_bank(k)
                    for kc in range(NKC):
                        S.op("pe", lambda e, b=b, m=m, kc=kc, t=t: e.matmul(
                            k.bank(b), W[:, kc, m * 128:(m + 1) * 128], k.H(kc, t * 512, 512),
                            start=(kc == 0), stop=(kc == NKC - 1)),
                            reads=[wkey, ("H", kc, t)], writes=[("ps", b)])
                    evac(k, fm(k, dst_off, c_base + m, t * 512, 512), k.bank(b), ("ps", b),
                         ("fm", dst_off, c_base + m, t), scale=scale, func=func)
        return handler
    return mk


def h_tm(k, vbuf_fn, nh, dv):
    def handler(W, wkey):
        S = k.S
        for tt in range(NT128):
            b = next_bank(k)
            for kc in range(NKC):
                S.op("pe", lambda e, b=b, kc=kc, tt=tt: e.matmul(
                    k.bank(b), k.H(kc, tt * 128, 128), W[:, kc, 0:512],
                    start=(kc == 0), stop=(kc == NKC - 1)),
                    reads=[wkey, ("H", kc, tt // 4)], writes=[("ps", b)])
            dst = vbuf_fn(tt)[:, :, 0:dv]
            src = k.bank(b).rearrange("p (h e) -> p h e", h=nh)
            evac(k, dst, src, ("ps", b), ("V", tt))
    return handler


class AttnPass:
    def __init__(self, k, *, qf, kf, aqf, akf, bias_fn, vf, vkey, dv, mode, fin, sbanks, obanks, scale=1.0, augkey=None, pre=None, act_func=None, scale_fn=None, mask_mul=False):
        self.__dict__.update(locals())
        self.state = {}
        self.per_bank = 2 if dv == 128 else 4

    def blocks(self):
        return [(self, j, i) for j in range(4) for i in range(4 * j + 4)]

    def O(self, j, r):
        pb = self.per_bank
        ob = self.obanks[j % len(self.obanks)][r // pb]
        c0 = (r % pb) * (self.dv + 1)
        return self.k.bank(ob)[:, c0:c0 + self.dv + 1], ("ps", ob)

    def emit_s(self, j, i):
        k, S = self.k, self.k.S
        if self.pre is not None:
            pl = self.pre if isinstance(self.pre, list) else [self.pre]
            self.pre = None
            for f_ in pl:
                f_()
        r0 = max(0, i - 4 * j)
        n0 = r0 * 128
        ncol = 512 - n0
        diag = i >= 4 * j
        k._blk = getattr(k, "_blk", 0) + 1
        pti = k._blk % 4
        pt = k.PT(pti)
        ptk = ("PT", pti)
        if self.mode == "exp":
            sb = self.sbanks[k._blk % len(self.sbanks)]
            has_aug = self.akf is not None
            for rep in range(1 + WARM_FILL):
                real = (rep == WARM_FILL)
                S.op("pe", lambda e, real=real: e.matmul(k.bank(sb)[:, n0:512], self.kf(i), self.qf(j * 512 + n0, ncol),
                                                         start=True, stop=((not has_aug and (not diag or self.mask_mul)) or not real)),
                     reads=([self.augkey] if (self.augkey is not None and not has_aug) else []), writes=[("ps", sb)])
            if has_aug:
                S.op("pe", lambda e: e.matmul(k.bank(sb)[:, n0:512], self.akf(i), self.aqf(j, n0, ncol),
                                              start=False, stop=(not diag)),
                     reads=[self.augkey], writes=[("ps", sb)])
            if diag and not self.mask_mul:
                S.op("pe", lambda e: e.matmul(k.bank(sb)[:, n0:n0 + 128], k.ident_bf, k.negtri_bf, start=False, stop=True),
                     reads=["identbf", "negtri"], writes=[("ps", sb)])
            bias = float(self.bias_fn(i, j)) if self.bias_fn is not None else 0.0
            if self.act_func is None:
                S.op("act", lambda e: e.activation(out=pt[:, n0:512], in_=k.bank(sb)[:, n0:512], func=AF.Exp, bias=bias, scale=1.0),
                     reads=[("ps", sb)], writes=[ptk])
            else:
                sc_ap = self.scale_fn(i, j)
                S.op("act", lambda e: e.activation(out=pt[:, n0:512], in_=k.bank(sb)[:, n0:512], func=self.act_func, scale=sc_ap),
                     reads=[("ps", sb), "UC"], writes=[ptk])
            if diag and self.mask_mul:
                S.op("pool", lambda e: e.tensor_tensor(out=pt[:, n0:n0 + 128], in0=pt[:, n0:n0 + 128], in1=k.tri_bf, op=ALU.mult),
                     reads=[ptk, "tri"], writes=[ptk])
        else:
            pr = self.sbanks[k._blk % len(self.sbanks)]
            sb, eb = pr
            S.op("pe", lambda e: e.matmul(k.bank(sb)[:, n0:512], self.kf(i), self.qf(j * 512 + n0, ncol), start=True, stop=True),
                 reads=[], writes=[("ps", sb)])
            S.op("pe", lambda e: e.matmul(k.bank(eb)[:, n0:512], self.akf(i), self.aqf(j, n0, ncol), start=True, stop=(not diag)),
                 reads=[self.augkey], writes=[("ps", eb)])
            if diag:
                S.op("pe", lambda e: e.matmul(k.bank(eb)[:, n0:n0 + 128], k.ident_bf, k.negtri_bf, start=False, stop=True),
                     reads=["identbf", "negtri"], writes=[("ps", eb)])
            eti = k._blk % 2
            et = k.f32(SB + eti * 512, 512)
            S.op("act", lambda e: e.activation(out=et[:, n0:512], in_=k.bank(eb)[:, n0:512], func=AF.Exp),
                 reads=[("ps", eb)], writes=[("ET", eti)])
            sc = self.scale
            S.op("dve", lambda e: e.scalar_tensor_tensor(out=pt[:, n0:512], in0=k.bank(sb)[:, n0:512], scalar=sc,
                                                         in1=et[:, n0:512], op0=ALU.mult, op1=ALU.mult),
                 reads=[("ps", sb), ("ET", eti)], writes=[ptk])
        self.state[(j, i)] = (pt, ptk, r0)

    def emit_pv(self, j, i):
        k, S = self.k, self.k.S
        pt, ptk, r0 = self.state.pop((j, i))
        for r in range(r0, 4):
            o, okey = self.O(j, r)
            last = (i == 4 * j + r)
            S.op("pe", lambda e, r=r, o=o, last=last: e.matmul(o, pt[:, r * 128:(r + 1) * 128], self.vf(i), start=(i == 0 and r % self.per_bank == 0), stop=last, skip_group_check=True),
                 reads=[ptk, self.vkey], writes=[okey])
            if last:
                self.fin(j, r, o, okey)


def run_blocks(k, blocks, L=2):
    n = len(blocks)
    for idx in range(n + L):
        if idx < n:
            p, j, i = blocks[idx]
            p.emit_s(j, i)
        if idx >= L:
            p, j, i = blocks[idx - L]
            p.emit_pv(j, i)
        tick(k)
    flush_deferred(k)


def defer(k, n, fn):
    k._dq = getattr(k, "_dq", [])
    k._dq.append([n, fn])


def tick(k):
    dq = getattr(k, "_dq", [])
    k._dq = []
    keep = []
    for it in dq:
        it[0] -= 1
        if it[0] <= 0:
            it[1]()
        else:
            keep.append(it)
    k._dq = keep + k._dq


def flush_deferred(k):
    while getattr(k, "_dq", []):
        dq = k._dq
        k._dq = []
        for it in dq:
            it[1]()


def transpose_to_fm(k, src, src_key, dst, dst_key, post=None):
    S = k.S
    q = getattr(k, "_tq", 0)
    k._tq = q + 1
    tb = k.tbanks[q % len(k.tbanks)]
    pq = k.bank(tb)[:, 0:128]
    pkey = ("ps", tb)
    S.op("pe", lambda e: e.transpose(pq, src, k.ident), reads=[src_key, "ident"], writes=[pkey])
    if post is None:
        evac(k, dst, pq, pkey, dst_key, eng="act")
    else:
        post(pq, pkey)


def softplus_neg(k, T, nh, banks, negb_col, key):
    S = k.S
    for t in range(NT):
        b = banks[t]
        S.op("act", lambda e, b=b, t=t: e.activation(out=T[0:nh, t * 512:(t + 1) * 512], in_=k.bank(b)[0:nh, :],
                                                    func=AF.Exp, bias=negb_col, scale=-1.0),
             reads=[("ps", b), "gcols"], writes=[key])
    S.op("act", lambda e: e.activation(out=T[0:nh, :], in_=T[0:nh, :], func=AF.Ln, bias=1.0, scale=1.0),
         reads=[key], writes=[key])


def build_aug(k, kvec, kkey, qvec, qkey, nh, spl_off, tag):
    S, d = k.S, k.d
    dk, dq = d["augk_" + tag], d["augq_" + tag]
    SPL = [k.bf(spl_off + i * 1024, 2048) for i in range(4)]
    ones = SPL[3]
    S.op("dve", lambda e: e.memset(ones[0:nh, :], 1.0), writes=[("SPL", 3)])
    for r in range(3):
        S.dma("sp", "augw", lambda e, r=r: e.dma_start(out=dk[0:nh, 3 + r, :], in_=ones[0:nh, :]), reads=[("SPL", 3)], writes=[("augd", tag)])
        S.dma("sp", "augw", lambda e, r=r: e.dma_start(out=dq[0:nh, r, :], in_=ones[0:nh, :]), reads=[("SPL", 3)], writes=[("augd", tag)])
    cnt = 0
    for vec, vkey, dram, r0 in ((kvec, kkey, dk, 0), (qvec, qkey, dq, 3)):
        for r in range(3):
            si = cnt % 3
            cnt += 1
            spl = SPL[si]
            S.op("dve", lambda e, spl=spl, vec=vec: e.tensor_copy(spl[0:nh, :], vec[0:nh, :]), reads=[vkey], writes=[("SPL", si)])
            if r < 2:
                S.op("dve", lambda e, spl=spl, vec=vec: e.tensor_tensor(out=vec[0:nh, :], in0=vec[0:nh, :], in1=spl[0:nh, :], op=ALU.subtract),
                     reads=[vkey, ("SPL", si)], writes=[vkey])
            S.dma("sp", "augw", lambda e, spl=spl, dram=dram, rr=r0 + r: e.dma_start(out=dram[0:nh, rr, :], in_=spl[0:nh, :]),
                  reads=[("SPL", si)], writes=[("augd", tag)])


def aug_views(k, slot):
    ak = k.bf(W_W + 5120 + slot * 2048, 2048)
    aq = k.bf(W_W + 5120 + slot * 2048 + 1024, 2048)
    akf = lambda i: ak[0:6, i * 128:(i + 1) * 128]
    aqf = lambda j, n0, n: aq[0:6, j * 512 + n0:j * 512 + n0 + n]
    return akf, aqf


def load_aug(k, tag, h, slot):
    S, d = k.S, k.d
    ak = k.bf(W_W + 5120 + slot * 2048, 2048)
    aq = k.bf(W_W + 5120 + slot * 2048 + 1024, 2048)
    S.dma("sp", f"augl{slot}", lambda e: e.dma_start(out=ak[0:6, :], in_=d["augk_" + tag][h, :, :]), reads=[("augd", tag)], writes=[("AUGS", slot)])
    S.dma("sp", f"augl{slot}", lambda e: e.dma_start(out=aq[0:6, :], in_=d["augq_" + tag][h, :, :]), reads=[("augd", tag)], writes=[("AUGS", slot)])


HB_OFF = [W_W + 0, W_W + 2048, W_W + 5120, W_W + 7168]


def hb_views(k, slot):
    return k.bf(HB_OFF[slot], 2048), k.bf(HB_OFF[slot] + 1024, 2048)


def hb_init(k):
    S = k.S
    for s_ in range(4):
        qb, kb = hb_views(k, s_)
        S.op("act", lambda e, qb=qb: e.memzero(qb), writes=[("HB", s_)])
        S.op("act", lambda e, kb=kb: e.memzero(kb), writes=[("HB", s_)])


def hb_build(k, slot, par, q_src, k_src, augq_ap, augk_ap, R, queue):
    S = k.S
    qb, kb = hb_views(k, slot)
    r0 = par * 64
    a0 = 64 if par == 0 else 0
    S.dma("sp", f"hbsp{slot}", lambda e: e.dma_start(out=qb[r0:r0 + 64, :], in_=q_src[r0:r0 + 64, :]), writes=[("HB", slot)])
    S.dma("sp", f"hbsp{slot}", lambda e: e.dma_start(out=kb[r0:r0 + 64, :], in_=k_src[r0:r0 + 64, :]), writes=[("HB", slot)])
    S.dma(queue, f"hb{queue}{slot}", lambda e: e.dma_start(out=qb[a0:a0 + R, :], in_=augq_ap), writes=[("HB", slot)])
    S.dma(queue, f"hb{queue}{slot}", lambda e: e.dma_start(out=kb[a0:a0 + R, :], in_=augk_ap), writes=[("HB", slot)])


def layer_consts(k, l):
    S, d = k.S, k.d
    o = k.const_end
    k.gb = k.f32(o, 24); o += 24
    k.convw = k.f32(o, 16); o += 16
    k.convb = k.f32(o, 4); o += 4
    k.skipc = k.f32(o, 4); o += 4
    k.normc = k.f32(o, 4); o += 4
    k.gcols = k.f32(o, 8); o += 8
    k.lamt = k.f32(o, 256); o += 256
    k.lamc = k.f32(o, 8); o += 8
    k.subln = k.f32(o, 128); o += 128
    k.normB = k.f32(o, 512); o += 512
    k.AKd = k.bf(o, 4 * 128); o += 256
    k.AQd = k.bf(o, 4 * 512); o += 1024
    k.ident_bf = k.bf(o, 128); o += 64
    k.negtri_bf = k.bf(o, 128); o += 64
    assert o <= W_CONST + 3072, o
    S.dma("sp", "lc", lambda e: e.dma_start(out=k.gb, in_=d["gbcols"][l, :, :]), writes=["lconst"])
    S.dma("sp", "lc", lambda e: e.dma_start(out=k.convw, in_=d["convw"][l, :, :]), writes=["lconst"])
    S.dma("sp", "lc", lambda e: e.dma_start(out=k.convb, in_=d["convb"][l, :, :]), writes=["lconst"])
    S.dma("sp", "lc", lambda e: e.dma_start(out=k.skipc, in_=d["skipc"][l, :, :]), writes=["lconst"])
    S.dma("sp", "lc", lambda e: e.dma_start(out=k.normc, in_=d["normc"][l, :, :]), writes=["lconst"])
    S.dma("sp", "lc", lambda e: e.dma_start(out=k.gcols[0:8, :], in_=d["gcols"][l, :, :]), writes=["gcols"])
    S.dma("sp", "lc", lambda e: e.dma_start(out=k.lamt, in_=d["difflam"][l:l + 1, :].partition_broadcast(128)), writes=["lamt"])
    S.dma("sp", "lc", lambda e: e.dma_start(out=k.subln, in_=d["diff_subln"][l:l + 1, :].partition_broadcast(128)), writes=["subln"])
    S.dma("sp", "lc", lambda e: e.dma_start(out=k.normB, in_=d["mlstm_norm"][l:l + 1, :].partition_broadcast(128)), writes=["normB"])
    S.op("dve", lambda e: e.tensor_scalar(k.gcols[0:8, 4:5], k.gcols[0:8, 0:1], -1.0, None, ALU.mult), reads=["gcols"], writes=["gcols"])
    S.op("dve", lambda e: e.tensor_scalar(k.gcols[0:8, 5:6], k.gcols[0:8, 2:3], -1.0, None, ALU.mult), reads=["gcols"], writes=["gcols"])
    if l == 0:
        S.dma("pool", "lc2", lambda e: e.dma_start(out=k.AKd[0:3, :], in_=d["alibi_k"][:, :]), writes=["alibi"])
        S.dma("pool", "lc2", lambda e: e.dma_start(out=k.AQd[0:3, :], in_=d["alibi_q"][:, :]), writes=["alibi"])
        S.dma("pool", "lc2", lambda e: e.dma_start(out=k.ident_bf, in_=d["ident"][:, :]), writes=["identbf"])
        S.dma("pool", "lc2", lambda e: e.dma_start(out=k.negtri_bf, in_=d["negtri"][:, :]), writes=["negtri"])
    import math
    lam_init = 0.8 - 0.6 * math.exp(-0.3 * l)
    lt, lc = k.lamt, k.lamc
    S.op("dve", lambda e: e.tensor_tensor(out=lt[:, 0:64], in0=lt[:, 0:64], in1=lt[:, 64:128], op=ALU.mult), reads=["lamt"], writes=["lamt"])
    S.op("dve", lambda e: e.tensor_tensor(out=lt[:, 128:192], in0=lt[:, 128:192], in1=lt[:, 192:256], op=ALU.mult), reads=["lamt"], writes=["lamt"])
    S.op("dve", lambda e: e.reduce_sum(out=lc[:, 0:1], in_=lt[:, 0:64], axis=AX.X), reads=["lamt"], writes=["lamc"])
    S.op("dve", lambda e: e.reduce_sum(out=lc[:, 1:2], in_=lt[:, 128:192], axis=AX.X), reads=["lamt"], writes=["lamc"])
    S.op("act", lambda e: e.activation(out=lc[:, 2:4], in_=lc[:, 0:2], func=AF.Exp), reads=["lamc"], writes=["lamc"])
    S.op("dve", lambda e: e.tensor_tensor(out=lc[:, 4:5], in0=lc[:, 3:4], in1=lc[:, 2:3], op=ALU.subtract), reads=["lamc"], writes=["lamc"])
    S.op("dve", lambda e: e.tensor_scalar(lc[:, 5:6], lc[:, 4:5], -lam_init, None, ALU.add), reads=["lamc"], writes=["neglam"])
    S.op("dve", lambda e: e.tensor_scalar(k.subln, k.subln, 1.0 - lam_init, None, ALU.mult), reads=["subln"], writes=["subln"])
    k.neglam = lc[:, 5:6]


def diff_branch(k, l):
    S = k.S
    QT, KT, VO, OD = DU(0), DU(1), DU(2), NU(0)
    V = k.bf(VO, 16 * 4 * 129).rearrange("p (t h e) -> p t h e", t=16, h=4)
    S.op("dve", lambda e: e.memset(V[:, :, :, 128:129], 1.0), writes=[("V", "ones")])
    wcol_jobs(k, l, [
        (C_DQ, 512, h_fm(QT)(k, 0)),
        (C_DK, 512, h_fm(KT, scale=0.125)(k, 0)),
        (C_DV, 512, h_tm(k, lambda tt: V[:, tt, :, :], 4, 128)),
    ])
    S.barrier()
    k.tbanks = [3]
    slopes = [2.0 ** (-8.0 * (h + 1) / 4) for h in range(4)]
    A1 = [[k.f32(SB + (p * 4 + r) * 128, 128) for r in range(4)] for p in range(2)]
    TB = SB + 1024
    blocks = []
    pres = []
    first_pass = []
    hb_init(k)
    for h in range(4):
        passes = []
        for c in range(2):
            slot = c + 2 * (h % 2)
            qb, kb = hb_views(k, slot)

            def qf(t0, n, qb=qb):
                return qb[:, t0:t0 + n]

            def kf(i, kb=kb):
                return kb[:, i * 128:(i + 1) * 128]

            pres.append(lambda h=h, c=c, slot=slot: hb_build(k, slot, c, fm(k, QT, h), fm(k, KT, h),
                                                              k.d["alibi_qf"][h, :, :], k.d["alibi_kf"][h, :, :], 3, "pool"))

            def bias_fn(i, j, h=h):
                return slopes[h] * (128.0 * i - 512.0 * j)

            def vf(i, h=h):
                return V[:, i, h, :]

            if c == 0:
                def fin(j, r, o, okey, h=h):
                    rc, rk = col(k)
                    a1 = A1[j % 2][r]
                    S.op("dve", lambda e: e.reciprocal(rc, o[:, 128:129]), reads=[okey], writes=[rk])
                    S.op("dve", lambda e: e.tensor_scalar(a1, o[:, 0:128], rc, None, ALU.mult), reads=[okey, rk], writes=[("A1", j % 2, r)])
            else:
                def fin(j, r, o, okey, h=h):
                    rc, rk = col(k)
                    sc, sk = col(k)
                    k._fp = getattr(k, "_fp", 0) + 1
                    n = k._fp
                    tmp = k.f32(TB + (n % 5) * 128, 128)
                    sq = k.f32(TB + 640 + (n % 2) * 128, 128)
                    ot = k.f32(TB + 896 + (n % 4) * 128, 128)
                    tk_, sqk, otk = ("TMP", n % 5), ("SQ", n % 2), ("OT", n % 4)
                    a1 = A1[j % 2][r]
                    tt = 4 * j + r
                    S.op("dve", lambda e: e.reciprocal(rc, o[:, 128:129]), reads=[okey], writes=[rk])
                    S.op("dve", lambda e: e.tensor_scalar(tmp, o[:, 0:128], rc, None, ALU.mult), reads=[okey, rk], writes=[tk_])
                    S.op("dve", lambda e: e.scalar_tensor_tensor(out=tmp, in0=tmp, scalar=k.neglam, in1=a1, op0=ALU.mult, op1=ALU.add),
                         reads=[tk_, ("A1", j % 2, r), "neglam"], writes=[tk_])

                    def stage_b():
                        S.op("act", lambda e: e.activation(out=sq, in_=tmp, func=AF.Square, accum_out=sc), reads=[tk_], writes=[sqk, sk])
                        S.op("act", lambda e: e.activation(out=sc, in_=sc, func=AF.Ln, bias=RMS_EPS, scale=1.0 / 128), reads=[sk], writes=[sk])
                        S.op("act", lambda e: e.activation(out=sc, in_=sc, func=AF.Exp, scale=-0.5), reads=[sk], writes=[sk])

                    def stage_c():
                        S.op("dve", lambda e: e.scalar_tensor_tensor(out=ot, in0=tmp, scalar=sc, in1=k.subln, op0=ALU.mult, op1=ALU.mult),
                             reads=[tk_, sk, "subln"], writes=[otk])
                        defer(k, 2, lambda: transpose_to_fm(k, ot, otk, fm(k, OD, h, tt * 128, 128), ("fm", OD, h, tt)))
                    defer(k, 2, stage_b)
                    defer(k, 3, stage_c)
            passes.append(AttnPass(k, qf=qf, kf=kf, aqf=None, akf=None, bias_fn=bias_fn, vf=vf, vkey=("V", "x"), dv=128,
                                   mode="exp", fin=fin, sbanks=[0, 1, 2], obanks=[(4, 5)] if c == 0 else [(6, 7)], augkey=("HB", slot)))
        first_pass.append(passes[0])
        b0, b1 = passes[0].blocks(), passes[1].blocks()
        for j in range(4):
            blocks += [b for b in b0 if b[1] == j]
            blocks += [b for b in b1 if b[1] == j]
    for u in range(4):
        first_pass[u].pre = (pres[0:4] if u == 0 else (pres[2 * u + 2:2 * u + 4] if u < 3 else None))
    run_blocks(k, blocks)
    S.barrier()


def fox_branch(k, l):
    S, d = k.S, k.d
    QT, KT, VO, OF = DU(0), DU(1), DU(2), NU(2)
    V = k.bf(VO, 16 * 8 * 65).rearrange("p (t h e) -> p t h e", t=16, h=8)
    WFF = k.bf(W_W + 4096, 64).rearrange("p (k c) -> p k c", k=8)
    S.op("dve", lambda e: e.memset(V[:, :, :, 64:65], 1.0), writes=[("V", "ones")])
    S.dma("pool", "wff", lambda e: e.dma_start(out=WFF, in_=d["w_in"][l, :, :, C_FF:C_FF + 8]), writes=["WFF"])
    S.barrier()
    wcol_jobs(k, l, [
        (C_FQ, 512, h_fm(QT)(k, 0)),
        (C_FK, 512, h_fm(KT, scale=0.125)(k, 0)),
        (C_FV, 512, h_tm(k, lambda tt: V[:, tt, :, :], 8, 64)),
    ])
    for t in range(NT):
        for kc in range(NKC):
            S.op("pe", lambda e, kc=kc, t=t: e.matmul(k.bank(t)[0:8, :], WFF[:, kc, :], k.H(kc, t * 512, 512),
                                                      start=(kc == 0), stop=(kc == NKC - 1)),
                 reads=["WFF", ("H", kc, t)], writes=[("ps", t)])
    S.barrier()
    T1 = k.f32(W_W, 2048)
    T2 = k.f32(W_W + 2048, 2048)
    ONES = k.f32(W_W + 7168, 2048)
    S.op("dve", lambda e: e.memset(ONES[0:8, :], 1.0), writes=["ONES"])
    softplus_neg(k, T1, 8, [0, 1, 2, 3], k.gcols[0:8, 4:5], "T1")
    S.op("dve", lambda e: e.tensor_tensor_scan(T2[0:8, :], ONES[0:8, :], T1[0:8, :], 0.0, ALU.mult, ALU.add), reads=["T1", "ONES"], writes=["T2"])
    S.op("dve", lambda e: e.tensor_scalar(T1[0:8, :], T2[0:8, :], -1.0, None, ALU.mult), reads=["T2"], writes=["T1"])
    build_aug(k, T2, "T2", T1, "T1", 8, NU(2), "fox")
    S.barrier()
    k.tbanks = [3, 7]
    OTB = [[k.f32(SB + (p * 4 + r) * 128, 128) for r in range(4)] for p in range(2)]
    blocks = []
    pres = []
    first_pass = []
    hb_init(k)
    for pr in range(4):
        passes = []
        for c in range(2):
            hd = 2 * pr + c
            slot = c + 2 * (pr % 2)
            qb, kb = hb_views(k, slot)

            def qf(t0, n, qb=qb):
                return qb[:, t0:t0 + n]

            def kf(i, kb=kb):
                return kb[:, i * 128:(i + 1) * 128]

            pres.append(lambda pr=pr, c=c, slot=slot, hd=hd: hb_build(k, slot, c, fm(k, QT, pr), fm(k, KT, pr),
                                                                      k.d["augq_fox"][hd, :, :], k.d["augk_fox"][hd, :, :], 6, "sp"))

            def vf(i, hd=hd):
                return V[:, i, hd, :]

            def fin(j, r, o, okey, pr=pr, c=c):
                rc, rk = col(k)
                ot = OTB[j % 2][r]
                otk = ("OTB", j % 2, r)
                S.op("dve", lambda e: e.reciprocal(rc, o[:, 64:65]), reads=[okey], writes=[rk])
                S.op("dve", lambda e: e.tensor_scalar(ot[:, c * 64:(c + 1) * 64], o[:, 0:64], rc, None, ALU.mult), reads=[okey, rk], writes=[otk])
                if c == 1:
                    tt = 4 * j + r
                    defer(k, 3, lambda: transpose_to_fm(k, ot, otk, fm(k, OF, pr, tt * 128, 128), ("fm", OF, pr, tt)))
            passes.append(AttnPass(k, qf=qf, kf=kf, aqf=None, akf=None, bias_fn=None, vf=vf, vkey=("V", "x"), dv=64,
                                   mode="exp", fin=fin, sbanks=[0, 1, 2, 6], obanks=[(4,)] if c == 0 else [(5,)], augkey=("HB", slot)))
        first_pass.append(passes[0])
        b0, b1 = passes[0].blocks(), passes[1].blocks()
        for j in range(4):
            blocks += [b for b in b0 if b[1] == j]
            blocks += [b for b in b1 if b[1] == j]
    for u in range(4):
        first_pass[u].pre = (pres[0:4] if u == 0 else (pres[2 * u + 2:2 * u + 4] if u < 3 else None))
    run_blocks(k, blocks, L=3)
    S.barrier()


def mlstm_branch(k, l):
    S, d = k.S, k.d
    QT, KT, VT, MX, XC, SZ, VA = DU(0), DU(1), DU(2), DU(3), NU(0), NU(1), NU(2)
    Vall = k.bf(VA, 16 * 4 * 129).rearrange("p (t h e) -> p t h e", t=16, h=4)
    WQKV = k.bf(W_W + 4096, 1536).rearrange("p (g h e) -> p g h e", g=3, h=4)
    WIF = k.bf(W_W + 4096 + 768, 96).rearrange("p (j o) -> p j o", j=12)
    S.op("dve", lambda e: e.memset(Vall[:, :, :, 128:129], 1.0), writes=[("V", "ones")])
    S.dma("pool", "wqkv", lambda e: e.dma_start(out=k.bf(W_W + 4096, 1536), in_=d["wqkv"][l, :, :]), writes=["WQKV"])
    S.dma("pool", "wqkv", lambda e: e.dma_start(out=k.bf(W_W + 4096 + 768, 96), in_=d["wif"][l, :, :]), writes=["WIF"])
    S.barrier()
    wcol_jobs(k, l, [
        (C_MX, 512, h_fm(MX)(k, 0)),
        (C_MZ, 512, h_fm(SZ, func=AF.Silu)(k, 0)),
    ])
    ACC = k.f32(SB, 2048)
    for c in range(4):
        mx = fm(k, MX, c)
        mkeys = [("fm", MX, c, t) for t in range(NT)]
        w = lambda j, c=c: k.convw[:, j * 4 + c:j * 4 + c + 1]
        S.op("dve", lambda e, mx=mx, c=c, w=w: e.tensor_scalar(ACC, mx, w(3), k.convb[:, c:c + 1], ALU.mult, ALU.add),
             reads=mkeys + ["lconst"], writes=["ACC"])
        for sh in (1, 2, 3):
            S.op("dve", lambda e, mx=mx, sh=sh, w=w: e.scalar_tensor_tensor(
                out=ACC[:, sh:], in0=mx[:, 0:S_LEN - sh], scalar=w(3 - sh), in1=ACC[:, sh:], op0=ALU.mult, op1=ALU.add),
                reads=mkeys + ["ACC", "lconst"], writes=["ACC"])
        S.op("act", lambda e, c=c: e.activation(out=fm(k, XC, c), in_=ACC, func=AF.Silu), reads=["ACC"],
             writes=[("fm", XC, c, t) for t in range(NT)])
    for h in range(4):
        for t in range(NT):
            for g, src, dst in ((0, XC, QT), (1, XC, KT), (2, MX, VT)):
                b = next_bank(k)
                S.op("pe", lambda e, b=b, g=g, src=src, h=h, t=t: e.matmul(
                    k.bank(b), WQKV[:, g, h, :], fm(k, src, h, t * 512, 512), start=True, stop=True),
                    reads=["WQKV", ("fm", src, h, t)], writes=[("ps", b)])
                evac(k, fm(k, dst, h, t * 512, 512), k.bank(b), ("ps", b), ("fm", dst, h, t))
        for tt in range(NT128):
            b = next_bank(k)
            S.op("pe", lambda e, b=b, h=h, tt=tt: e.matmul(
                k.bank(b)[:, 0:128], fm(k, MX, h, tt * 128, 128), WQKV[:, 2, h, :], start=True, stop=True),
                reads=["WQKV", ("fm", MX, h, tt // 4)], writes=[("ps", b)])
            evac(k, Vall[:, tt, h, 0:128], k.bank(b)[:, 0:128], ("ps", b), ("V", tt, h))
    for h in range(4):
        S.op("dve", lambda e, h=h: e.tensor_scalar(fm(k, XC, h), fm(k, XC, h), k.skipc[:, h:h + 1], None, ALU.mult),
             reads=[("fm", XC, h, t) for t in range(NT)] + ["lconst"], writes=[("fm", XC, h, t) for t in range(NT)])
    S.barrier()
    for t in range(NT):
        for g, bb in ((0, t), (1, 4 + t)):
            for jj in range(12):
                src = (QT, KT, VT)[jj // 4]
                S.op("pe", lambda e, g=g, bb=bb, jj=jj, src=src, t=t: e.matmul(
                    k.bank(bb)[0:4, :], WIF[:, jj, g * 4:(g + 1) * 4], fm(k, src, jj % 4, t * 512, 512),
                    start=(jj == 0), stop=(jj == 11)), reads=["WIF"], writes=[("ps", bb)])
    T1 = k.f32(W_W, 2048)
    T2 = k.f32(W_W + 2048, 2048)
    T3 = k.f32(W_W + 5120, 2048)
    ONES = k.f32(W_W + 7168, 2048)
    S.op("dve", lambda e: e.memset(ONES[0:4, :], 1.0), writes=["ONES"])
    softplus_neg(k, T1, 4, [4, 5, 6, 7], k.gcols[0:4, 5:6], "T1")
    S.op("dve", lambda e: e.tensor_tensor_scan(T2[0:4, :], ONES[0:4, :], T1[0:4, :], 0.0, ALU.mult, ALU.add), reads=["T1", "ONES"], writes=["T2"])
    for t in range(NT):
        S.op("dve", lambda e, t=t: e.tensor_scalar(T1[0:4, t * 512:(t + 1) * 512], k.bank(t)[0:4, :], k.gcols[0:4, 1:2], None, ALU.add),
             reads=[("ps", t), "gcols", "T2"], writes=["T1"])
    S.op("dve", lambda e: e.tensor_tensor(out=T1[0:4, :], in0=T1[0:4, :], in1=T2[0:4, :], op=ALU.add), reads=["T1", "T2"], writes=["T1"])
    S.op("dve", lambda e: e.tensor_tensor_scan(T3[0:4, :], ONES[0:4, :], T1[0:4, :], 0.0, ALU.mult, ALU.max), reads=["T1", "ONES"], writes=["T3"])
    S.op("dve", lambda e: e.tensor_scalar(T3[0:4, :], T3[0:4, :], -1.0, None, ALU.mult), reads=["T3"], writes=["T3"])
    UR = ONES
    EM = k.f32(W_CONST + 2700, 64)
    UC = k.f32(W_CONST + 2780, 160)
    for j in range(NT):
        nb = T3[0:4, 512 * j - 1:512 * j] if j > 0 else 0.0
        S.op("act", lambda e, j=j, nb=nb: e.activation(out=T2[0:4, j * 512:(j + 1) * 512], in_=T2[0:4, j * 512:(j + 1) * 512],
                                                     func=AF.Exp, bias=nb, scale=1.0), reads=["T2", "T3"], writes=["T2"])
    for tt in range(NT128):
        S.op("pe", lambda e, tt=tt: e.transpose(k.bank(3)[:, tt * 4:(tt + 1) * 4], T2[0:4, tt * 128:(tt + 1) * 128], k.ident[0:4, 0:4]),
             reads=["T2", "ident"], writes=[("ps", 3)])
    S.op("dve", lambda e: e.tensor_copy(EM, k.bank(3)[:, 0:64]), reads=[("ps", 3)], writes=["EM"])
    for j in range(NT):
        nb = T3[0:4, 512 * j - 1:512 * j] if j > 0 else 0.0
        ncol = (j + 1) * 512
        S.op("act", lambda e, nb=nb, ncol=ncol: e.activation(out=UR[0:4, 0:ncol], in_=T1[0:4, 0:ncol], func=AF.Exp, bias=nb, scale=1.0),
             reads=["T1", "T3", "ONES"], writes=["ONES"])
        for i in range(4 * j + 4):
            idx = 2 * j * (j + 1) + i
            S.op("pe", lambda e, i=i, idx=idx: e.transpose(k.bank(2)[:, idx * 4:(idx + 1) * 4], UR[0:4, i * 128:(i + 1) * 128], k.ident[0:4, 0:4]),
                 reads=["ONES", "ident"], writes=[("ps", 2)])
    S.op("dve", lambda e: e.tensor_scalar(UC, k.bank(2)[:, 0:160], 128.0 ** -0.5, None, ALU.mult), reads=[("ps", 2)], writes=["UC"])
    S.barrier()
    k.tbanks = [3, 7]
    TB = SB + 1024
    blocks = []
    for h in range(4):

        def qf(t0, n, h=h):
            return fm(k, QT, h, t0, n)

        def kf(i, h=h):
            return fm(k, KT, h, i * 128, 128)

        def vf(i, h=h):
            return Vall[:, i, h, :]

        def fin(j, r, o, okey, h=h):
            tt = 4 * j + r
            c1, k1 = col(k)
            c2, k2 = col(k)
            c3, k3 = col(k)
            k._fp = getattr(k, "_fp", 0) + 1
            n = k._fp
            NUM = k.f32(TB + (n % 4) * 128, 128)
            TF = k.f32(TB + 640 + (n % 2) * 128, 128)
            HN = k.f32(TB + 896 + (n % 4) * 128, 128)
            ST6 = k.f32(TB + 512 + (n % 8) * 8, 6)
            MV = k.f32(TB + 576 + (n % 8) * 4, 2)
            hk, nk, fk, stk, mvk = ("HH", n % 4), ("HN", n % 4), ("TF", n % 2), ("ST6", n % 8), ("MV", n % 8)
            den = o[:, 128:129]
            S.op("dve", lambda e: e.tensor_tensor(out=c1, in0=den, in1=EM[:, tt * 4 + h:tt * 4 + h + 1], op=ALU.max), reads=[okey, "EM"], writes=[k1])
            S.op("dve", lambda e: e.scalar_tensor_tensor(out=c1, in0=den, scalar=-1.0, in1=c1, op0=ALU.mult, op1=ALU.max), reads=[okey, k1], writes=[k1])
            S.op("dve", lambda e: e.reciprocal(c1, c1), reads=[k1], writes=[k1])
            S.op("dve", lambda e: e.tensor_scalar(NUM, o[:, 0:128], c1, None, ALU.mult), reads=[okey, k1], writes=[hk])
            S.op("dve", lambda e: e.bn_stats(ST6, NUM), reads=[hk], writes=[stk])
            S.op("dve", lambda e: e.bn_aggr(MV, ST6), reads=[stk], writes=[mvk])

            def stage_b():
                S.op("act", lambda e: e.activation(out=c2, in_=MV[:, 1:2], func=AF.Ln, bias=LN_EPS, scale=1.0), reads=[mvk], writes=[k2])
                S.op("act", lambda e: e.activation(out=c2, in_=c2, func=AF.Exp, scale=-0.5), reads=[k2], writes=[k2])

            def post(pq, pkey):
                xs = fm(k, XC, h, tt * 128, 128)
                sz = fm(k, SZ, h, tt * 128, 128)
                S.op("dve", lambda e: e.scalar_tensor_tensor(out=TF, in0=pq, scalar=k.normc[:, h:h + 1], in1=xs, op0=ALU.mult, op1=ALU.add),
                     reads=[pkey, "lconst"], writes=[fk])
                S.op("dve", lambda e: e.tensor_tensor(out=sz, in0=TF, in1=sz, op=ALU.mult), reads=[fk], writes=[("fm", SZ, h, tt)])

            def stage_c():
                S.op("dve", lambda e: e.tensor_scalar(HN, NUM, MV[:, 0:1], c2, ALU.subtract, ALU.mult), reads=[hk, mvk, k2], writes=[nk])
                defer(k, 2, lambda: transpose_to_fm(k, HN, nk, None, None, post=post))
            defer(k, 2, stage_b)
            defer(k, 3, stage_c)

        def scale_fn(i, j, h=h):
            idx = 2 * j * (j + 1) + i
            return UC[:, idx * 4 + h:idx * 4 + h + 1]
        ps_ = AttnPass(k, qf=qf, kf=kf, aqf=None, akf=None, bias_fn=None, vf=vf, vkey=("V", "x"), dv=128, mode="exp", fin=fin,
                       sbanks=[0, 1, 2, 6], obanks=[(4, 5)], act_func=AF.Copy, scale_fn=scale_fn, mask_mul=True)
        blocks += ps_.blocks()
    run_blocks(k, blocks, L=3)
    S.barrier()


def merge_and_out(k, l):
    S, d = k.S, k.d
    OB = [NU(0), NU(2), NU(1)]
    MG = DU(0)
    GT = [k.f32(SB + i * 512, 512) for i in range(2)]
    ACC = k.f32(SB + 1024, 512)
    PB = k.f32(SB + 1536, 512)

    def load_mp(dd):
        s = dd % 2
        tile = k.bf(W_W + s * 2304, 4608)
        S.dma("pool", f"mp{s}", lambda e: e.dma_start(out=tile, in_=d["mpack"][l, dd, :, :]), writes=[("MP", s)])
        return tile

    tiles = {0: load_mp(0), 1: load_mp(1)}
    cnt = 0
    for dd in range(NKC):
        MP = tiles[dd]
        mkey = ("MP", dd % 2)
        for t in range(NT):
            for b in range(3):
                bg, bo = next_bank(k), next_bank(k)
                for kc in range(NKC):
                    S.op("pe", lambda e, bg=bg, b=b, kc=kc, t=t, MP=MP: e.matmul(
                        k.bank(bg), MP[:, b * 1536 + kc * 128:b * 1536 + (kc + 1) * 128], k.H(kc, t * 512, 512),
                        start=(kc == 0), stop=(kc == NKC - 1)), reads=[mkey, ("H", kc, t)], writes=[("ps", bg)])
                for kc in range(4):
                    S.op("pe", lambda e, bo=bo, b=b, kc=kc, t=t, MP=MP: e.matmul(
                        k.bank(bo), MP[:, b * 1536 + 1024 + kc * 128:b * 1536 + 1024 + (kc + 1) * 128], fm(k, OB[b], kc, t * 512, 512),
                        start=(kc == 0), stop=(kc == 3)), reads=[mkey], writes=[("ps", bo)])
                gt = GT[cnt % 2]
                gk = ("GT", cnt % 2)
                cnt += 1
                gbc = k.gb[:, b * 8 + dd:b * 8 + dd + 1]
                S.op("act", lambda e, gt=gt, bg=bg, gbc=gbc: e.activation(out=gt, in_=k.bank(bg), func=AF.Sigmoid, bias=gbc, scale=1.0),
                     reads=[("ps", bg), "lconst"], writes=[gk])
                if b == 0:
                    S.op("dve", lambda e, gt=gt, bo=bo: e.tensor_tensor(out=ACC, in0=gt, in1=k.bank(bo), op=ALU.mult),
                         reads=[gk, ("ps", bo)], writes=["MACC"])
                else:
                    S.op("dve", lambda e, gt=gt, bo=bo: e.tensor_tensor(out=PB, in0=gt, in1=k.bank(bo), op=ALU.mult),
                         reads=[gk, ("ps", bo)], writes=["MPB"])
                    dst = ACC if b == 1 else fm(k, MG, dd, t * 512, 512)
                    dkey = "MACC" if b == 1 else ("fm", MG, dd, t)
                    S.op("dve", lambda e, dst=dst: e.tensor_tensor(out=dst, in0=ACC, in1=PB, op=ALU.add),
                         reads=["MACC", "MPB"], writes=[dkey] + (["MACC"] if b == 2 else []))
        if dd + 2 < NKC:
            tiles[dd + 2] = load_mp(dd + 2)
    S.barrier()
    MN = NU(0)
    for i in range(4):
        S.dma("sp", "mncp", lambda e, i=i: e.dma_start(out=k.bf(MN + i * 2048, 4096), in_=k.bf(MG + i * 2048, 4096)),
              writes=[("MN", i)])
    S.barrier()
    XS = [k.f32(SB + i * 512, 512) for i in range(2)]

    WOT = [k.bf(W_W + dd * 512, 1024) for dd in range(NKC)]
    for dd in range(NKC):
        S.dma("pool", "wo", lambda e, dd=dd: e.dma_start(out=WOT[dd], in_=d["wout"][l, dd, :, :]), writes=[("WO", dd)])
    cnt = 0
    for t in range(NT):
        for dd in range(NKC):
            WO = WOT[dd]
            b = next_bank(k)
            xs = XS[cnt % 2]
            xk = ("XS", cnt % 2)
            cnt += 1
            S.dma("sp", f"xs{cnt % 2}", lambda e, xs=xs, dd=dd, t=t: e.dma_start(out=xs, in_=d["xspill"][:, dd, t * 512:(t + 1) * 512]),
                  writes=[xk])
            for kc in range(NKC):
                S.op("pe", lambda e, b=b, kc=kc, t=t, WO=WO: e.matmul(
                    k.bank(b), WO[:, kc * 128:(kc + 1) * 128], fm(k, MN, kc, t * 512, 512),
                    start=(kc == 0), stop=(kc == NKC - 1)), reads=[("WO", dd)], writes=[("ps", b)])
            S.op("dve", lambda e, b=b, xs=xs, dd=dd, t=t: e.tensor_tensor(out=k.X(dd, t), in0=k.bank(b), in1=xs, op=ALU.add),
                 reads=[("ps", b), xk], writes=[("X", dd, t)])


def spill_x(k):
    S, d = k.S, k.d
    for c in range(NKC):
        o = W_X + c * S_LEN
        S.dma("sp", "xsp", lambda e, c=c, o=o: e.dma_start(out=d["xspill"][:, c, :], in_=k.arena[:, o:o + S_LEN]),
              reads=[("X", c, t) for t in range(NT)], writes=["xspill"])


def mixer(k, l, branches=("ml", "diff", "fox")):
    S = k.S
    layer_consts(k, l)
    rmsnorm(k, l * 3 + 1, "H")
    spill_x(k)
    S.barrier()
    if "ml" in branches:
        mlstm_branch(k, l)
    if "diff" in branches:
        diff_branch(k, l)
    if "fox" in branches:
        fox_branch(k, l)
    merge_and_out(k, l)


def _chunk_rows(w):
    K_, N_ = w.shape
    return w.reshape(K_ // 128, 128, N_).transpose(1, 0, 2)


def _cols(v):
    return v.reshape(-1, 128).T


def prep_shared(inp):
    f = np.float32
    sh = {}
    for which in (1, 2):
        wg, wu, wd = inp[f"ffn{which}_w_gate"], inp[f"ffn{which}_w_up"], inp[f"ffn{which}_w_down"]
        gu = np.empty((DEPTH, NFC, 128, 2, NKC, 128), f)
        for l in range(DEPTH):
            for g, w in enumerate((wg[l], wu[l])):
                gu[l, :, :, g] = w.reshape(NKC, 128, NFC, 128).transpose(2, 1, 0, 3)
        sh[f"ffn{which}_gu"] = gu.reshape(DEPTH, NFC, 128, 2 * NKC * 128)
        sh[f"ffn{which}_wd"] = np.ascontiguousarray(wd.reshape(DEPTH, NFC, 128, D))
    vecs = []
    for l in range(DEPTH):
        vecs += [inp["ffn1_norm"][l], inp["mix_norm"][l], inp["ffn2_norm"][l]]
    vecs.append(inp["final_norm"])
    sh["normw"] = np.ascontiguousarray(np.stack([_cols(v) for v in vecs], axis=1).reshape(128, 56)).astype(f)
    sh["ident"] = np.eye(128, dtype=f)
    sh["ones"] = np.ones((128, 128), f)
    sh["tri"] = np.triu(np.ones((128, 128), f))
    sh["negtri"] = (np.tril(np.ones((128, 128), f), -1) * NEG_BIG).astype(f)
    w_in = inp["w_in"]
    sh["w_in"] = np.ascontiguousarray(np.stack([_chunk_rows(w_in[l]) for l in range(DEPTH)]))
    gb = np.empty((DEPTH, 128, 24), f)
    cw = np.empty((DEPTH, 128, 16), f)
    cb = np.empty((DEPTH, 128, 4), f)
    sk = np.empty((DEPTH, 128, 4), f)
    nmc = np.empty((DEPTH, 128, 4), f)
    gc = np.zeros((DEPTH, 8, 8), f)
    for l in range(DEPTH):
        for b in range(3):
            gb[l, :, b * 8:(b + 1) * 8] = _cols(inp["gate_bias"][l, b])
        for j in range(4):
            cw[l, :, j * 4:(j + 1) * 4] = _cols(inp["mlstm_conv_w"][l, j])
        cb[l] = _cols(inp["mlstm_conv_b"][l])
        sk[l] = _cols(inp["mlstm_skip"][l])
        nmc[l] = _cols(inp["mlstm_norm"][l])
        gc[l, :, 0] = inp["fox_b_f"][l]
        gc[l, 0:4, 1] = inp["mlstm_b_if"][l, 0:4]
        gc[l, 0:4, 2] = inp["mlstm_b_if"][l, 4:8]
    sh["gbcols"], sh["convw"], sh["convb"], sh["skipc"], sh["gcols"] = gb, cw, cb, sk, gc
    sh["normc"] = nmc
    sh["difflam"] = np.ascontiguousarray(np.concatenate(
        [inp["diff_lq1"], inp["diff_lk1"], inp["diff_lq2"], inp["diff_lk2"]], axis=1)).astype(f)
    sh["diff_subln"] = np.ascontiguousarray(inp["diff_subln"]).astype(f)
    sh["mlstm_norm"] = np.ascontiguousarray(inp["mlstm_norm"]).astype(f)
    slopes = [2.0 ** (-8.0 * (h + 1) / 4) for h in range(4)]
    ak = np.zeros((3, 4, 128), f)
    aq = np.zeros((3, 4, 512), f)
    relq = np.arange(512)
    for h in range(4):
        ak[0, h] = slopes[h] * np.arange(128)
        ak[1, h] = 1.0
        ak[2, h] = 1.0
        aq[0, h] = 1.0
        aq[1, h] = -slopes[h] * (128 * (relq // 128))
        aq[2, h] = -slopes[h] * (relq % 128)
    sh["alibi_k"] = ak.reshape(3, 512)
    sh["alibi_q"] = aq.reshape(3, 2048)
    tpos = np.arange(S_LEN)
    akf = np.zeros((4, 3, S_LEN), f)
    aqf = np.zeros((4, 3, S_LEN), f)
    for h in range(4):
        akf[h, 0] = slopes[h] * (tpos % 128)
        akf[h, 1] = 1.0
        akf[h, 2] = 1.0
        aqf[h, 0] = 1.0
        aqf[h, 1] = -slopes[h] * (128 * ((tpos % 512) // 128))
        aqf[h, 2] = -slopes[h] * (tpos % 128)
    sh["alibi_kf"], sh["alibi_qf"] = akf, aqf
    wqkv = np.empty((DEPTH, 128, 3, 4, 128), f)
    for g, nm in enumerate(("mlstm_wq", "mlstm_wk", "mlstm_wv")):
        wqkv[:, :, g] = inp[nm].transpose(0, 2, 1, 3)
    sh["wqkv"] = wqkv.reshape(DEPTH, 128, 1536)
    sh["wif"] = np.ascontiguousarray(inp["mlstm_w_if"].reshape(DEPTH, 12, 128, 8).transpose(0, 2, 1, 3)).reshape(DEPTH, 128, 96)
    mp = np.empty((DEPTH, NKC, 128, 3, 1536), f)
    wbs = (inp["w_branch_diff"], inp["w_branch_fox"], inp["w_branch_mlstm"])
    for l in range(DEPTH):
        for b in range(3):
            g = w_in[l][:, C_G + b * D:C_G + (b + 1) * D]
            mp[l, :, :, b, 0:1024] = g.reshape(NKC, 128, NKC, 128).transpose(2, 1, 0, 3).reshape(NKC, 128, 1024)
            mp[l, :, :, b, 1024:1536] = wbs[b][l].reshape(4, 128, NKC, 128).transpose(2, 1, 0, 3).reshape(NKC, 128, 512)
    sh["mpack"] = mp.reshape(DEPTH, NKC, 128, 4608)
    wo = np.empty((DEPTH, NKC, 128, 1024), f)
    for l in range(DEPTH):
        wo[l] = inp["w_out"][l].reshape(NKC, 128, NKC, 128).transpose(2, 1, 0, 3).reshape(NKC, 128, 1024)
    sh["wout"] = wo
    return sh


def build_program(shapes, plan):
    from contextlib import ExitStack
    nc = bass.Bass("TRN2", target_bir_lowering=False)
    dram = {}
    for name, shp in shapes.items():
        dram[name] = nc.dram_tensor(name, list(shp), F32, kind="ExternalInput").ap()
    dram["outT"] = nc.dram_tensor("outT", [128, NKC, S_LEN], F32, kind="ExternalOutput").ap()
    dram["xspill"] = nc.dram_tensor("xspill", [128, NKC, S_LEN], F32, kind="Internal").ap()
    for nm in ("augk_fox", "augq_fox", "augk_ml", "augq_ml"):
        dram[nm] = nc.dram_tensor(nm, [8, 6, S_LEN], BF16, kind="Internal").ap()
    with ExitStack() as es:
        sems = [es.enter_context(nc.semaphore(f"s{i}")) for i in range(70)]
        S = Sched(nc, sems)
        arena = nc.alloc_sbuf_tensor("arena", [128, ARENA_WORDS], F32)
        ps = es.enter_context(nc.psum_tensor("ps", [128, 4096], F32))
        k = K(nc, S, arena, ps, dram)
        plan(k)
        with nc.Block() as block:
            S.emit(block)
    return nc


def full_plan(k):
    build_consts(k)
    load_x(k)
    for l in range(DEPTH):
        rmsnorm(k, l * 3 + 0, "H")
        ffn(k, l, 1)
        mixer(k, l)
        rmsnorm(k, l * 3 + 2, "H")
        k.S.barrier()
        ffn(k, l, 2)
    rmsnorm(k, 6, "X")
    store_out(k)


def run(inputs, plan=full_plan, trace=False, cores=NB):
    sh = prep_shared(inputs)
    x = np.asarray(inputs["x"], np.float32)
    in_maps = []
    for b in range(cores):
        m = dict(sh)
        m["xT"] = np.ascontiguousarray(x[b].T.reshape(NKC, 128, S_LEN).transpose(1, 0, 2))
        in_maps.append(m)
    shapes = {n: a.shape for n, a in in_maps[0].items()}
    nc = build_program(shapes, plan)
    res = run_bass_kernel_spmd(nc, in_maps, core_ids=list(range(cores)), trace=trace)
    out = np.empty((cores, S_LEN, D), np.float32)
    for b in range(cores):
        o = res.results[b]["outT"]
        out[b] = o.transpose(1, 0, 2).reshape(D, S_LEN).T
    return out, res


def kernel(**inputs):
    out, _ = run(inputs)
    return out
```

```python
import bisect
import numpy as np
import concourse.bass as bass
import concourse.mybir as mybir
from concourse.bass_utils import run_bass_kernel_spmd

F32 = mybir.dt.float32
BF16 = mybir.dt.bfloat16
AF = mybir.ActivationFunctionType
ALU = mybir.AluOpType
AX = mybir.AxisListType


class _Ev:
    __slots__ = ("eng", "idx", "val", "clock")

    def __init__(self, eng, idx, val, clock):
        self.eng, self.idx, self.val, self.clock = eng, idx, val, clock


class _Eng:
    def __init__(self, name, sem, self_sync):
        self.name, self.sem, self.self_sync = name, sem, self_sync
        self.ops = []
        self.count = 0
        self.sig_idx = []
        self.sig_val = []
        self.n_inst = 0
        self.inst_rec = []
        self.clock = {}
        self.last_compute = None


class _Slot:
    def __init__(self, name, sem):
        self.name, self.sem, self.total = name, sem, 0


class _Res:
    __slots__ = ("w", "rs")

    def __init__(self):
        self.w, self.rs = None, []


class Sched:
    def __init__(self, nc, sems):
        self.nc = nc
        self._sems = list(sems)
        self.engs = {}
        for name, ss in (("pe", False), ("act", True), ("dve", True), ("pool", True), ("sp", False)):
            self.engs[name] = _Eng(name, self._sems.pop(), ss)
        self.slots = {}
        self.resd = {}

    def slot(self, name):
        s = self.slots.get(name)
        if s is None:
            s = _Slot(name, self._sems.pop())
            self.slots[name] = s
        return s

    def res(self, key):
        r = self.resd.get(key)
        if r is None:
            r = _Res()
            self.resd[key] = r
        return r

    def _value(self, ev):
        if ev.val is not None:
            return ev.val
        E = self.engs[ev.eng]
        j = bisect.bisect_left(E.sig_idx, ev.idx)
        if j < len(E.sig_idx):
            return E.sig_val[j]
        E.count += 1
        rec = E.inst_rec[ev.idx]
        rec[2], rec[3] = E.sem, 1
        E.sig_idx.append(ev.idx)
        E.sig_val.append(E.count)
        ev.val = E.count
        return ev.val

    def _wait_for(self, E, evs):
        need = {}
        for ev in evs:
            if ev is None:
                continue
            if ev.eng == E.name and not E.self_sync:
                continue
            if ev.eng in self.slots:
                v = self.slots[ev.eng].total
            else:
                v = self._value(ev)
            if E.clock.get(ev.eng, 0) >= v:
                continue
            if need.get(ev.eng, (0, None))[0] < v:
                need[ev.eng] = (v, ev)
        for name, (v, ev) in need.items():
            if E.clock.get(name, 0) >= v:
                continue
            sem = self.slots[name].sem if name in self.slots else self.engs[name].sem
            E.ops.append(["w", sem, v])
            E.clock[name] = v
            for k, cv in ev.clock.items():
                if E.clock.get(k, 0) < cv:
                    E.clock[k] = cv

    def _deps(self, reads, writes):
        evs = []
        for k in reads:
            r = self.res(k)
            if r.w is not None:
                evs.append(r.w)
        for k in writes:
            r = self.res(k)
            if r.w is not None:
                evs.append(r.w)
            evs.extend(r.rs)
        return evs

    def _commit(self, ev, reads, writes):
        for k in reads:
            self.res(k).rs.append(ev)
        for k in writes:
            r = self.res(k)
            r.w, r.rs = ev, []

    def op(self, eng, fn, reads=(), writes=()):
        E = self.engs[eng]
        self._wait_for(E, self._deps(reads, writes))
        rec = ["i", fn, None, 0]
        E.ops.append(rec)
        E.inst_rec.append(rec)
        idx = E.n_inst
        E.n_inst += 1
        E.last_compute = idx
        clk = dict(E.clock)
        ev = _Ev(eng, idx, None, clk)
        self._commit(ev, reads, writes)
        return ev

    def dma(self, queue, slot, fn, reads=(), writes=()):
        E = self.engs[queue]
        S = self.slot(slot) if isinstance(slot, str) else slot
        self._wait_for(E, self._deps(reads, writes))
        S.total += 16
        rec = ["i", fn, S.sem, 16]
        E.ops.append(rec)
        E.inst_rec.append(rec)
        E.n_inst += 1
        ev = _Ev(S.name, -1, S.total, dict(E.clock))
        self._commit(ev, reads, writes)
        return ev

    def barrier(self):
        evs = []
        for E in self.engs.values():
            if E.last_compute is not None:
                ev = _Ev(E.name, E.last_compute, None, dict(E.clock))
                self._value(ev)
                evs.append(ev)
        for S in self.slots.values():
            if S.total:
                evs.append(_Ev(S.name, -1, S.total, {}))
        for E in self.engs.values():
            self._wait_for(E, evs)
        self.resd = {}

    def wait_all_dma(self, eng, slots):
        E = self.engs[eng]
        evs = [_Ev(self.slots[s].name, -1, self.slots[s].total, {}) for s in slots if self.slots[s].total]
        self._wait_for(E, evs)

    def emit(self, block):
        def replay(E):
            def run(e):
                for rec in E.ops:
                    if rec[0] == "w":
                        e.wait_ge(rec[1], rec[2])
                    else:
                        ins = rec[1](e)
                        if rec[2] is not None:
                            ins.then_inc(rec[2], rec[3])
            return run
        block.tensor(replay(self.engs["pe"]))
        block.scalar(replay(self.engs["act"]))
        block.vector(replay(self.engs["dve"]))
        block.gpsimd(replay(self.engs["pool"]))
        block.sync(replay(self.engs["sp"]))


D = 1024
S_LEN = 2048
NB = 8
DEPTH = 2
DFF = 2816
NFC = DFF // 128
NKC = D // 128
NT = S_LEN // 512
NT128 = S_LEN // 128
RMS_EPS = 1e-6
LN_EPS = 1e-5
N_IN = 7176
FFN_GROUPS = [(0, 6), (6, 12), (12, 17), (17, 22)]

W_X = 0
W_H = W_X + 16384
W_CONST = W_H + 8192
W_PT = W_CONST + 3072
W_W = W_PT + 1024
W_N = W_W + 9216
W_END = W_N + 12288
ARENA_WORDS = W_END


class K:
    def __init__(self, nc, S, arena, ps, dram):
        self.nc, self.S, self.arena, self.ps, self.d = nc, S, arena, ps, dram
        self.ps_rr = 0

    def f32(self, off, n):
        return self.arena[:, off:off + n]

    def bf(self, off, n):
        return self.arena[:, off:off + (n + 1) // 2].bitcast(BF16)

    def bank(self, b):
        return self.ps[:, b * 512:(b + 1) * 512]

    def X(self, c, t):
        o = W_X + c * S_LEN + t * 512
        return self.arena[:, o:o + 512]

    def H(self, c, t0=0, n=S_LEN):
        return self.bf(W_H + c * (S_LEN // 2), S_LEN)[:, t0:t0 + n]

    def PT(self, i):
        return self.bf(W_PT + i * 256, 512)


def build_consts(k):
    S, d = k.S, k.d
    o = W_CONST
    k.ident = k.f32(o, 128); o += 128
    k.ones_bf = k.bf(o, 128); o += 64
    k.tri_bf = k.bf(o, 128); o += 64
    k.normw = k.f32(o, 7 * 8); o += 56
    k.const_end = o
    S.dma("sp", "c0", lambda e: e.dma_start(out=k.ident, in_=d["ident"][:, :]), writes=["ident"])
    S.dma("pool", "c1", lambda e: e.dma_start(out=k.ones_bf, in_=d["ones"][:, :]), writes=["ones"])
    S.dma("pool", "c1", lambda e: e.dma_start(out=k.tri_bf, in_=d["tri"][:, :]), writes=["tri"])
    S.dma("sp", "c0", lambda e: e.dma_start(out=k.normw, in_=d["normw"][:, :]), writes=["normw"])


def load_x(k):
    S, d = k.S, k.d
    for c in range(NKC):
        o = W_X + c * S_LEN
        S.dma("sp", "xld", lambda e, c=c, o=o: e.dma_start(out=k.arena[:, o:o + S_LEN], in_=d["xT"][:, c, :]),
              writes=[("X", c, t) for t in range(NT)])


def rmsnorm(k, widx, out_mode):
    S = k.S
    R0 = W_N + 8192
    for t in range(NT):
        for c in range(NKC):
            pt = k.PT((t * NKC + c) % 4)
            key = ("PT", (t * NKC + c) % 4)
            if c % 2 == 0:
                S.op("act", lambda e, pt=pt, c=c, t=t: e.activation(out=pt, in_=k.X(c, t), func=AF.Square),
                     reads=[("X", c, t)], writes=[key])
            else:
                S.op("dve", lambda e, pt=pt, c=c, t=t: e.tensor_tensor(out=pt, in0=k.X(c, t), in1=k.X(c, t), op=ALU.mult),
                     reads=[("X", c, t)], writes=[key])
            S.op("pe", lambda e, pt=pt, c=c, t=t: e.matmul(k.bank(t), k.ones_bf, pt, start=(c == 0), stop=(c == NKC - 1)),
                 reads=[key, "ones"], writes=[("ps", t)])
    for t in range(NT):
        R = k.f32(R0 + t * 512, 512)
        S.op("act", lambda e, R=R, t=t: e.activation(out=R, in_=k.bank(t), func=AF.Ln, bias=RMS_EPS, scale=1.0 / D),
             reads=[("ps", t)], writes=[("R", t)])
        S.op("act", lambda e, R=R: e.activation(out=R, in_=R, func=AF.Exp, scale=-0.5), reads=[("R", t)], writes=[("R", t)])
        for c in range(NKC):
            wcol = k.normw[:, widx * 8 + c:widx * 8 + c + 1]
            if out_mode == "H":
                S.op("dve", lambda e, R=R, c=c, t=t, wcol=wcol: e.scalar_tensor_tensor(
                    out=k.H(c, t * 512, 512), in0=k.X(c, t), scalar=wcol, in1=R, op0=ALU.mult, op1=ALU.mult),
                    reads=[("X", c, t), ("R", t), "normw"], writes=[("H", c, t)])
            else:
                S.op("dve", lambda e, R=R, c=c, t=t, wcol=wcol: e.scalar_tensor_tensor(
                    out=k.X(c, t), in0=k.X(c, t), scalar=wcol, in1=R, op0=ALU.mult, op1=ALU.mult),
                    reads=[("X", c, t), ("R", t), "normw"], writes=[("X", c, t)])


def ffn(k, l, which):
    S, d = k.S, k.d
    gu_d = d[f"ffn{which}_gu"]
    wd_d = d[f"ffn{which}_wd"]
    GU = [k.bf(W_W + i * 1024, 2048) for i in range(3)]
    WD = [k.bf(W_W + 3072 + i * 3072, 6 * 1024) for i in range(2)]
    A = [k.bf(W_N + i * 1024, 2048) for i in range(7)]
    SG = [k.f32(W_N + 7168 + i * 512, 512) for i in range(2)]

    def load_gu(c):
        s = c % 3
        S.dma("pool", f"gu{s}", lambda e: e.dma_start(out=GU[s], in_=gu_d[l, c, :, :]), writes=[("GU", s)])

    def load_wd(g):
        s = g % 2
        c0, c1 = FFN_GROUPS[g]
        for c in range(c0, c1):
            S.dma("pool", f"wd{s}", lambda e, c=c: e.dma_start(out=WD[s][:, (c - c0) * 1024:(c - c0 + 1) * 1024], in_=wd_d[l, c, :, :]),
                  writes=[("WD", s)])

    def gu_chunk(c):
        s = c % 3
        for t in range(NT):
            pr = (c * NT + t) % 3
            bg, bu = 2 * pr, 2 * pr + 1
            for g, b in ((0, bg), (1, bu)):
                for kc in range(NKC):
                    S.op("pe", lambda e, g=g, b=b, kc=kc, t=t: e.matmul(
                        k.bank(b), GU[s][:, (g * 8 + kc) * 128:(g * 8 + kc + 1) * 128], k.H(kc, t * 512, 512),
                        start=(kc == 0), stop=(kc == NKC - 1)),
                        reads=[("GU", s), ("H", kc, t)], writes=[("ps", b)])
            sg = SG[(c * NT + t) % 2]
            sgk = ("SG", (c * NT + t) % 2)
            S.op("act", lambda e, sg=sg, bg=bg: e.activation(out=sg, in_=k.bank(bg), func=AF.Silu),
                 reads=[("ps", bg)], writes=[sgk])
            S.op("dve", lambda e, sg=sg, bu=bu, t=t: e.tensor_tensor(
                out=A[c % 7][:, t * 512:(t + 1) * 512], in0=sg, in1=k.bank(bu), op=ALU.mult),
                reads=[sgk, ("ps", bu)], writes=[("A", c % 7, t)])

    dn_cnt = [0]

    def down(g):
        s = g % 2
        c0, c1 = FFN_GROUPS[g]
        for t in range(NT):
            for dd in range(NKC):
                b = 6 + dn_cnt[0] % 2
                dn_cnt[0] += 1
                for c in range(c0, c1):
                    S.op("pe", lambda e, b=b, c=c, dd=dd, t=t: e.matmul(
                        k.bank(b), WD[s][:, (c - c0) * 1024 + dd * 128:(c - c0) * 1024 + (dd + 1) * 128],
                        A[c % 7][:, t * 512:(t + 1) * 512], start=(c == c0), stop=(c == c1 - 1)),
                        reads=[("WD", s), ("A", c % 7, t)], writes=[("ps", b)])
                S.op("dve", lambda e, b=b, dd=dd, t=t: e.scalar_tensor_tensor(
                    out=k.X(dd, t), in0=k.bank(b), scalar=0.5, in1=k.X(dd, t), op0=ALU.mult, op1=ALU.add),
                    reads=[("ps", b), ("X", dd, t)], writes=[("X", dd, t)])

    for c in range(3):
        load_gu(c)
    load_wd(0)
    load_wd(1)
    grp_of = {}
    for g, (c0, c1) in enumerate(FFN_GROUPS):
        for c in range(c0, c1):
            grp_of[c] = g
    for c in range(NFC):
        gu_chunk(c)
        if c + 3 < NFC:
            load_gu(c + 3)
        g = grp_of[c]
        if c == FFN_GROUPS[g][0] and g > 0:
            down(g - 1)
            if g + 1 < len(FFN_GROUPS):
                load_wd(g + 1)
    down(len(FFN_GROUPS) - 1)


def store_out(k):
    S, d = k.S, k.d
    for c in range(NKC):
        o = W_X + c * S_LEN
        S.dma("sp", "ost", lambda e, c=c, o=o: e.dma_start(out=d["outT"][:, c, :], in_=k.arena[:, o:o + S_LEN]),
              reads=[("X", c, t) for t in range(NT)])
    S.wait_all_dma("sp", ["ost"])


U = 4096
W_SCR = W_END + 64
ARENA_WORDS = W_END + 2560
SB = W_SCR + 64
NEG_BIG = -30000.0
WARM_FILL = 0
C_DQ, C_DK, C_DV = 0, 512, 1024
C_FQ, C_FK, C_FV, C_FF = 1536, 2048, 2560, 3072
C_MX, C_MZ, C_G = 3080, 3592, 4104


def DU(i):
    return W_X + i * U


def NU(i):
    return W_N + i * U


def fm(k, off, c, t0=0, n=S_LEN):
    return k.bf(off + c * (S_LEN // 2), S_LEN)[:, t0:t0 + n]


def next_bank(k):
    b = k.ps_rr % 8
    k.ps_rr += 1
    return b


def col(k):
    i = getattr(k, "_col_rr", 0)
    k._col_rr = i + 1
    i %= 48
    return k.arena[:, W_SCR + i:W_SCR + i + 1], ("col", i)


def evac(k, dst, src, src_key, dst_key, scale=None, func=None, eng=None):
    S = k.S
    if eng is None:
        k._ev_rr = getattr(k, "_ev_rr", 0) + 1
        eng = "act" if (func is not None or k._ev_rr % 2 == 0) else "dve"
    if eng == "act":
        f = func if func is not None else AF.Copy
        sc = 1.0 if scale is None else scale
        S.op("act", lambda e: e.activation(out=dst, in_=src, func=f, scale=sc), reads=[src_key], writes=[dst_key])
    else:
        if scale is None:
            S.op("dve", lambda e: e.tensor_copy(dst, src), reads=[src_key], writes=[dst_key])
        else:
            S.op("dve", lambda e: e.tensor_scalar(dst, src, scale, None, ALU.mult), reads=[src_key], writes=[dst_key])


def wcol_jobs(k, l, jobs):
    S, d = k.S, k.d

    def load(i):
        c0, nc_, _ = jobs[i]
        s = i % 2
        tile = k.bf(W_W + s * 2048, 4096).rearrange("p (k c) -> p k c", k=8)
        S.dma("pool", f"wc{s}", lambda e: e.dma_start(out=tile[:, :, 0:nc_], in_=d["w_in"][l, :, :, c0:c0 + nc_]),
              writes=[("WC", s)])
        return tile

    tiles = {}
    for i in range(min(2, len(jobs))):
        tiles[i] = load(i)
    for i in range(len(jobs)):
        jobs[i][2](tiles[i], ("WC", i % 2))
        if i + 2 < len(jobs):
            tiles[i + 2] = load(i + 2)


def h_fm(dst_off, scale=None, func=None):
    def mk(k, c_base, nchunks=4):
        def handler(W, wkey):
            S = k.S
            for m in range(nchunks):
                for t in range(NT):
                    b = next_bank(k)
                    for kc in range(NKC):
                        S.op("pe", lambda e, b=b, m=m, kc=kc, t=t: e.matmul(
                            k.bank(b), W[:, kc, m * 128:(m + 1) * 128], k.H(kc, t * 512, 512),
                            start=(kc == 0), stop=(kc == NKC - 1)),
                            reads=[wkey, ("H", kc, t)], writes=[("ps", b)])
                    evac(k, fm(k, dst_off, c_base + m, t * 512, 512), k.bank(b), ("ps", b),
                         ("fm", dst_off, c_base + m, t), scale=scale, func=func)
        return handler
    return mk


def h_tm(k, vbuf_fn, nh, dv):
    def handler(W, wkey):
        S = k.S
        for tt in range(NT128):
            b = next_bank(k)
            for kc in range(NKC):
                S.op("pe", lambda e, b=b, kc=kc, tt=tt: e.matmul(
                    k.bank(b), k.H(kc, tt * 128, 128), W[:, kc, 0:512],
                    start=(kc == 0), stop=(kc == NKC - 1)),
                    reads=[wkey, ("H", kc, tt // 4)], writes=[("ps", b)])
            dst = vbuf_fn(tt)[:, :, 0:dv]
            src = k.bank(b).rearrange("p (h e) -> p h e", h=nh)
            evac(k, dst, src, ("ps", b), ("V", tt))
    return handler


class AttnPass:
    def __init__(self, k, *, qf, kf, aqf, akf, bias_fn, vf, vkey, dv, mode, fin, sbanks, obanks, scale=1.0, augkey=None, pre=None, act_func=None, scale_fn=None, mask_mul=False):
        self.__dict__.update(locals())
        self.state = {}
        self.per_bank = 2 if dv == 128 else 4

    def blocks(self):
        return [(self, j, i) for j in range(4) for i in range(4 * j + 4)]

    def O(self, j, r):
        pb = self.per_bank
        ob = self.obanks[j % len(self.obanks)][r // pb]
        c0 = (r % pb) * (self.dv + 1)
        return self.k.bank(ob)[:, c0:c0 + self.dv + 1], ("ps", ob)

    def emit_s(self, j, i):
        k, S = self.k, self.k.S
        if self.pre is not None:
            pl = self.pre if isinstance(self.pre, list) else [self.pre]
            self.pre = None
            for f_ in pl:
                f_()
        r0 = max(0, i - 4 * j)
        n0 = r0 * 128
        ncol = 512 - n0
        diag = i >= 4 * j
        k._blk = getattr(k, "_blk", 0) + 1
        pti = k._blk % 4
        pt = k.PT(pti)
        ptk = ("PT", pti)
        if self.mode == "exp":
            sb = self.sbanks[k._blk % len(self.sbanks)]
            has_aug = self.akf is not None
            for rep in range(1 + WARM_FILL):
                real = (rep == WARM_FILL)
                S.op("pe", lambda e, real=real: e.matmul(k.bank(sb)[:, n0:512], self.kf(i), self.qf(j * 512 + n0, ncol),
                                                         start=True, stop=((not has_aug and (not diag or self.mask_mul)) or not real)),
                     reads=([self.augkey] if (self.augkey is not None and not has_aug) else []), writes=[("ps", sb)])
            if has_aug:
                S.op("pe", lambda e: e.matmul(k.bank(sb)[:, n0:512], self.akf(i), self.aqf(j, n0, ncol),
                                              start=False, stop=(not diag)),
                     reads=[self.augkey], writes=[("ps", sb)])
            if diag and not self.mask_mul:
                S.op("pe", lambda e: e.matmul(k.bank(sb)[:, n0:n0 + 128], k.ident_bf, k.negtri_bf, start=False, stop=True),
                     reads=["identbf", "negtri"], writes=[("ps", sb)])
            bias = float(self.bias_fn(i, j)) if self.bias_fn is not None else 0.0
            if self.act_func is None:
                S.op("act", lambda e: e.activation(out=pt[:, n0:512], in_=k.bank(sb)[:, n0:512], func=AF.Exp, bias=bias, scale=1.0),
                     reads=[("ps", sb)], writes=[ptk])
            else:
                sc_ap = self.scale_fn(i, j)
                S.op("act", lambda e: e.activation(out=pt[:, n0:512], in_=k.bank(sb)[:, n0:512], func=self.act_func, scale=sc_ap),
                     reads=[("ps", sb), "UC"], writes=[ptk])
            if diag and self.mask_mul:
                S.op("pool", lambda e: e.tensor_tensor(out=pt[:, n0:n0 + 128], in0=pt[:, n0:n0 + 128], in1=k.tri_bf, op=ALU.mult),
                     reads=[ptk, "tri"], writes=[ptk])
        else:
            pr = self.sbanks[k._blk % len(self.sbanks)]
            sb, eb = pr
            S.op("pe", lambda e: e.matmul(k.bank(sb)[:, n0:512], self.kf(i), self.qf(j * 512 + n0, ncol), start=True, stop=True),
                 reads=[], writes=[("ps", sb)])
            S.op("pe", lambda e: e.matmul(k.bank(eb)[:, n0:512], self.akf(i), self.aqf(j, n0, ncol), start=True, stop=(not diag)),
                 reads=[self.augkey], writes=[("ps", eb)])
            if diag:
                S.op("pe", lambda e: e.matmul(k.bank(eb)[:, n0:n0 + 128], k.ident_bf, k.negtri_bf, start=False, stop=True),
                     reads=["identbf", "negtri"], writes=[("ps", eb)])
            eti = k._blk % 2
            et = k.f32(SB + eti * 512, 512)
            S.op("act", lambda e: e.activation(out=et[:, n0:512], in_=k.bank(eb)[:, n0:512], func=AF.Exp),
                 reads=[("ps", eb)], writes=[("ET", eti)])
            sc = self.scale
            S.op("dve", lambda e: e.scalar_tensor_tensor(out=pt[:, n0:512], in0=k.bank(sb)[:, n0:512], scalar=sc,
                                                         in1=et[:, n0:512], op0=ALU.mult, op1=ALU.mult),
                 reads=[("ps", sb), ("ET", eti)], writes=[ptk])
        self.state[(j, i)] = (pt, ptk, r0)

    def emit_pv(self, j, i):
        k, S = self.k, self.k.S
        pt, ptk, r0 = self.state.pop((j, i))
        for r in range(r0, 4):
            o, okey = self.O(j, r)
            last = (i == 4 * j + r)
            S.op("pe", lambda e, r=r, o=o, last=last: e.matmul(o, pt[:, r * 128:(r + 1) * 128], self.vf(i), start=(i == 0 and r % self.per_bank == 0), stop=last, skip_group_check=True),
                 reads=[ptk, self.vkey], writes=[okey])
            if last:
                self.fin(j, r, o, okey)


def run_blocks(k, blocks, L=2):
    n = len(blocks)
    for idx in range(n + L):
        if idx < n:
            p, j, i = blocks[idx]
            p.emit_s(j, i)
        if idx >= L:
            p, j, i = blocks[idx - L]
            p.emit_pv(j, i)
        tick(k)
    flush_deferred(k)


def defer(k, n, fn):
    k._dq = getattr(k, "_dq", [])
    k._dq.append([n, fn])


def tick(k):
    dq = getattr(k, "_dq", [])
    k._dq = []
    keep = []
    for it in dq:
        it[0] -= 1
        if it[0] <= 0:
            it[1]()
        else:
            keep.append(it)
    k._dq = keep + k._dq


def flush_deferred(k):
    while getattr(k, "_dq", []):
        dq = k._dq
        k._dq = []
        for it in dq:
            it[1]()


def transpose_to_fm(k, src, src_key, dst, dst_key, post=None):
    S = k.S
    q = getattr(k, "_tq", 0)
    k._tq = q + 1
    tb = k.tbanks[q % len(k.tbanks)]
    pq = k.bank(tb)[:, 0:128]
    pkey = ("ps", tb)
    S.op("pe", lambda e: e.transpose(pq, src, k.ident), reads=[src_key, "ident"], writes=[pkey])
    if post is None:
        evac(k, dst, pq, pkey, dst_key, eng="act")
    else:
        post(pq, pkey)


def softplus_neg(k, T, nh, banks, negb_col, key):
    S = k.S
    for t in range(NT):
        b = banks[t]
        S.op("act", lambda e, b=b, t=t: e.activation(out=T[0:nh, t * 512:(t + 1) * 512], in_=k.bank(b)[0:nh, :],
                                                    func=AF.Exp, bias=negb_col, scale=-1.0),
             reads=[("ps", b), "gcols"], writes=[key])
    S.op("act", lambda e: e.activation(out=T[0:nh, :], in_=T[0:nh, :], func=AF.Ln, bias=1.0, scale=1.0),
         reads=[key], writes=[key])


def build_aug(k, kvec, kkey, qvec, qkey, nh, spl_off, tag):
    S, d = k.S, k.d
    dk, dq = d["augk_" + tag], d["augq_" + tag]
    SPL = [k.bf(spl_off + i * 1024, 2048) for i in range(4)]
    ones = SPL[3]
    S.op("dve", lambda e: e.memset(ones[0:nh, :], 1.0), writes=[("SPL", 3)])
    for r in range(3):
        S.dma("sp", "augw", lambda e, r=r: e.dma_start(out=dk[0:nh, 3 + r, :], in_=ones[0:nh, :]), reads=[("SPL", 3)], writes=[("augd", tag)])
        S.dma("sp", "augw", lambda e, r=r: e.dma_start(out=dq[0:nh, r, :], in_=ones[0:nh, :]), reads=[("SPL", 3)], writes=[("augd", tag)])
    cnt = 0
    for vec, vkey, dram, r0 in ((kvec, kkey, dk, 0), (qvec, qkey, dq, 3)):
        for r in range(3):
            si = cnt % 3
            cnt += 1
            spl = SPL[si]
            S.op("dve", lambda e, spl=spl, vec=vec: e.tensor_copy(spl[0:nh, :], vec[0:nh, :]), reads=[vkey], writes=[("SPL", si)])
            if r < 2:
                S.op("dve", lambda e, spl=spl, vec=vec: e.tensor_tensor(out=vec[0:nh, :], in0=vec[0:nh, :], in1=spl[0:nh, :], op=ALU.subtract),
                     reads=[vkey, ("SPL", si)], writes=[vkey])
            S.dma("sp", "augw", lambda e, spl=spl, dram=dram, rr=r0 + r: e.dma_start(out=dram[0:nh, rr, :], in_=spl[0:nh, :]),
                  reads=[("SPL", si)], writes=[("augd", tag)])


def aug_views(k, slot):
    ak = k.bf(W_W + 5120 + slot * 2048, 2048)
    aq = k.bf(W_W + 5120 + slot * 2048 + 1024, 2048)
    akf = lambda i: ak[0:6, i * 128:(i + 1) * 128]
    aqf = lambda j, n0, n: aq[0:6, j * 512 + n0:j * 512 + n0 + n]
    return akf, aqf


def load_aug(k, tag, h, slot):
    S, d = k.S, k.d
    ak = k.bf(W_W + 5120 + slot * 2048, 2048)
    aq = k.bf(W_W + 5120 + slot * 2048 + 1024, 2048)
    S.dma("sp", f"augl{slot}", lambda e: e.dma_start(out=ak[0:6, :], in_=d["augk_" + tag][h, :, :]), reads=[("augd", tag)], writes=[("AUGS", slot)])
    S.dma("sp", f"augl{slot}", lambda e: e.dma_start(out=aq[0:6, :], in_=d["augq_" + tag][h, :, :]), reads=[("augd", tag)], writes=[("AUGS", slot)])


HB_OFF = [W_W + 0, W_W + 2048, W_W + 5120, W_W + 7168]


def hb_views(k, slot):
    return k.bf(HB_OFF[slot], 2048), k.bf(HB_OFF[slot] + 1024, 2048)


def hb_init(k):
    S = k.S
    for s_ in range(4):
        qb, kb = hb_views(k, s_)
        S.op("act", lambda e, qb=qb: e.memzero(qb), writes=[("HB", s_)])
        S.op("act", lambda e, kb=kb: e.memzero(kb), writes=[("HB", s_)])


def hb_build(k, slot, par, q_src, k_src, augq_ap, augk_ap, R, queue):
    S = k.S
    qb, kb = hb_views(k, slot)
    r0 = par * 64
    a0 = 64 if par == 0 else 0
    S.dma("sp", f"hbsp{slot}", lambda e: e.dma_start(out=qb[r0:r0 + 64, :], in_=q_src[r0:r0 + 64, :]), writes=[("HB", slot)])
    S.dma("sp", f"hbsp{slot}", lambda e: e.dma_start(out=kb[r0:r0 + 64, :], in_=k_src[r0:r0 + 64, :]), writes=[("HB", slot)])
    S.dma(queue, f"hb{queue}{slot}", lambda e: e.dma_start(out=qb[a0:a0 + R, :], in_=augq_ap), writes=[("HB", slot)])
    S.dma(queue, f"hb{queue}{slot}", lambda e: e.dma_start(out=kb[a0:a0 + R, :], in_=augk_ap), writes=[("HB", slot)])


def layer_consts(k, l):
    S, d = k.S, k.d
    o = k.const_end
    k.gb = k.f32(o, 24); o += 24
    k.convw = k.f32(o, 16); o += 16
    k.convb = k.f32(o, 4); o += 4
    k.skipc = k.f32(o, 4); o += 4
    k.normc = k.f32(o, 4); o += 4
    k.gcols = k.f32(o, 8); o += 8
    k.lamt = k.f32(o, 256); o += 256
    k.lamc = k.f32(o, 8); o += 8
    k.subln = k.f32(o, 128); o += 128
    k.normB = k.f32(o, 512); o += 512
    k.AKd = k.bf(o, 4 * 128); o += 256
    k.AQd = k.bf(o, 4 * 512); o += 1024
    k.ident_bf = k.bf(o, 128); o += 64
    k.negtri_bf = k.bf(o, 128); o += 64
    assert o <= W_CONST + 3072, o
    S.dma("sp", "lc", lambda e: e.dma_start(out=k.gb, in_=d["gbcols"][l, :, :]), writes=["lconst"])
    S.dma("sp", "lc", lambda e: e.dma_start(out=k.convw, in_=d["convw"][l, :, :]), writes=["lconst"])
    S.dma("sp", "lc", lambda e: e.dma_start(out=k.convb, in_=d["convb"][l, :, :]), writes=["lconst"])
    S.dma("sp", "lc", lambda e: e.dma_start(out=k.skipc, in_=d["skipc"][l, :, :]), writes=["lconst"])
    S.dma("sp", "lc", lambda e: e.dma_start(out=k.normc, in_=d["normc"][l, :, :]), writes=["lconst"])
    S.dma("sp", "lc", lambda e: e.dma_start(out=k.gcols[0:8, :], in_=d["gcols"][l, :, :]), writes=["gcols"])
    S.dma("sp", "lc", lambda e: e.dma_start(out=k.lamt, in_=d["difflam"][l:l + 1, :].partition_broadcast(128)), writes=["lamt"])
    S.dma("sp", "lc", lambda e: e.dma_start(out=k.subln, in_=d["diff_subln"][l:l + 1, :].partition_broadcast(128)), writes=["subln"])
    S.dma("sp", "lc", lambda e: e.dma_start(out=k.normB, in_=d["mlstm_norm"][l:l + 1, :].partition_broadcast(128)), writes=["normB"])
    S.op("dve", lambda e: e.tensor_scalar(k.gcols[0:8, 4:5], k.gcols[0:8, 0:1], -1.0, None, ALU.mult), reads=["gcols"], writes=["gcols"])
    S.op("dve", lambda e: e.tensor_scalar(k.gcols[0:8, 5:6], k.gcols[0:8, 2:3], -1.0, None, ALU.mult), reads=["gcols"], writes=["gcols"])
    if l == 0:
        S.dma("pool", "lc2", lambda e: e.dma_start(out=k.AKd[0:3, :], in_=d["alibi_k"][:, :]), writes=["alibi"])
        S.dma("pool", "lc2", lambda e: e.dma_start(out=k.AQd[0:3, :], in_=d["alibi_q"][:, :]), writes=["alibi"])
        S.dma("pool", "lc2", lambda e: e.dma_start(out=k.ident_bf, in_=d["ident"][:, :]), writes=["identbf"])
        S.dma("pool", "lc2", lambda e: e.dma_start(out=k.negtri_bf, in_=d["negtri"][:, :]), writes=["negtri"])
    import math
    lam_init = 0.8 - 0.6 * math.exp(-0.3 * l)
    lt, lc = k.lamt, k.lamc
    S.op("dve", lambda e: e.tensor_tensor(out=lt[:, 0:64], in0=lt[:, 0:64], in1=lt[:, 64:128], op=ALU.mult), reads=["lamt"], writes=["lamt"])
    S.op("dve", lambda e: e.tensor_tensor(out=lt[:, 128:192], in0=lt[:, 128:192], in1=lt[:, 192:256], op=ALU.mult), reads=["lamt"], writes=["lamt"])
    S.op("dve", lambda e: e.reduce_sum(out=lc[:, 0:1], in_=lt[:, 0:64], axis=AX.X), reads=["lamt"], writes=["lamc"])
    S.op("dve", lambda e: e.reduce_sum(out=lc[:, 1:2], in_=lt[:, 128:192], axis=AX.X), reads=["lamt"], writes=["lamc"])
    S.op("act", lambda e: e.activation(out=lc[:, 2:4], in_=lc[:, 0:2], func=AF.Exp), reads=["lamc"], writes=["lamc"])
    S.op("dve", lambda e: e.tensor_tensor(out=lc[:, 4:5], in0=lc[:, 3:4], in1=lc[:, 2:3], op=ALU.subtract), reads=["lamc"], writes=["lamc"])
    S.op("dve", lambda e: e.tensor_scalar(lc[:, 5:6], lc[:, 4:5], -lam_init, None, ALU.add), reads=["lamc"], writes=["neglam"])
    S.op("dve", lambda e: e.tensor_scalar(k.subln, k.subln, 1.0 - lam_init, None, ALU.mult), reads=["subln"], writes=["subln"])
    k.neglam = lc[:, 5:6]


def diff_branch(k, l):
    S = k.S
    QT, KT, VO, OD = DU(0), DU(1), DU(2), NU(0)
    V = k.bf(VO, 16 * 4 * 129).rearrange("p (t h e) -> p t h e", t=16, h=4)
    S.op("dve", lambda e: e.memset(V[:, :, :, 128:129], 1.0), writes=[("V", "ones")])
    wcol_jobs(k, l, [
        (C_DQ, 512, h_fm(QT)(k, 0)),
        (C_DK, 512, h_fm(KT, scale=0.125)(k, 0)),
        (C_DV, 512, h_tm(k, lambda tt: V[:, tt, :, :], 4, 128)),
    ])
    S.barrier()
    k.tbanks = [3]
    slopes = [2.0 ** (-8.0 * (h + 1) / 4) for h in range(4)]
    A1 = [[k.f32(SB + (p * 4 + r) * 128, 128) for r in range(4)] for p in range(2)]
    TB = SB + 1024
    blocks = []
    pres = []
    first_pass = []
    hb_init(k)
    for h in range(4):
        passes = []
        for c in range(2):
            slot = c + 2 * (h % 2)
            qb, kb = hb_views(k, slot)

            def qf(t0, n, qb=qb):
                return qb[:, t0:t0 + n]

            def kf(i, kb=kb):
                return kb[:, i * 128:(i + 1) * 128]

            pres.append(lambda h=h, c=c, slot=slot: hb_build(k, slot, c, fm(k, QT, h), fm(k, KT, h),
                                                              k.d["alibi_qf"][h, :, :], k.d["alibi_kf"][h, :, :], 3, "pool"))

            def bias_fn(i, j, h=h):
                return slopes[h] * (128.0 * i - 512.0 * j)

            def vf(i, h=h):
                return V[:, i, h, :]

            if c == 0:
                def fin(j, r, o, okey, h=h):
                    rc, rk = col(k)
                    a1 = A1[j % 2][r]
                    S.op("dve", lambda e: e.reciprocal(rc, o[:, 128:129]), reads=[okey], writes=[rk])
                    S.op("dve", lambda e: e.tensor_scalar(a1, o[:, 0:128], rc, None, ALU.mult), reads=[okey, rk], writes=[("A1", j % 2, r)])
            else:
                def fin(j, r, o, okey, h=h):
                    rc, rk = col(k)
                    sc, sk = col(k)
                    k._fp = getattr(k, "_fp", 0) + 1
                    n = k._fp
                    tmp = k.f32(TB + (n % 5) * 128, 128)
                    sq = k.f32(TB + 640 + (n % 2) * 128, 128)
                    ot = k.f32(TB + 896 + (n % 4) * 128, 128)
                    tk_, sqk, otk = ("TMP", n % 5), ("SQ", n % 2), ("OT", n % 4)
                    a1 = A1[j % 2][r]
                    tt = 4 * j + r
                    S.op("dve", lambda e: e.reciprocal(rc, o[:, 128:129]), reads=[okey], writes=[rk])
                    S.op("dve", lambda e: e.tensor_scalar(tmp, o[:, 0:128], rc, None, ALU.mult), reads=[okey, rk], writes=[tk_])
                    S.op("dve", lambda e: e.scalar_tensor_tensor(out=tmp, in0=tmp, scalar=k.neglam, in1=a1, op0=ALU.mult, op1=ALU.add),
                         reads=[tk_, ("A1", j % 2, r), "neglam"], writes=[tk_])

                    def stage_b():
                        S.op("act", lambda e: e.activation(out=sq, in_=tmp, func=AF.Square, accum_out=sc), reads=[tk_], writes=[sqk, sk])
                        S.op("act", lambda e: e.activation(out=sc, in_=sc, func=AF.Ln, bias=RMS_EPS, scale=1.0 / 128), reads=[sk], writes=[sk])
                        S.op("act", lambda e: e.activation(out=sc, in_=sc, func=AF.Exp, scale=-0.5), reads=[sk], writes=[sk])

                    def stage_c():
                        S.op("dve", lambda e: e.scalar_tensor_tensor(out=ot, in0=tmp, scalar=sc, in1=k.subln, op0=ALU.mult, op1=ALU.mult),
                             reads=[tk_, sk, "subln"], writes=[otk])
                        defer(k, 2, lambda: transpose_to_fm(k, ot, otk, fm(k, OD, h, tt * 128, 128), ("fm", OD, h, tt)))
                    defer(k, 2, stage_b)
                    defer(k, 3, stage_c)
            passes.append(AttnPass(k, qf=qf, kf=kf, aqf=None, akf=None, bias_fn=bias_fn, vf=vf, vkey=("V", "x"), dv=128,
                                   mode="exp", fin=fin, sbanks=[0, 1, 2], obanks=[(4, 5)] if c == 0 else [(6, 7)], augkey=("HB", slot)))
        first_pass.append(passes[0])
        b0, b1 = passes[0].blocks(), passes[1].blocks()
        for j in range(4):
            blocks += [b for b in b0 if b[1] == j]
            blocks += [b for b in b1 if b[1] == j]
    for u in range(4):
        first_pass[u].pre = (pres[0:4] if u == 0 else (pres[2 * u + 2:2 * u + 4] if u < 3 else None))
    run_blocks(k, blocks)
    S.barrier()


def fox_branch(k, l):
    S, d = k.S, k.d
    QT, KT, VO, OF = DU(0), DU(1), DU(2), NU(2)
    V = k.bf(VO, 16 * 8 * 65).rearrange("p (t h e) -> p t h e", t=16, h=8)
    WFF = k.bf(W_W + 4096, 64).rearrange("p (k c) -> p k c", k=8)
    S.op("dve", lambda e: e.memset(V[:, :, :, 64:65], 1.0), writes=[("V", "ones")])
    S.dma("pool", "wff", lambda e: e.dma_start(out=WFF, in_=d["w_in"][l, :, :, C_FF:C_FF + 8]), writes=["WFF"])
    S.barrier()
    wcol_jobs(k, l, [
        (C_FQ, 512, h_fm(QT)(k, 0)),
        (C_FK, 512, h_fm(KT, scale=0.125)(k, 0)),
        (C_FV, 512, h_tm(k, lambda tt: V[:, tt, :, :], 8, 64)),
    ])
    for t in range(NT):
        for kc in range(NKC):
            S.op("pe", lambda e, kc=kc, t=t: e.matmul(k.bank(t)[0:8, :], WFF[:, kc, :], k.H(kc, t * 512, 512),
                                                      start=(kc == 0), stop=(kc == NKC - 1)),
                 reads=["WFF", ("H", kc, t)], writes=[("ps", t)])
    S.barrier()
    T1 = k.f32(W_W, 2048)
    T2 = k.f32(W_W + 2048, 2048)
    ONES = k.f32(W_W + 7168, 2048)
    S.op("dve", lambda e: e.memset(ONES[0:8, :], 1.0), writes=["ONES"])
    softplus_neg(k, T1, 8, [0, 1, 2, 3], k.gcols[0:8, 4:5], "T1")
    S.op("dve", lambda e: e.tensor_tensor_scan(T2[0:8, :], ONES[0:8, :], T1[0:8, :], 0.0, ALU.mult, ALU.add), reads=["T1", "ONES"], writes=["T2"])
    S.op("dve", lambda e: e.tensor_scalar(T1[0:8, :], T2[0:8, :], -1.0, None, ALU.mult), reads=["T2"], writes=["T1"])
    build_aug(k, T2, "T2", T1, "T1", 8, NU(2), "fox")
    S.barrier()
    k.tbanks = [3]
    OTB = [[k.f32(SB + (p * 4 + r) * 128, 128) for r in range(4)] for p in range(2)]
    blocks = []
    pres = []
    first_pass = []
    hb_init(k)
    for pr in range(4):
        passes = []
        for c in range(2):
            hd = 2 * pr + c
            slot = c + 2 * (pr % 2)
            qb, kb = hb_views(k, slot)

            def qf(t0, n, qb=qb):
                return qb[:, t0:t0 + n]

            def kf(i, kb=kb):
                return kb[:, i * 128:(i + 1) * 128]

            pres.append(lambda pr=pr, c=c, slot=slot, hd=hd: hb_build(k, slot, c, fm(k, QT, pr), fm(k, KT, pr),
                                                                      k.d["augq_fox"][hd, :, :], k.d["augk_fox"][hd, :, :], 6, "sp"))

            def vf(i, hd=hd):
                return V[:, i, hd, :]

            def fin(j, r, o, okey, pr=pr, c=c):
                rc, rk = col(k)
                ot = OTB[j % 2][r]
                otk = ("OTB", j % 2, r)
                S.op("dve", lambda e: e.reciprocal(rc, o[:, 64:65]), reads=[okey], writes=[rk])
                S.op("dve", lambda e: e.tensor_scalar(ot[:, c * 64:(c + 1) * 64], o[:, 0:64], rc, None, ALU.mult), reads=[okey, rk], writes=[otk])
                if c == 1:
                    tt = 4 * j + r
                    defer(k, 3, lambda: transpose_to_fm(k, ot, otk, fm(k, OF, pr, tt * 128, 128), ("fm", OF, pr, tt)))
            passes.append(AttnPass(k, qf=qf, kf=kf, aqf=None, akf=None, bias_fn=None, vf=vf, vkey=("V", "x"), dv=64,
                                   mode="exp", fin=fin, sbanks=[0, 1, 2], obanks=[(4,), (5,)] if c == 0 else [(6,), (7,)], augkey=("HB", slot)))
        first_pass.append(passes[0])
        b0, b1 = passes[0].blocks(), passes[1].blocks()
        for j in range(4):
            blocks += [b for b in b0 if b[1] == j]
            blocks += [b for b in b1 if b[1] == j]
    for u in range(4):
        first_pass[u].pre = (pres[0:4] if u == 0 else (pres[2 * u + 2:2 * u + 4] if u < 3 else None))
    run_blocks(k, blocks, L=3)
    S.barrier()


def mlstm_branch(k, l):
    S, d = k.S, k.d
    QT, KT, VT, MX, XC, SZ, VA = DU(0), DU(1), DU(2), DU(3), NU(0), NU(1), NU(2)
    Vall = k.bf(VA, 16 * 4 * 129).rearrange("p (t h e) -> p t h e", t=16, h=4)
    WQKV = k.bf(W_W + 4096, 1536).rearrange("p (g h e) -> p g h e", g=3, h=4)
    WIF = k.bf(W_W + 4096 + 768, 96).rearrange("p (j o) -> p j o", j=12)
    S.op("dve", lambda e: e.memset(Vall[:, :, :, 128:129], 1.0), writes=[("V", "ones")])
    S.dma("pool", "wqkv", lambda e: e.dma_start(out=k.bf(W_W + 4096, 1536), in_=d["wqkv"][l, :, :]), writes=["WQKV"])
    S.dma("pool", "wqkv", lambda e: e.dma_start(out=k.bf(W_W + 4096 + 768, 96), in_=d["wif"][l, :, :]), writes=["WIF"])
    S.barrier()
    wcol_jobs(k, l, [
        (C_MX, 512, h_fm(MX)(k, 0)),
        (C_MZ, 512, h_fm(SZ, func=AF.Silu)(k, 0)),
    ])
    ACC = k.f32(SB, 2048)
    for c in range(4):
        mx = fm(k, MX, c)
        mkeys = [("fm", MX, c, t) for t in range(NT)]
        w = lambda j, c=c: k.convw[:, j * 4 + c:j * 4 + c + 1]
        S.op("dve", lambda e, mx=mx, c=c, w=w: e.tensor_scalar(ACC, mx, w(3), k.convb[:, c:c + 1], ALU.mult, ALU.add),
             reads=mkeys + ["lconst"], writes=["ACC"])
        for sh in (1, 2, 3):
            S.op("dve", lambda e, mx=mx, sh=sh, w=w: e.scalar_tensor_tensor(
                out=ACC[:, sh:], in0=mx[:, 0:S_LEN - sh], scalar=w(3 - sh), in1=ACC[:, sh:], op0=ALU.mult, op1=ALU.add),
                reads=mkeys + ["ACC", "lconst"], writes=["ACC"])
        S.op("act", lambda e, c=c: e.activation(out=fm(k, XC, c), in_=ACC, func=AF.Silu), reads=["ACC"],
             writes=[("fm", XC, c, t) for t in range(NT)])
    for h in range(4):
        for t in range(NT):
            for g, src, dst in ((0, XC, QT), (1, XC, KT), (2, MX, VT)):
                b = next_bank(k)
                S.op("pe", lambda e, b=b, g=g, src=src, h=h, t=t: e.matmul(
                    k.bank(b), WQKV[:, g, h, :], fm(k, src, h, t * 512, 512), start=True, stop=True),
                    reads=["WQKV", ("fm", src, h, t)], writes=[("ps", b)])
                evac(k, fm(k, dst, h, t * 512, 512), k.bank(b), ("ps", b), ("fm", dst, h, t))
        for tt in range(NT128):
            b = next_bank(k)
            S.op("pe", lambda e, b=b, h=h, tt=tt: e.matmul(
                k.bank(b)[:, 0:128], fm(k, MX, h, tt * 128, 128), WQKV[:, 2, h, :], start=True, stop=True),
                reads=["WQKV", ("fm", MX, h, tt // 4)], writes=[("ps", b)])
            evac(k, Vall[:, tt, h, 0:128], k.bank(b)[:, 0:128], ("ps", b), ("V", tt, h))
    for h in range(4):
        S.op("dve", lambda e, h=h: e.tensor_scalar(fm(k, XC, h), fm(k, XC, h), k.skipc[:, h:h + 1], None, ALU.mult),
             reads=[("fm", XC, h, t) for t in range(NT)] + ["lconst"], writes=[("fm", XC, h, t) for t in range(NT)])
    S.barrier()
    for t in range(NT):
        for g, bb in ((0, t), (1, 4 + t)):
            for jj in range(12):
                src = (QT, KT, VT)[jj // 4]
                S.op("pe", lambda e, g=g, bb=bb, jj=jj, src=src, t=t: e.matmul(
                    k.bank(bb)[0:4, :], WIF[:, jj, g * 4:(g + 1) * 4], fm(k, src, jj % 4, t * 512, 512),
                    start=(jj == 0), stop=(jj == 11)), reads=["WIF"], writes=[("ps", bb)])
    T1 = k.f32(W_W, 2048)
    T2 = k.f32(W_W + 2048, 2048)
    T3 = k.f32(W_W + 5120, 2048)
    ONES = k.f32(W_W + 7168, 2048)
    S.op("dve", lambda e: e.memset(ONES[0:4, :], 1.0), writes=["ONES"])
    softplus_neg(k, T1, 4, [4, 5, 6, 7], k.gcols[0:4, 5:6], "T1")
    S.op("dve", lambda e: e.tensor_tensor_scan(T2[0:4, :], ONES[0:4, :], T1[0:4, :], 0.0, ALU.mult, ALU.add), reads=["T1", "ONES"], writes=["T2"])
    for t in range(NT):
        S.op("dve", lambda e, t=t: e.tensor_scalar(T1[0:4, t * 512:(t + 1) * 512], k.bank(t)[0:4, :], k.gcols[0:4, 1:2], None, ALU.add),
             reads=[("ps", t), "gcols", "T2"], writes=["T1"])
    S.op("dve", lambda e: e.tensor_tensor(out=T1[0:4, :], in0=T1[0:4, :], in1=T2[0:4, :], op=ALU.add), reads=["T1", "T2"], writes=["T1"])
    S.op("dve", lambda e: e.tensor_tensor_scan(T3[0:4, :], ONES[0:4, :], T1[0:4, :], 0.0, ALU.mult, ALU.max), reads=["T1", "ONES"], writes=["T3"])
    S.op("dve", lambda e: e.tensor_scalar(T3[0:4, :], T3[0:4, :], -1.0, None, ALU.mult), reads=["T3"], writes=["T3"])
    UR = ONES
    EM = k.f32(W_CONST + 2700, 64)
    UC = k.f32(W_CONST + 2780, 160)
    for j in range(NT):
        nb = T3[0:4, 512 * j - 1:512 * j] if j > 0 else 0.0
        S.op("act", lambda e, j=j, nb=nb: e.activation(out=T2[0:4, j * 512:(j + 1) * 512], in_=T2[0:4, j * 512:(j + 1) * 512],
                                                     func=AF.Exp, bias=nb, scale=1.0), reads=["T2", "T3"], writes=["T2"])
    for tt in range(NT128):
        S.op("pe", lambda e, tt=tt: e.transpose(k.bank(3)[:, tt * 4:(tt + 1) * 4], T2[0:4, tt * 128:(tt + 1) * 128], k.ident[0:4, 0:4]),
             reads=["T2", "ident"], writes=[("ps", 3)])
    S.op("dve", lambda e: e.tensor_copy(EM, k.bank(3)[:, 0:64]), reads=[("ps", 3)], writes=["EM"])
    for j in range(NT):
        nb = T3[0:4, 512 * j - 1:512 * j] if j > 0 else 0.0
        ncol = (j + 1) * 512
        S.op("act", lambda e, nb=nb, ncol=ncol: e.activation(out=UR[0:4, 0:ncol], in_=T1[0:4, 0:ncol], func=AF.Exp, bias=nb, scale=1.0),
             reads=["T1", "T3", "ONES"], writes=["ONES"])
        for i in range(4 * j + 4):
            idx = 2 * j * (j + 1) + i
            S.op("pe", lambda e, i=i, idx=idx: e.transpose(k.bank(2)[:, idx * 4:(idx + 1) * 4], UR[0:4, i * 128:(i + 1) * 128], k.ident[0:4, 0:4]),
                 reads=["ONES", "ident"], writes=[("ps", 2)])
    S.op("dve", lambda e: e.tensor_scalar(UC, k.bank(2)[:, 0:160], 128.0 ** -0.5, None, ALU.mult), reads=[("ps", 2)], writes=["UC"])
    S.barrier()
    k.tbanks = [3, 7]
    TB = SB + 1024
    blocks = []
    for h in range(4):

        def qf(t0, n, h=h):
            return fm(k, QT, h, t0, n)

        def kf(i, h=h):
            return fm(k, KT, h, i * 128, 128)

        def vf(i, h=h):
            return Vall[:, i, h, :]

        def fin(j, r, o, okey, h=h):
            tt = 4 * j + r
            c1, k1 = col(k)
            c2, k2 = col(k)
            c3, k3 = col(k)
            k._fp = getattr(k, "_fp", 0) + 1
            n = k._fp
            NUM = k.f32(TB + (n % 4) * 128, 128)
            TF = k.f32(TB + 640 + (n % 2) * 128, 128)
            HN = k.f32(TB + 896 + (n % 4) * 128, 128)
            ST6 = k.f32(TB + 512 + (n % 8) * 8, 6)
            MV = k.f32(TB + 576 + (n % 8) * 4, 2)
            hk, nk, fk, stk, mvk = ("HH", n % 4), ("HN", n % 4), ("TF", n % 2), ("ST6", n % 8), ("MV", n % 8)
            den = o[:, 128:129]
            S.op("dve", lambda e: e.tensor_tensor(out=c1, in0=den, in1=EM[:, tt * 4 + h:tt * 4 + h + 1], op=ALU.max), reads=[okey, "EM"], writes=[k1])
            S.op("dve", lambda e: e.scalar_tensor_tensor(out=c1, in0=den, scalar=-1.0, in1=c1, op0=ALU.mult, op1=ALU.max), reads=[okey, k1], writes=[k1])
            S.op("dve", lambda e: e.reciprocal(c1, c1), reads=[k1], writes=[k1])
            S.op("dve", lambda e: e.tensor_scalar(NUM, o[:, 0:128], c1, None, ALU.mult), reads=[okey, k1], writes=[hk])
            S.op("dve", lambda e: e.bn_stats(ST6, NUM), reads=[hk], writes=[stk])
            S.op("dve", lambda e: e.bn_aggr(MV, ST6), reads=[stk], writes=[mvk])

            def stage_b():
                S.op("act", lambda e: e.activation(out=c2, in_=MV[:, 1:2], func=AF.Ln, bias=LN_EPS, scale=1.0), reads=[mvk], writes=[k2])
                S.op("act", lambda e: e.activation(out=c2, in_=c2, func=AF.Exp, scale=-0.5), reads=[k2], writes=[k2])

            def post(pq, pkey):
                xs = fm(k, XC, h, tt * 128, 128)
                sz = fm(k, SZ, h, tt * 128, 128)
                S.op("dve", lambda e: e.scalar_tensor_tensor(out=TF, in0=pq, scalar=k.normc[:, h:h + 1], in1=xs, op0=ALU.mult, op1=ALU.add),
                     reads=[pkey, "lconst"], writes=[fk])
                S.op("dve", lambda e: e.tensor_tensor(out=sz, in0=TF, in1=sz, op=ALU.mult), reads=[fk], writes=[("fm", SZ, h, tt)])

            def stage_c():
                S.op("dve", lambda e: e.tensor_scalar(HN, NUM, MV[:, 0:1], c2, ALU.subtract, ALU.mult), reads=[hk, mvk, k2], writes=[nk])
                defer(k, 2, lambda: transpose_to_fm(k, HN, nk, None, None, post=post))
            defer(k, 2, stage_b)
            defer(k, 3, stage_c)

        def scale_fn(i, j, h=h):
            idx = 2 * j * (j + 1) + i
            return UC[:, idx * 4 + h:idx * 4 + h + 1]
        ps_ = AttnPass(k, qf=qf, kf=kf, aqf=None, akf=None, bias_fn=None, vf=vf, vkey=("V", "x"), dv=128, mode="exp", fin=fin,
                       sbanks=[0, 1, 2, 6], obanks=[(4, 5)], act_func=AF.Copy, scale_fn=scale_fn, mask_mul=True)
        blocks += ps_.blocks()
    run_blocks(k, blocks, L=3)
    S.barrier()


def merge_and_out(k, l):
    S, d = k.S, k.d
    OB = [NU(0), NU(2), NU(1)]
    MG = DU(0)
    GT = [k.f32(SB + i * 512, 512) for i in range(2)]
    ACC = k.f32(SB + 1024, 512)
    PB = k.f32(SB + 1536, 512)

    def load_mp(dd):
        s = dd % 2
        tile = k.bf(W_W + s * 2304, 4608)
        S.dma("pool", f"mp{s}", lambda e: e.dma_start(out=tile, in_=d["mpack"][l, dd, :, :]), writes=[("MP", s)])
        return tile

    tiles = {0: load_mp(0), 1: load_mp(1)}
    cnt = 0
    for dd in range(NKC):
        MP = tiles[dd]
        mkey = ("MP", dd % 2)
        for t in range(NT):
            for b in range(3):
                bg, bo = next_bank(k), next_bank(k)
                for kc in range(NKC):
                    S.op("pe", lambda e, bg=bg, b=b, kc=kc, t=t, MP=MP: e.matmul(
                        k.bank(bg), MP[:, b * 1536 + kc * 128:b * 1536 + (kc + 1) * 128], k.H(kc, t * 512, 512),
                        start=(kc == 0), stop=(kc == NKC - 1)), reads=[mkey, ("H", kc, t)], writes=[("ps", bg)])
                for kc in range(4):
                    S.op("pe", lambda e, bo=bo, b=b, kc=kc, t=t, MP=MP: e.matmul(
                        k.bank(bo), MP[:, b * 1536 + 1024 + kc * 128:b * 1536 + 1024 + (kc + 1) * 128], fm(k, OB[b], kc, t * 512, 512),
                        start=(kc == 0), stop=(kc == 3)), reads=[mkey], writes=[("ps", bo)])
                gt = GT[cnt % 2]
                gk = ("GT", cnt % 2)
                cnt += 1
                gbc = k.gb[:, b * 8 + dd:b * 8 + dd + 1]
                S.op("act", lambda e, gt=gt, bg=bg, gbc=gbc: e.activation(out=gt, in_=k.bank(bg), func=AF.Sigmoid, bias=gbc, scale=1.0),
                     reads=[("ps", bg), "lconst"], writes=[gk])
                if b == 0:
                    S.op("dve", lambda e, gt=gt, bo=bo: e.tensor_tensor(out=ACC, in0=gt, in1=k.bank(bo), op=ALU.mult),
                         reads=[gk, ("ps", bo)], writes=["MACC"])
                else:
                    S.op("dve", lambda e, gt=gt, bo=bo: e.tensor_tensor(out=PB, in0=gt, in1=k.bank(bo), op=ALU.mult),
                         reads=[gk, ("ps", bo)], writes=["MPB"])
                    dst = ACC if b == 1 else fm(k, MG, dd, t * 512, 512)
                    dkey = "MACC" if b == 1 else ("fm", MG, dd, t)
                    S.op("dve", lambda e, dst=dst: e.tensor_tensor(out=dst, in0=ACC, in1=PB, op=ALU.add),
                         reads=["MACC", "MPB"], writes=[dkey] + (["MACC"] if b == 2 else []))
        if dd + 2 < NKC:
            tiles[dd + 2] = load_mp(dd + 2)
    S.barrier()
    MN = NU(0)
    for i in range(4):
        S.dma("sp", "mncp", lambda e, i=i: e.dma_start(out=k.bf(MN + i * 2048, 4096), in_=k.bf(MG + i * 2048, 4096)),
              writes=[("MN", i)])
    S.barrier()
    XS = [k.f32(SB + i * 512, 512) for i in range(2)]

    WOT = [k.bf(W_W + dd * 512, 1024) for dd in range(NKC)]
    for dd in range(NKC):
        S.dma("pool", "wo", lambda e, dd=dd: e.dma_start(out=WOT[dd], in_=d["wout"][l, dd, :, :]), writes=[("WO", dd)])
    cnt = 0
    for t in range(NT):
        for dd in range(NKC):
            WO = WOT[dd]
            b = next_bank(k)
            xs = XS[cnt % 2]
            xk = ("XS", cnt % 2)
            cnt += 1
            S.dma("sp", f"xs{cnt % 2}", lambda e, xs=xs, dd=dd, t=t: e.dma_start(out=xs, in_=d["xspill"][:, dd, t * 512:(t + 1) * 512]),
                  writes=[xk])
            for kc in range(NKC):
                S.op("pe", lambda e, b=b, kc=kc, t=t, WO=WO: e.matmul(
                    k.bank(b), WO[:, kc * 128:(kc + 1) * 128], fm(k, MN, kc, t * 512, 512),
                    start=(kc == 0), stop=(kc == NKC - 1)), reads=[("WO", dd)], writes=[("ps", b)])
            S.op("dve", lambda e, b=b, xs=xs, dd=dd, t=t: e.tensor_tensor(out=k.X(dd, t), in0=k.bank(b), in1=xs, op=ALU.add),
                 reads=[("ps", b), xk], writes=[("X", dd, t)])


def spill_x(k):
    S, d = k.S, k.d
    for c in range(NKC):
        o = W_X + c * S_LEN
        S.dma("sp", "xsp", lambda e, c=c, o=o: e.dma_start(out=d["xspill"][:, c, :], in_=k.arena[:, o:o + S_LEN]),
              reads=[("X", c, t) for t in range(NT)], writes=["xspill"])


def mixer(k, l, branches=("ml", "diff", "fox")):
    S = k.S
    layer_consts(k, l)
    rmsnorm(k, l * 3 + 1, "H")
    spill_x(k)
    S.barrier()
    if "ml" in branches:
        mlstm_branch(k, l)
    if "diff" in branches:
        diff_branch(k, l)
    if "fox" in branches:
        fox_branch(k, l)
    merge_and_out(k, l)


def _chunk_rows(w):
    K_, N_ = w.shape
    return w.reshape(K_ // 128, 128, N_).transpose(1, 0, 2)


def _cols(v):
    return v.reshape(-1, 128).T


def prep_shared(inp):
    f = np.float32
    sh = {}
    for which in (1, 2):
        wg, wu, wd = inp[f"ffn{which}_w_gate"], inp[f"ffn{which}_w_up"], inp[f"ffn{which}_w_down"]
        gu = np.empty((DEPTH, NFC, 128, 2, NKC, 128), f)
        for l in range(DEPTH):
            for g, w in enumerate((wg[l], wu[l])):
                gu[l, :, :, g] = w.reshape(NKC, 128, NFC, 128).transpose(2, 1, 0, 3)
        sh[f"ffn{which}_gu"] = gu.reshape(DEPTH, NFC, 128, 2 * NKC * 128)
        sh[f"ffn{which}_wd"] = np.ascontiguousarray(wd.reshape(DEPTH, NFC, 128, D))
    vecs = []
    for l in range(DEPTH):
        vecs += [inp["ffn1_norm"][l], inp["mix_norm"][l], inp["ffn2_norm"][l]]
    vecs.append(inp["final_norm"])
    sh["normw"] = np.ascontiguousarray(np.stack([_cols(v) for v in vecs], axis=1).reshape(128, 56)).astype(f)
    sh["ident"] = np.eye(128, dtype=f)
    sh["ones"] = np.ones((128, 128), f)
    sh["tri"] = np.triu(np.ones((128, 128), f))
    sh["negtri"] = (np.tril(np.ones((128, 128), f), -1) * NEG_BIG).astype(f)
    w_in = inp["w_in"]
    sh["w_in"] = np.ascontiguousarray(np.stack([_chunk_rows(w_in[l]) for l in range(DEPTH)]))
    gb = np.empty((DEPTH, 128, 24), f)
    cw = np.empty((DEPTH, 128, 16), f)
    cb = np.empty((DEPTH, 128, 4), f)
    sk = np.empty((DEPTH, 128, 4), f)
    nmc = np.empty((DEPTH, 128, 4), f)
    gc = np.zeros((DEPTH, 8, 8), f)
    for l in range(DEPTH):
        for b in range(3):
            gb[l, :, b * 8:(b + 1) * 8] = _cols(inp["gate_bias"][l, b])
        for j in range(4):
            cw[l, :, j * 4:(j + 1) * 4] = _cols(inp["mlstm_conv_w"][l, j])
        cb[l] = _cols(inp["mlstm_conv_b"][l])
        sk[l] = _cols(inp["mlstm_skip"][l])
        nmc[l] = _cols(inp["mlstm_norm"][l])
        gc[l, :, 0] = inp["fox_b_f"][l]
        gc[l, 0:4, 1] = inp["mlstm_b_if"][l, 0:4]
        gc[l, 0:4, 2] = inp["mlstm_b_if"][l, 4:8]
    sh["gbcols"], sh["convw"], sh["convb"], sh["skipc"], sh["gcols"] = gb, cw, cb, sk, gc
    sh["normc"] = nmc
    sh["difflam"] = np.ascontiguousarray(np.concatenate(
        [inp["diff_lq1"], inp["diff_lk1"], inp["diff_lq2"], inp["diff_lk2"]], axis=1)).astype(f)
    sh["diff_subln"] = np.ascontiguousarray(inp["diff_subln"]).astype(f)
    sh["mlstm_norm"] = np.ascontiguousarray(inp["mlstm_norm"]).astype(f)
    slopes = [2.0 ** (-8.0 * (h + 1) / 4) for h in range(4)]
    ak = np.zeros((3, 4, 128), f)
    aq = np.zeros((3, 4, 512), f)
    relq = np.arange(512)
    for h in range(4):
        ak[0, h] = slopes[h] * np.arange(128)
        ak[1, h] = 1.0
        ak[2, h] = 1.0
        aq[0, h] = 1.0
        aq[1, h] = -slopes[h] * (128 * (relq // 128))
        aq[2, h] = -slopes[h] * (relq % 128)
    sh["alibi_k"] = ak.reshape(3, 512)
    sh["alibi_q"] = aq.reshape(3, 2048)
    tpos = np.arange(S_LEN)
    akf = np.zeros((4, 3, S_LEN), f)
    aqf = np.zeros((4, 3, S_LEN), f)
    for h in range(4):
        akf[h, 0] = slopes[h] * (tpos % 128)
        akf[h, 1] = 1.0
        akf[h, 2] = 1.0
        aqf[h, 0] = 1.0
        aqf[h, 1] = -slopes[h] * (128 * ((tpos % 512) // 128))
        aqf[h, 2] = -slopes[h] * (tpos % 128)
    sh["alibi_kf"], sh["alibi_qf"] = akf, aqf
    wqkv = np.empty((DEPTH, 128, 3, 4, 128), f)
    for g, nm in enumerate(("mlstm_wq", "mlstm_wk", "mlstm_wv")):
        wqkv[:, :, g] = inp[nm].transpose(0, 2, 1, 3)
    sh["wqkv"] = wqkv.reshape(DEPTH, 128, 1536)
    sh["wif"] = np.ascontiguousarray(inp["mlstm_w_if"].reshape(DEPTH, 12, 128, 8).transpose(0, 2, 1, 3)).reshape(DEPTH, 128, 96)
    mp = np.empty((DEPTH, NKC, 128, 3, 1536), f)
    wbs = (inp["w_branch_diff"], inp["w_branch_fox"], inp["w_branch_mlstm"])
    for l in range(DEPTH):
        for b in range(3):
            g = w_in[l][:, C_G + b * D:C_G + (b + 1) * D]
            mp[l, :, :, b, 0:1024] = g.reshape(NKC, 128, NKC, 128).transpose(2, 1, 0, 3).reshape(NKC, 128, 1024)
            mp[l, :, :, b, 1024:1536] = wbs[b][l].reshape(4, 128, NKC, 128).transpose(2, 1, 0, 3).reshape(NKC, 128, 512)
    sh["mpack"] = mp.reshape(DEPTH, NKC, 128, 4608)
    wo = np.empty((DEPTH, NKC, 128, 1024), f)
    for l in range(DEPTH):
        wo[l] = inp["w_out"][l].reshape(NKC, 128, NKC, 128).transpose(2, 1, 0, 3).reshape(NKC, 128, 1024)
    sh["wout"] = wo
    return sh


def build_program(shapes, plan):
    from contextlib import ExitStack
    nc = bass.Bass("TRN2", target_bir_lowering=False)
    dram = {}
    for name, shp in shapes.items():
        dram[name] = nc.dram_tensor(name, list(shp), F32, kind="ExternalInput").ap()
    dram["outT"] = nc.dram_tensor("outT", [128, NKC, S_LEN], F32, kind="ExternalOutput").ap()
    dram["xspill"] = nc.dram_tensor("xspill", [128, NKC, S_LEN], F32, kind="Internal").ap()
    for nm in ("augk_fox", "augq_fox", "augk_ml", "augq_ml"):
        dram[nm] = nc.dram_tensor(nm, [8, 6, S_LEN], BF16, kind="Internal").ap()
    with ExitStack() as es:
        sems = [es.enter_context(nc.semaphore(f"s{i}")) for i in range(70)]
        S = Sched(nc, sems)
        arena = nc.alloc_sbuf_tensor("arena", [128, ARENA_WORDS], F32)
        ps = es.enter_context(nc.psum_tensor("ps", [128, 4096], F32))
        k = K(nc, S, arena, ps, dram)
        plan(k)
        with nc.Block() as block:
            S.emit(block)
    return nc


def full_plan(k):
    build_consts(k)
    load_x(k)
    for l in range(DEPTH):
        rmsnorm(k, l * 3 + 0, "H")
        ffn(k, l, 1)
        mixer(k, l)
        rmsnorm(k, l * 3 + 2, "H")
        k.S.barrier()
        ffn(k, l, 2)
    rmsnorm(k, 6, "X")
    store_out(k)


def run(inputs, plan=full_plan, trace=False, cores=NB):
    sh = prep_shared(inputs)
    x = np.asarray(inputs["x"], np.float32)
    in_maps = []
    for b in range(cores):
        m = dict(sh)
        m["xT"] = np.ascontiguousarray(x[b].T.reshape(NKC, 128, S_LEN).transpose(1, 0, 2))
        in_maps.append(m)
    shapes = {n: a.shape for n, a in in_maps[0].items()}
    nc = build_program(shapes, plan)
    res = run_bass_kernel_spmd(nc, in_maps, core_ids=list(range(cores)), trace=trace)
    out = np.empty((cores, S_LEN, D), np.float32)
    for b in range(cores):
        o = res.results[b]["outT"]
        out[b] = o.transpose(1, 0, 2).reshape(D, S_LEN).T
    return out, res


def kernel(**inputs):
    out, _ = run(inputs)
    return out
```
